# Optimizing a Trainium2 kernel written in Bass

```python
import math
import jax
import jax.numpy as jnp
from jax import lax
import numpy as np

D_MODEL = 1024
BATCH = 8
SEQ = 4096
DEPTH = 2

N_EVEN = (DEPTH + 1) // 2
N_ODD = DEPTH // 2
NORM_EPS = 1e-6

MLSTM_HEADS = 4
MLSTM_HD = D_MODEL // 8
MLSTM_W = MLSTM_HEADS * MLSTM_HD
MLSTM_CHUNK = 128
CONV_W = 5
POOL_WINDOWS = (2, 4, 8, 16)
POOL_GROUPS = len(POOL_WINDOWS)
POOL_GC = D_MODEL // 8
POOL_W = POOL_GROUPS * POOL_GC
MIX_W = MLSTM_W + POOL_W
N_GATE = 2 * 2 * MLSTM_HEADS
EVEN_IN = 4 * MLSTM_W + POOL_W + N_GATE
ATT_HD = 128
ATT_HEADS = D_MODEL // ATT_HD
ATT_KV_HEADS = ATT_HEADS // 4
ATT_GROUP = ATT_HEADS // ATT_KV_HEADS
ATT_W = ATT_HEADS * ATT_HD
ODD_IN = (ATT_HEADS + 2 * ATT_KV_HEADS) * ATT_HD
Q_BLOCK = 128
ROPE_THETA = 10000.0
GRID_W = 64
PEER_HEADS = 8
PEER_NKEYS = 128
PEER_EXPERTS = PEER_NKEYS * PEER_NKEYS
PEER_QDIM = 256
PEER_TOPK = 16
PEER_CHUNK = 128

kernel_name = 'hybrid_mlstm_pool_axialgqa_peer_encoder'

f32 = jnp.float32


def _unit_rms(x):
    xf = x.astype(f32)
    return xf * lax.rsqrt(jnp.mean(xf * xf, axis=-1, keepdims=True) + NORM_EPS)


def _rms_norm(x, g):
    return (_unit_rms(x) * g.astype(f32)).astype(x.dtype)


def _dwconv_centred(a, w, b):
    y = lax.conv_general_dilated(a, w.astype(a.dtype), window_strides=(1,), padding='SAME',
                                 dimension_numbers=('NWC', 'WIO', 'NWC'),
                                 feature_group_count=a.shape[-1])
    return y + b


def _mlstm_bidir_scan(q, k, v, log_i, log_f):
    lead = q.shape[:3]
    S = q.shape[3]
    L = MLSTM_CHUNK
    nc = S // L

    def to_chunks(a):
        a = a.reshape(lead + (nc, L) + a.shape[4:])
        return jnp.moveaxis(a, 3, 0)

    in_chunk_mask = jnp.tril(jnp.ones((L, L), dtype=bool))

    def step(carry, inp):
        C, n, m = carry
        qc, kc, vc, li, lf = inp
        b = jnp.cumsum(lf, axis=-1)
        b_end = b[..., -1]
        log_w = jnp.where(in_chunk_mask, b[..., :, None] - b[..., None, :] + li[..., None, :], -jnp.inf)
        log_carry = b + m[..., None]
        m_t = jnp.maximum(log_carry, jnp.max(log_w, axis=-1))
        w_intra = jnp.exp(log_w - m_t[..., None])
        w_carry = jnp.exp(log_carry - m_t)
        sc = jnp.einsum('...jd,...sd->...js', qc, kc) * w_intra
        num = jnp.einsum('...js,...sv->...jv', sc, vc) + w_carry[..., None] * jnp.einsum('...jd,...dv->...jv', qc, C)
        den = jnp.sum(sc, axis=-1) + w_carry * jnp.einsum('...jd,...d->...j', qc, n)
        h = num / jnp.maximum(jnp.abs(den), jnp.exp(-m_t))[..., None]
        log_end = b_end[..., None] - b + li
        m_new = jnp.maximum(b_end + m, jnp.max(log_end, axis=-1))
        w_end = jnp.exp(log_end - m_new[..., None])
        decay = jnp.exp(b_end + m - m_new)
        kw = kc * w_end[..., None]
        C_new = decay[..., None, None] * C + jnp.einsum('...sd,...sv->...dv', kw, vc)
        n_new = decay[..., None] * n + jnp.sum(kw, axis=-2)
        return (C_new, n_new, m_new), h

    d = q.shape[-1]
    dv = v.shape[-1]
    init = (jnp.zeros(lead + (d, dv), f32), jnp.zeros(lead + (d,), f32), jnp.zeros(lead, f32))
    _, hs = lax.scan(step, init, (to_chunks(q), to_chunks(k), to_chunks(v), to_chunks(log_i), to_chunks(log_f)))
    return jnp.moveaxis(hs, 0, 3).reshape(lead + (S, dv))


def _multiscale_pool(p, pool_w, pool_scale):
    B, S, _ = p.shape
    pf = p.astype(f32)
    csum = jnp.concatenate([jnp.zeros((B, 1, POOL_W), f32), jnp.cumsum(pf, axis=1)], axis=1)
    t = jnp.arange(S)
    outs = []
    for gi, win in enumerate(POOL_WINDOWS):
        lo = jnp.clip(t - win // 2, 0, S)
        hi = jnp.clip(t + win // 2, 0, S)
        sl = slice(gi * POOL_GC, (gi + 1) * POOL_GC)
        cg = csum[..., sl]
        mean = (cg[:, hi] - cg[:, lo]) / (hi - lo).astype(f32)[None, :, None]
        y = (mean - pf[..., sl]).astype(p.dtype)
        outs.append(jnp.einsum('bsc,ce->bse', y, pool_w[gi]))
    return jnp.concatenate(outs, axis=-1) * pool_scale


def _mlstm_pool_mixer(h, w_in, b_in, conv_w, conv_b, mnorm_g, pool_w, pool_scale, w_out):
    B, S, _ = h.shape
    z = jnp.einsum('bsd,de->bse', h, w_in) + b_in
    qk, v, o, p, g = jnp.split(z, [2 * MLSTM_W, 3 * MLSTM_W, 4 * MLSTM_W, 4 * MLSTM_W + POOL_W], axis=-1)
    qk = jax.nn.silu(_dwconv_centred(qk, conv_w, conv_b))
    q, k = jnp.split(qk, 2, axis=-1)

    def heads(a):
        return a.reshape(B, S, MLSTM_HEADS, MLSTM_HD).transpose(0, 2, 1, 3).astype(f32)

    q, k, v = heads(q), heads(k) * (MLSTM_HD ** -0.5), heads(v)

    def both(a):
        return jnp.stack([a, jnp.flip(a, axis=2)])

    g = g.astype(f32).reshape(B, S, 2, 2, MLSTM_HEADS).transpose(2, 3, 0, 4, 1)
    log_i = jnp.stack([g[0, 0], jnp.flip(g[0, 1], axis=-1)])
    log_f = jax.nn.log_sigmoid(jnp.stack([g[1, 0], jnp.flip(g[1, 1], axis=-1)]))
    hs = _mlstm_bidir_scan(both(q), both(k), both(v), log_i, log_f)
    hm = (hs[0] + jnp.flip(hs[1], axis=2)).transpose(0, 2, 1, 3)
    hm = _rms_norm(hm, mnorm_g.reshape(MLSTM_HEADS, MLSTM_HD)).astype(h.dtype)
    hm = hm.reshape(B, S, MLSTM_W) * jax.nn.sigmoid(o)
    yp = _multiscale_pool(p, pool_w, pool_scale)
    return jnp.einsum('bse,ed->bsd', jnp.concatenate([hm, yp], axis=-1), w_out)


def _axial_rope_tables(S):
    rows = S // GRID_W
    r, cidx = jnp.meshgrid(jnp.arange(rows), jnp.arange(GRID_W), indexing='ij')
    axis_dim = ATT_HD // 2
    freqs = ROPE_THETA ** (-jnp.arange(0, axis_dim, 2, dtype=f32) / axis_dim)
    ang = jnp.concatenate([r.reshape(-1, 1).astype(f32) * freqs,
                           cidx.reshape(-1, 1).astype(f32) * freqs], axis=-1)
    return jnp.cos(ang), jnp.sin(ang)


def _apply_rope(x, cos, sin):
    xr = x.astype(f32).reshape(x.shape[:-1] + (ATT_HD // 2, 2))
    x0, x1 = xr[..., 0], xr[..., 1]
    c = cos[None, :, None, :]
    s = sin[None, :, None, :]
    out = jnp.stack([x0 * c - x1 * s, x0 * s + x1 * c], axis=-1).reshape(x.shape)
    return out.astype(x.dtype)


def _gqa_axial_mixer(h, w_in, qn_g, kn_g, w_out):
    B, S, _ = h.shape
    z = jnp.einsum('bsd,de->bse', h, w_in)
    q, k, v = jnp.split(z, [ATT_W, ATT_W + ATT_KV_HEADS * ATT_HD], axis=-1)
    q = _rms_norm(q.reshape(B, S, ATT_HEADS, ATT_HD), qn_g)
    k = _rms_norm(k.reshape(B, S, ATT_KV_HEADS, ATT_HD), kn_g)
    v = v.reshape(B, S, ATT_KV_HEADS, ATT_HD)
    cos, sin = _axial_rope_tables(S)
    q = _apply_rope(q, cos, sin)
    k = _apply_rope(k, cos, sin)
    qb = jnp.moveaxis(q.reshape(B, S // Q_BLOCK, Q_BLOCK, ATT_KV_HEADS, ATT_GROUP, ATT_HD), 1, 0)
    scale = ATT_HD ** -0.5

    def attend(q_blk):
        s = jnp.einsum('bqkgd,bskd->bkgqs', q_blk, k).astype(f32) * scale
        pr = jax.nn.softmax(s, axis=-1).astype(v.dtype)
        return jnp.einsum('bkgqs,bskd->bqkgd', pr, v)

    out = lax.map(attend, qb)
    out = jnp.moveaxis(out, 0, 1).reshape(B, S, ATT_W)
    return jnp.einsum('bse,ed->bsd', out, w_out)


def _peer(h, w_q, sub_keys, u, v):
    B, S, D = h.shape
    xt = h.reshape(B * S // PEER_CHUNK, PEER_CHUNK, D)
    half = PEER_QDIM // 2
    nk2 = PEER_TOPK * PEER_TOPK

    def chunk(xc):
        q = jnp.einsum('td,de->te', xc, w_q).reshape(PEER_CHUNK, PEER_HEADS, 2, half)
        q = _unit_rms(q)
        s = jnp.einsum('thpc,pnc->thpn', q, sub_keys.astype(f32))
        s1, i1 = lax.top_k(s[:, :, 0], PEER_TOPK)
        s2, i2 = lax.top_k(s[:, :, 1], PEER_TOPK)
        cand_s = (s1[..., :, None] + s2[..., None, :]).reshape(PEER_CHUNK, PEER_HEADS, nk2)
        cand_i = (i1[..., :, None] * PEER_NKEYS + i2[..., None, :]).reshape(PEER_CHUNK, PEER_HEADS, nk2)
        top_s, pos = lax.top_k(cand_s, PEER_TOPK)
        idx = jnp.take_along_axis(cand_i, pos, axis=-1)
        gate = jax.nn.softmax(top_s, axis=-1)
        act = jax.nn.gelu(jnp.einsum('thkd,td->thk', u[idx], xc).astype(f32), approximate=False)
        coef = (gate * act).astype(xc.dtype)
        return jnp.einsum('thk,thkd->td', coef, v[idx])

    return lax.map(chunk, xt).reshape(B, S, D)


def setup_inputs(seed: int = 0) -> dict:
    key = jax.random.key(seed)
    ks = jax.random.split(key, 28)
    D = D_MODEL

    def nrm(k, shape, s):
        return jax.random.normal(k, shape, f32) * s

    ev_b_in = nrm(ks[7], (N_EVEN, EVEN_IN), 0.02)
    f0 = 4 * MLSTM_W + POOL_W + 2 * MLSTM_HEADS
    ev_b_in = ev_b_in.at[:, f0:f0 + 2 * MLSTM_HEADS].add(
        jax.random.uniform(ks[8], (N_EVEN, 2 * MLSTM_HEADS), f32, 3.0, 6.0))
    return {
        'x': nrm(ks[0], (BATCH, SEQ, D), 1.0),
        'c': nrm(ks[1], (BATCH, D), 1.0),
        'ada_w': nrm(ks[2], (DEPTH, D, 6 * D), 0.5 * D ** -0.5),
        'ada_b': nrm(ks[3], (DEPTH, 6 * D), 0.02),
        'norm_mix_g': 1.0 + nrm(ks[4], (DEPTH, D), 0.05),
        'norm_ffn_g': 1.0 + nrm(ks[5], (DEPTH, D), 0.05),
        'ev_w_in': nrm(ks[6], (N_EVEN, D, EVEN_IN), D ** -0.5),
        'ev_b_in': ev_b_in,
        'ev_conv_w': nrm(ks[9], (N_EVEN, CONV_W, 1, 2 * MLSTM_W), CONV_W ** -0.5),
        'ev_conv_b': nrm(ks[10], (N_EVEN, 2 * MLSTM_W), 0.02),
        'ev_mnorm_g': 1.0 + nrm(ks[11], (N_EVEN, MLSTM_W), 0.05),
        'ev_pool_w': nrm(ks[12], (N_EVEN, POOL_GROUPS, POOL_GC, POOL_GC), POOL_GC ** -0.5),
        'ev_pool_scale': 1.0 + nrm(ks[13], (N_EVEN, POOL_W), 0.1),
        'ev_w_out': nrm(ks[14], (N_EVEN, MIX_W, D), MIX_W ** -0.5),
        'od_w_in': nrm(ks[15], (N_ODD, D, ODD_IN), D ** -0.5),
        'od_qnorm_g': 1.0 + nrm(ks[16], (N_ODD, ATT_HD), 0.05),
        'od_knorm_g': 1.0 + nrm(ks[17], (N_ODD, ATT_HD), 0.05),
        'od_w_out': nrm(ks[18], (N_ODD, ATT_W, D), ATT_W ** -0.5),
        'peer_w_q': nrm(ks[19], (DEPTH, D, PEER_HEADS * PEER_QDIM), D ** -0.5),
        'peer_keys': nrm(ks[20], (DEPTH, 2, PEER_NKEYS, PEER_QDIM // 2), (PEER_QDIM // 2) ** -0.5),
        'peer_u': nrm(ks[21], (DEPTH, PEER_EXPERTS, D), D ** -0.5),
        'peer_v': nrm(ks[22], (DEPTH, PEER_EXPERTS, D), 0.5),
        'final_g': 1.0 + nrm(ks[23], (D,), 0.05),
    }


def reference(x, c, ada_w, ada_b, norm_mix_g, norm_ffn_g, ev_w_in, ev_b_in, ev_conv_w, ev_conv_b,
              ev_mnorm_g, ev_pool_w, ev_pool_scale, ev_w_out, od_w_in, od_qnorm_g, od_knorm_g, od_w_out,
              peer_w_q, peer_keys, peer_u, peer_v, final_g):
    cond = jax.nn.silu(c)
    for i in range(DEPTH):
        mod = jnp.einsum('bd,de->be', cond, ada_w[i]) + ada_b[i]
        sh1, sc1, g1, sh2, sc2, g2 = [m[:, None, :] for m in jnp.split(mod, 6, axis=-1)]
        hmix = _rms_norm(x, norm_mix_g[i]) * (1 + sc1) + sh1
        if i % 2 == 0:
            j = i // 2
            y = _mlstm_pool_mixer(hmix, ev_w_in[j], ev_b_in[j], ev_conv_w[j], ev_conv_b[j], ev_mnorm_g[j],
                                  ev_pool_w[j], ev_pool_scale[j], ev_w_out[j])
        else:
            j = i // 2
            y = _gqa_axial_mixer(hmix, od_w_in[j], od_qnorm_g[j], od_knorm_g[j], od_w_out[j])
        x = x + g1 * y
        hffn = _rms_norm(x, norm_ffn_g[i]) * (1 + sc2) + sh2
        x = x + g2 * _peer(hffn, peer_w_q[i], peer_keys[i], peer_u[i], peer_v[i])
    return _rms_norm(x, final_g)
```

```python
import contextlib
import numpy as np
import concourse.bass as bass
import concourse.mybir as mybir
from concourse.bass_utils import run_bass_kernel_spmd

F32 = mybir.dt.float32
BF16 = mybir.dt.bfloat16
I32 = mybir.dt.int32
U32 = mybir.dt.uint32
AF = mybir.ActivationFunctionType
ALU = mybir.AluOpType
AX = mybir.AxisListType

S = 4096
D = 1024
NT = S // 128
EPS = 1e-6


class Tok:
    __slots__ = ("w", "r", "name")

    def __init__(self, name=""):
        self.w = None
        self.r = {}
        self.name = name


class _Eng:
    def __init__(self, name, sem):
        self.name = name
        self.sem = sem
        self.cnt = 0
        self.seen = {}
        self.ops = []


NDMA = 6


class _Rec:
    def __getattr__(self, name):
        def f(*a, **k):
            return (name, a, k)
        return f


_REC = _Rec()


class Prog:
    ENG = ("pe", "act", "dve", "pool", "sp")

    def __init__(self, nc, stack):
        self.nc = nc
        self.stack = stack
        self.e = {}
        self.sems = {}
        for n in self.ENG:
            self.e[n] = _Eng(n, stack.enter_context(nc.semaphore("s_" + n)))
            self.sems[n] = (self.e[n].sem, 1)
        self.dslot = {}
        for q in ("sp", "pool", "act"):
            sl = []
            for j in range(NDMA):
                key = "d_%s%d" % (q, j)
                sem = stack.enter_context(nc.semaphore(key))
                self.sems[key] = (sem, 16)
                sl.append([key, 0])
            self.dslot[q] = [sl, 0]

    def _deps(self, e, rd, wr):
        deps = {}

        def need(dep, same_ok):
            if dep is None:
                return
            en, c = dep
            if en == e and (e == "pe" or not same_ok):
                return
            if deps.get(en, 0) < c:
                deps[en] = c

        for t in rd:
            need(t.w, True)
        for t in wr:
            need(t.w, False)
            for en, c in t.r.items():
                need((en, c), False)
        return deps

    def _emit_waits(self, E, deps):
        for en, c in deps.items():
            if E.seen.get(en, 0) < c:
                sem, step = self.sems[en]
                E.ops.append(lambda h, sem=sem, v=c * step: h.wait_ge(sem, v))
                E.seen[en] = c

    def op(self, e, fn, rd=(), wr=()):
        E = self.e[e]
        self._emit_waits(E, self._deps(e, rd, wr))
        E.cnt += 1
        sem = E.sem
        rec = fn(_REC)
        E.ops.append(lambda h, rec=rec, sem=sem: getattr(h, rec[0])(*rec[1], **rec[2]).then_inc(sem, 1))
        me = (e, E.cnt)
        for t in wr:
            t.w = me
            t.r = {}
        for t in rd:
            t.r[e] = E.cnt

    def dma(self, q, fn, rd=(), wr=()):
        E = self.e[q]
        slots, nxt = self.dslot[q]
        slot = slots[nxt % NDMA]
        self.dslot[q][1] = nxt + 1
        key = slot[0]
        deps = self._deps(key, rd, wr)
        if slot[1] > 0:
            deps[key] = max(deps.get(key, 0), slot[1])
        self._emit_waits(E, deps)
        slot[1] += 1
        sem = self.sems[key][0]
        rec = fn(_REC)
        E.ops.append(lambda h, rec=rec, sem=sem: getattr(h, rec[0])(*rec[1], **rec[2]).then_inc(sem, 16))
        me = (key, slot[1])
        for t in wr:
            t.w = me
            t.r = {}
        for t in rd:
            t.r[key] = slot[1]

    def barrier(self):
        tgt = {n: self.e[n].cnt for n in self.ENG}
        for q in self.dslot:
            for key, c in self.dslot[q][0]:
                tgt[key] = c
        for n in self.ENG:
            E = self.e[n]
            d = {k: v for k, v in tgt.items() if v > 0 and not (k == n and n == "pe")}
            self._emit_waits(E, d)

    def flush(self):
        nc = self.nc
        with nc.Block() as block:
            @block.tensor
            def _(h):
                for f in self.e["pe"].ops:
                    f(h)

            @block.scalar
            def _(h):
                for f in self.e["act"].ops:
                    f(h)

            @block.vector
            def _(h):
                for f in self.e["dve"].ops:
                    f(h)

            @block.gpsimd
            def _(h):
                for f in self.e["pool"].ops:
                    f(h)

            @block.sync
            def _(h):
                for f in self.e["sp"].ops:
                    f(h)
        for n in self.ENG:
            self.e[n].ops = []


class Buf:
    def __init__(self, t):
        self.t = t
        self.k = Tok()

    def __getitem__(self, i):
        return self.t[i]


def _ks(xs):
    return [x.k if isinstance(x, Buf) else x for x in xs]


_NAME = [0]


class Ctx:
    def __init__(self, P, nc, st):
        self.P, self.nc, self.st = P, nc, st
        self.n = 0

    def T(self, shape, dt, name=None):
        _NAME[0] += 1
        return Buf(self.st.enter_context(self.nc.sbuf_tensor("%s_%d" % (name or "t", _NAME[0]), list(shape), dt)))

    def PS(self, shape, dt=F32, name=None):
        _NAME[0] += 1
        return Buf(self.st.enter_context(self.nc.psum_tensor("%s_%d" % (name or "p", _NAME[0]), list(shape), dt)))

    def V(self, fn, rd=(), wr=()):
        self.P.op("dve", fn, _ks(rd), _ks(wr))

    def A(self, fn, rd=(), wr=()):
        self.P.op("act", fn, _ks(rd), _ks(wr))

    def G(self, fn, rd=(), wr=()):
        self.P.op("pool", fn, _ks(rd), _ks(wr))

    def M(self, fn, rd=(), wr=()):
        self.P.op("pe", fn, _ks(rd), _ks(wr))

    def dma(self, q, out, in_, rd=(), wr=()):
        self.P.dma(q, lambda h, out=out, in_=in_: h.dma_start(out=out, in_=in_), _ks(rd), _ks(wr))


def load_cast(cx, q, dst_bf, src_ap, stage, eng="pool"):
    cx.dma(q, stage.t[:] if not isinstance(stage, tuple) else stage[1], src_ap, wr=[stage if not isinstance(stage, tuple) else stage[0]])


def emit_norm_tile(cx, xt, gs, sh, hb, sq, st2, idb, ptr, hT_dst, hT_buf, hf=None):
    cx.V(lambda h: h.memset(st2[:, 0:1], 0.0), wr=[st2])
    cx.A(lambda h: h.activation(out=sq[:], in_=xt[:], func=AF.Square, accum_out=st2[:, 0:1]), rd=[xt, st2], wr=[sq, st2])
    cx.A(lambda h: h.activation(out=st2[:, 1:2], in_=st2[:, 0:1], func=AF.Sqrt, scale=1.0 / D, bias=EPS_AP[0][:, 0:1]), rd=[st2, EPS_AP[0]], wr=[st2])
    cx.V(lambda h: h.reciprocal(out=st2[:, 2:3], in_=st2[:, 1:2]), rd=[st2], wr=[st2])
    cx.V(lambda h: h.scalar_tensor_tensor(out=sq[:], in0=xt[:], scalar=st2[:, 2:3], in1=gs[:], op0=ALU.mult, op1=ALU.mult), rd=[xt, st2, gs], wr=[sq])
    if hf is not None:
        cx.V(lambda h: h.tensor_tensor(out=hf[:], in0=sq[:], in1=sh[:], op=ALU.add), rd=[sq, sh], wr=[hf])
        cx.G(lambda h: h.tensor_copy(out=hb[:], in_=hf[:]), rd=[hf], wr=[hb])
    else:
        cx.V(lambda h: h.tensor_tensor(out=hb[:], in0=sq[:], in1=sh[:], op=ALU.add), rd=[sq, sh], wr=[hb])
    for k in range(8):
        cx.M(lambda h, k=k: h.transpose(out=ptr[:, k, :], in_=hb[:, k * 128:(k + 1) * 128], identity=idb[:]), rd=[hb, idb], wr=[ptr])
    cx.A(lambda h: h.activation(out=hT_dst, in_=ptr[:], func=AF.Copy), rd=[ptr], wr=[hT_buf])


EPS_AP = [None]


def load_consts(cx, CN):
    idf = cx.T([128, 128], F32)
    idb = cx.T([128, 128], BF16)
    eps = cx.T([128, 1], F32)
    cx.dma("sp", idf[:], CN["ident"], wr=[idf])
    cx.V(lambda h: h.tensor_copy(out=idb[:], in_=idf[:]), rd=[idf], wr=[idb])
    cx.V(lambda h: h.memset(eps[:], EPS), wr=[eps])
    EPS_AP[0] = eps
    return idf, idb, eps


def phase_mods(P, nc, IN, MODS):
    with contextlib.ExitStack() as st:
        cx = Ctx(P, nc, st)
        cT = cx.T([128, 8], F32)
        cond = cx.T([128, 8], F32)
        crep = cx.T([128, 8, 128], F32)
        cx.dma("sp", cT[:], IN["cT"], wr=[cT])
        cx.A(lambda h: h.activation(out=cond[:], in_=cT[:], func=AF.Silu), rd=[cT], wr=[cond])
        cx.V(lambda h: h.tensor_copy(out=crep[:], in_=cond[:].unsqueeze(2).broadcast_to([128, 8, 128])), rd=[cond], wr=[crep])
        wb = [cx.T([128, 8, 512], F32) for _ in range(2)]
        ps = [cx.PS([128, 512]) for _ in range(2)]
        mod = cx.T([128, 6144], F32)
        ab = cx.T([128, 6144], F32)
        gm = cx.T([128, 1024], F32)
        gf = cx.T([128, 1024], F32)
        n = 0
        for i in range(2):
            cx.dma("act", ab[:], IN["ada_b"][i:i + 1, :].broadcast_to([128, 6144]), wr=[ab])
            cx.dma("act", gm[:], IN["norm_mix_g"][i:i + 1, :].broadcast_to([128, 1024]), wr=[gm])
            cx.dma("act", gf[:], IN["norm_ffn_g"][i:i + 1, :].broadcast_to([128, 1024]), wr=[gf])
            for nb in range(12):
                w = wb[n % 2]
                p = ps[n % 2]
                n += 1
                cx.dma("sp" if nb % 2 == 0 else "pool", w[:], IN["ada_w"][i, :, nb * 512:(nb + 1) * 512].rearrange("(k p) n -> p k n", p=128), wr=[w])
                for k in range(8):
                    cx.M(lambda h, k=k, w=w, p=p: h.matmul(p[:], lhsT=crep[:, k, :], rhs=w[:, k, :], start=(k == 0), stop=(k == 7)), rd=[crep, w], wr=[p])
                cx.V(lambda h, p=p, nb=nb: h.tensor_tensor(out=mod[:, nb * 512:(nb + 1) * 512], in0=p[:], in1=ab[:, nb * 512:(nb + 1) * 512], op=ALU.add), rd=[p, ab], wr=[mod])
            cx.V(lambda h: h.scalar_tensor_tensor(out=mod[:, 1024:2048], in0=mod[:, 1024:2048], scalar=1.0, in1=gm[:], op0=ALU.add, op1=ALU.mult), rd=[mod, gm], wr=[mod])
            cx.V(lambda h: h.scalar_tensor_tensor(out=mod[:, 4096:5120], in0=mod[:, 4096:5120], scalar=1.0, in1=gf[:], op0=ALU.add, op1=ALU.mult), rd=[mod, gf], wr=[mod])
            for j, off in enumerate([1024, 0, 2048, 4096, 3072, 5120]):
                cx.dma("sp", MODS.t[i, j], mod[:, off:off + 1024], rd=[mod], wr=[MODS])
        P.barrier()
        P.flush()


def load_mods(cx, MODS, i, js, q="act"):
    out = []
    for j in js:
        b = cx.T([128, 1024], F32)
        cx.dma(q, b[:], MODS.t[i, j], rd=[MODS], wr=[b])
        out.append(b)
    return out


def emit_hT(cx, Xin, gs, sh, idb, hT):
    xb = [cx.T([128, 1024], F32) for _ in range(2)]
    sq = cx.T([128, 1024], F32)
    hb = [cx.T([128, 1024], BF16) for _ in range(2)]
    st2 = [cx.T([128, 4], F32) for _ in range(2)]
    ptr = [cx.PS([128, 8, 128], BF16) for _ in range(2)]
    for i in range(NT):
        x = xb[i % 2]
        cx.dma("sp", x[:], Xin.t[i * 128:(i + 1) * 128, :], rd=[Xin], wr=[x])
        emit_norm_tile(cx, x, gs, sh, hb[i % 2], sq, st2[i % 2], idb, ptr[i % 2], hT[:, :, i * 128:(i + 1) * 128], hT)


def emit_outproj(cx, Xin, Xout, catT_src, w_ap, g1, stage_f, final=None):
    wob = cx.T([128, 8, 1024], BF16)
    for k in range(8):
        cx.dma("sp", stage_f[:], w_ap[k * 128:(k + 1) * 128, :], wr=[stage_f])
        cx.G(lambda h, k=k: h.tensor_copy(out=wob[:, k, :], in_=stage_f[:]), rd=[stage_f], wr=[wob])
    py = [cx.PS([128, 1024]) for _ in range(2)]
    xb = [cx.T([128, 1024], F32) for _ in range(2)]
    yb = [cx.T([128, 1024], F32) for _ in range(2)]
    for i in range(NT):
        ap, tok = catT_src(i)
        p = py[i % 2]
        x = xb[i % 2]
        y = yb[i % 2]
        cx.dma("act", x[:], Xin.t[i * 128:(i + 1) * 128, :], rd=[Xin], wr=[x])
        for nb in range(2):
            for k in range(8):
                cx.M(lambda h, k=k, nb=nb, p=p, ap=ap: h.matmul(p[:, nb * 512:(nb + 1) * 512], lhsT=ap[:, k, :], rhs=wob[:, k, nb * 512:(nb + 1) * 512],
                                                               start=(k == 0), stop=(k == 7)), rd=[tok, wob], wr=[p])
        cx.V(lambda h, p=p, y=y: h.tensor_tensor(out=y[:], in0=p[:], in1=g1[:], op=ALU.mult), rd=[p, g1], wr=[y])
        cx.G(lambda h, x=x, y=y: h.tensor_tensor(out=y[:], in0=y[:], in1=x[:], op=ALU.add), rd=[y, x], wr=[y])
        cx.dma("sp", Xout.t[i * 128:(i + 1) * 128, :], y[:], rd=[y], wr=[Xout])


def phase_even(P, nc, IN, MODS, li, Xin, Xout, SC):
    CATT, V1D, SIGD, HFD = SC["CATT"], SC["V1D"], SC["SIGD"], SC["HFD"]
    with contextlib.ExitStack() as st0:
        c0 = Ctx(P, nc, st0)
        idf, idb, eps = load_consts(c0, IN)
        QK = c0.T([128, 8, S], BF16)
        GT = c0.T([128, NT, 16], F32)
        EB = c0.T([128, NT, 8], F32)
        ES = c0.T([128, NT, 8], F32)
        EE = c0.T([128, NT, 8], F32)
        with contextlib.ExitStack() as st1:
            cx1 = Ctx(P, nc, st1)
            hT = cx1.T([128, 8, S], BF16)
            with contextlib.ExitStack() as st2:
                c2 = Ctx(P, nc, st2)
                gs1, sh1 = load_mods(c2, MODS, li, [0, 1])
                emit_hT(c2, Xin, gs1, sh1, idb, hT)
                P.barrier()
            with contextlib.ExitStack() as stf:
                cx = Ctx(P, nc, stf)
                bT = cx.T([128, 20], F32)
                cw = cx.T([128, 8, 5], F32)
                cb = cx.T([128, 8], F32)
                edge = cx.T([128, 4, 32], F32)
                cx.dma("sp", bT[:], IN["ev_b_inT"], wr=[bT])
                cx.dma("sp", cw[:], IN["ev_conv_wT"], wr=[cw])
                cx.dma("sp", cb[:], IN["ev_conv_bT"], wr=[cb])
                cx.dma("sp", edge[:], IN["pooledge"].broadcast_to([128, 4, 32]), wr=[edge])
                wst = [cx.T([128, 8, 128], F32) for _ in range(2)]
                wcb = [cx.T([128, 8, 128], BF16) for _ in range(2)]
                zc = cx.T([128, S + 16], F32)
                pa = cx.T([128, S + 16], F32)
                yb = cx.T([128, S + 16], F32)
                ybf = cx.T([128, S], BF16)
                yo = [cx.T([128, 512], BF16) for _ in range(2)]
                pw = cx.T([128, 128], F32)
                psc = cx.T([128, 128], F32)
                pwb = cx.T([128, 128], BF16)
                pz = [cx.PS([128, 512]) for _ in range(2)]
                cx.V(lambda h: h.memset(zc[:], 0.0), wr=[zc])
                nps = 0
                for c in range(12):
                    col0 = c * 128 if c < 8 else 2048 + (c - 8) * 128
                    bcol = c if c < 8 else 16 + (c - 8)
                    ws, wc = wst[c % 2], wcb[c % 2]
                    cx.dma("sp", ws[:], IN["ev_w_in"][:, col0:col0 + 128].rearrange("(k p) n -> p k n", p=128), wr=[ws])
                    cx.G(lambda h, ws=ws, wc=wc: h.tensor_copy(out=wc[:], in_=ws[:]), rd=[ws], wr=[wc])
                    for tb in range(8):
                        p = pz[nps % 2]
                        nps += 1
                        for k in range(8):
                            cx.M(lambda h, k=k, p=p, wc=wc, tb=tb: h.matmul(p[:], lhsT=wc[:, k, :], rhs=hT[:, k, tb * 512:(tb + 1) * 512], start=(k == 0), stop=(k == 7)),
                                 rd=[wc, hT], wr=[p])
                        cx.A(lambda h, p=p, tb=tb, bcol=bcol: h.activation(out=zc[:, 8 + tb * 512:8 + (tb + 1) * 512], in_=p[:], func=AF.Identity, bias=bT[:, bcol:bcol + 1]),
                             rd=[p, bT], wr=[zc])
                    if c < 8:
                        cx.V(lambda h, c=c: h.tensor_scalar(out=yb[:, 0:S], in0=zc[:, 6:6 + S], scalar1=cw[:, c, 0:1], scalar2=None, op0=ALU.mult), rd=[zc, cw], wr=[yb])
                        for j in range(1, 5):
                            cx.V(lambda h, c=c, j=j: h.scalar_tensor_tensor(out=yb[:, 0:S], in0=zc[:, 6 + j:6 + j + S], scalar=cw[:, c, j:j + 1], in1=yb[:, 0:S], op0=ALU.mult, op1=ALU.add),
                                 rd=[zc, cw, yb], wr=[yb])
                        cx.A(lambda h, c=c: h.activation(out=QK[:, c, :], in_=yb[:, 0:S], func=AF.Silu, bias=cb[:, c:c + 1]), rd=[yb, cb], wr=[QK])
                    else:
                        g = c - 8
                        win = (2, 4, 8, 16)[g]
                        half = win // 2
                        n_el = S + 15
                        cur = zc
                        bufs = [pa, yb]
                        bi = 0
                        step = 1
                        while step < win:
                            d = bufs[bi % 2]
                            bi += 1
                            cx.V(lambda h, cur=cur, d=d, step=step, n_el=n_el: h.tensor_tensor(out=d[:, 0:n_el - step + 1], in0=cur[:, 0:n_el - step + 1], in1=cur[:, step:n_el + 1], op=ALU.add),
                                 rd=[cur], wr=[d])
                            n_el = n_el - step
                            cur = d
                            step *= 2
                        o = bufs[bi % 2]
                        cx.V(lambda h, cur=cur, half=half, o=o, win=win: h.tensor_scalar(out=o[:, 0:S], in0=cur[:, 8 - half:8 - half + S], scalar1=1.0 / win, scalar2=None, op0=ALU.mult), rd=[cur], wr=[o])
                        cx.V(lambda h, o=o, g=g: h.tensor_tensor(out=o[:, 0:16], in0=o[:, 0:16], in1=edge[:, g, 0:16], op=ALU.mult), rd=[o, edge], wr=[o])
                        cx.V(lambda h, o=o, g=g: h.tensor_tensor(out=o[:, S - 16:S], in0=o[:, S - 16:S], in1=edge[:, g, 16:32], op=ALU.mult), rd=[o, edge], wr=[o])
                        cx.V(lambda h, o=o: h.tensor_tensor(out=ybf[:], in0=o[:, 0:S], in1=zc[:, 8:8 + S], op=ALU.subtract), rd=[o, zc], wr=[ybf])
                        cx.dma("sp", pw[:], IN["ev_pool_w"][g], wr=[pw])
                        cx.dma("sp", psc[:], IN["ev_pool_scale"][0:1, g * 128:(g + 1) * 128].broadcast_to([128, 128]), wr=[psc])
                        cx.V(lambda h: h.tensor_tensor(out=pwb[:], in0=pw[:], in1=psc[:], op=ALU.mult), rd=[pw, psc], wr=[pwb])
                        for tb in range(8):
                            p = pz[nps % 2]
                            y_ = yo[nps % 2]
                            nps += 1
                            cx.M(lambda h, p=p, tb=tb: h.matmul(p[:], lhsT=pwb[:], rhs=ybf[:, tb * 512:(tb + 1) * 512], start=True, stop=True), rd=[pwb, ybf], wr=[p])
                            cx.A(lambda h, p=p, y_=y_: h.activation(out=y_[:], in_=p[:], func=AF.Copy), rd=[p], wr=[y_])
                            cx.dma("sp", CATT.t[4 + g, :, tb * 512:(tb + 1) * 512], y_[:], rd=[y_], wr=[CATT])
                P.barrier()
            with contextlib.ExitStack() as stt:
                cx = Ctx(P, nc, stt)
                stage_f = cx.T([128, 1024], F32)
                wtm = cx.T([128, 8, 1040], BF16)
                for k in range(8):
                    cx.dma("sp", stage_f[:], IN["ev_w_in"][k * 128:(k + 1) * 128, 1024:2048], wr=[stage_f])
                    cx.V(lambda h, k=k: h.tensor_copy(out=wtm[:, k, 0:1024], in_=stage_f[:]), rd=[stage_f], wr=[wtm])
                gst = cx.T([128, 8, 16], F32)
                cx.dma("sp", gst[:], IN["ev_w_in"][:, 2560:2576].rearrange("(k p) n -> p k n", p=128), wr=[gst])
                cx.V(lambda h: h.tensor_copy(out=wtm[:, :, 1024:1040], in_=gst[:]), rd=[gst], wr=[wtm])
                bvo = cx.T([128, 1024], F32)
                bg = cx.T([128, 16], F32)
                cx.dma("sp", bvo[:], IN["ev_b_in"][0:1, 1024:2048].broadcast_to([128, 1024]), wr=[bvo])
                cx.dma("sp", bg[:], IN["ev_b_in"][0:1, 2560:2576].broadcast_to([128, 16]), wr=[bg])
                pv = [cx.PS([128, 512]) for _ in range(2)]
                po = [cx.PS([128, 512]) for _ in range(2)]
                pg = [cx.PS([128, 16]) for _ in range(2)]
                v1 = [cx.T([128, 4, 130], BF16) for _ in range(2)]
                of = [cx.T([128, 512], F32) for _ in range(2)]
                ob = [cx.T([128, 512], BF16) for _ in range(2)]
                for b_ in v1:
                    cx.V(lambda h, b_=b_: h.memset(b_[:], 1.0), wr=[b_])
                for i in range(NT):
                    a = i % 2
                    for (p, c0_, c1_) in ((pv[a], 0, 512), (po[a], 512, 1024), (pg[a], 1024, 1040)):
                        for k in range(8):
                            cx.M(lambda h, k=k, p=p, c0_=c0_, c1_=c1_, i=i: h.matmul(p[:], lhsT=hT[:, k, i * 128:(i + 1) * 128], rhs=wtm[:, k, c0_:c1_], start=(k == 0), stop=(k == 7)),
                                 rd=[hT, wtm], wr=[p])
                    for hh in range(4):
                        cx.V(lambda h, a=a, hh=hh: h.tensor_tensor(out=v1[a][:, hh, 0:128], in0=pv[a][:, hh * 128:(hh + 1) * 128],
                                                                   in1=bvo[:, hh * 128:(hh + 1) * 128], op=ALU.add), rd=[pv[a], bvo], wr=[v1[a]])
                    cx.dma("sp", V1D.t[i], v1[a][:], rd=[v1[a]], wr=[V1D])
                    cx.V(lambda h, a=a: h.tensor_tensor(out=of[a][:], in0=po[a][:], in1=bvo[:, 512:1024], op=ALU.add), rd=[po[a], bvo], wr=[of[a]])
                    cx.A(lambda h, a=a: h.activation(out=ob[a][:], in_=of[a][:], func=AF.Sigmoid), rd=[of[a]], wr=[ob[a]])
                    if i == 0:
                        cx.dma("sp", SC["DBG2"].t, of[a][:], rd=[of[a]], wr=[SC["DBG2"]])
                    cx.dma("sp", SIGD.t[i], ob[a][:], rd=[ob[a]], wr=[SIGD])
                    cx.V(lambda h, a=a, i=i: h.tensor_tensor(out=GT[:, i, :], in0=pg[a][:], in1=bg[:], op=ALU.add), rd=[pg[a], bg], wr=[GT])
                cx.dma("sp", SC["DBG1"].t, GT[:], rd=[GT], wr=[SC["DBG1"]])
                P.barrier()
            with contextlib.ExitStack() as stg:
                cx = Ctx(P, nc, stg)
                LF = cx.T([128, NT, 8], F32)
                t8 = cx.T([128, NT, 8], F32)
                BC = cx.T([128, NT, 16], F32)
                cx.A(lambda h: h.activation(out=t8[:], in_=GT[:, :, 8:16], func=AF.Exp, scale=-1.0), rd=[GT], wr=[t8])
                cx.V(lambda h: h.tensor_scalar(out=t8[:], in0=t8[:], scalar1=1.0, scalar2=None, op0=ALU.add), rd=[t8], wr=[t8])
                cx.A(lambda h: h.activation(out=LF[:], in_=t8[:], func=AF.Ln), rd=[t8], wr=[LF])
                cx.V(lambda h: h.tensor_scalar(out=LF[:], in0=LF[:], scalar1=-1.0, scalar2=None, op0=ALU.mult), rd=[LF], wr=[LF])
                triU = cx.T([128, 128], F32)
                triL = cx.T([128, 128], F32)
                ones = cx.T([128, 128], F32)
                cx.dma("sp", triU[:], IN["triU"], wr=[triU])
                cx.dma("sp", triL[:], IN["triL"], wr=[triL])
                cx.V(lambda h: h.memset(ones[:], 1.0), wr=[ones])
                pc = cx.PS([128, NT, 16])
                for i in range(NT):
                    cx.M(lambda h, i=i: h.matmul(pc[:, i, 0:4], lhsT=triU[:], rhs=LF[:, i, 0:4], start=True, stop=True), rd=[triU, LF], wr=[pc])
                    cx.M(lambda h, i=i: h.matmul(pc[:, i, 4:8], lhsT=triL[:], rhs=LF[:, i, 4:8], start=True, stop=True), rd=[triL, LF], wr=[pc])
                    cx.M(lambda h, i=i: h.matmul(pc[:, i, 8:16], lhsT=ones[:], rhs=LF[:, i, 0:8], start=True, stop=True), rd=[ones, LF], wr=[pc])
                cx.V(lambda h: h.tensor_copy(out=BC[:], in_=pc[:]), rd=[pc], wr=[BC])
                cx.A(lambda h: h.activation(out=EB[:], in_=BC[:, :, 0:8], func=AF.Exp), rd=[BC], wr=[EB])
                cx.A(lambda h: h.activation(out=EE[:], in_=BC[:, :, 8:16], func=AF.Exp), rd=[BC], wr=[EE])
                cx.V(lambda h: h.tensor_tensor(out=t8[:], in0=GT[:, :, 0:8], in1=BC[:, :, 0:8], op=ALU.subtract), rd=[GT, BC], wr=[t8])
                cx.V(lambda h: h.tensor_scalar(out=t8[:], in0=t8[:], scalar1=float(-0.5 * np.log(128.0)), scalar2=None, op0=ALU.add), rd=[t8], wr=[t8])
                cx.A(lambda h: h.activation(out=ES[:], in_=t8[:], func=AF.Exp), rd=[t8], wr=[ES])
                P.barrier()
        with contextlib.ExitStack() as st3:
            cx = Ctx(P, nc, st3)
            mk = []
            for nm in ("triU", "triL"):
                f = cx.T([128, 128], F32)
                cx.dma("sp", f[:], IN[nm], wr=[f])
                mk.append(f)
            Cst = cx.T([128, 8, 129], F32)
            Cb = cx.T([128, 8, 129], BF16)
            cx.V(lambda h: h.memset(Cst[:], 0.0), wr=[Cst])
            cx.V(lambda h: h.memset(Cb[:], 0.0), wr=[Cb])
            v1 = [cx.T([128, 4, 130], BF16) for _ in range(3)]
            ps_s = [cx.PS([128, 128]) for _ in range(2)]
            ps_a = [cx.PS([128, 129]) for _ in range(2)]
            ps_t = cx.PS([128, 128], BF16)
            ps_d = cx.PS([128, 129])
            ps_h = cx.PS([128, 4, 128], BF16)
            AT = [cx.T([128, 128], BF16) for _ in range(2)]
            ksb = [cx.T([128, 128], BF16) for _ in range(2)]
            sm = [cx.T([128, 4], F32) for _ in range(2)]
            hcur = [cx.T([128, 4, 128], F32) for _ in range(2)]
            hfl = [cx.T([128, 512], F32) for _ in range(2)]
            sgl = [cx.T([128, 512], BF16) for _ in range(2)]
            sq = cx.T([128, 512], F32)
            st4 = [cx.T([128, 12], F32) for _ in range(2)]
            mg = cx.T([128, 512], F32)
            hmb = [cx.T([128, 512], BF16) for _ in range(2)]
            hmT = [cx.T([128, 4, 128], BF16) for _ in range(2)]
            cx.dma("sp", mg[:], IN["ev_mnorm_g"][0:1, :].broadcast_to([128, 512]), wr=[mg])
            nst = 0
            for d in range(2):
                order = range(NT) if d == 0 else range(NT - 1, -1, -1)
                for i in order:
                    vb = v1[nst % 3]
                    hc = hcur[nst % 2]
                    nst += 1
                    cx.dma("sp", vb[:], V1D.t[i], rd=[V1D], wr=[vb])
                    tsl = slice(i * 128, (i + 1) * 128)
                    for hh in range(4):
                        col = d * 4 + hh
                        a = (nst * 4 + hh) % 2
                        cx.M(lambda h, a=a, hh=hh, tsl=tsl: h.matmul(ps_s[a][:], lhsT=QK[:, 4 + hh, tsl], rhs=QK[:, hh, tsl], start=True, stop=True), rd=[QK], wr=[ps_s[a]])
                        cx.V(lambda h, a=a, i=i, col=col, d=d: h.scalar_tensor_tensor(out=AT[a][:], in0=ps_s[a][:], scalar=ES[:, i, col:col + 1], in1=mk[d][:], op0=ALU.mult, op1=ALU.mult),
                             rd=[ps_s[a], ES, mk[d]], wr=[AT[a]])
                        cx.M(lambda h, a=a, hh=hh, vb=vb: h.matmul(ps_a[a][:], lhsT=AT[a][:], rhs=vb[:, hh, 0:129], start=True, stop=False), rd=[AT[a], vb], wr=[ps_a[a]])
                        cx.M(lambda h, a=a, hh=hh, tsl=tsl, col=col: h.matmul(ps_a[a][:], lhsT=QK[:, hh, tsl], rhs=Cb[:, col, :], start=False, stop=True), rd=[QK, Cb], wr=[ps_a[a]])
                        cx.A(lambda h, a=a, i=i, col=col: h.activation(out=sm[a][:, 2:3], in_=ps_a[a][:, 128:129], func=AF.Abs, scale=EB[:, i, col:col + 1]),
                             rd=[ps_a[a], EB], wr=[sm[a]])
                        cx.V(lambda h, a=a: h.tensor_scalar(out=sm[a][:, 0:1], in0=sm[a][:, 2:3], scalar1=1.0, scalar2=None, op0=ALU.max), rd=[sm[a]], wr=[sm[a]])
                        cx.V(lambda h, a=a: h.reciprocal(out=sm[a][:, 3:4], in_=sm[a][:, 0:1]), rd=[sm[a]], wr=[sm[a]])
                        cx.V(lambda h, a=a, i=i, col=col: h.tensor_tensor(out=sm[a][:, 1:2], in0=EB[:, i, col:col + 1], in1=sm[a][:, 3:4], op=ALU.mult), rd=[sm[a], EB], wr=[sm[a]])
                        cx.A(lambda h, a=a, hh=hh, hc=hc: h.activation(out=hc[:, hh, :], in_=ps_a[a][:, 0:128], func=AF.Copy, scale=sm[a][:, 1:2]), rd=[ps_a[a], sm[a]], wr=[hc])
                        cx.M(lambda h, hh=hh, tsl=tsl: h.transpose(out=ps_t[:], in_=QK[:, 4 + hh, tsl], identity=idb[:]), rd=[QK, idb], wr=[ps_t])
                        cx.A(lambda h, a=a, i=i, col=col: h.activation(out=ksb[a][:], in_=ps_t[:], func=AF.Copy, scale=ES[:, i, col:col + 1]), rd=[ps_t, ES], wr=[ksb[a]])
                        cx.M(lambda h, a=a, hh=hh, vb=vb: h.matmul(ps_d[:], lhsT=ksb[a][:], rhs=vb[:, hh, 0:129], start=True, stop=True), rd=[ksb[a], vb], wr=[ps_d])
                        cx.V(lambda h, i=i, col=col: h.tensor_scalar(out=Cst[:, col, :], in0=Cst[:, col, :], scalar1=EE[:, i, col:col + 1], scalar2=None, op0=ALU.mult), rd=[Cst, EE], wr=[Cst])
                        cx.V(lambda h, i=i, col=col: h.scalar_tensor_tensor(out=Cst[:, col, :], in0=ps_d[:], scalar=EE[:, i, col:col + 1], in1=Cst[:, col, :], op0=ALU.mult, op1=ALU.add),
                             rd=[ps_d, EE, Cst], wr=[Cst])
                        cx.A(lambda h, col=col: h.activation(out=Cb[:, col, :], in_=Cst[:, col, :], func=AF.Copy), rd=[Cst], wr=[Cb])
                    if d == 0:
                        cx.dma("act", HFD.t[i], hc[:].rearrange("p h d -> p (h d)"), rd=[hc], wr=[HFD])
                    else:
                        a = i % 2
                        cx.dma("sp", hfl[a][:], HFD.t[i], rd=[HFD], wr=[hfl[a]])
                        cx.dma("sp", sgl[a][:], SIGD.t[i], rd=[SIGD], wr=[sgl[a]])
                        cx.V(lambda h, a=a, hc=hc: h.tensor_tensor(out=hfl[a][:], in0=hfl[a][:], in1=hc[:].rearrange("p h d -> p (h d)"), op=ALU.add), rd=[hfl[a], hc], wr=[hfl[a]])
                        cx.G(lambda h, a=a: h.tensor_tensor(out=sq[:], in0=hfl[a][:], in1=hfl[a][:], op=ALU.mult), rd=[hfl[a]], wr=[sq])
                        cx.V(lambda h, a=a: h.tensor_reduce(out=st4[a][:, 0:4], in_=sq[:].rearrange("p (h d) -> p h d", h=4), axis=AX.X, op=ALU.add), rd=[sq], wr=[st4[a]])
                        cx.A(lambda h, a=a: h.activation(out=st4[a][:, 4:8], in_=st4[a][:, 0:4], func=AF.Sqrt, scale=1.0 / 128, bias=eps[:, 0:1]), rd=[st4[a], eps], wr=[st4[a]])
                        cx.V(lambda h, a=a: h.reciprocal(out=st4[a][:, 8:12], in_=st4[a][:, 4:8]), rd=[st4[a]], wr=[st4[a]])
                        cx.V(lambda h, a=a: h.tensor_tensor(out=hfl[a][:].rearrange("p (h d) -> p h d", h=4), in0=hfl[a][:].rearrange("p (h d) -> p h d", h=4),
                                                            in1=st4[a][:, 8:12].unsqueeze(2).broadcast_to([128, 4, 128]), op=ALU.mult), rd=[hfl[a], st4[a]], wr=[hfl[a]])
                        cx.G(lambda h, a=a: h.tensor_tensor(out=sq[:], in0=mg[:], in1=sgl[a][:], op=ALU.mult), rd=[mg, sgl[a]], wr=[sq])
                        cx.V(lambda h, a=a: h.tensor_tensor(out=hmb[a][:], in0=hfl[a][:], in1=sq[:], op=ALU.mult), rd=[hfl[a], sq], wr=[hmb[a]])
                        for k in range(4):
                            cx.M(lambda h, a=a, k=k: h.transpose(out=ps_h[:, k, :], in_=hmb[a][:, k * 128:(k + 1) * 128], identity=idb[:]), rd=[hmb[a], idb], wr=[ps_h])
                        cx.A(lambda h, a=a: h.activation(out=hmT[a][:], in_=ps_h[:], func=AF.Copy), rd=[ps_h], wr=[hmT[a]])
                        cx.dma("act", CATT.t[0:4, :, i * 128:(i + 1) * 128].rearrange("c f t -> f c t"), hmT[a][:], rd=[hmT[a]], wr=[CATT])
            P.barrier()
        with contextlib.ExitStack() as st4_:
            cx = Ctx(P, nc, st4_)
            cb_ = [cx.T([128, 8, 128], BF16) for _ in range(3)]
            g1, = load_mods(cx, MODS, li, [2])
            stage_f = cx.T([128, 1024], F32)

            def src(i):
                b = cb_[i % 3]
                cx.dma("pool", b[:], CATT.t[:, :, i * 128:(i + 1) * 128].rearrange("c f t -> f c t"), rd=[CATT], wr=[b])
                return b, b
            emit_outproj(cx, Xin, Xout, src, IN["ev_w_out"], g1, stage_f)
            P.barrier()
        P.flush()


W_SPECS = {
    "ada_w": [2, 1024, 6144], "ada_b": [2, 6144], "norm_mix_g": [2, 1024], "norm_ffn_g": [2, 1024],
    "ev_w_in": [1024, 2576], "ev_b_in": [1, 2576], "ev_b_inT": [128, 20], "ev_conv_wT": [128, 8, 5], "ev_conv_bT": [128, 8],
    "ev_mnorm_g": [1, 512], "ev_pool_w": [4, 128, 128], "ev_pool_scale": [1, 512], "ev_w_out": [1024, 1024],
    "od_w_in": [1024, 1536], "od_qnorm_g": [1, 128], "od_knorm_g": [1, 128], "od_w_out": [1024, 1024],
    "peer_w_q": [2, 1024, 2048], "peer_keys": [2, 2, 128, 128], "peer_u": [2, 16384, 1024], "peer_v": [2, 16384, 1024],
    "final_g": [1, 1024],
    "ident": [128, 128], "triU": [128, 128], "triL": [128, 128], "pooledge": [1, 4, 32], "ropecs": [128, 2, NT, 64], "iota16": [1, 16],
    "cT": [128, 8],
}


def host_consts():
    cn = {}
    cn["ident"] = np.eye(128, dtype=np.float32)
    s_ = np.arange(128)
    cn["triU"] = (s_[:, None] <= s_[None, :]).astype(np.float32)
    cn["triL"] = (s_[:, None] >= s_[None, :]).astype(np.float32)
    pe = np.zeros((1, 4, 32), np.float32)
    for g, w in enumerate((2, 4, 8, 16)):
        for j in range(32):
            t = j if j < 16 else S - 32 + j
            lo = max(t - w // 2, 0)
            hi = min(t + w // 2, S)
            pe[0, g, j] = w / float(hi - lo)
    cn["pooledge"] = pe
    t = np.arange(S)
    r, c = t // 64, t % 64
    freqs = (10000.0 ** (-np.arange(0, 64, 2, dtype=np.float32) / 64.0)).astype(np.float32)
    ang = np.concatenate([r[:, None].astype(np.float32) * freqs, c[:, None].astype(np.float32) * freqs], axis=-1).astype(np.float32)
    cs = np.stack([np.cos(ang), np.sin(ang)], 0).astype(np.float32)
    cn["ropecs"] = np.ascontiguousarray(cs.reshape(2, NT, 128, 64).transpose(2, 0, 1, 3))
    cn["iota16"] = np.arange(16, dtype=np.float32)[None, :]
    return cn


DEBUG = [False]


def build(first, last):
    nc = bass.Bass("TRN2", target_bir_lowering=False)
    IK = "ExternalOutput" if DEBUG[0] else "Internal"
    IN = {k: nc.dram_tensor(k, v, F32, kind="ExternalInput").ap() for k, v in W_SPECS.items()}
    xin = nc.dram_tensor("xin", [S, D], F32, kind="ExternalInput").ap()
    out = nc.dram_tensor("out", [S, D], F32, kind="ExternalOutput").ap()
    X = {}
    for k in range(0, 5):
        if k == first - 1:
            X[k] = Buf(xin)
        elif k == last:
            X[k] = Buf(out)
        elif first <= k < last:
            X[k] = Buf(nc.dram_tensor("X%d" % k, [S, D], F32, kind="Internal").ap())
    SC = {
        "CATT": Buf(nc.dram_tensor("CATT", [8, 128, S], BF16, kind=IK).ap()),
        "V1D": Buf(nc.dram_tensor("V1D", [NT, 128, 4, 130], BF16, kind=IK).ap()),
        "SIGD": Buf(nc.dram_tensor("SIGD", [NT, 128, 512], BF16, kind=IK).ap()),
        "HFD": Buf(nc.dram_tensor("HFD", [NT, 128, 512], F32, kind=IK).ap()),
        "TAB": [Buf(nc.dram_tensor("TAB%d" % i, [16384, 2048], BF16, kind="Internal").ap()) for i in range(2)],
    }
    SC["DBG1"] = Buf(nc.dram_tensor("DBG1", [128, NT, 16], F32, kind=IK).ap())
    SC["DBG2"] = Buf(nc.dram_tensor("DBG2", [128, 512], F32, kind=IK).ap())
    MODS = Buf(nc.dram_tensor("MODS", [2, 6, 128, 1024], F32, kind=IK).ap())
    with contextlib.ExitStack() as st:
        P = Prog(nc, st)
        phase_mods(P, nc, IN, MODS)
        for ph in range(first, last + 1):
            if ph == 1:
                phase_even(P, nc, IN, MODS, 0, X[0], X[1], SC)
            elif ph == 2:
                phase_peer(P, nc, IN, MODS, 0, X[1], X[2], SC, final=False)
            elif ph == 3:
                phase_attn(P, nc, IN, MODS, 1, X[2], X[3], SC)
            elif ph == 4:
                phase_peer(P, nc, IN, MODS, 1, X[3], X[4], SC, final=True)
    return nc


def host_inputs(inputs):
    f = lambda a: np.ascontiguousarray(np.asarray(a, dtype=np.float32))
    sh = {}
    sh["ada_w"] = f(inputs["ada_w"]); sh["ada_b"] = f(inputs["ada_b"])
    sh["norm_mix_g"] = f(inputs["norm_mix_g"]); sh["norm_ffn_g"] = f(inputs["norm_ffn_g"])
    sh["ev_w_in"] = f(inputs["ev_w_in"][0]); sh["ev_b_in"] = f(inputs["ev_b_in"])
    sh["ev_b_inT"] = f(np.asarray(inputs["ev_b_in"])[0, :2560].reshape(20, 128).T)
    cw = np.asarray(inputs["ev_conv_w"])[0, :, 0, :]
    sh["ev_conv_wT"] = f(cw.T.reshape(8, 128, 5).transpose(1, 0, 2))
    sh["ev_conv_bT"] = f(np.asarray(inputs["ev_conv_b"])[0].reshape(8, 128).T)
    sh["ev_mnorm_g"] = f(inputs["ev_mnorm_g"]); sh["ev_pool_w"] = f(inputs["ev_pool_w"][0]); sh["ev_pool_scale"] = f(inputs["ev_pool_scale"])
    sh["ev_w_out"] = f(inputs["ev_w_out"][0])
    sh["od_w_in"] = f(inputs["od_w_in"][0]); sh["od_qnorm_g"] = f(inputs["od_qnorm_g"]); sh["od_knorm_g"] = f(inputs["od_knorm_g"])
    sh["od_w_out"] = f(inputs["od_w_out"][0])
    sh["peer_w_q"] = f(inputs["peer_w_q"]); sh["peer_keys"] = f(inputs["peer_keys"])
    sh["peer_u"] = f(inputs["peer_u"]); sh["peer_v"] = f(inputs["peer_v"])
    sh["final_g"] = f(np.asarray(inputs["final_g"])[None, :])
    sh.update(host_consts())
    return sh


def run_phases(inputs, first, last, xin_list, cores):
    nc = build(first, last)
    sh = host_inputs(inputs)
    c = np.asarray(inputs["c"], dtype=np.float32)
    in_maps = []
    for j, b in enumerate(cores):
        m = dict(sh)
        m["cT"] = np.ascontiguousarray(c[b].reshape(8, 128).T)
        m["xin"] = np.ascontiguousarray(xin_list[j], dtype=np.float32)
        in_maps.append(m)
    res = run_bass_kernel_spmd(nc, in_maps, core_ids=list(range(len(cores))))
    if DEBUG[0]:
        return res.results
    return [r["out"] for r in res.results]


def kernel(**inputs):
    x = np.asarray(inputs["x"], dtype=np.float32)
    outs = run_phases(inputs, 1, 4, [x[b] for b in range(8)], list(range(8)))
    return np.stack(outs, 0).astype(np.float32)


def phase_peer(P, nc, IN, MODS, li, Xin, Xout, SC, final):
    TAB = SC["TAB"][li]
    with contextlib.ExitStack() as st:
        cx = Ctx(P, nc, st)
        sf = [cx.T([128, 4, 1024], F32) for _ in range(3)]
        sb = [cx.T([128, 4, 1024], BF16) for _ in range(3)]
        n = 0
        for half, nm in enumerate(("peer_u", "peer_v")):
            for blk in range(32):
                f, b = sf[n % 3], sb[n % 3]
                rows = slice(blk * 512, (blk + 1) * 512)
                cx.dma("sp", f[:], IN[nm][li, rows, :].rearrange("(n p) d -> p n d", p=128), wr=[f])
                if n % 3 == 0:
                    cx.A(lambda h: h.activation(out=b[:], in_=f[:], func=AF.Copy), rd=[f], wr=[b])
                elif n % 3 == 1:
                    cx.V(lambda h: h.tensor_copy(out=b[:], in_=f[:]), rd=[f], wr=[b])
                else:
                    cx.G(lambda h: h.tensor_copy(out=b[:], in_=f[:]), rd=[f], wr=[b])
                cx.dma("act", TAB.t[rows, half * 1024:(half + 1) * 1024].rearrange("(n p) d -> p n d", p=128), b[:], rd=[b], wr=[TAB])
                n += 1
        P.barrier()
        P.flush()
    with contextlib.ExitStack() as st:
        cx = Ctx(P, nc, st)
        idf, idb, eps = load_consts(cx, IN)
        gs2, sh2, g2 = load_mods(cx, MODS, li, [3, 4, 5])
        io16 = cx.T([128, 16], F32)
        th16 = cx.T([128, 16], F32)
        cx.dma("sp", io16[:], IN["iota16"].broadcast_to([128, 16]), wr=[io16])
        cx.V(lambda h: h.tensor_scalar(out=th16[:], in0=io16[:], scalar1=16.0, scalar2=None, op0=ALU.mult), rd=[io16], wr=[th16])
        if final:
            fg = cx.T([128, 1024], F32)
            cx.dma("sp", fg[:], IN["final_g"].broadcast_to([128, 1024]), wr=[fg])
        wq = cx.T([128, 8, 2048], BF16)
        ptr = cx.PS([128, 8, 128], BF16)
        keysT = cx.T([128, 2, 128], BF16)
        with contextlib.ExitStack() as stw:
            cw_ = Ctx(P, nc, stw)
            stg = [cw_.T([128, 2048], F32) for _ in range(2)]
            for k in range(8):
                cw_.dma("sp", stg[k % 2][:], IN["peer_w_q"][li, k * 128:(k + 1) * 128, :], wr=[stg[k % 2]])
                cw_.V(lambda h: h.tensor_copy(out=wq[:, k, :], in_=stg[k % 2][:]), rd=[stg[k % 2]], wr=[wq])
            kf = cw_.T([128, 2, 128], F32)
            kb = cw_.T([128, 2, 128], BF16)
            cw_.dma("sp", kf[:], IN["peer_keys"][li].rearrange("t n c -> n t c"), wr=[kf])
            cw_.V(lambda h: h.tensor_copy(out=kb[:], in_=kf[:]), rd=[kf], wr=[kb])
            for t in range(2):
                cw_.M(lambda h: h.transpose(out=ptr[:, t, :], in_=kb[:, t, :], identity=idb[:]), rd=[kb, idb], wr=[ptr])
            cw_.A(lambda h: h.activation(out=keysT[:], in_=ptr[:, 0:2, :], func=AF.Copy), rd=[ptr], wr=[keysT])
            P.barrier()
        pq = [cx.PS([128, 512]) for _ in range(2)]
        psc = [cx.PS([128, 4, 128]) for _ in range(2)]
        po = cx.PS([128, 1024])
        xb = [cx.T([128, 1024], F32) for _ in range(2)]
        sq = cx.T([128, 1024], F32)
        hf = cx.T([128, 1024], F32)
        hb = cx.T([128, 1024], BF16)
        hTi = cx.T([128, 8, 128], BF16)
        st2 = cx.T([128, 4], F32)
        qb = cx.T([128, 2048], BF16)
        sqq = cx.T([128, 2048], F32)
        rs = cx.T([128, 48], F32)
        qT = cx.T([128, 16, 128], BF16)
        S1 = cx.T([128, 16, 128], F32)
        wk = [cx.T([128, 128], F32) for _ in range(2)]
        m = cx.T([128, 16, 16], F32)
        ix = cx.T([128, 16, 16], U32)
        ixf = cx.T([128, 16, 16], F32)
        CS = cx.T([128, 8, 256], F32)
        wk2 = [cx.T([128, 256], F32) for _ in range(2)]
        tops = cx.T([128, 8, 16], F32)
        pos = cx.T([128, 8, 16], U32)
        posf = cx.T([128, 8, 16], F32)
        af = cx.T([128, 8, 16], F32)
        bf_ = cx.T([128, 8, 16], F32)
        oh = cx.T([128, 8, 16, 16], F32)
        i12 = cx.T([128, 2, 128], F32)
        idxf = cx.T([128, 128], F32)
        idx = cx.T([128, 128], I32)
        ge = cx.T([128, 8, 16], F32)
        gsum = cx.T([128, 16], F32)
        gate = cx.T([128, 128], F32)
        act = cx.T([128, 128], F32)
        gl = cx.T([128, 128], F32)
        coef = cx.T([128, 128], F32)
        NG = 3
        Gb = [cx.T([128, 4, 2048], BF16) for _ in range(NG)]
        Gtok = [[Tok() for _ in range(4)] for _ in range(NG)]
        diag = [cx.T([128, 4, 128], BF16) for _ in range(2)]
        junk = cx.T([128, 1024], BF16)
        yb = [cx.T([128, 1024], F32) for _ in range(2)]
        sgn = 0
        for i in range(NT):
            x = xb[i % 2]
            cx.dma("sp", x[:], Xin.t[i * 128:(i + 1) * 128, :], rd=[Xin], wr=[x])
            emit_norm_tile(cx, x, gs2, sh2, hb, sq, st2, idb, ptr, hTi[:], hTi, hf=hf)
            for nb in range(4):
                p = pq[nb % 2]
                for k in range(8):
                    cx.M(lambda h: h.matmul(p[:], lhsT=hTi[:, k, :], rhs=wq[:, k, nb * 512:(nb + 1) * 512], start=(k == 0), stop=(k == 7)), rd=[hTi, wq], wr=[p])
                cx.A(lambda h: h.activation(out=qb[:, nb * 512:(nb + 1) * 512], in_=p[:], func=AF.Copy), rd=[p], wr=[qb])
            cx.V(lambda h: h.tensor_tensor(out=sqq[:], in0=qb[:], in1=qb[:], op=ALU.mult), rd=[qb], wr=[sqq])
            cx.V(lambda h: h.tensor_reduce(out=rs[:, 0:16], in_=sqq[:].rearrange("p (g c) -> p g c", g=16), axis=AX.X, op=ALU.add), rd=[sqq], wr=[rs])
            cx.A(lambda h: h.activation(out=rs[:, 16:32], in_=rs[:, 0:16], func=AF.Sqrt, scale=1.0 / 128, bias=eps[:, 0:1]), rd=[rs, eps], wr=[rs])
            cx.V(lambda h: h.reciprocal(out=rs[:, 32:48], in_=rs[:, 16:32]), rd=[rs], wr=[rs])
            for r in range(2):
                for j in range(8):
                    g_ = r * 8 + j
                    cx.M(lambda h: h.transpose(out=ptr[:, j, :], in_=qb[:, g_ * 128:(g_ + 1) * 128], identity=idb[:]), rd=[qb, idb], wr=[ptr])
                cx.A(lambda h: h.activation(out=qT[:, r * 8:(r + 1) * 8, :], in_=ptr[:], func=AF.Copy), rd=[ptr], wr=[qT])
            for r in range(4):
                ps_ = psc[r % 2]
                for j in range(4):
                    hp = r * 4 + j
                    cx.M(lambda h: h.matmul(ps_[:, j, :], lhsT=qT[:, hp, :], rhs=keysT[:, hp % 2, :], start=True, stop=True), rd=[qT, keysT], wr=[ps_])
                cx.V(lambda h: h.tensor_tensor(out=S1[:, r * 4:(r + 1) * 4, :], in0=ps_[:], in1=rs[:, 32 + r * 4:32 + (r + 1) * 4].unsqueeze(2).broadcast_to([128, 4, 128]), op=ALU.mult),
                     rd=[ps_, rs], wr=[S1])
            for hp in range(16):
                w_ = wk[hp % 2]
                cx.V(lambda h: h.max(out=m[:, hp, 0:8], in_=S1[:, hp, :]), rd=[S1], wr=[m])
                cx.V(lambda h: h.max_index(out=ix[:, hp, 0:8], in_max=m[:, hp, 0:8], in_values=S1[:, hp, :]), rd=[m, S1], wr=[ix])
                cx.V(lambda h: h.match_replace(out=w_[:], in_to_replace=m[:, hp, 0:8], in_values=S1[:, hp, :], imm_value=-1e30), rd=[m, S1], wr=[w_])
                cx.V(lambda h: h.max(out=m[:, hp, 8:16], in_=w_[:]), rd=[w_], wr=[m])
                cx.V(lambda h: h.max_index(out=ix[:, hp, 8:16], in_max=m[:, hp, 8:16], in_values=w_[:]), rd=[m, w_], wr=[ix])
            mv = m[:].rearrange("p (h t) k -> p h t k", t=2)
            cx.V(lambda h: h.tensor_tensor(out=CS[:].rearrange("p h (a b) -> p h a b", a=16), in0=mv[:, :, 0, :].unsqueeze(3).broadcast_to([128, 8, 16, 16]),
                                           in1=mv[:, :, 1, :].unsqueeze(2).broadcast_to([128, 8, 16, 16]), op=ALU.add), rd=[m], wr=[CS])
            for hh in range(8):
                w_ = wk2[hh % 2]
                cx.V(lambda h: h.max(out=tops[:, hh, 0:8], in_=CS[:, hh, :]), rd=[CS], wr=[tops])
                cx.V(lambda h: h.max_index(out=pos[:, hh, 0:8], in_max=tops[:, hh, 0:8], in_values=CS[:, hh, :]), rd=[tops, CS], wr=[pos])
                cx.V(lambda h: h.match_replace(out=w_[:], in_to_replace=tops[:, hh, 0:8], in_values=CS[:, hh, :], imm_value=-1e30), rd=[tops, CS], wr=[w_])
                cx.V(lambda h: h.max(out=tops[:, hh, 8:16], in_=w_[:]), rd=[w_], wr=[tops])
                cx.V(lambda h: h.max_index(out=pos[:, hh, 8:16], in_max=tops[:, hh, 8:16], in_values=w_[:]), rd=[tops, w_], wr=[pos])
            cx.V(lambda h: h.tensor_copy(out=posf[:], in_=pos[:]), rd=[pos], wr=[posf])
            cx.V(lambda h: h.tensor_copy(out=ixf[:], in_=ix[:]), rd=[ix], wr=[ixf])
            bc4 = lambda ap3: ap3.unsqueeze(3).broadcast_to([128, 8, 16, 16])
            io4 = io16[:].unsqueeze(1).unsqueeze(1).broadcast_to([128, 8, 16, 16])
            th4 = th16[:].unsqueeze(1).unsqueeze(1).broadcast_to([128, 8, 16, 16])
            cx.V(lambda h: h.tensor_tensor(out=oh[:], in0=bc4(posf[:]), in1=th4, op=ALU.is_ge), rd=[posf, th16], wr=[oh])
            cx.V(lambda h: h.tensor_reduce(out=af[:], in_=oh[:], axis=AX.X, op=ALU.add), rd=[oh], wr=[af])
            cx.V(lambda h: h.tensor_scalar(out=af[:], in0=af[:], scalar1=-1.0, scalar2=None, op0=ALU.add), rd=[af], wr=[af])
            cx.V(lambda h: h.scalar_tensor_tensor(out=bf_[:], in0=af[:], scalar=-16.0, in1=posf[:], op0=ALU.mult, op1=ALU.add), rd=[af, posf], wr=[bf_])
            ixv = ixf[:].rearrange("p (h t) k -> p h t k", t=2)
            for t, src in ((0, af), (1, bf_)):
                cx.V(lambda h: h.tensor_tensor(out=oh[:], in0=bc4(src[:]), in1=io4, op=ALU.is_equal), rd=[src, io16], wr=[oh])
                cx.V(lambda h: h.tensor_tensor(out=oh[:], in0=oh[:], in1=ixv[:, :, t, :].unsqueeze(2).broadcast_to([128, 8, 16, 16]), op=ALU.mult), rd=[oh, ixf], wr=[oh])
                cx.V(lambda h: h.tensor_reduce(out=i12[:, t, :].rearrange("p (h k) -> p h k", h=8), in_=oh[:], axis=AX.X, op=ALU.add), rd=[oh], wr=[i12])
            cx.V(lambda h: h.scalar_tensor_tensor(out=idxf[:], in0=i12[:, 0, :], scalar=128.0, in1=i12[:, 1, :], op0=ALU.mult, op1=ALU.add), rd=[i12], wr=[idxf])
            cx.V(lambda h: h.tensor_copy(out=idx[:], in_=idxf[:]), rd=[idxf], wr=[idx])
            cx.V(lambda h: h.tensor_tensor(out=ge[:], in0=tops[:], in1=tops[:, :, 0:1].broadcast_to([128, 8, 16]), op=ALU.subtract), rd=[tops], wr=[ge])
            cx.A(lambda h: h.activation(out=ge[:], in_=ge[:], func=AF.Exp), rd=[ge], wr=[ge])
            cx.V(lambda h: h.tensor_reduce(out=gsum[:, 0:8], in_=ge[:], axis=AX.X, op=ALU.add), rd=[ge], wr=[gsum])
            cx.V(lambda h: h.reciprocal(out=gsum[:, 8:16], in_=gsum[:, 0:8]), rd=[gsum], wr=[gsum])
            cx.V(lambda h: h.tensor_tensor(out=gate[:].rearrange("p (h k) -> p h k", h=8), in0=ge[:], in1=gsum[:, 8:16].unsqueeze(2).broadcast_to([128, 8, 16]), op=ALU.mult),
                 rd=[ge, gsum], wr=[gate])
            cx.V(lambda h: h.memset(act[:], 0.0), wr=[act])
            for sg in range(32):
                b_ = sgn % NG
                sgn += 1
                G_ = Gb[b_]
                dg = diag[sg % 2]
                for jj in range(4):
                    j = sg * 4 + jj
                    P.dma("pool", lambda h: h.indirect_dma_start(out=G_[:, jj, :], out_offset=None, in_=TAB.t,
                                                                 in_offset=bass.IndirectOffsetOnAxis(ap=idx[:, j:j + 1], axis=0)),
                          _ks([idx, TAB]), [Gtok[b_][jj]])
                for jj in range(4):
                    j = sg * 4 + jj
                    cx.V(lambda h: h.scalar_tensor_tensor(out=junk[:], in0=G_[:, jj, 0:1024], scalar=1.0, in1=hb[:], op0=ALU.mult, op1=ALU.mult, accum_out=act[:, j:j + 1]),
                         rd=[Gtok[b_][jj], hb, act], wr=[junk, act])
                cs = slice(sg * 4, (sg + 1) * 4)
                cx.A(lambda h: h.activation(out=gl[:, cs], in_=act[:, cs], func=AF.Gelu), rd=[act], wr=[gl])
                cx.V(lambda h: h.tensor_tensor(out=coef[:, cs], in0=gl[:, cs], in1=gate[:, cs], op=ALU.mult), rd=[gl, gate], wr=[coef])
                for jj in range(4):
                    j = sg * 4 + jj
                    cx.V(lambda h: h.tensor_scalar(out=dg[:, jj, :], in0=idf[:], scalar1=coef[:, j:j + 1], scalar2=None, op0=ALU.mult), rd=[idf, coef], wr=[dg])
                for jj in range(4):
                    j = sg * 4 + jj
                    for hv in range(2):
                        cx.M(lambda h: h.matmul(po[:, hv * 512:(hv + 1) * 512], lhsT=dg[:, jj, :], rhs=G_[:, jj, 1024 + hv * 512:1024 + (hv + 1) * 512],
                                                start=(j == 0), stop=(j == 127)), rd=[dg, Gtok[b_][jj]], wr=[po])
            y = yb[i % 2]
            cx.V(lambda h: h.tensor_tensor(out=y[:], in0=po[:], in1=g2[:], op=ALU.mult), rd=[po, g2], wr=[y])
            cx.V(lambda h: h.tensor_tensor(out=y[:], in0=y[:], in1=x[:], op=ALU.add), rd=[y, x], wr=[y])
            if final:
                cx.V(lambda h: h.memset(st2[:, 0:1], 0.0), wr=[st2])
                cx.A(lambda h: h.activation(out=sq[:], in_=y[:], func=AF.Square, accum_out=st2[:, 0:1]), rd=[y, st2], wr=[sq, st2])
                cx.A(lambda h: h.activation(out=st2[:, 1:2], in_=st2[:, 0:1], func=AF.Sqrt, scale=1.0 / D, bias=eps[:, 0:1]), rd=[st2, eps], wr=[st2])
                cx.V(lambda h: h.reciprocal(out=st2[:, 2:3], in_=st2[:, 1:2]), rd=[st2], wr=[st2])
                cx.V(lambda h: h.scalar_tensor_tensor(out=y[:], in0=y[:], scalar=st2[:, 2:3], in1=fg[:], op0=ALU.mult, op1=ALU.mult), rd=[y, st2, fg], wr=[y])
            cx.dma("sp", Xout.t[i * 128:(i + 1) * 128, :], y[:], rd=[y], wr=[Xout])
        P.barrier()
        P.flush()


def phase_attn(P, nc, IN, MODS, li, Xin, Xout, SC):
    with contextlib.ExitStack() as st0:
        c0 = Ctx(P, nc, st0)
        idf, idb, eps = load_consts(c0, IN)
        bigT = c0.T([128, 8, S], BF16)
        qT = c0.T([128, 8, S], BF16)
        kT = c0.T([128, 2, S], BF16)
        V1 = c0.T([128, NT, 2, 130], BF16)
        with contextlib.ExitStack() as st1:
            c1 = Ctx(P, nc, st1)
            gs1, sh1 = load_mods(c1, MODS, li, [0, 1])
            emit_hT(c1, Xin, gs1, sh1, idb, bigT)
            P.barrier()
        with contextlib.ExitStack() as st2:
            cx = Ctx(P, nc, st2)
            w = cx.T([128, 8, 1536], BF16)
            with contextlib.ExitStack() as stw:
                cw_ = Ctx(P, nc, stw)
                stg = [cw_.T([128, 1536], F32) for _ in range(2)]
                for k in range(8):
                    cw_.dma("sp", stg[k % 2][:], IN["od_w_in"][k * 128:(k + 1) * 128, :], wr=[stg[k % 2]])
                    cw_.V(lambda h: h.tensor_copy(out=w[:, k, :], in_=stg[k % 2][:]), rd=[stg[k % 2]], wr=[w])
                P.barrier()
            csb = [cx.T([128, 2, 64], F32) for _ in range(2)]
            gq = cx.T([128, 10, 128], F32)
            g1_ = cx.T([128, 128], F32)
            g2_ = cx.T([128, 128], F32)
            cx.dma("sp", g1_[:], IN["od_qnorm_g"].broadcast_to([128, 128]), wr=[g1_])
            cx.dma("sp", g2_[:], IN["od_knorm_g"].broadcast_to([128, 128]), wr=[g2_])
            cx.V(lambda h: h.tensor_scalar(out=g1_[:], in0=g1_[:], scalar1=float(128 ** -0.5), scalar2=None, op0=ALU.mult), rd=[g1_], wr=[g1_])
            cx.V(lambda h: h.tensor_copy(out=gq[:, 0:8, :], in_=g1_[:].unsqueeze(1).broadcast_to([128, 8, 128])), rd=[g1_], wr=[gq])
            cx.V(lambda h: h.tensor_copy(out=gq[:, 8:10, :], in_=g2_[:].unsqueeze(1).broadcast_to([128, 2, 128])), rd=[g2_], wr=[gq])
            cx.V(lambda h: h.memset(V1[:], 1.0), wr=[V1])
            pz = [cx.PS([128, 512]) for _ in range(3)]
            ptq = cx.PS([128, 8, 128], BF16)
            ptk = cx.PS([128, 2, 128], BF16)
            rs = cx.T([128, 32], F32)
            qn = cx.T([128, 10, 128], F32)
            qr = cx.T([128, 10, 128], BF16)
            t1 = cx.T([128, 10, 64], F32)
            t2 = cx.T([128, 10, 64], F32)
            for i in range(NT):
                tsl = slice(i * 128, (i + 1) * 128)
                for nb in range(3):
                    for k in range(8):
                        cx.M(lambda h: h.matmul(pz[nb][:], lhsT=bigT[:, k, tsl], rhs=w[:, k, nb * 512:(nb + 1) * 512], start=(k == 0), stop=(k == 7)), rd=[bigT, w], wr=[pz[nb]])
                cx.A(lambda h: h.activation(out=V1[:, i, :, 0:128], in_=pz[2][:, 256:512].rearrange("p (g d) -> p g d", g=2), func=AF.Copy), rd=[pz[2]], wr=[V1])
                cs = csb[i % 2]
                cx.dma("act", cs[:], IN["ropecs"][:, :, i, :], wr=[cs])
                zsrc = ((pz[0], 0, 4, 512), (pz[1], 4, 8, 512), (pz[2], 8, 10, 256))
                for (pp, g0, g1x, wd) in zsrc:
                    cx.A(lambda h: h.activation(out=qn[:, g0:g1x, :], in_=pp[:, 0:wd].rearrange("p (g d) -> p g d", d=128), func=AF.Square), rd=[pp], wr=[qn])
                cx.V(lambda h: h.tensor_reduce(out=rs[:, 0:10], in_=qn[:], axis=AX.X, op=ALU.add), rd=[qn], wr=[rs])
                cx.A(lambda h: h.activation(out=rs[:, 10:20], in_=rs[:, 0:10], func=AF.Sqrt, scale=1.0 / 128, bias=eps[:, 0:1]), rd=[rs, eps], wr=[rs])
                cx.V(lambda h: h.reciprocal(out=rs[:, 20:30], in_=rs[:, 10:20]), rd=[rs], wr=[rs])
                for (pp, g0, g1x, wd) in zsrc:
                    cx.V(lambda h: h.tensor_tensor(out=qn[:, g0:g1x, :], in0=pp[:, 0:wd].rearrange("p (g d) -> p g d", d=128),
                                                   in1=rs[:, 20 + g0:20 + g1x].unsqueeze(2).broadcast_to([128, g1x - g0, 128]), op=ALU.mult), rd=[pp, rs], wr=[qn])
                cx.V(lambda h: h.tensor_tensor(out=qn[:], in0=qn[:], in1=gq[:], op=ALU.mult), rd=[qn, gq], wr=[qn])
                qv = qn[:].rearrange("p g (d t) -> p g d t", t=2)
                qo = qr[:].rearrange("p g (d t) -> p g d t", t=2)
                cc = cs[:, 0, :].unsqueeze(1).broadcast_to([128, 10, 64])
                ss_ = cs[:, 1, :].unsqueeze(1).broadcast_to([128, 10, 64])
                cx.V(lambda h: h.tensor_tensor(out=t1[:], in0=qv[:, :, :, 0], in1=cc, op=ALU.mult), rd=[qn, cs], wr=[t1])
                cx.V(lambda h: h.tensor_tensor(out=t2[:], in0=qv[:, :, :, 1], in1=ss_, op=ALU.mult), rd=[qn, cs], wr=[t2])
                cx.V(lambda h: h.tensor_tensor(out=qo[:, :, :, 0], in0=t1[:], in1=t2[:], op=ALU.subtract), rd=[t1, t2], wr=[qr])
                cx.V(lambda h: h.tensor_tensor(out=t1[:], in0=qv[:, :, :, 0], in1=ss_, op=ALU.mult), rd=[qn, cs, qr], wr=[t1])
                cx.V(lambda h: h.tensor_tensor(out=t2[:], in0=qv[:, :, :, 1], in1=cc, op=ALU.mult), rd=[qn, cs, qr], wr=[t2])
                cx.V(lambda h: h.tensor_tensor(out=qo[:, :, :, 1], in0=t1[:], in1=t2[:], op=ALU.add), rd=[t1, t2], wr=[qr])
                for g_ in range(8):
                    cx.M(lambda h: h.transpose(out=ptq[:, g_, :], in_=qr[:, g_, :], identity=idb[:]), rd=[qr, idb], wr=[ptq])
                for g_ in range(2):
                    cx.M(lambda h: h.transpose(out=ptk[:, g_, :], in_=qr[:, 8 + g_, :], identity=idb[:]), rd=[qr, idb], wr=[ptk])
                cx.A(lambda h: h.activation(out=qT[:, :, tsl], in_=ptq[:], func=AF.Copy), rd=[ptq], wr=[qT])
                cx.A(lambda h: h.activation(out=kT[:, :, tsl], in_=ptk[:], func=AF.Copy), rd=[ptk], wr=[kT])
            P.barrier()
        import os as _os
        if _os.environ.get("ATT_STOP") == "2":
            P.barrier()
            P.flush()
            return
        with contextlib.ExitStack() as st3:
            cx = Ctx(P, nc, st3)
            pss = [cx.PS([128, 512]) for _ in range(2)]
            pacc = [cx.PS([128, 512]) for _ in range(4)]
            pto = cx.PS([128, 8, 128], BF16)
            pT = [cx.T([128, 512], BF16) for _ in range(3)]
            ao = [cx.T([128, 8, 128], BF16) for _ in range(4)]
            rinv = cx.T([128, 8], F32)
            nsc = 0
            nac = 0
            for qb_ in range(8):
                qsl = slice(qb_ * 512, (qb_ + 1) * 512)
                for hd_ in range(8):
                    g_ = hd_ // 4
                    accs = pacc
                    for sj in range(NT):
                        ps_ = pss[nsc % 2]
                        pt_ = pT[nsc % 3]
                        nsc += 1
                        cx.M(lambda h: h.matmul(ps_[:], lhsT=kT[:, g_, sj * 128:(sj + 1) * 128], rhs=qT[:, hd_, qsl], start=True, stop=True), rd=[kT, qT], wr=[ps_])
                        cx.A(lambda h: h.activation(out=pt_[:], in_=ps_[:], func=AF.Exp), rd=[ps_], wr=[pt_])
                        for qs in range(4):
                            cx.M(lambda h: h.matmul(accs[qs][:, 0:129], lhsT=pt_[:, qs * 128:(qs + 1) * 128], rhs=V1[:, sj, g_, 0:129], start=(sj == 0), stop=(sj == NT - 1)),
                                 rd=[pt_, V1], wr=[accs[qs]])
                    for qs in range(4):
                        a_ = accs[qs]
                        cx.V(lambda h: h.reciprocal(out=rinv[:, qs:qs + 1], in_=a_[:, 128:129]), rd=[a_], wr=[rinv])
                        cx.V(lambda h: h.tensor_scalar(out=ao[qs][:, hd_, :], in0=a_[:, 0:128], scalar1=rinv[:, qs:qs + 1], scalar2=None, op0=ALU.mult), rd=[a_, rinv], wr=[ao[qs]])
                for qs in range(4):
                    ti = qb_ * 4 + qs
                    for k in range(8):
                        cx.M(lambda h: h.transpose(out=pto[:, k, :], in_=ao[qs][:, k, :], identity=idb[:]), rd=[ao[qs], idb], wr=[pto])
                    cx.V(lambda h: h.tensor_copy(out=bigT[:, :, ti * 128:(ti + 1) * 128], in_=pto[:]), rd=[pto], wr=[bigT])
            P.barrier()
        if _os.environ.get("ATT_STOP") == "3":
            P.barrier()
            P.flush()
            return
        with contextlib.ExitStack() as st4:
            cx = Ctx(P, nc, st4)
            g1, = load_mods(cx, MODS, li, [2])
            stage_f = cx.T([128, 1024], F32)
            emit_outproj(cx, Xin, Xout, lambda i: (bigT[:, :, i * 128:(i + 1) * 128], bigT), IN["od_w_out"], g1, stage_f)
            P.barrier()
        P.flush()
```

```python
import contextlib
import numpy as np
import concourse.bass as bass
import concourse.mybir as mybir
from concourse.bass_utils import run_bass_kernel_spmd

F32 = mybir.dt.float32
BF16 = mybir.dt.bfloat16
I32 = mybir.dt.int32
U32 = mybir.dt.uint32
AF = mybir.ActivationFunctionType
ALU = mybir.AluOpType
AX = mybir.AxisListType

S = 4096
D = 1024
NT = S // 128
EPS = 1e-6


class Tok:
    __slots__ = ("w", "r", "name")

    def __init__(self, name=""):
        self.w = None
        self.r = {}
        self.name = name


class _Eng:
    def __init__(self, name, sem):
        self.name = name
        self.sem = sem
        self.cnt = 0
        self.seen = {}
        self.ops = []


NDMA = 6


class _Rec:
    def __getattr__(self, name):
        def f(*a, **k):
            return (name, a, k)
        return f


_REC = _Rec()


class Prog:
    ENG = ("pe", "act", "dve", "pool", "sp")

    def __init__(self, nc, stack):
        self.nc = nc
        self.stack = stack
        self.e = {}
        self.sems = {}
        for n in self.ENG:
            self.e[n] = _Eng(n, stack.enter_context(nc.semaphore("s_" + n)))
            self.sems[n] = (self.e[n].sem, 1)
        self.dslot = {}
        for q in ("sp", "pool", "act"):
            sl = []
            for j in range(NDMA):
                key = "d_%s%d" % (q, j)
                sem = stack.enter_context(nc.semaphore(key))
                self.sems[key] = (sem, 16)
                sl.append([key, 0])
            self.dslot[q] = [sl, 0]

    def _deps(self, e, rd, wr):
        deps = {}

        def need(dep, same_ok):
            if dep is None:
                return
            en, c = dep
            if en == e and (e == "pe" or not same_ok):
                return
            if deps.get(en, 0) < c:
                deps[en] = c

        for t in rd:
            need(t.w, True)
        for t in wr:
            need(t.w, False)
            for en, c in t.r.items():
                need((en, c), False)
        return deps

    def _emit_waits(self, E, deps):
        for en, c in deps.items():
            if E.seen.get(en, 0) < c:
                sem, step = self.sems[en]
                E.ops.append(lambda h, sem=sem, v=c * step: h.wait_ge(sem, v))
                E.seen[en] = c

    def op(self, e, fn, rd=(), wr=()):
        E = self.e[e]
        self._emit_waits(E, self._deps(e, rd, wr))
        E.cnt += 1
        sem = E.sem
        rec = fn(_REC)
        E.ops.append(lambda h, rec=rec, sem=sem: getattr(h, rec[0])(*rec[1], **rec[2]).then_inc(sem, 1))
        me = (e, E.cnt)
        for t in wr:
            t.w = me
            t.r = {}
        for t in rd:
            t.r[e] = E.cnt

    def dma(self, q, fn, rd=(), wr=()):
        E = self.e[q]
        slots, nxt = self.dslot[q]
        slot = slots[nxt % NDMA]
        self.dslot[q][1] = nxt + 1
        key = slot[0]
        deps = self._deps(key, rd, wr)
        if slot[1] > 0:
            deps[key] = max(deps.get(key, 0), slot[1])
        self._emit_waits(E, deps)
        slot[1] += 1
        sem = self.sems[key][0]
        rec = fn(_REC)
        E.ops.append(lambda h, rec=rec, sem=sem: getattr(h, rec[0])(*rec[1], **rec[2]).then_inc(sem, 16))
        me = (key, slot[1])
        for t in wr:
            t.w = me
            t.r = {}
        for t in rd:
            t.r[key] = slot[1]

    def barrier(self):
        tgt = {n: self.e[n].cnt for n in self.ENG}
        for q in self.dslot:
            for key, c in self.dslot[q][0]:
                tgt[key] = c
        for n in self.ENG:
            E = self.e[n]
            d = {k: v for k, v in tgt.items() if v > 0 and not (k == n and n == "pe")}
            self._emit_waits(E, d)

    def flush(self):
        nc = self.nc
        with nc.Block() as block:
            @block.tensor
            def _(h):
                for f in self.e["pe"].ops:
                    f(h)

            @block.scalar
            def _(h):
                for f in self.e["act"].ops:
                    f(h)

            @block.vector
            def _(h):
                for f in self.e["dve"].ops:
                    f(h)

            @block.gpsimd
            def _(h):
                for f in self.e["pool"].ops:
                    f(h)

            @block.sync
            def _(h):
                for f in self.e["sp"].ops:
                    f(h)
        for n in self.ENG:
            self.e[n].ops = []


class Buf:
    def __init__(self, t):
        self.t = t
        self.k = Tok()

    def __getitem__(self, i):
        return self.t[i]


def _ks(xs):
    return [x.k if isinstance(x, Buf) else x for x in xs]


_NAME = [0]


class Ctx:
    def __init__(self, P, nc, st):
        self.P, self.nc, self.st = P, nc, st
        self.n = 0

    def T(self, shape, dt, name=None):
        _NAME[0] += 1
        return Buf(self.st.enter_context(self.nc.sbuf_tensor("%s_%d" % (name or "t", _NAME[0]), list(shape), dt)))

    def PS(self, shape, dt=F32, name=None):
        _NAME[0] += 1
        return Buf(self.st.enter_context(self.nc.psum_tensor("%s_%d" % (name or "p", _NAME[0]), list(shape), dt)))

    def V(self, fn, rd=(), wr=()):
        self.P.op("dve", fn, _ks(rd), _ks(wr))

    def A(self, fn, rd=(), wr=()):
        self.P.op("act", fn, _ks(rd), _ks(wr))

    def G(self, fn, rd=(), wr=()):
        self.P.op("pool", fn, _ks(rd), _ks(wr))

    def M(self, fn, rd=(), wr=()):
        self.P.op("pe", fn, _ks(rd), _ks(wr))

    def dma(self, q, out, in_, rd=(), wr=()):
        self.P.dma(q, lambda h, out=out, in_=in_: h.dma_start(out=out, in_=in_), _ks(rd), _ks(wr))


def load_cast(cx, q, dst_bf, src_ap, stage, eng="pool"):
    cx.dma(q, stage.t[:] if not isinstance(stage, tuple) else stage[1], src_ap, wr=[stage if not isinstance(stage, tuple) else stage[0]])


def emit_norm_tile(cx, xt, gs, sh, hb, sq, st2, idb, ptr, hT_dst, hT_buf, hf=None):
    cx.V(lambda h: h.memset(st2[:, 0:1], 0.0), wr=[st2])
    cx.A(lambda h: h.activation(out=sq[:], in_=xt[:], func=AF.Square, accum_out=st2[:, 0:1]), rd=[xt, st2], wr=[sq, st2])
    cx.A(lambda h: h.activation(out=st2[:, 1:2], in_=st2[:, 0:1], func=AF.Sqrt, scale=1.0 / D, bias=EPS_AP[0][:, 0:1]), rd=[st2, EPS_AP[0]], wr=[st2])
    cx.V(lambda h: h.reciprocal(out=st2[:, 2:3], in_=st2[:, 1:2]), rd=[st2], wr=[st2])
    cx.V(lambda h: h.scalar_tensor_tensor(out=sq[:], in0=xt[:], scalar=st2[:, 2:3], in1=gs[:], op0=ALU.mult, op1=ALU.mult), rd=[xt, st2, gs], wr=[sq])
    if hf is not None:
        cx.V(lambda h: h.tensor_tensor(out=hf[:], in0=sq[:], in1=sh[:], op=ALU.add), rd=[sq, sh], wr=[hf])
        cx.G(lambda h: h.tensor_copy(out=hb[:], in_=hf[:]), rd=[hf], wr=[hb])
    else:
        cx.V(lambda h: h.tensor_tensor(out=hb[:], in0=sq[:], in1=sh[:], op=ALU.add), rd=[sq, sh], wr=[hb])
    for k in range(8):
        cx.M(lambda h, k=k: h.transpose(out=ptr[:, k, :], in_=hb[:, k * 128:(k + 1) * 128], identity=idb[:]), rd=[hb, idb], wr=[ptr])
    cx.A(lambda h: h.activation(out=hT_dst, in_=ptr[:], func=AF.Copy), rd=[ptr], wr=[hT_buf])


EPS_AP = [None]


def load_consts(cx, CN):
    idf = cx.T([128, 128], F32)
    idb = cx.T([128, 128], BF16)
    eps = cx.T([128, 1], F32)
    cx.dma("sp", idf[:], CN["ident"], wr=[idf])
    cx.V(lambda h: h.tensor_copy(out=idb[:], in_=idf[:]), rd=[idf], wr=[idb])
    cx.V(lambda h: h.memset(eps[:], EPS), wr=[eps])
    EPS_AP[0] = eps
    return idf, idb, eps


def phase_mods(P, nc, IN, MODS):
    with contextlib.ExitStack() as st:
        cx = Ctx(P, nc, st)
        cT = cx.T([128, 8], F32)
        cond = cx.T([128, 8], F32)
        crep = cx.T([128, 8, 128], F32)
        cx.dma("sp", cT[:], IN["cT"], wr=[cT])
        cx.A(lambda h: h.activation(out=cond[:], in_=cT[:], func=AF.Silu), rd=[cT], wr=[cond])
        cx.V(lambda h: h.tensor_copy(out=crep[:], in_=cond[:].unsqueeze(2).broadcast_to([128, 8, 128])), rd=[cond], wr=[crep])
        wb = [cx.T([128, 8, 512], F32) for _ in range(2)]
        ps = [cx.PS([128, 512]) for _ in range(2)]
        mod = cx.T([128, 6144], F32)
        ab = cx.T([128, 6144], F32)
        gm = cx.T([128, 1024], F32)
        gf = cx.T([128, 1024], F32)
        n = 0
        for i in range(2):
            cx.dma("act", ab[:], IN["ada_b"][i:i + 1, :].broadcast_to([128, 6144]), wr=[ab])
            cx.dma("act", gm[:], IN["norm_mix_g"][i:i + 1, :].broadcast_to([128, 1024]), wr=[gm])
            cx.dma("act", gf[:], IN["norm_ffn_g"][i:i + 1, :].broadcast_to([128, 1024]), wr=[gf])
            for nb in range(12):
                w = wb[n % 2]
                p = ps[n % 2]
                n += 1
                cx.dma("sp" if nb % 2 == 0 else "pool", w[:], IN["ada_w"][i, :, nb * 512:(nb + 1) * 512].rearrange("(k p) n -> p k n", p=128), wr=[w])
                for k in range(8):
                    cx.M(lambda h, k=k, w=w, p=p: h.matmul(p[:], lhsT=crep[:, k, :], rhs=w[:, k, :], start=(k == 0), stop=(k == 7)), rd=[crep, w], wr=[p])
                cx.V(lambda h, p=p, nb=nb: h.tensor_tensor(out=mod[:, nb * 512:(nb + 1) * 512], in0=p[:], in1=ab[:, nb * 512:(nb + 1) * 512], op=ALU.add), rd=[p, ab], wr=[mod])
            cx.V(lambda h: h.scalar_tensor_tensor(out=mod[:, 1024:2048], in0=mod[:, 1024:2048], scalar=1.0, in1=gm[:], op0=ALU.add, op1=ALU.mult), rd=[mod, gm], wr=[mod])
            cx.V(lambda h: h.scalar_tensor_tensor(out=mod[:, 4096:5120], in0=mod[:, 4096:5120], scalar=1.0, in1=gf[:], op0=ALU.add, op1=ALU.mult), rd=[mod, gf], wr=[mod])
            for j, off in enumerate([1024, 0, 2048, 4096, 3072, 5120]):
                cx.dma("sp", MODS.t[i, j], mod[:, off:off + 1024], rd=[mod], wr=[MODS])
        P.barrier()
        P.flush()


def load_mods(cx, MODS, i, js, q="act"):
    out = []
    for j in js:
        b = cx.T([128, 1024], F32)
        cx.dma(q, b[:], MODS.t[i, j], rd=[MODS], wr=[b])
        out.append(b)
    return out


def emit_hT(cx, Xin, gs, sh, idb, hT):
    xb = [cx.T([128, 1024], F32) for _ in range(2)]
    sq = cx.T([128, 1024], F32)
    hb = [cx.T([128, 1024], BF16) for _ in range(2)]
    st2 = [cx.T([128, 4], F32) for _ in range(2)]
    ptr = [cx.PS([128, 8, 128], BF16) for _ in range(2)]
    for i in range(NT):
        x = xb[i % 2]
        cx.dma("sp", x[:], Xin.t[i * 128:(i + 1) * 128, :], rd=[Xin], wr=[x])
        emit_norm_tile(cx, x, gs, sh, hb[i % 2], sq, st2[i % 2], idb, ptr[i % 2], hT[:, :, i * 128:(i + 1) * 128], hT)


def emit_outproj(cx, Xin, Xout, catT_src, w_ap, g1, stage_f, final=None):
    wob = cx.T([128, 8, 1024], BF16)
    for k in range(8):
        cx.dma("sp", stage_f[:], w_ap[k * 128:(k + 1) * 128, :], wr=[stage_f])
        cx.G(lambda h, k=k: h.tensor_copy(out=wob[:, k, :], in_=stage_f[:]), rd=[stage_f], wr=[wob])
    py = [cx.PS([128, 1024]) for _ in range(2)]
    xb = [cx.T([128, 1024], F32) for _ in range(2)]
    yb = [cx.T([128, 1024], F32) for _ in range(2)]
    for i in range(NT):
        ap, tok = catT_src(i)
        p = py[i % 2]
        x = xb[i % 2]
        y = yb[i % 2]
        cx.dma("act", x[:], Xin.t[i * 128:(i + 1) * 128, :], rd=[Xin], wr=[x])
        for nb in range(2):
            for k in range(8):
                cx.M(lambda h, k=k, nb=nb, p=p, ap=ap: h.matmul(p[:, nb * 512:(nb + 1) * 512], lhsT=ap[:, k, :], rhs=wob[:, k, nb * 512:(nb + 1) * 512],
                                                               start=(k == 0), stop=(k == 7)), rd=[tok, wob], wr=[p])
        cx.V(lambda h, p=p, y=y: h.tensor_tensor(out=y[:], in0=p[:], in1=g1[:], op=ALU.mult), rd=[p, g1], wr=[y])
        cx.G(lambda h, x=x, y=y: h.tensor_tensor(out=y[:], in0=y[:], in1=x[:], op=ALU.add), rd=[y, x], wr=[y])
        cx.dma("sp", Xout.t[i * 128:(i + 1) * 128, :], y[:], rd=[y], wr=[Xout])


def phase_even(P, nc, IN, MODS, li, Xin, Xout, SC):
    CATT, V1D, SIGD, HFD = SC["CATT"], SC["V1D"], SC["SIGD"], SC["HFD"]
    with contextlib.ExitStack() as st0:
        c0 = Ctx(P, nc, st0)
        idf, idb, eps = load_consts(c0, IN)
        QK = c0.T([128, 8, S], BF16)
        GT = c0.T([128, NT, 16], F32)
        EB = c0.T([128, NT, 8], F32)
        ES = c0.T([128, NT, 8], F32)
        EE = c0.T([128, NT, 8], F32)
        with contextlib.ExitStack() as st1:
            cx1 = Ctx(P, nc, st1)
            hT = cx1.T([128, 8, S], BF16)
            with contextlib.ExitStack() as st2:
                c2 = Ctx(P, nc, st2)
                gs1, sh1 = load_mods(c2, MODS, li, [0, 1])
                emit_hT(c2, Xin, gs1, sh1, idb, hT)
                P.barrier()
            with contextlib.ExitStack() as stf:
                cx = Ctx(P, nc, stf)
                bT = cx.T([128, 20], F32)
                cw = cx.T([128, 8, 5], F32)
                cb = cx.T([128, 8], F32)
                edge = cx.T([128, 4, 32], F32)
                cx.dma("sp", bT[:], IN["ev_b_inT"], wr=[bT])
                cx.dma("sp", cw[:], IN["ev_conv_wT"], wr=[cw])
                cx.dma("sp", cb[:], IN["ev_conv_bT"], wr=[cb])
                cx.dma("sp", edge[:], IN["pooledge"].broadcast_to([128, 4, 32]), wr=[edge])
                wst = [cx.T([128, 8, 128], F32) for _ in range(2)]
                wcb = [cx.T([128, 8, 128], BF16) for _ in range(2)]
                zc = cx.T([128, S + 16], F32)
                pa = cx.T([128, S + 16], F32)
                yb = cx.T([128, S + 16], F32)
                ybf = cx.T([128, S], BF16)
                yo = [cx.T([128, 512], BF16) for _ in range(2)]
                pw = cx.T([128, 128], F32)
                psc = cx.T([128, 128], F32)
                pwb = cx.T([128, 128], BF16)
                pz = [cx.PS([128, 512]) for _ in range(2)]
                cx.V(lambda h: h.memset(zc[:], 0.0), wr=[zc])
                nps = 0
                for c in range(12):
                    col0 = c * 128 if c < 8 else 2048 + (c - 8) * 128
                    bcol = c if c < 8 else 16 + (c - 8)
                    ws, wc = wst[c % 2], wcb[c % 2]
                    cx.dma("sp", ws[:], IN["ev_w_in"][:, col0:col0 + 128].rearrange("(k p) n -> p k n", p=128), wr=[ws])
                    cx.G(lambda h, ws=ws, wc=wc: h.tensor_copy(out=wc[:], in_=ws[:]), rd=[ws], wr=[wc])
                    for tb in range(8):
                        p = pz[nps % 2]
                        nps += 1
                        for k in range(8):
                            cx.M(lambda h, k=k, p=p, wc=wc, tb=tb: h.matmul(p[:], lhsT=wc[:, k, :], rhs=hT[:, k, tb * 512:(tb + 1) * 512], start=(k == 0), stop=(k == 7)),
                                 rd=[wc, hT], wr=[p])
                        cx.A(lambda h, p=p, tb=tb, bcol=bcol: h.activation(out=zc[:, 8 + tb * 512:8 + (tb + 1) * 512], in_=p[:], func=AF.Identity, bias=bT[:, bcol:bcol + 1]),
                             rd=[p, bT], wr=[zc])
                    if c < 8:
                        cx.V(lambda h, c=c: h.tensor_scalar(out=yb[:, 0:S], in0=zc[:, 6:6 + S], scalar1=cw[:, c, 0:1], scalar2=None, op0=ALU.mult), rd=[zc, cw], wr=[yb])
                        for j in range(1, 5):
                            cx.V(lambda h, c=c, j=j: h.scalar_tensor_tensor(out=yb[:, 0:S], in0=zc[:, 6 + j:6 + j + S], scalar=cw[:, c, j:j + 1], in1=yb[:, 0:S], op0=ALU.mult, op1=ALU.add),
                                 rd=[zc, cw, yb], wr=[yb])
                        cx.A(lambda h, c=c: h.activation(out=QK[:, c, :], in_=yb[:, 0:S], func=AF.Silu, bias=cb[:, c:c + 1]), rd=[yb, cb], wr=[QK])
                    else:
                        g = c - 8
                        win = (2, 4, 8, 16)[g]
                        half = win // 2
                        n_el = S + 15
                        cur = zc
                        bufs = [pa, yb]
                        bi = 0
                        step = 1
                        while step < win:
                            d = bufs[bi % 2]
                            bi += 1
                            cx.V(lambda h, cur=cur, d=d, step=step, n_el=n_el: h.tensor_tensor(out=d[:, 0:n_el - step + 1], in0=cur[:, 0:n_el - step + 1], in1=cur[:, step:n_el + 1], op=ALU.add),
                                 rd=[cur], wr=[d])
                            n_el = n_el - step
                            cur = d
                            step *= 2
                        o = bufs[bi % 2]
                        cx.V(lambda h, cur=cur, half=half, o=o, win=win: h.tensor_scalar(out=o[:, 0:S], in0=cur[:, 8 - half:8 - half + S], scalar1=1.0 / win, scalar2=None, op0=ALU.mult), rd=[cur], wr=[o])
                        cx.V(lambda h, o=o, g=g: h.tensor_tensor(out=o[:, 0:16], in0=o[:, 0:16], in1=edge[:, g, 0:16], op=ALU.mult), rd=[o, edge], wr=[o])
                        cx.V(lambda h, o=o, g=g: h.tensor_tensor(out=o[:, S - 16:S], in0=o[:, S - 16:S], in1=edge[:, g, 16:32], op=ALU.mult), rd=[o, edge], wr=[o])
                        cx.V(lambda h, o=o: h.tensor_tensor(out=ybf[:], in0=o[:, 0:S], in1=zc[:, 8:8 + S], op=ALU.subtract), rd=[o, zc], wr=[ybf])
                        cx.dma("sp", pw[:], IN["ev_pool_w"][g], wr=[pw])
                        cx.dma("sp", psc[:], IN["ev_pool_scale"][0:1, g * 128:(g + 1) * 128].broadcast_to([128, 128]), wr=[psc])
                        cx.V(lambda h: h.tensor_tensor(out=pwb[:], in0=pw[:], in1=psc[:], op=ALU.mult), rd=[pw, psc], wr=[pwb])
                        for tb in range(8):
                            p = pz[nps % 2]
                            y_ = yo[nps % 2]
                            nps += 1
                            cx.M(lambda h, p=p, tb=tb: h.matmul(p[:], lhsT=pwb[:], rhs=ybf[:, tb * 512:(tb + 1) * 512], start=True, stop=True), rd=[pwb, ybf], wr=[p])
                            cx.A(lambda h, p=p, y_=y_: h.activation(out=y_[:], in_=p[:], func=AF.Copy), rd=[p], wr=[y_])
                            cx.dma("sp", CATT.t[4 + g, :, tb * 512:(tb + 1) * 512], y_[:], rd=[y_], wr=[CATT])
                P.barrier()
            with contextlib.ExitStack() as stt:
                cx = Ctx(P, nc, stt)
                stage_f = cx.T([128, 1024], F32)
                wtm = cx.T([128, 8, 1040], BF16)
                for k in range(8):
                    cx.dma("sp", stage_f[:], IN["ev_w_in"][k * 128:(k + 1) * 128, 1024:2048], wr=[stage_f])
                    cx.V(lambda h, k=k: h.tensor_copy(out=wtm[:, k, 0:1024], in_=stage_f[:]), rd=[stage_f], wr=[wtm])
                gst = cx.T([128, 8, 16], F32)
                cx.dma("sp", gst[:], IN["ev_w_in"][:, 2560:2576].rearrange("(k p) n -> p k n", p=128), wr=[gst])
                cx.V(lambda h: h.tensor_copy(out=wtm[:, :, 1024:1040], in_=gst[:]), rd=[gst], wr=[wtm])
                bvo = cx.T([128, 1024], F32)
                bg = cx.T([128, 16], F32)
                cx.dma("sp", bvo[:], IN["ev_b_in"][0:1, 1024:2048].broadcast_to([128, 1024]), wr=[bvo])
                cx.dma("sp", bg[:], IN["ev_b_in"][0:1, 2560:2576].broadcast_to([128, 16]), wr=[bg])
                pv = [cx.PS([128, 512]) for _ in range(2)]
                po = [cx.PS([128, 512]) for _ in range(2)]
                pg = [cx.PS([128, 16]) for _ in range(2)]
                v1 = [cx.T([128, 4, 130], BF16) for _ in range(2)]
                of = [cx.T([128, 512], F32) for _ in range(2)]
                ob = [cx.T([128, 512], BF16) for _ in range(2)]
                for b_ in v1:
                    cx.V(lambda h, b_=b_: h.memset(b_[:], 1.0), wr=[b_])
                for i in range(NT):
                    a = i % 2
                    for (p, c0_, c1_) in ((pv[a], 0, 512), (po[a], 512, 1024), (pg[a], 1024, 1040)):
                        for k in range(8):
                            cx.M(lambda h, k=k, p=p, c0_=c0_, c1_=c1_, i=i: h.matmul(p[:], lhsT=hT[:, k, i * 128:(i + 1) * 128], rhs=wtm[:, k, c0_:c1_], start=(k == 0), stop=(k == 7)),
                                 rd=[hT, wtm], wr=[p])
                    for hh in range(4):
                        cx.V(lambda h, a=a, hh=hh: h.tensor_tensor(out=v1[a][:, hh, 0:128], in0=pv[a][:, hh * 128:(hh + 1) * 128],
                                                                   in1=bvo[:, hh * 128:(hh + 1) * 128], op=ALU.add), rd=[pv[a], bvo], wr=[v1[a]])
                    cx.dma("sp", V1D.t[i], v1[a][:], rd=[v1[a]], wr=[V1D])
                    cx.V(lambda h, a=a: h.tensor_tensor(out=of[a][:], in0=po[a][:], in1=bvo[:, 512:1024], op=ALU.add), rd=[po[a], bvo], wr=[of[a]])
                    cx.A(lambda h, a=a: h.activation(out=ob[a][:], in_=of[a][:], func=AF.Sigmoid), rd=[of[a]], wr=[ob[a]])
                    if i == 0:
                        cx.dma("sp", SC["DBG2"].t, of[a][:], rd=[of[a]], wr=[SC["DBG2"]])
                    cx.dma("sp", SIGD.t[i], ob[a][:], rd=[ob[a]], wr=[SIGD])
                    cx.V(lambda h, a=a, i=i: h.tensor_tensor(out=GT[:, i, :], in0=pg[a][:], in1=bg[:], op=ALU.add), rd=[pg[a], bg], wr=[GT])
                cx.dma("sp", SC["DBG1"].t, GT[:], rd=[GT], wr=[SC["DBG1"]])
                P.barrier()
            with contextlib.ExitStack() as stg:
                cx = Ctx(P, nc, stg)
                LF = cx.T([128, NT, 8], F32)
                t8 = cx.T([128, NT, 8], F32)
                BC = cx.T([128, NT, 16], F32)
                cx.A(lambda h: h.activation(out=t8[:], in_=GT[:, :, 8:16], func=AF.Exp, scale=-1.0), rd=[GT], wr=[t8])
                cx.V(lambda h: h.tensor_scalar(out=t8[:], in0=t8[:], scalar1=1.0, scalar2=None, op0=ALU.add), rd=[t8], wr=[t8])
                cx.A(lambda h: h.activation(out=LF[:], in_=t8[:], func=AF.Ln), rd=[t8], wr=[LF])
                cx.V(lambda h: h.tensor_scalar(out=LF[:], in0=LF[:], scalar1=-1.0, scalar2=None, op0=ALU.mult), rd=[LF], wr=[LF])
                triU = cx.T([128, 128], F32)
                triL = cx.T([128, 128], F32)
                ones = cx.T([128, 128], F32)
                cx.dma("sp", triU[:], IN["triU"], wr=[triU])
                cx.dma("sp", triL[:], IN["triL"], wr=[triL])
                cx.V(lambda h: h.memset(ones[:], 1.0), wr=[ones])
                pc = cx.PS([128, NT, 16])
                for i in range(NT):
                    cx.M(lambda h, i=i: h.matmul(pc[:, i, 0:4], lhsT=triU[:], rhs=LF[:, i, 0:4], start=True, stop=True), rd=[triU, LF], wr=[pc])
                    cx.M(lambda h, i=i: h.matmul(pc[:, i, 4:8], lhsT=triL[:], rhs=LF[:, i, 4:8], start=True, stop=True), rd=[triL, LF], wr=[pc])
                    cx.M(lambda h, i=i: h.matmul(pc[:, i, 8:16], lhsT=ones[:], rhs=LF[:, i, 0:8], start=True, stop=True), rd=[ones, LF], wr=[pc])
                cx.V(lambda h: h.tensor_copy(out=BC[:], in_=pc[:]), rd=[pc], wr=[BC])
                cx.A(lambda h: h.activation(out=EB[:], in_=BC[:, :, 0:8], func=AF.Exp), rd=[BC], wr=[EB])
                cx.A(lambda h: h.activation(out=EE[:], in_=BC[:, :, 8:16], func=AF.Exp), rd=[BC], wr=[EE])
                cx.V(lambda h: h.tensor_tensor(out=t8[:], in0=GT[:, :, 0:8], in1=BC[:, :, 0:8], op=ALU.subtract), rd=[GT, BC], wr=[t8])
                cx.V(lambda h: h.tensor_scalar(out=t8[:], in0=t8[:], scalar1=float(-0.5 * np.log(128.0)), scalar2=None, op0=ALU.add), rd=[t8], wr=[t8])
                cx.A(lambda h: h.activation(out=ES[:], in_=t8[:], func=AF.Exp), rd=[t8], wr=[ES])
                P.barrier()
        with contextlib.ExitStack() as st3:
            cx = Ctx(P, nc, st3)
            mk = []
            for nm in ("triU", "triL"):
                f = cx.T([128, 128], F32)
                cx.dma("sp", f[:], IN[nm], wr=[f])
                mk.append(f)
            Cst = cx.T([128, 8, 129], F32)
            Cb = cx.T([128, 8, 129], BF16)
            cx.V(lambda h: h.memset(Cst[:], 0.0), wr=[Cst])
            cx.V(lambda h: h.memset(Cb[:], 0.0), wr=[Cb])
            NV = 6
            v1 = [cx.T([128, 4, 130], BF16) for _ in range(NV)]
            psS = [cx.PS([128, 4, 128]) for _ in range(2)]
            psA = [cx.PS([128, 3, 129]) for _ in range(3)]
            ps_t = cx.PS([128, 8, 128], BF16)
            ps_h = cx.PS([128, 4, 128], BF16)
            AT = cx.T([128, 8, 128], BF16)
            ksb = cx.T([128, 8, 128], BF16)
            sm = cx.T([128, 8, 4], F32)
            hacc = cx.T([128, NT, 512], F32)
            NSG = 6
            sgl = [cx.T([128, 512], BF16) for _ in range(NSG)]
            sq = cx.T([128, 512], F32)
            st4 = [cx.T([128, 12], F32) for _ in range(2)]
            mg = cx.T([128, 512], F32)
            hmb = [cx.T([128, 512], BF16) for _ in range(2)]
            hmT = [cx.T([128, 4, 128], BF16) for _ in range(2)]
            cx.dma("sp", mg[:], IN["ev_mnorm_g"][0:1, :].broadcast_to([128, 512]), wr=[mg])
            bg = SC.get("BG0")
            if bg is not None:
                bg.attach(cx)
            chains = [(d, hh) for d in range(2) for hh in range(4)]

            def aslot(c):
                return psA[c // 3], c % 3

            def tile_of(s, d):
                return s if d == 0 else NT - 1 - s

            PDV = 2
            nfin = 0
            for s in range(NT + PDV):
                if s < NT:
                    for d in range(2):
                        vb = v1[(2 * s + d) % NV]
                        cx.dma("sp", vb[:], V1D.t[tile_of(s, d)], rd=[V1D], wr=[vb])
                    if s >= NT // 2:
                        for d in range(2):
                            sg_ = sgl[(2 * s + d) % NSG]
                            cx.dma("act", sg_[:], SIGD.t[tile_of(s, d)], rd=[SIGD], wr=[sg_])
                s_ = s - PDV
                if s_ < 0:
                    continue
                s = s_
                if bg is not None:
                    bg.step(4)
                vbs = [v1[(2 * s + d) % NV] for d in range(2)]
                tls = [tile_of(s, d) for d in range(2)]
                for c, (d, hh) in enumerate(chains):
                    tsl = slice(tls[d] * 128, (tls[d] + 1) * 128)
                    cx.M(lambda h: h.matmul(psS[d][:, hh, :], lhsT=QK[:, 4 + hh, tsl], rhs=QK[:, hh, tsl], start=True, stop=True), rd=[QK], wr=[psS[d]])
                for c, (d, hh) in enumerate(chains):
                    col = d * 4 + hh
                    cx.V(lambda h: h.scalar_tensor_tensor(out=AT[:, c, :], in0=psS[d][:, hh, :], scalar=ES[:, tls[d], col:col + 1], in1=mk[d][:], op0=ALU.mult, op1=ALU.mult),
                         rd=[psS[d], ES, mk[d]], wr=[AT])
                for c, (d, hh) in enumerate(chains):
                    col = d * 4 + hh
                    tsl = slice(tls[d] * 128, (tls[d] + 1) * 128)
                    pa, sl = aslot(c)
                    cx.M(lambda h: h.matmul(pa[:, sl, :], lhsT=AT[:, c, :], rhs=vbs[d][:, hh, 0:129], start=True, stop=False), rd=[AT, vbs[d]], wr=[pa])
                    cx.M(lambda h: h.matmul(pa[:, sl, :], lhsT=QK[:, hh, tsl], rhs=Cb[:, col, :], start=False, stop=True), rd=[QK, Cb], wr=[pa])
                for c, (d, hh) in enumerate(chains):
                    col = d * 4 + hh
                    pa, sl = aslot(c)
                    cx.A(lambda h: h.activation(out=sm[:, c, 2:3], in_=pa[:, sl, 128:129], func=AF.Abs, scale=EB[:, tls[d], col:col + 1]), rd=[pa, EB], wr=[sm])
                cx.V(lambda h: h.tensor_scalar(out=sm[:, :, 0:1], in0=sm[:, :, 2:3], scalar1=1.0, scalar2=None, op0=ALU.max), rd=[sm], wr=[sm])
                cx.V(lambda h: h.reciprocal(out=sm[:, :, 3:4], in_=sm[:, :, 0:1]), rd=[sm], wr=[sm])
                for d in range(2):
                    cx.V(lambda h: h.tensor_tensor(out=sm[:, d * 4:(d + 1) * 4, 1:2], in0=EB[:, tls[d], d * 4:(d + 1) * 4].unsqueeze(2), in1=sm[:, d * 4:(d + 1) * 4, 3:4], op=ALU.mult),
                         rd=[sm, EB], wr=[sm])
                for c, (d, hh) in enumerate(chains):
                    pa, sl = aslot(c)
                    dst = hacc[:, tls[d], hh * 128:(hh + 1) * 128]
                    if s < NT // 2:
                        cx.A(lambda h: h.activation(out=dst, in_=pa[:, sl, 0:128], func=AF.Copy, scale=sm[:, c, 1:2]), rd=[pa, sm], wr=[hacc])
                    else:
                        cx.V(lambda h: h.scalar_tensor_tensor(out=dst, in0=pa[:, sl, 0:128], scalar=sm[:, c, 1:2], in1=dst, op0=ALU.mult, op1=ALU.add), rd=[pa, sm, hacc], wr=[hacc])
                for c, (d, hh) in enumerate(chains):
                    tsl = slice(tls[d] * 128, (tls[d] + 1) * 128)
                    cx.M(lambda h: h.transpose(out=ps_t[:, c, :], in_=QK[:, 4 + hh, tsl], identity=idb[:]), rd=[QK, idb], wr=[ps_t])
                for c, (d, hh) in enumerate(chains):
                    col = d * 4 + hh
                    cx.A(lambda h: h.activation(out=ksb[:, c, :], in_=ps_t[:, c, :], func=AF.Copy, scale=ES[:, tls[d], col:col + 1]), rd=[ps_t, ES], wr=[ksb])
                for c, (d, hh) in enumerate(chains):
                    pa, sl = aslot(c)
                    cx.M(lambda h: h.matmul(pa[:, sl, :], lhsT=ksb[:, c, :], rhs=vbs[d][:, hh, 0:129], start=True, stop=True), rd=[ksb, vbs[d]], wr=[pa])
                for c, (d, hh) in enumerate(chains):
                    col = d * 4 + hh
                    pa, sl = aslot(c)
                    cx.V(lambda h: h.tensor_scalar(out=Cst[:, col, :], in0=Cst[:, col, :], scalar1=EE[:, tls[d], col:col + 1], scalar2=None, op0=ALU.mult), rd=[Cst, EE], wr=[Cst])
                    cx.V(lambda h: h.scalar_tensor_tensor(out=Cst[:, col, :], in0=pa[:, sl, :], scalar=EE[:, tls[d], col:col + 1], in1=Cst[:, col, :], op0=ALU.mult, op1=ALU.add),
                         rd=[pa, EE, Cst], wr=[Cst])
                cx.A(lambda h: h.activation(out=Cb[:], in_=Cst[:], func=AF.Copy), rd=[Cst], wr=[Cb])
                if s >= NT // 2:
                    for d in range(2):
                        i = tls[d]
                        a = nfin % 2
                        nfin += 1
                        sg_ = sgl[(2 * s + d) % NSG]
                        hv = hacc[:, i, :]
                        cx.V(lambda h: h.tensor_tensor(out=sq[:], in0=hv, in1=hv, op=ALU.mult), rd=[hacc], wr=[sq])
                        cx.V(lambda h: h.tensor_reduce(out=st4[a][:, 0:4], in_=sq[:].rearrange("p (h d) -> p h d", h=4), axis=AX.X, op=ALU.add), rd=[sq], wr=[st4[a]])
                        cx.A(lambda h: h.activation(out=st4[a][:, 4:8], in_=st4[a][:, 0:4], func=AF.Sqrt, scale=1.0 / 128, bias=eps[:, 0:1]), rd=[st4[a], eps], wr=[st4[a]])
                        cx.V(lambda h: h.reciprocal(out=st4[a][:, 8:12], in_=st4[a][:, 4:8]), rd=[st4[a]], wr=[st4[a]])
                        cx.V(lambda h: h.tensor_tensor(out=hv.rearrange("p (h d) -> p h d", h=4), in0=hv.rearrange("p (h d) -> p h d", h=4),
                                                       in1=st4[a][:, 8:12].unsqueeze(2).broadcast_to([128, 4, 128]), op=ALU.mult), rd=[hacc, st4[a]], wr=[hacc])
                        cx.V(lambda h: h.tensor_tensor(out=sq[:], in0=mg[:], in1=sg_[:], op=ALU.mult), rd=[mg, sg_], wr=[sq])
                        cx.V(lambda h: h.tensor_tensor(out=hmb[a][:], in0=hv, in1=sq[:], op=ALU.mult), rd=[hacc, sq], wr=[hmb[a]])
                        for k in range(4):
                            cx.M(lambda h: h.transpose(out=ps_h[:, k, :], in_=hmb[a][:, k * 128:(k + 1) * 128], identity=idb[:]), rd=[hmb[a], idb], wr=[ps_h])
                        cx.A(lambda h: h.activation(out=hmT[a][:], in_=ps_h[:], func=AF.Copy), rd=[ps_h], wr=[hmT[a]])
                        cx.dma("act", CATT.t[0:4, :, i * 128:(i + 1) * 128].rearrange("c f t -> f c t"), hmT[a][:], rd=[hmT[a]], wr=[CATT])
            if bg is not None:
                bg.finish()
                SC.setdefault("TABDONE", {})[0] = True
            P.barrier()
        with contextlib.ExitStack() as st4_:
            cx = Ctx(P, nc, st4_)
            cb_ = [cx.T([128, 8, 128], BF16) for _ in range(3)]
            g1, = load_mods(cx, MODS, li, [2])
            stage_f = cx.T([128, 1024], F32)

            def src(i):
                b = cb_[i % 3]
                cx.dma("pool", b[:], CATT.t[:, :, i * 128:(i + 1) * 128].rearrange("c f t -> f c t"), rd=[CATT], wr=[b])
                return b, b
            emit_outproj(cx, Xin, Xout, src, IN["ev_w_out"], g1, stage_f)
            P.barrier()
        P.flush()


W_SPECS = {
    "ada_w": [2, 1024, 6144], "ada_b": [2, 6144], "norm_mix_g": [2, 1024], "norm_ffn_g": [2, 1024],
    "ev_w_in": [1024, 2576], "ev_b_in": [1, 2576], "ev_b_inT": [128, 20], "ev_conv_wT": [128, 8, 5], "ev_conv_bT": [128, 8],
    "ev_mnorm_g": [1, 512], "ev_pool_w": [4, 128, 128], "ev_pool_scale": [1, 512], "ev_w_out": [1024, 1024],
    "od_w_in": [1024, 1536], "od_qnorm_g": [1, 128], "od_knorm_g": [1, 128], "od_w_out": [1024, 1024],
    "peer_w_q": [2, 1024, 2048], "peer_keys": [2, 2, 128, 128], "peer_u": [2, 16384, 1024], "peer_v": [2, 16384, 1024],
    "final_g": [1, 1024],
    "ident": [128, 128], "triU": [128, 128], "triL": [128, 128], "pooledge": [1, 4, 32], "ropecs": [128, 2, NT, 64], "iota16": [1, 16],
    "cT": [128, 8],
}


def host_consts():
    cn = {}
    cn["ident"] = np.eye(128, dtype=np.float32)
    s_ = np.arange(128)
    cn["triU"] = (s_[:, None] <= s_[None, :]).astype(np.float32)
    cn["triL"] = (s_[:, None] >= s_[None, :]).astype(np.float32)
    pe = np.zeros((1, 4, 32), np.float32)
    for g, w in enumerate((2, 4, 8, 16)):
        for j in range(32):
            t = j if j < 16 else S - 32 + j
            lo = max(t - w // 2, 0)
            hi = min(t + w // 2, S)
            pe[0, g, j] = w / float(hi - lo)
    cn["pooledge"] = pe
    t = np.arange(S)
    r, c = t // 64, t % 64
    freqs = (10000.0 ** (-np.arange(0, 64, 2, dtype=np.float32) / 64.0)).astype(np.float32)
    ang = np.concatenate([r[:, None].astype(np.float32) * freqs, c[:, None].astype(np.float32) * freqs], axis=-1).astype(np.float32)
    cs = np.stack([np.cos(ang), np.sin(ang)], 0).astype(np.float32)
    cn["ropecs"] = np.ascontiguousarray(cs.reshape(2, NT, 128, 64).transpose(2, 0, 1, 3))
    cn["iota16"] = np.arange(16, dtype=np.float32)[None, :]
    return cn


DEBUG = [False]


def build(first, last):
    nc = bass.Bass("TRN2", target_bir_lowering=False)
    IK = "ExternalOutput" if DEBUG[0] else "Internal"
    IN = {k: nc.dram_tensor(k, v, F32, kind="ExternalInput").ap() for k, v in W_SPECS.items()}
    xin = nc.dram_tensor("xin", [S, D], F32, kind="ExternalInput").ap()
    out = nc.dram_tensor("out", [S, D], F32, kind="ExternalOutput").ap()
    X = {}
    for k in range(0, 5):
        if k == first - 1:
            X[k] = Buf(xin)
        elif k == last:
            X[k] = Buf(out)
        elif first <= k < last:
            X[k] = Buf(nc.dram_tensor("X%d" % k, [S, D], F32, kind="Internal").ap())
    SC = {
        "CATT": Buf(nc.dram_tensor("CATT", [8, 128, S], BF16, kind=IK).ap()),
        "V1D": Buf(nc.dram_tensor("V1D", [NT, 128, 4, 130], BF16, kind=IK).ap()),
        "SIGD": Buf(nc.dram_tensor("SIGD", [NT, 128, 512], BF16, kind=IK).ap()),
        "HFD": Buf(nc.dram_tensor("HFD", [NT, 128, 512], F32, kind=IK).ap()),
        "TAB": [Buf(nc.dram_tensor("TAB%d" % i, [16384, 2048], BF16, kind="Internal").ap()) for i in range(2)],
    }
    SC["DBG1"] = Buf(nc.dram_tensor("DBG1", [128, NT, 16], F32, kind=IK).ap())
    SC["DBG2"] = Buf(nc.dram_tensor("DBG2", [128, 512], F32, kind=IK).ap())
    MODS = Buf(nc.dram_tensor("MODS", [2, 6, 128, 1024], F32, kind=IK).ap())
    with contextlib.ExitStack() as st:
        P = Prog(nc, st)
        phase_mods(P, nc, IN, MODS)
        if first <= 1 and last >= 2:
            SC["BG0"] = TableBuilder(P, nc, IN, 0, SC["TAB"][0])
        if first <= 3 and last >= 4:
            SC["BG1"] = TableBuilder(P, nc, IN, 1, SC["TAB"][1])
        for ph in range(first, last + 1):
            if ph == 1:
                phase_even(P, nc, IN, MODS, 0, X[0], X[1], SC)
            elif ph == 2:
                phase_peer(P, nc, IN, MODS, 0, X[1], X[2], SC, final=False)
            elif ph == 3:
                phase_attn(P, nc, IN, MODS, 1, X[2], X[3], SC)
            elif ph == 4:
                phase_peer(P, nc, IN, MODS, 1, X[3], X[4], SC, final=True)
    return nc


def host_inputs(inputs):
    f = lambda a: np.ascontiguousarray(np.asarray(a, dtype=np.float32))
    sh = {}
    sh["ada_w"] = f(inputs["ada_w"]); sh["ada_b"] = f(inputs["ada_b"])
    sh["norm_mix_g"] = f(inputs["norm_mix_g"]); sh["norm_ffn_g"] = f(inputs["norm_ffn_g"])
    sh["ev_w_in"] = f(inputs["ev_w_in"][0]); sh["ev_b_in"] = f(inputs["ev_b_in"])
    sh["ev_b_inT"] = f(np.asarray(inputs["ev_b_in"])[0, :2560].reshape(20, 128).T)
    cw = np.asarray(inputs["ev_conv_w"])[0, :, 0, :]
    sh["ev_conv_wT"] = f(cw.T.reshape(8, 128, 5).transpose(1, 0, 2))
    sh["ev_conv_bT"] = f(np.asarray(inputs["ev_conv_b"])[0].reshape(8, 128).T)
    sh["ev_mnorm_g"] = f(inputs["ev_mnorm_g"]); sh["ev_pool_w"] = f(inputs["ev_pool_w"][0]); sh["ev_pool_scale"] = f(inputs["ev_pool_scale"])
    sh["ev_w_out"] = f(inputs["ev_w_out"][0])
    sh["od_w_in"] = f(inputs["od_w_in"][0]); sh["od_qnorm_g"] = f(inputs["od_qnorm_g"]); sh["od_knorm_g"] = f(inputs["od_knorm_g"])
    sh["od_w_out"] = f(inputs["od_w_out"][0])
    sh["peer_w_q"] = f(inputs["peer_w_q"]); sh["peer_keys"] = f(inputs["peer_keys"])
    sh["peer_u"] = f(inputs["peer_u"]); sh["peer_v"] = f(inputs["peer_v"])
    sh["final_g"] = f(np.asarray(inputs["final_g"])[None, :])
    sh.update(host_consts())
    return sh


def run_phases(inputs, first, last, xin_list, cores):
    nc = build(first, last)
    sh = host_inputs(inputs)
    c = np.asarray(inputs["c"], dtype=np.float32)
    in_maps = []
    for j, b in enumerate(cores):
        m = dict(sh)
        m["cT"] = np.ascontiguousarray(c[b].reshape(8, 128).T)
        m["xin"] = np.ascontiguousarray(xin_list[j], dtype=np.float32)
        in_maps.append(m)
    res = run_bass_kernel_spmd(nc, in_maps, core_ids=list(range(len(cores))))
    if DEBUG[0]:
        return res.results
    return [r["out"] for r in res.results]


def kernel(**inputs):
    x = np.asarray(inputs["x"], dtype=np.float32)
    outs = run_phases(inputs, 1, 4, [x[b] for b in range(8)], list(range(8)))
    return np.stack(outs, 0).astype(np.float32)


class TableBuilder:
    def __init__(self, P, nc, IN, li, TAB):
        self.P, self.nc, self.IN, self.li, self.TAB = P, nc, IN, li, TAB
        self.blocks = [(half, blk) for half in range(2) for blk in range(64)]
        self.k = 0
        self.loaded = 0
        self.cx = None

    def attach(self, cx):
        self.cx = cx
        self.sf = [cx.T([128, 2, 1024], F32) for _ in range(2)]
        self.sb = [cx.T([128, 2, 1024], BF16) for _ in range(2)]

    def _load(self):
        if self.loaded >= len(self.blocks):
            return
        half, blk = self.blocks[self.loaded]
        f = self.sf[self.loaded % 2]
        rows = slice(blk * 256, (blk + 1) * 256)
        self.cx.dma("pool", f[:], self.IN[("peer_u", "peer_v")[half]][self.li, rows, :].rearrange("(n p) d -> p n d", p=128), wr=[f])
        self.loaded += 1

    def step(self, n=1):
        for _ in range(n):
            if self.k >= len(self.blocks):
                return
            if self.loaded == self.k:
                self._load()
            self._load()
            half, blk = self.blocks[self.k]
            f, b = self.sf[self.k % 2], self.sb[self.k % 2]
            rows = slice(blk * 256, (blk + 1) * 256)
            self.cx.G(lambda h: h.tensor_copy(out=b[:], in_=f[:]), rd=[f], wr=[b])
            self.cx.dma("pool", self.TAB.t[rows, half * 1024:(half + 1) * 1024].rearrange("(n p) d -> p n d", p=128), b[:], rd=[b], wr=[self.TAB])
            self.k += 1

    def finish(self):
        self.step(len(self.blocks))
        self.cx = None

    @property
    def done(self):
        return self.k >= len(self.blocks)


POOL_DOTS = False


def phase_peer(P, nc, IN, MODS, li, Xin, Xout, SC, final):
    TAB = SC["TAB"][li]
    with contextlib.ExitStack() as st:
        cx = Ctx(P, nc, st)
        prebuilt = bool(SC.get("TABDONE", {}).get(li))
        sf = [cx.T([128, 4, 1024], F32) for _ in range(0 if prebuilt else 3)]
        sb = [cx.T([128, 4, 1024], BF16) for _ in range(0 if prebuilt else 3)]
        n = 0
        for half, nm in enumerate(("peer_u", "peer_v")):
            for blk in range(0 if prebuilt else 32):
                f, b = sf[n % 3], sb[n % 3]
                rows = slice(blk * 512, (blk + 1) * 512)
                cx.dma("sp", f[:], IN[nm][li, rows, :].rearrange("(n p) d -> p n d", p=128), wr=[f])
                if n % 3 == 0:
                    cx.A(lambda h: h.activation(out=b[:], in_=f[:], func=AF.Copy), rd=[f], wr=[b])
                elif n % 3 == 1:
                    cx.V(lambda h: h.tensor_copy(out=b[:], in_=f[:]), rd=[f], wr=[b])
                else:
                    cx.G(lambda h: h.tensor_copy(out=b[:], in_=f[:]), rd=[f], wr=[b])
                cx.dma("act", TAB.t[rows, half * 1024:(half + 1) * 1024].rearrange("(n p) d -> p n d", p=128), b[:], rd=[b], wr=[TAB])
                n += 1
        P.barrier()
        P.flush()
    with contextlib.ExitStack() as st:
        cx = Ctx(P, nc, st)
        idf, idb, eps = load_consts(cx, IN)
        gs2, sh2, g2 = load_mods(cx, MODS, li, [3, 4, 5])
        io16 = cx.T([128, 16], F32)
        th16 = cx.T([128, 16], F32)
        cx.dma("sp", io16[:], IN["iota16"].broadcast_to([128, 16]), wr=[io16])
        cx.V(lambda h: h.tensor_scalar(out=th16[:], in0=io16[:], scalar1=16.0, scalar2=None, op0=ALU.mult), rd=[io16], wr=[th16])
        if final:
            fg = cx.T([128, 1024], F32)
            cx.dma("sp", fg[:], IN["final_g"].broadcast_to([128, 1024]), wr=[fg])
        wq = cx.T([128, 8, 2048], BF16)
        ptr = cx.PS([128, 8, 128], BF16)
        keysT = cx.T([128, 2, 128], BF16)
        with contextlib.ExitStack() as stw:
            cw_ = Ctx(P, nc, stw)
            stg = [cw_.T([128, 2048], F32) for _ in range(2)]
            for k in range(8):
                cw_.dma("sp", stg[k % 2][:], IN["peer_w_q"][li, k * 128:(k + 1) * 128, :], wr=[stg[k % 2]])
                cw_.V(lambda h: h.tensor_copy(out=wq[:, k, :], in_=stg[k % 2][:]), rd=[stg[k % 2]], wr=[wq])
            kf = cw_.T([128, 2, 128], F32)
            kb = cw_.T([128, 2, 128], BF16)
            cw_.dma("sp", kf[:], IN["peer_keys"][li].rearrange("t n c -> n t c"), wr=[kf])
            cw_.V(lambda h: h.tensor_copy(out=kb[:], in_=kf[:]), rd=[kf], wr=[kb])
            for t in range(2):
                cw_.M(lambda h: h.transpose(out=ptr[:, t, :], in_=kb[:, t, :], identity=idb[:]), rd=[kb, idb], wr=[ptr])
            cw_.A(lambda h: h.activation(out=keysT[:], in_=ptr[:, 0:2, :], func=AF.Copy), rd=[ptr], wr=[keysT])
            P.barrier()
        pq = [cx.PS([128, 512]) for _ in range(2)]
        psc = [cx.PS([128, 4, 128]) for _ in range(2)]
        po = cx.PS([128, 1024])
        xb = [cx.T([128, 1024], F32) for _ in range(2)]
        sq = cx.T([128, 1024], F32)
        hb = cx.T([128, 1024], BF16)
        hTi = cx.T([128, 8, 128], BF16)
        st2 = cx.T([128, 4], F32)
        qb = cx.T([128, 2048], BF16)
        sqq = cx.T([128, 2048], F32)
        rs = cx.T([128, 48], F32)
        qT = cx.T([128, 16, 128], BF16)
        S1 = cx.T([128, 16, 128], F32)
        wk = [cx.T([128, 128], F32) for _ in range(2)]
        m = cx.T([128, 16, 16], F32)
        ix = cx.T([128, 16, 16], U32)
        ixf = cx.T([128, 16, 16], F32)
        CS = cx.T([128, 8, 256], F32)
        wk2 = [cx.T([128, 256], F32) for _ in range(2)]
        tops = cx.T([128, 8, 16], F32)
        pos = cx.T([128, 8, 16], U32)
        posf = cx.T([128, 8, 16], F32)
        af = cx.T([128, 8, 16], F32)
        bf_ = cx.T([128, 8, 16], F32)
        oh = cx.T([128, 8, 16, 16], F32)
        i12 = cx.T([128, 2, 128], F32)
        idxf = cx.T([128, 128], F32)
        idx = cx.T([128, 128], I32)
        ge = cx.T([128, 8, 16], F32)
        gsum = cx.T([128, 16], F32)
        gate = cx.T([128, 128], F32)
        act = cx.T([128, 128], F32)
        gl = cx.T([128, 128], F32)
        coef = cx.T([128, 128], F32)
        NG = 4
        actT = [Tok() for _ in range(4)]
        glT = [Tok() for _ in range(4)]
        coefT = [Tok() for _ in range(4)]
        Gb = [cx.T([128, 4, 2048], BF16) for _ in range(NG)]
        Gtok = [[Tok() for _ in range(4)] for _ in range(NG)]
        diag = [cx.T([128, 4, 128], BF16) for _ in range(2)]
        junk = cx.T([128, 1024], BF16)
        yb = [cx.T([128, 1024], F32) for _ in range(2)]
        sgn = 0
        hbs = [hb, cx.T([128, 1024], BF16)]
        idxs = [idx, cx.T([128, 128], I32)]
        gates = [gate, cx.T([128, 128], F32)]
        st3 = cx.T([128, 4], F32)
        sq2 = cx.T([128, 1024], F32) if final else None
        fg_ = fg if final else None
        FSTEP = 2

        def front(i):
            x = xb[i % 2]
            hb = hbs[i % 2]
            idx = idxs[i % 2]
            gate = gates[i % 2]
            x = xb[i % 2]
            yield
            cx.dma("sp", x[:], Xin.t[i * 128:(i + 1) * 128, :], rd=[Xin], wr=[x])
            yield
            emit_norm_tile(cx, x, gs2, sh2, hb, sq, st2, idb, ptr, hTi[:], hTi)
            for nb in range(4):
                p = pq[nb % 2]
                for k in range(8):
                    yield
                    cx.M(lambda h: h.matmul(p[:], lhsT=hTi[:, k, :], rhs=wq[:, k, nb * 512:(nb + 1) * 512], start=(k == 0), stop=(k == 7)), rd=[hTi, wq], wr=[p])
                yield
                cx.A(lambda h: h.activation(out=qb[:, nb * 512:(nb + 1) * 512], in_=p[:], func=AF.Copy), rd=[p], wr=[qb])
            yield
            cx.V(lambda h: h.tensor_tensor(out=sqq[:], in0=qb[:], in1=qb[:], op=ALU.mult), rd=[qb], wr=[sqq])
            yield
            cx.V(lambda h: h.tensor_reduce(out=rs[:, 0:16], in_=sqq[:].rearrange("p (g c) -> p g c", g=16), axis=AX.X, op=ALU.add), rd=[sqq], wr=[rs])
            yield
            cx.A(lambda h: h.activation(out=rs[:, 16:32], in_=rs[:, 0:16], func=AF.Sqrt, scale=1.0 / 128, bias=eps[:, 0:1]), rd=[rs, eps], wr=[rs])
            yield
            cx.V(lambda h: h.reciprocal(out=rs[:, 32:48], in_=rs[:, 16:32]), rd=[rs], wr=[rs])
            for r in range(2):
                for j in range(8):
                    g_ = r * 8 + j
                    yield
                    cx.M(lambda h: h.transpose(out=ptr[:, j, :], in_=qb[:, g_ * 128:(g_ + 1) * 128], identity=idb[:]), rd=[qb, idb], wr=[ptr])
                yield
                cx.A(lambda h: h.activation(out=qT[:, r * 8:(r + 1) * 8, :], in_=ptr[:], func=AF.Copy), rd=[ptr], wr=[qT])
            for r in range(4):
                ps_ = psc[r % 2]
                for j in range(4):
                    hp = r * 4 + j
                    yield
                    cx.M(lambda h: h.matmul(ps_[:, j, :], lhsT=qT[:, hp, :], rhs=keysT[:, hp % 2, :], start=True, stop=True), rd=[qT, keysT], wr=[ps_])
                yield
                cx.V(lambda h: h.tensor_tensor(out=S1[:, r * 4:(r + 1) * 4, :], in0=ps_[:], in1=rs[:, 32 + r * 4:32 + (r + 1) * 4].unsqueeze(2).broadcast_to([128, 4, 128]), op=ALU.mult),
                     rd=[ps_, rs], wr=[S1])
            for hp in range(16):
                w_ = wk[hp % 2]
                yield
                cx.V(lambda h: h.max(out=m[:, hp, 0:8], in_=S1[:, hp, :]), rd=[S1], wr=[m])
                yield
                cx.V(lambda h: h.max_index(out=ix[:, hp, 0:8], in_max=m[:, hp, 0:8], in_values=S1[:, hp, :]), rd=[m, S1], wr=[ix])
                yield
                cx.V(lambda h: h.match_replace(out=w_[:], in_to_replace=m[:, hp, 0:8], in_values=S1[:, hp, :], imm_value=-1e30), rd=[m, S1], wr=[w_])
                yield
                cx.V(lambda h: h.max(out=m[:, hp, 8:16], in_=w_[:]), rd=[w_], wr=[m])
                yield
                cx.V(lambda h: h.max_index(out=ix[:, hp, 8:16], in_max=m[:, hp, 8:16], in_values=w_[:]), rd=[m, w_], wr=[ix])
            mv = m[:].rearrange("p (h t) k -> p h t k", t=2)
            yield
            cx.V(lambda h: h.tensor_tensor(out=CS[:].rearrange("p h (a b) -> p h a b", a=16), in0=mv[:, :, 0, :].unsqueeze(3).broadcast_to([128, 8, 16, 16]),
                                           in1=mv[:, :, 1, :].unsqueeze(2).broadcast_to([128, 8, 16, 16]), op=ALU.add), rd=[m], wr=[CS])
            for hh in range(8):
                w_ = wk2[hh % 2]
                yield
                cx.V(lambda h: h.max(out=tops[:, hh, 0:8], in_=CS[:, hh, :]), rd=[CS], wr=[tops])
                yield
                cx.V(lambda h: h.max_index(out=pos[:, hh, 0:8], in_max=tops[:, hh, 0:8], in_values=CS[:, hh, :]), rd=[tops, CS], wr=[pos])
                yield
                cx.V(lambda h: h.match_replace(out=w_[:], in_to_replace=tops[:, hh, 0:8], in_values=CS[:, hh, :], imm_value=-1e30), rd=[tops, CS], wr=[w_])
                yield
                cx.V(lambda h: h.max(out=tops[:, hh, 8:16], in_=w_[:]), rd=[w_], wr=[tops])
                yield
                cx.V(lambda h: h.max_index(out=pos[:, hh, 8:16], in_max=tops[:, hh, 8:16], in_values=w_[:]), rd=[tops, w_], wr=[pos])
            yield
            cx.V(lambda h: h.tensor_copy(out=posf[:], in_=pos[:]), rd=[pos], wr=[posf])
            yield
            cx.V(lambda h: h.tensor_copy(out=ixf[:], in_=ix[:]), rd=[ix], wr=[ixf])
            bc4 = lambda ap3: ap3.unsqueeze(3).broadcast_to([128, 8, 16, 16])
            io4 = io16[:].unsqueeze(1).unsqueeze(1).broadcast_to([128, 8, 16, 16])
            th4 = th16[:].unsqueeze(1).unsqueeze(1).broadcast_to([128, 8, 16, 16])
            yield
            cx.V(lambda h: h.tensor_tensor(out=oh[:], in0=bc4(posf[:]), in1=th4, op=ALU.is_ge), rd=[posf, th16], wr=[oh])
            yield
            cx.V(lambda h: h.tensor_reduce(out=af[:], in_=oh[:], axis=AX.X, op=ALU.add), rd=[oh], wr=[af])
            yield
            cx.V(lambda h: h.tensor_scalar(out=af[:], in0=af[:], scalar1=-1.0, scalar2=None, op0=ALU.add), rd=[af], wr=[af])
            yield
            cx.V(lambda h: h.scalar_tensor_tensor(out=bf_[:], in0=af[:], scalar=-16.0, in1=posf[:], op0=ALU.mult, op1=ALU.add), rd=[af, posf], wr=[bf_])
            ixv = ixf[:].rearrange("p (h t) k -> p h t k", t=2)
            for t, src in ((0, af), (1, bf_)):
                yield
                cx.V(lambda h: h.tensor_tensor(out=oh[:], in0=bc4(src[:]), in1=io4, op=ALU.is_equal), rd=[src, io16], wr=[oh])
                yield
                cx.V(lambda h: h.tensor_tensor(out=oh[:], in0=oh[:], in1=ixv[:, :, t, :].unsqueeze(2).broadcast_to([128, 8, 16, 16]), op=ALU.mult), rd=[oh, ixf], wr=[oh])
                yield
                cx.V(lambda h: h.tensor_reduce(out=i12[:, t, :].rearrange("p (h k) -> p h k", h=8), in_=oh[:], axis=AX.X, op=ALU.add), rd=[oh], wr=[i12])
            yield
            cx.V(lambda h: h.scalar_tensor_tensor(out=idxf[:], in0=i12[:, 0, :], scalar=128.0, in1=i12[:, 1, :], op0=ALU.mult, op1=ALU.add), rd=[i12], wr=[idxf])
            yield
            cx.V(lambda h: h.tensor_copy(out=idx[:], in_=idxf[:]), rd=[idxf], wr=[idx])
            yield
            cx.V(lambda h: h.tensor_tensor(out=ge[:], in0=tops[:], in1=tops[:, :, 0:1].broadcast_to([128, 8, 16]), op=ALU.subtract), rd=[tops], wr=[ge])
            yield
            cx.A(lambda h: h.activation(out=ge[:], in_=ge[:], func=AF.Exp), rd=[ge], wr=[ge])
            yield
            cx.V(lambda h: h.tensor_reduce(out=gsum[:, 0:8], in_=ge[:], axis=AX.X, op=ALU.add), rd=[ge], wr=[gsum])
            yield
            cx.V(lambda h: h.reciprocal(out=gsum[:, 8:16], in_=gsum[:, 0:8]), rd=[gsum], wr=[gsum])
            yield
            cx.V(lambda h: h.tensor_tensor(out=gate[:].rearrange("p (h k) -> p h k", h=8), in0=ge[:], in1=gsum[:, 8:16].unsqueeze(2).broadcast_to([128, 8, 16]), op=ALU.mult),
                 rd=[ge, gsum], wr=[gate])


        def back(i, fg):
            nonlocal sgn
            x = xb[i % 2]
            hb = hbs[i % 2]
            idx = idxs[i % 2]
            gate = gates[i % 2]
            cx.V(lambda h: h.memset(act[:], 0.0), wr=actT)
            PF = NG - 2
            for sgi in range(32 + PF + 1):
                if sgi < 32:
                    bq_ = (sgn + sgi) % NG
                    for jj in range(4):
                        j = sgi * 4 + jj
                        P.dma("pool", lambda h: h.indirect_dma_start(out=Gb[bq_][:, jj, :], out_offset=None, in_=TAB.t,
                                                                     in_offset=bass.IndirectOffsetOnAxis(ap=idx[:, j:j + 1], axis=0)),
                              _ks([idx, TAB]), [Gtok[bq_][jj]])
                sg = sgi - PF
                if 0 <= sg < 32:
                    b_ = (sgn + sg) % NG
                    G_ = Gb[b_]
                    for jj in range(4):
                        j = sg * 4 + jj
                        cx.V(lambda h: h.scalar_tensor_tensor(out=junk[:], in0=G_[:, jj, 0:1024], scalar=1.0, in1=hb[:], op0=ALU.mult, op1=ALU.mult, accum_out=act[:, j:j + 1]),
                             rd=[Gtok[b_][jj], hb], wr=[junk, actT[sg % 4]])
                        for _ in range(FSTEP):
                            next(fg, None)
                    cs = slice(sg * 4, (sg + 1) * 4)
                    cx.A(lambda h: h.activation(out=gl[:, cs], in_=act[:, cs], func=AF.Gelu), rd=[actT[sg % 4]], wr=[glT[sg % 4]])
                sg = sgi - PF - 1
                if 0 <= sg < 32:
                    b_ = (sgn + sg) % NG
                    G_ = Gb[b_]
                    dg = diag[sg % 2]
                    cs = slice(sg * 4, (sg + 1) * 4)
                    cx.V(lambda h: h.tensor_tensor(out=coef[:, cs], in0=gl[:, cs], in1=gate[:, cs], op=ALU.mult), rd=[glT[sg % 4], gate], wr=[coefT[sg % 4]])
                    for jj in range(4):
                        j = sg * 4 + jj
                        cx.A(lambda h: h.activation(out=dg[:, jj, :], in_=idf[:], func=AF.Copy, scale=coef[:, j:j + 1]), rd=[idf, coefT[sg % 4]], wr=[dg])
                    for jj in range(4):
                        j = sg * 4 + jj
                        for hv in range(2):
                            cx.M(lambda h: h.matmul(po[:, hv * 512:(hv + 1) * 512], lhsT=dg[:, jj, :], rhs=G_[:, jj, 1024 + hv * 512:1024 + (hv + 1) * 512],
                                                    start=(j == 0), stop=(j == 127)), rd=[dg, Gtok[b_][jj]], wr=[po])
            sgn += 32
            y = yb[i % 2]
            cx.V(lambda h: h.tensor_tensor(out=y[:], in0=po[:], in1=g2[:], op=ALU.mult), rd=[po, g2], wr=[y])
            cx.V(lambda h: h.tensor_tensor(out=y[:], in0=y[:], in1=x[:], op=ALU.add), rd=[y, x], wr=[y])
            if final:
                cx.V(lambda h: h.memset(st3[:, 0:1], 0.0), wr=[st3])
                cx.A(lambda h: h.activation(out=sq2[:], in_=y[:], func=AF.Square, accum_out=st3[:, 0:1]), rd=[y, st3], wr=[sq2, st3])
                cx.A(lambda h: h.activation(out=st3[:, 1:2], in_=st3[:, 0:1], func=AF.Sqrt, scale=1.0 / D, bias=eps[:, 0:1]), rd=[st3, eps], wr=[st3])
                cx.V(lambda h: h.reciprocal(out=st3[:, 2:3], in_=st3[:, 1:2]), rd=[st3], wr=[st3])
                cx.V(lambda h: h.scalar_tensor_tensor(out=y[:], in0=y[:], scalar=st3[:, 2:3], in1=fg_[:], op0=ALU.mult, op1=ALU.mult), rd=[y, st3, fg_], wr=[y])
            cx.dma("sp", Xout.t[i * 128:(i + 1) * 128, :], y[:], rd=[y], wr=[Xout])

        fgen = front(0)
        for _ in fgen:
            pass
        for i in range(NT):
            fgen = front(i + 1) if i + 1 < NT else iter(())
            back(i, fgen)
            for _ in fgen:
                pass
        P.barrier()
        P.flush()


def phase_attn(P, nc, IN, MODS, li, Xin, Xout, SC):
    with contextlib.ExitStack() as st0:
        c0 = Ctx(P, nc, st0)
        idf, idb, eps = load_consts(c0, IN)
        bigT = c0.T([128, 8, S], BF16)
        qT = c0.T([128, 8, S], BF16)
        kT = c0.T([128, 2, S], BF16)
        V1 = c0.T([128, NT, 2, 130], BF16)
        with contextlib.ExitStack() as st1:
            c1 = Ctx(P, nc, st1)
            gs1, sh1 = load_mods(c1, MODS, li, [0, 1])
            emit_hT(c1, Xin, gs1, sh1, idb, bigT)
            P.barrier()
        with contextlib.ExitStack() as st2:
            cx = Ctx(P, nc, st2)
            w = cx.T([128, 8, 1536], BF16)
            with contextlib.ExitStack() as stw:
                cw_ = Ctx(P, nc, stw)
                stg = [cw_.T([128, 1536], F32) for _ in range(2)]
                for k in range(8):
                    cw_.dma("sp", stg[k % 2][:], IN["od_w_in"][k * 128:(k + 1) * 128, :], wr=[stg[k % 2]])
                    cw_.V(lambda h: h.tensor_copy(out=w[:, k, :], in_=stg[k % 2][:]), rd=[stg[k % 2]], wr=[w])
                P.barrier()
            csb = [cx.T([128, 2, 64], F32) for _ in range(2)]
            gq = cx.T([128, 10, 128], F32)
            g1_ = cx.T([128, 128], F32)
            g2_ = cx.T([128, 128], F32)
            cx.dma("sp", g1_[:], IN["od_qnorm_g"].broadcast_to([128, 128]), wr=[g1_])
            cx.dma("sp", g2_[:], IN["od_knorm_g"].broadcast_to([128, 128]), wr=[g2_])
            cx.V(lambda h: h.tensor_scalar(out=g1_[:], in0=g1_[:], scalar1=float(128 ** -0.5), scalar2=None, op0=ALU.mult), rd=[g1_], wr=[g1_])
            cx.V(lambda h: h.tensor_copy(out=gq[:, 0:8, :], in_=g1_[:].unsqueeze(1).broadcast_to([128, 8, 128])), rd=[g1_], wr=[gq])
            cx.V(lambda h: h.tensor_copy(out=gq[:, 8:10, :], in_=g2_[:].unsqueeze(1).broadcast_to([128, 2, 128])), rd=[g2_], wr=[gq])
            cx.V(lambda h: h.memset(V1[:], 1.0), wr=[V1])
            pz = [cx.PS([128, 512]) for _ in range(3)]
            ptq = cx.PS([128, 8, 128], BF16)
            ptk = cx.PS([128, 2, 128], BF16)
            rs = cx.T([128, 32], F32)
            qn = cx.T([128, 10, 128], F32)
            qr = cx.T([128, 10, 128], BF16)
            t1 = cx.T([128, 10, 64], F32)
            t2 = cx.T([128, 10, 64], F32)
            for i in range(NT):
                tsl = slice(i * 128, (i + 1) * 128)
                for nb in range(3):
                    for k in range(8):
                        cx.M(lambda h: h.matmul(pz[nb][:], lhsT=bigT[:, k, tsl], rhs=w[:, k, nb * 512:(nb + 1) * 512], start=(k == 0), stop=(k == 7)), rd=[bigT, w], wr=[pz[nb]])
                cx.A(lambda h: h.activation(out=V1[:, i, :, 0:128], in_=pz[2][:, 256:512].rearrange("p (g d) -> p g d", g=2), func=AF.Copy), rd=[pz[2]], wr=[V1])
                cs = csb[i % 2]
                cx.dma("act", cs[:], IN["ropecs"][:, :, i, :], wr=[cs])
                zsrc = ((pz[0], 0, 4, 512), (pz[1], 4, 8, 512), (pz[2], 8, 10, 256))
                for (pp, g0, g1x, wd) in zsrc:
                    cx.A(lambda h: h.activation(out=qn[:, g0:g1x, :], in_=pp[:, 0:wd].rearrange("p (g d) -> p g d", d=128), func=AF.Square), rd=[pp], wr=[qn])
                cx.V(lambda h: h.tensor_reduce(out=rs[:, 0:10], in_=qn[:], axis=AX.X, op=ALU.add), rd=[qn], wr=[rs])
                cx.A(lambda h: h.activation(out=rs[:, 10:20], in_=rs[:, 0:10], func=AF.Sqrt, scale=1.0 / 128, bias=eps[:, 0:1]), rd=[rs, eps], wr=[rs])
                cx.V(lambda h: h.reciprocal(out=rs[:, 20:30], in_=rs[:, 10:20]), rd=[rs], wr=[rs])
                for (pp, g0, g1x, wd) in zsrc:
                    cx.V(lambda h: h.tensor_tensor(out=qn[:, g0:g1x, :], in0=pp[:, 0:wd].rearrange("p (g d) -> p g d", d=128),
                                                   in1=rs[:, 20 + g0:20 + g1x].unsqueeze(2).broadcast_to([128, g1x - g0, 128]), op=ALU.mult), rd=[pp, rs], wr=[qn])
                cx.V(lambda h: h.tensor_tensor(out=qn[:], in0=qn[:], in1=gq[:], op=ALU.mult), rd=[qn, gq], wr=[qn])
                qv = qn[:].rearrange("p g (d t) -> p g d t", t=2)
                qo = qr[:].rearrange("p g (d t) -> p g d t", t=2)
                cc = cs[:, 0, :].unsqueeze(1).broadcast_to([128, 10, 64])
                ss_ = cs[:, 1, :].unsqueeze(1).broadcast_to([128, 10, 64])
                cx.V(lambda h: h.tensor_tensor(out=t1[:], in0=qv[:, :, :, 0], in1=cc, op=ALU.mult), rd=[qn, cs], wr=[t1])
                cx.V(lambda h: h.tensor_tensor(out=t2[:], in0=qv[:, :, :, 1], in1=ss_, op=ALU.mult), rd=[qn, cs], wr=[t2])
                cx.V(lambda h: h.tensor_tensor(out=qo[:, :, :, 0], in0=t1[:], in1=t2[:], op=ALU.subtract), rd=[t1, t2], wr=[qr])
                cx.V(lambda h: h.tensor_tensor(out=t1[:], in0=qv[:, :, :, 0], in1=ss_, op=ALU.mult), rd=[qn, cs, qr], wr=[t1])
                cx.V(lambda h: h.tensor_tensor(out=t2[:], in0=qv[:, :, :, 1], in1=cc, op=ALU.mult), rd=[qn, cs, qr], wr=[t2])
                cx.V(lambda h: h.tensor_tensor(out=qo[:, :, :, 1], in0=t1[:], in1=t2[:], op=ALU.add), rd=[t1, t2], wr=[qr])
                for g_ in range(8):
                    cx.M(lambda h: h.transpose(out=ptq[:, g_, :], in_=qr[:, g_, :], identity=idb[:]), rd=[qr, idb], wr=[ptq])
                for g_ in range(2):
                    cx.M(lambda h: h.transpose(out=ptk[:, g_, :], in_=qr[:, 8 + g_, :], identity=idb[:]), rd=[qr, idb], wr=[ptk])
                cx.A(lambda h: h.activation(out=qT[:, :, tsl], in_=ptq[:], func=AF.Copy), rd=[ptq], wr=[qT])
                cx.A(lambda h: h.activation(out=kT[:, :, tsl], in_=ptk[:], func=AF.Copy), rd=[ptk], wr=[kT])
            P.barrier()
        import os as _os
        if _os.environ.get("ATT_STOP") == "2":
            P.barrier()
            P.flush()
            return
        with contextlib.ExitStack() as st3:
            cx = Ctx(P, nc, st3)
            pss = [cx.PS([128, 512]) for _ in range(2)]
            pacc = [cx.PS([128, 512]) for _ in range(4)]
            pto = cx.PS([128, 8, 128], BF16)
            pT = [cx.T([128, 512], BF16) for _ in range(3)]
            ao = [cx.T([128, 8, 128], BF16) for _ in range(4)]
            rinv = cx.T([128, 8], F32)
            steps = [(qb_, hd_, sj) for qb_ in range(8) for hd_ in range(8) for sj in range(NT)]
            accs = pacc

            def emit_score(n):
                qb_, hd_, sj = steps[n]
                ps_ = pss[n % 2]
                cx.M(lambda h: h.matmul(ps_[:], lhsT=kT[:, hd_ // 4, sj * 128:(sj + 1) * 128], rhs=qT[:, hd_, qb_ * 512:(qb_ + 1) * 512], start=True, stop=True), rd=[kT, qT], wr=[ps_])

            bg = SC.get("BG1")
            if bg is not None:
                bg.attach(cx)
            emit_score(0)
            for n, (qb_, hd_, sj) in enumerate(steps):
                g_ = hd_ // 4
                if bg is not None and n % 14 == 0:
                    bg.step(1)
                if n + 1 < len(steps):
                    emit_score(n + 1)
                ps_ = pss[n % 2]
                pt_ = pT[n % 3]
                cx.A(lambda h: h.activation(out=pt_[:], in_=ps_[:], func=AF.Exp), rd=[ps_], wr=[pt_])
                for qs in range(4):
                    cx.M(lambda h: h.matmul(accs[qs][:, 0:129], lhsT=pt_[:, qs * 128:(qs + 1) * 128], rhs=V1[:, sj, g_, 0:129], start=(sj == 0), stop=(sj == NT - 1)),
                         rd=[pt_, V1], wr=[accs[qs]])
                if sj == NT - 1:
                    for qs in range(4):
                        a_ = accs[qs]
                        cx.V(lambda h: h.reciprocal(out=rinv[:, qs:qs + 1], in_=a_[:, 128:129]), rd=[a_], wr=[rinv])
                        cx.V(lambda h: h.tensor_scalar(out=ao[qs][:, hd_, :], in0=a_[:, 0:128], scalar1=rinv[:, qs:qs + 1], scalar2=None, op0=ALU.mult), rd=[a_, rinv], wr=[ao[qs]])
                    if hd_ == 7:
                        for qs in range(4):
                            ti = qb_ * 4 + qs
                            for k in range(8):
                                cx.M(lambda h: h.transpose(out=pto[:, k, :], in_=ao[qs][:, k, :], identity=idb[:]), rd=[ao[qs], idb], wr=[pto])
                            cx.V(lambda h: h.tensor_copy(out=bigT[:, :, ti * 128:(ti + 1) * 128], in_=pto[:]), rd=[pto], wr=[bigT])
            if bg is not None:
                bg.finish()
                SC.setdefault("TABDONE", {})[1] = True
            P.barrier()
        if _os.environ.get("ATT_STOP") == "3":
            P.barrier()
            P.flush()
            return
        with contextlib.ExitStack() as st4:
            cx = Ctx(P, nc, st4)
            g1, = load_mods(cx, MODS, li, [2])
            stage_f = cx.T([128, 1024], F32)
            emit_outproj(cx, Xin, Xout, lambda i: (bigT[:, :, i * 128:(i + 1) * 128], bigT), IN["od_w_out"], g1, stage_f)
            P.barrier()
        P.flush()
```

```python
import contextlib
import numpy as np
import concourse.bass as bass
import concourse.mybir as mybir
from concourse.bass_utils import run_bass_kernel_spmd

F32 = mybir.dt.float32
BF16 = mybir.dt.bfloat16
I32 = mybir.dt.int32
U32 = mybir.dt.uint32
AF = mybir.ActivationFunctionType
ALU = mybir.AluOpType
AX = mybir.AxisListType

S = 4096
D = 1024
NT = S // 128
EPS = 1e-6


class Tok:
    __slots__ = ("w", "r", "name")

    def __init__(self, name=""):
        self.w = None
        self.r = {}
        self.name = name


class _Eng:
    def __init__(self, name, sem):
        self.name = name
        self.sem = sem
        self.cnt = 0
        self.seen = {}
        self.ops = []


NDMA = 12


class _Rec:
    def __getattr__(self, name):
        def f(*a, **k):
            return (name, a, k)
        return f


_REC = _Rec()


class Prog:
    ENG = ("pe", "act", "dve", "pool", "sp")

    def __init__(self, nc, stack):
        self.nc = nc
        self.stack = stack
        self.e = {}
        self.sems = {}
        for n in self.ENG:
            self.e[n] = _Eng(n, stack.enter_context(nc.semaphore("s_" + n)))
            self.sems[n] = (self.e[n].sem, 1)
        self.dslot = {}
        for q in ("sp", "pool", "act"):
            sl = []
            for j in range(NDMA):
                key = "d_%s%d" % (q, j)
                sem = stack.enter_context(nc.semaphore(key))
                self.sems[key] = (sem, 16)
                sl.append([key, 0])
            self.dslot[q] = [sl, 0]

    def _deps(self, e, rd, wr):
        deps = {}

        def need(dep, same_ok):
            if dep is None:
                return
            en, c = dep
            if en == e and (e == "pe" or not same_ok):
                return
            if deps.get(en, 0) < c:
                deps[en] = c

        for t in rd:
            need(t.w, True)
        for t in wr:
            need(t.w, False)
            for en, c in t.r.items():
                need((en, c), False)
        return deps

    def _emit_waits(self, E, deps):
        for en, c in deps.items():
            if E.seen.get(en, 0) < c:
                sem, step = self.sems[en]
                E.ops.append(lambda h, sem=sem, v=c * step: h.wait_ge(sem, v))
                E.seen[en] = c

    def op(self, e, fn, rd=(), wr=()):
        E = self.e[e]
        self._emit_waits(E, self._deps(e, rd, wr))
        E.cnt += 1
        sem = E.sem
        rec = fn(_REC)
        E.ops.append(lambda h, rec=rec, sem=sem: getattr(h, rec[0])(*rec[1], **rec[2]).then_inc(sem, 1))
        me = (e, E.cnt)
        for t in wr:
            t.w = me
            t.r = {}
        for t in rd:
            t.r[e] = E.cnt

    def dma(self, q, fn, rd=(), wr=()):
        E = self.e[q]
        slots, nxt = self.dslot[q]
        slot = slots[nxt % NDMA]
        self.dslot[q][1] = nxt + 1
        key = slot[0]
        deps = self._deps(key, rd, wr)
        if slot[1] > 0:
            deps[key] = max(deps.get(key, 0), slot[1])
        self._emit_waits(E, deps)
        slot[1] += 1
        sem = self.sems[key][0]
        rec = fn(_REC)
        E.ops.append(lambda h, rec=rec, sem=sem: getattr(h, rec[0])(*rec[1], **rec[2]).then_inc(sem, 16))
        me = (key, slot[1])
        for t in wr:
            t.w = me
            t.r = {}
        for t in rd:
            t.r[key] = slot[1]

    def barrier(self):
        tgt = {n: self.e[n].cnt for n in self.ENG}
        for q in self.dslot:
            for key, c in self.dslot[q][0]:
                tgt[key] = c
        for n in self.ENG:
            E = self.e[n]
            d = {k: v for k, v in tgt.items() if v > 0 and not (k == n and n == "pe")}
            self._emit_waits(E, d)

    def flush(self):
        nc = self.nc
        with nc.Block() as block:
            @block.tensor
            def _(h):
                for f in self.e["pe"].ops:
                    f(h)

            @block.scalar
            def _(h):
                for f in self.e["act"].ops:
                    f(h)

            @block.vector
            def _(h):
                for f in self.e["dve"].ops:
                    f(h)

            @block.gpsimd
            def _(h):
                for f in self.e["pool"].ops:
                    f(h)

            @block.sync
            def _(h):
                for f in self.e["sp"].ops:
                    f(h)
        for n in self.ENG:
            self.e[n].ops = []


class Buf:
    def __init__(self, t):
        self.t = t
        self.k = Tok()

    def __getitem__(self, i):
        return self.t[i]


def _ks(xs):
    return [x.k if isinstance(x, Buf) else x for x in xs]


_NAME = [0]


class Ctx:
    def __init__(self, P, nc, st):
        self.P, self.nc, self.st = P, nc, st
        self.n = 0

    def T(self, shape, dt, name=None):
        _NAME[0] += 1
        return Buf(self.st.enter_context(self.nc.sbuf_tensor("%s_%d" % (name or "t", _NAME[0]), list(shape), dt)))

    def PS(self, shape, dt=F32, name=None):
        _NAME[0] += 1
        return Buf(self.st.enter_context(self.nc.psum_tensor("%s_%d" % (name or "p", _NAME[0]), list(shape), dt)))

    def V(self, fn, rd=(), wr=()):
        self.P.op("dve", fn, _ks(rd), _ks(wr))

    def A(self, fn, rd=(), wr=()):
        self.P.op("act", fn, _ks(rd), _ks(wr))

    def G(self, fn, rd=(), wr=()):
        self.P.op("pool", fn, _ks(rd), _ks(wr))

    def M(self, fn, rd=(), wr=()):
        self.P.op("pe", fn, _ks(rd), _ks(wr))

    def dma(self, q, out, in_, rd=(), wr=()):
        self.P.dma(q, lambda h, out=out, in_=in_: h.dma_start(out=out, in_=in_), _ks(rd), _ks(wr))


def load_cast(cx, q, dst_bf, src_ap, stage, eng="pool"):
    cx.dma(q, stage.t[:] if not isinstance(stage, tuple) else stage[1], src_ap, wr=[stage if not isinstance(stage, tuple) else stage[0]])


def emit_norm_tile(cx, xt, gs, sh, hb, sq, st2, idb, ptr, hT_dst, hT_buf, hf=None):
    cx.V(lambda h: h.memset(st2[:, 0:1], 0.0), wr=[st2])
    cx.A(lambda h: h.activation(out=sq[:], in_=xt[:], func=AF.Square, accum_out=st2[:, 0:1]), rd=[xt, st2], wr=[sq, st2])
    cx.A(lambda h: h.activation(out=st2[:, 1:2], in_=st2[:, 0:1], func=AF.Sqrt, scale=1.0 / D, bias=EPS_AP[0][:, 0:1]), rd=[st2, EPS_AP[0]], wr=[st2])
    cx.V(lambda h: h.reciprocal(out=st2[:, 2:3], in_=st2[:, 1:2]), rd=[st2], wr=[st2])
    cx.V(lambda h: h.scalar_tensor_tensor(out=sq[:], in0=xt[:], scalar=st2[:, 2:3], in1=gs[:], op0=ALU.mult, op1=ALU.mult), rd=[xt, st2, gs], wr=[sq])
    if hf is not None:
        cx.V(lambda h: h.tensor_tensor(out=hf[:], in0=sq[:], in1=sh[:], op=ALU.add), rd=[sq, sh], wr=[hf])
        cx.G(lambda h: h.tensor_copy(out=hb[:], in_=hf[:]), rd=[hf], wr=[hb])
    else:
        cx.V(lambda h: h.tensor_tensor(out=hb[:], in0=sq[:], in1=sh[:], op=ALU.add), rd=[sq, sh], wr=[hb])
    for k in range(8):
        cx.M(lambda h, k=k: h.transpose(out=ptr[:, k, :], in_=hb[:, k * 128:(k + 1) * 128], identity=idb[:]), rd=[hb, idb], wr=[ptr])
    cx.A(lambda h: h.activation(out=hT_dst, in_=ptr[:], func=AF.Copy), rd=[ptr], wr=[hT_buf])


EPS_AP = [None]


def load_consts(cx, CN):
    idf = cx.T([128, 128], F32)
    idb = cx.T([128, 128], BF16)
    eps = cx.T([128, 1], F32)
    cx.dma("sp", idf[:], CN["ident"], wr=[idf])
    cx.V(lambda h: h.tensor_copy(out=idb[:], in_=idf[:]), rd=[idf], wr=[idb])
    cx.V(lambda h: h.memset(eps[:], EPS), wr=[eps])
    EPS_AP[0] = eps
    return idf, idb, eps


def phase_mods(P, nc, IN, MODS):
    with contextlib.ExitStack() as st:
        cx = Ctx(P, nc, st)
        cT = cx.T([128, 8], F32)
        cond = cx.T([128, 8], F32)
        crep = cx.T([128, 8, 128], F32)
        cx.dma("sp", cT[:], IN["cT"], wr=[cT])
        cx.A(lambda h: h.activation(out=cond[:], in_=cT[:], func=AF.Silu), rd=[cT], wr=[cond])
        cx.V(lambda h: h.tensor_copy(out=crep[:], in_=cond[:].unsqueeze(2).broadcast_to([128, 8, 128])), rd=[cond], wr=[crep])
        wb = [cx.T([128, 8, 512], F32) for _ in range(2)]
        ps = [cx.PS([128, 512]) for _ in range(2)]
        mod = cx.T([128, 6144], F32)
        ab = cx.T([128, 6144], F32)
        gm = cx.T([128, 1024], F32)
        gf = cx.T([128, 1024], F32)
        n = 0
        for i in range(2):
            cx.dma("act", ab[:], IN["ada_b"][i:i + 1, :].broadcast_to([128, 6144]), wr=[ab])
            cx.dma("act", gm[:], IN["norm_mix_g"][i:i + 1, :].broadcast_to([128, 1024]), wr=[gm])
            cx.dma("act", gf[:], IN["norm_ffn_g"][i:i + 1, :].broadcast_to([128, 1024]), wr=[gf])
            for nb in range(12):
                w = wb[n % 2]
                p = ps[n % 2]
                n += 1
                cx.dma("sp" if nb % 2 == 0 else "pool", w[:], IN["ada_w"][i, :, nb * 512:(nb + 1) * 512].rearrange("(k p) n -> p k n", p=128), wr=[w])
                for k in range(8):
                    cx.M(lambda h, k=k, w=w, p=p: h.matmul(p[:], lhsT=crep[:, k, :], rhs=w[:, k, :], start=(k == 0), stop=(k == 7)), rd=[crep, w], wr=[p])
                cx.V(lambda h, p=p, nb=nb: h.tensor_tensor(out=mod[:, nb * 512:(nb + 1) * 512], in0=p[:], in1=ab[:, nb * 512:(nb + 1) * 512], op=ALU.add), rd=[p, ab], wr=[mod])
            cx.V(lambda h: h.scalar_tensor_tensor(out=mod[:, 1024:2048], in0=mod[:, 1024:2048], scalar=1.0, in1=gm[:], op0=ALU.add, op1=ALU.mult), rd=[mod, gm], wr=[mod])
            cx.V(lambda h: h.scalar_tensor_tensor(out=mod[:, 4096:5120], in0=mod[:, 4096:5120], scalar=1.0, in1=gf[:], op0=ALU.add, op1=ALU.mult), rd=[mod, gf], wr=[mod])
            for j, off in enumerate([1024, 0, 2048, 4096, 3072, 5120]):
                cx.dma("sp", MODS.t[i, j], mod[:, off:off + 1024], rd=[mod], wr=[MODS])
        P.barrier()
        P.flush()


def load_mods(cx, MODS, i, js, q="act"):
    out = []
    for j in js:
        b = cx.T([128, 1024], F32)
        cx.dma(q, b[:], MODS.t[i, j], rd=[MODS], wr=[b])
        out.append(b)
    return out


def emit_hT(cx, Xin, gs, sh, idb, hT):
    xb = [cx.T([128, 1024], F32) for _ in range(2)]
    sq = cx.T([128, 1024], F32)
    hb = [cx.T([128, 1024], BF16) for _ in range(2)]
    st2 = [cx.T([128, 4], F32) for _ in range(2)]
    ptr = [cx.PS([128, 8, 128], BF16) for _ in range(2)]
    for i in range(NT):
        x = xb[i % 2]
        cx.dma("sp", x[:], Xin.t[i * 128:(i + 1) * 128, :], rd=[Xin], wr=[x])
        emit_norm_tile(cx, x, gs, sh, hb[i % 2], sq, st2[i % 2], idb, ptr[i % 2], hT[:, :, i * 128:(i + 1) * 128], hT)


def emit_outproj(cx, Xin, Xout, catT_src, w_ap, g1, stage_f, final=None):
    wob = cx.T([128, 8, 1024], BF16)
    for k in range(8):
        cx.dma("sp", stage_f[:], w_ap[k * 128:(k + 1) * 128, :], wr=[stage_f])
        cx.G(lambda h, k=k: h.tensor_copy(out=wob[:, k, :], in_=stage_f[:]), rd=[stage_f], wr=[wob])
    py = [cx.PS([128, 1024]) for _ in range(2)]
    xb = [cx.T([128, 1024], F32) for _ in range(2)]
    yb = [cx.T([128, 1024], F32) for _ in range(2)]
    for i in range(NT):
        ap, tok = catT_src(i)
        p = py[i % 2]
        x = xb[i % 2]
        y = yb[i % 2]
        cx.dma("act", x[:], Xin.t[i * 128:(i + 1) * 128, :], rd=[Xin], wr=[x])
        for nb in range(2):
            for k in range(8):
                cx.M(lambda h, k=k, nb=nb, p=p, ap=ap: h.matmul(p[:, nb * 512:(nb + 1) * 512], lhsT=ap[:, k, :], rhs=wob[:, k, nb * 512:(nb + 1) * 512],
                                                               start=(k == 0), stop=(k == 7)), rd=[tok, wob], wr=[p])
        cx.V(lambda h, p=p, y=y: h.tensor_tensor(out=y[:], in0=p[:], in1=g1[:], op=ALU.mult), rd=[p, g1], wr=[y])
        cx.G(lambda h, x=x, y=y: h.tensor_tensor(out=y[:], in0=y[:], in1=x[:], op=ALU.add), rd=[y, x], wr=[y])
        cx.dma("sp", Xout.t[i * 128:(i + 1) * 128, :], y[:], rd=[y], wr=[Xout])


def phase_even(P, nc, IN, MODS, li, Xin, Xout, SC):
    CATT, V1D, SIGD, HFD = SC["CATT"], SC["V1D"], SC["SIGD"], SC["HFD"]
    with contextlib.ExitStack() as st0:
        c0 = Ctx(P, nc, st0)
        idf, idb, eps = load_consts(c0, IN)
        QK = c0.T([128, 8, S], BF16)
        GT = c0.T([128, NT, 16], F32)
        EB = c0.T([128, NT, 8], F32)
        ES = c0.T([128, NT, 8], F32)
        EE = c0.T([128, NT, 8], F32)
        with contextlib.ExitStack() as st1:
            cx1 = Ctx(P, nc, st1)
            hT = cx1.T([128, 8, S], BF16)
            with contextlib.ExitStack() as st2:
                c2 = Ctx(P, nc, st2)
                gs1, sh1 = load_mods(c2, MODS, li, [0, 1])
                emit_hT(c2, Xin, gs1, sh1, idb, hT)
                P.barrier()
            with contextlib.ExitStack() as stf:
                cx = Ctx(P, nc, stf)
                bT = cx.T([128, 20], F32)
                cw = cx.T([128, 8, 5], F32)
                cb = cx.T([128, 8], F32)
                edge = cx.T([128, 4, 32], F32)
                cx.dma("sp", bT[:], IN["ev_b_inT"], wr=[bT])
                cx.dma("sp", cw[:], IN["ev_conv_wT"], wr=[cw])
                cx.dma("sp", cb[:], IN["ev_conv_bT"], wr=[cb])
                cx.dma("sp", edge[:], IN["pooledge"].broadcast_to([128, 4, 32]), wr=[edge])
                wst = [cx.T([128, 8, 128], F32) for _ in range(2)]
                wcb = [cx.T([128, 8, 128], BF16) for _ in range(2)]
                zc = cx.T([128, S + 16], F32)
                pa = cx.T([128, S + 16], F32)
                yb = cx.T([128, S + 16], F32)
                ybf = cx.T([128, S], BF16)
                yo = [cx.T([128, 512], BF16) for _ in range(2)]
                pw = cx.T([128, 128], F32)
                psc = cx.T([128, 128], F32)
                pwb = cx.T([128, 128], BF16)
                pz = [cx.PS([128, 512]) for _ in range(2)]
                cx.V(lambda h: h.memset(zc[:], 0.0), wr=[zc])
                nps = 0
                for c in range(12):
                    col0 = c * 128 if c < 8 else 2048 + (c - 8) * 128
                    bcol = c if c < 8 else 16 + (c - 8)
                    ws, wc = wst[c % 2], wcb[c % 2]
                    cx.dma("sp", ws[:], IN["ev_w_in"][:, col0:col0 + 128].rearrange("(k p) n -> p k n", p=128), wr=[ws])
                    cx.G(lambda h, ws=ws, wc=wc: h.tensor_copy(out=wc[:], in_=ws[:]), rd=[ws], wr=[wc])
                    for tb in range(8):
                        p = pz[nps % 2]
                        nps += 1
                        for k in range(8):
                            cx.M(lambda h, k=k, p=p, wc=wc, tb=tb: h.matmul(p[:], lhsT=wc[:, k, :], rhs=hT[:, k, tb * 512:(tb + 1) * 512], start=(k == 0), stop=(k == 7)),
                                 rd=[wc, hT], wr=[p])
                        cx.A(lambda h, p=p, tb=tb, bcol=bcol: h.activation(out=zc[:, 8 + tb * 512:8 + (tb + 1) * 512], in_=p[:], func=AF.Identity, bias=bT[:, bcol:bcol + 1]),
                             rd=[p, bT], wr=[zc])
                    if c < 8:
                        cx.V(lambda h, c=c: h.tensor_scalar(out=yb[:, 0:S], in0=zc[:, 6:6 + S], scalar1=cw[:, c, 0:1], scalar2=None, op0=ALU.mult), rd=[zc, cw], wr=[yb])
                        for j in range(1, 5):
                            cx.V(lambda h, c=c, j=j: h.scalar_tensor_tensor(out=yb[:, 0:S], in0=zc[:, 6 + j:6 + j + S], scalar=cw[:, c, j:j + 1], in1=yb[:, 0:S], op0=ALU.mult, op1=ALU.add),
                                 rd=[zc, cw, yb], wr=[yb])
                        cx.A(lambda h, c=c: h.activation(out=QK[:, c, :], in_=yb[:, 0:S], func=AF.Silu, bias=cb[:, c:c + 1]), rd=[yb, cb], wr=[QK])
                    else:
                        g = c - 8
                        win = (2, 4, 8, 16)[g]
                        half = win // 2
                        n_el = S + 15
                        cur = zc
                        bufs = [pa, yb]
                        bi = 0
                        step = 1
                        while step < win:
                            d = bufs[bi % 2]
                            bi += 1
                            cx.V(lambda h, cur=cur, d=d, step=step, n_el=n_el: h.tensor_tensor(out=d[:, 0:n_el - step + 1], in0=cur[:, 0:n_el - step + 1], in1=cur[:, step:n_el + 1], op=ALU.add),
                                 rd=[cur], wr=[d])
                            n_el = n_el - step
                            cur = d
                            step *= 2
                        o = bufs[bi % 2]
                        cx.V(lambda h, cur=cur, half=half, o=o, win=win: h.tensor_scalar(out=o[:, 0:S], in0=cur[:, 8 - half:8 - half + S], scalar1=1.0 / win, scalar2=None, op0=ALU.mult), rd=[cur], wr=[o])
                        cx.V(lambda h, o=o, g=g: h.tensor_tensor(out=o[:, 0:16], in0=o[:, 0:16], in1=edge[:, g, 0:16], op=ALU.mult), rd=[o, edge], wr=[o])
                        cx.V(lambda h, o=o, g=g: h.tensor_tensor(out=o[:, S - 16:S], in0=o[:, S - 16:S], in1=edge[:, g, 16:32], op=ALU.mult), rd=[o, edge], wr=[o])
                        cx.V(lambda h, o=o: h.tensor_tensor(out=ybf[:], in0=o[:, 0:S], in1=zc[:, 8:8 + S], op=ALU.subtract), rd=[o, zc], wr=[ybf])
                        cx.dma("sp", pw[:], IN["ev_pool_w"][g], wr=[pw])
                        cx.dma("sp", psc[:], IN["ev_pool_scale"][0:1, g * 128:(g + 1) * 128].broadcast_to([128, 128]), wr=[psc])
                        cx.V(lambda h: h.tensor_tensor(out=pwb[:], in0=pw[:], in1=psc[:], op=ALU.mult), rd=[pw, psc], wr=[pwb])
                        for tb in range(8):
                            p = pz[nps % 2]
                            y_ = yo[nps % 2]
                            nps += 1
                            cx.M(lambda h, p=p, tb=tb: h.matmul(p[:], lhsT=pwb[:], rhs=ybf[:, tb * 512:(tb + 1) * 512], start=True, stop=True), rd=[pwb, ybf], wr=[p])
                            cx.A(lambda h, p=p, y_=y_: h.activation(out=y_[:], in_=p[:], func=AF.Copy), rd=[p], wr=[y_])
                            cx.dma("sp", CATT.t[4 + g, :, tb * 512:(tb + 1) * 512], y_[:], rd=[y_], wr=[CATT])
                P.barrier()
            with contextlib.ExitStack() as stt:
                cx = Ctx(P, nc, stt)
                stage_f = cx.T([128, 1024], F32)
                wtm = cx.T([128, 8, 1040], BF16)
                for k in range(8):
                    cx.dma("sp", stage_f[:], IN["ev_w_in"][k * 128:(k + 1) * 128, 1024:2048], wr=[stage_f])
                    cx.V(lambda h, k=k: h.tensor_copy(out=wtm[:, k, 0:1024], in_=stage_f[:]), rd=[stage_f], wr=[wtm])
                gst = cx.T([128, 8, 16], F32)
                cx.dma("sp", gst[:], IN["ev_w_in"][:, 2560:2576].rearrange("(k p) n -> p k n", p=128), wr=[gst])
                cx.V(lambda h: h.tensor_copy(out=wtm[:, :, 1024:1040], in_=gst[:]), rd=[gst], wr=[wtm])
                bvo = cx.T([128, 1024], F32)
                bg = cx.T([128, 16], F32)
                cx.dma("sp", bvo[:], IN["ev_b_in"][0:1, 1024:2048].broadcast_to([128, 1024]), wr=[bvo])
                cx.dma("sp", bg[:], IN["ev_b_in"][0:1, 2560:2576].broadcast_to([128, 16]), wr=[bg])
                pv = [cx.PS([128, 512]) for _ in range(2)]
                po = [cx.PS([128, 512]) for _ in range(2)]
                pg = [cx.PS([128, 16]) for _ in range(2)]
                v1 = [cx.T([128, 4, 130], BF16) for _ in range(2)]
                of = [cx.T([128, 512], F32) for _ in range(2)]
                ob = [cx.T([128, 512], BF16) for _ in range(2)]
                for b_ in v1:
                    cx.V(lambda h, b_=b_: h.memset(b_[:], 1.0), wr=[b_])
                for i in range(NT):
                    a = i % 2
                    for (p, c0_, c1_) in ((pv[a], 0, 512), (po[a], 512, 1024), (pg[a], 1024, 1040)):
                        for k in range(8):
                            cx.M(lambda h, k=k, p=p, c0_=c0_, c1_=c1_, i=i: h.matmul(p[:], lhsT=hT[:, k, i * 128:(i + 1) * 128], rhs=wtm[:, k, c0_:c1_], start=(k == 0), stop=(k == 7)),
                                 rd=[hT, wtm], wr=[p])
                    for hh in range(4):
                        cx.V(lambda h, a=a, hh=hh: h.tensor_tensor(out=v1[a][:, hh, 0:128], in0=pv[a][:, hh * 128:(hh + 1) * 128],
                                                                   in1=bvo[:, hh * 128:(hh + 1) * 128], op=ALU.add), rd=[pv[a], bvo], wr=[v1[a]])
                    cx.dma("sp", V1D.t[i], v1[a][:], rd=[v1[a]], wr=[V1D])
                    cx.V(lambda h, a=a: h.tensor_tensor(out=of[a][:], in0=po[a][:], in1=bvo[:, 512:1024], op=ALU.add), rd=[po[a], bvo], wr=[of[a]])
                    cx.A(lambda h, a=a: h.activation(out=ob[a][:], in_=of[a][:], func=AF.Sigmoid), rd=[of[a]], wr=[ob[a]])
                    if i == 0:
                        cx.dma("sp", SC["DBG2"].t, of[a][:], rd=[of[a]], wr=[SC["DBG2"]])
                    cx.dma("sp", SIGD.t[i], ob[a][:], rd=[ob[a]], wr=[SIGD])
                    cx.V(lambda h, a=a, i=i: h.tensor_tensor(out=GT[:, i, :], in0=pg[a][:], in1=bg[:], op=ALU.add), rd=[pg[a], bg], wr=[GT])
                cx.dma("sp", SC["DBG1"].t, GT[:], rd=[GT], wr=[SC["DBG1"]])
                P.barrier()
            with contextlib.ExitStack() as stg:
                cx = Ctx(P, nc, stg)
                LF = cx.T([128, NT, 8], F32)
                t8 = cx.T([128, NT, 8], F32)
                BC = cx.T([128, NT, 16], F32)
                cx.A(lambda h: h.activation(out=t8[:], in_=GT[:, :, 8:16], func=AF.Exp, scale=-1.0), rd=[GT], wr=[t8])
                cx.V(lambda h: h.tensor_scalar(out=t8[:], in0=t8[:], scalar1=1.0, scalar2=None, op0=ALU.add), rd=[t8], wr=[t8])
                cx.A(lambda h: h.activation(out=LF[:], in_=t8[:], func=AF.Ln), rd=[t8], wr=[LF])
                cx.V(lambda h: h.tensor_scalar(out=LF[:], in0=LF[:], scalar1=-1.0, scalar2=None, op0=ALU.mult), rd=[LF], wr=[LF])
                triU = cx.T([128, 128], F32)
                triL = cx.T([128, 128], F32)
                ones = cx.T([128, 128], F32)
                cx.dma("sp", triU[:], IN["triU"], wr=[triU])
                cx.dma("sp", triL[:], IN["triL"], wr=[triL])
                cx.V(lambda h: h.memset(ones[:], 1.0), wr=[ones])
                pc = cx.PS([128, NT, 16])
                for i in range(NT):
                    cx.M(lambda h, i=i: h.matmul(pc[:, i, 0:4], lhsT=triU[:], rhs=LF[:, i, 0:4], start=True, stop=True), rd=[triU, LF], wr=[pc])
                    cx.M(lambda h, i=i: h.matmul(pc[:, i, 4:8], lhsT=triL[:], rhs=LF[:, i, 4:8], start=True, stop=True), rd=[triL, LF], wr=[pc])
                    cx.M(lambda h, i=i: h.matmul(pc[:, i, 8:16], lhsT=ones[:], rhs=LF[:, i, 0:8], start=True, stop=True), rd=[ones, LF], wr=[pc])
                cx.V(lambda h: h.tensor_copy(out=BC[:], in_=pc[:]), rd=[pc], wr=[BC])
                cx.A(lambda h: h.activation(out=EB[:], in_=BC[:, :, 0:8], func=AF.Exp), rd=[BC], wr=[EB])
                cx.A(lambda h: h.activation(out=EE[:], in_=BC[:, :, 8:16], func=AF.Exp), rd=[BC], wr=[EE])
                cx.V(lambda h: h.tensor_tensor(out=t8[:], in0=GT[:, :, 0:8], in1=BC[:, :, 0:8], op=ALU.subtract), rd=[GT, BC], wr=[t8])
                cx.V(lambda h: h.tensor_scalar(out=t8[:], in0=t8[:], scalar1=float(-0.5 * np.log(128.0)), scalar2=None, op0=ALU.add), rd=[t8], wr=[t8])
                cx.A(lambda h: h.activation(out=ES[:], in_=t8[:], func=AF.Exp), rd=[t8], wr=[ES])
                P.barrier()
        with contextlib.ExitStack() as st3:
            cx = Ctx(P, nc, st3)
            mk = []
            for nm in ("triU", "triL"):
                f = cx.T([128, 128], F32)
                cx.dma("sp", f[:], IN[nm], wr=[f])
                mk.append(f)
            Cst = cx.T([128, 8, 129], F32)
            Cb = cx.T([128, 8, 129], BF16)
            cx.V(lambda h: h.memset(Cst[:], 0.0), wr=[Cst])
            cx.V(lambda h: h.memset(Cb[:], 0.0), wr=[Cb])
            NV = 6
            v1 = [cx.T([128, 4, 130], BF16) for _ in range(NV)]
            psS = [cx.PS([128, 4, 128]) for _ in range(2)]
            psA = [cx.PS([128, 3, 129]) for _ in range(3)]
            ps_t = cx.PS([128, 8, 128], BF16)
            ps_h = cx.PS([128, 4, 128], BF16)
            AT = cx.T([128, 8, 128], BF16)
            ksb = cx.T([128, 8, 128], BF16)
            sm = cx.T([128, 8, 4], F32)
            hacc = cx.T([128, NT, 512], F32)
            NSG = 6
            sgl = [cx.T([128, 512], BF16) for _ in range(NSG)]
            sq = cx.T([128, 512], F32)
            st4 = [cx.T([128, 12], F32) for _ in range(2)]
            mg = cx.T([128, 512], F32)
            hmb = [cx.T([128, 512], BF16) for _ in range(2)]
            hmT = [cx.T([128, 4, 128], BF16) for _ in range(2)]
            cx.dma("sp", mg[:], IN["ev_mnorm_g"][0:1, :].broadcast_to([128, 512]), wr=[mg])
            bg = SC.get("BG0")
            if bg is not None:
                bg.attach(cx)
            chains = [(d, hh) for d in range(2) for hh in range(4)]

            def aslot(c):
                return psA[c // 3], c % 3

            def tile_of(s, d):
                return s if d == 0 else NT - 1 - s

            PDV = 2
            nfin = 0
            for s in range(NT + PDV):
                if s < NT:
                    for d in range(2):
                        vb = v1[(2 * s + d) % NV]
                        cx.dma("sp", vb[:], V1D.t[tile_of(s, d)], rd=[V1D], wr=[vb])
                    if s >= NT // 2:
                        for d in range(2):
                            sg_ = sgl[(2 * s + d) % NSG]
                            cx.dma("act", sg_[:], SIGD.t[tile_of(s, d)], rd=[SIGD], wr=[sg_])
                s_ = s - PDV
                if s_ < 0:
                    continue
                s = s_
                if bg is not None:
                    bg.step(4)
                vbs = [v1[(2 * s + d) % NV] for d in range(2)]
                tls = [tile_of(s, d) for d in range(2)]
                for c, (d, hh) in enumerate(chains):
                    tsl = slice(tls[d] * 128, (tls[d] + 1) * 128)
                    cx.M(lambda h: h.matmul(psS[d][:, hh, :], lhsT=QK[:, 4 + hh, tsl], rhs=QK[:, hh, tsl], start=True, stop=True), rd=[QK], wr=[psS[d]])
                for c, (d, hh) in enumerate(chains):
                    col = d * 4 + hh
                    cx.V(lambda h: h.scalar_tensor_tensor(out=AT[:, c, :], in0=psS[d][:, hh, :], scalar=ES[:, tls[d], col:col + 1], in1=mk[d][:], op0=ALU.mult, op1=ALU.mult),
                         rd=[psS[d], ES, mk[d]], wr=[AT])
                for c, (d, hh) in enumerate(chains):
                    col = d * 4 + hh
                    tsl = slice(tls[d] * 128, (tls[d] + 1) * 128)
                    pa, sl = aslot(c)
                    cx.M(lambda h: h.matmul(pa[:, sl, :], lhsT=AT[:, c, :], rhs=vbs[d][:, hh, 0:129], start=True, stop=False), rd=[AT, vbs[d]], wr=[pa])
                    cx.M(lambda h: h.matmul(pa[:, sl, :], lhsT=QK[:, hh, tsl], rhs=Cb[:, col, :], start=False, stop=True), rd=[QK, Cb], wr=[pa])
                for c, (d, hh) in enumerate(chains):
                    col = d * 4 + hh
                    pa, sl = aslot(c)
                    cx.A(lambda h: h.activation(out=sm[:, c, 2:3], in_=pa[:, sl, 128:129], func=AF.Abs, scale=EB[:, tls[d], col:col + 1]), rd=[pa, EB], wr=[sm])
                cx.V(lambda h: h.tensor_scalar(out=sm[:, :, 0:1], in0=sm[:, :, 2:3], scalar1=1.0, scalar2=None, op0=ALU.max), rd=[sm], wr=[sm])
                cx.V(lambda h: h.reciprocal(out=sm[:, :, 3:4], in_=sm[:, :, 0:1]), rd=[sm], wr=[sm])
                for d in range(2):
                    cx.V(lambda h: h.tensor_tensor(out=sm[:, d * 4:(d + 1) * 4, 1:2], in0=EB[:, tls[d], d * 4:(d + 1) * 4].unsqueeze(2), in1=sm[:, d * 4:(d + 1) * 4, 3:4], op=ALU.mult),
                         rd=[sm, EB], wr=[sm])
                for c, (d, hh) in enumerate(chains):
                    pa, sl = aslot(c)
                    dst = hacc[:, tls[d], hh * 128:(hh + 1) * 128]
                    if s < NT // 2:
                        cx.A(lambda h: h.activation(out=dst, in_=pa[:, sl, 0:128], func=AF.Copy, scale=sm[:, c, 1:2]), rd=[pa, sm], wr=[hacc])
                    else:
                        cx.V(lambda h: h.scalar_tensor_tensor(out=dst, in0=pa[:, sl, 0:128], scalar=sm[:, c, 1:2], in1=dst, op0=ALU.mult, op1=ALU.add), rd=[pa, sm, hacc], wr=[hacc])
                for c, (d, hh) in enumerate(chains):
                    tsl = slice(tls[d] * 128, (tls[d] + 1) * 128)
                    cx.M(lambda h: h.transpose(out=ps_t[:, c, :], in_=QK[:, 4 + hh, tsl], identity=idb[:]), rd=[QK, idb], wr=[ps_t])
                for c, (d, hh) in enumerate(chains):
                    col = d * 4 + hh
                    cx.A(lambda h: h.activation(out=ksb[:, c, :], in_=ps_t[:, c, :], func=AF.Copy, scale=ES[:, tls[d], col:col + 1]), rd=[ps_t, ES], wr=[ksb])
                for c, (d, hh) in enumerate(chains):
                    pa, sl = aslot(c)
                    cx.M(lambda h: h.matmul(pa[:, sl, :], lhsT=ksb[:, c, :], rhs=vbs[d][:, hh, 0:129], start=True, stop=True), rd=[ksb, vbs[d]], wr=[pa])
                for c, (d, hh) in enumerate(chains):
                    col = d * 4 + hh
                    pa, sl = aslot(c)
                    cx.V(lambda h: h.tensor_scalar(out=Cst[:, col, :], in0=Cst[:, col, :], scalar1=EE[:, tls[d], col:col + 1], scalar2=None, op0=ALU.mult), rd=[Cst, EE], wr=[Cst])
                    cx.V(lambda h: h.scalar_tensor_tensor(out=Cst[:, col, :], in0=pa[:, sl, :], scalar=EE[:, tls[d], col:col + 1], in1=Cst[:, col, :], op0=ALU.mult, op1=ALU.add),
                         rd=[pa, EE, Cst], wr=[Cst])
                cx.A(lambda h: h.activation(out=Cb[:], in_=Cst[:], func=AF.Copy), rd=[Cst], wr=[Cb])
                if s >= NT // 2:
                    for d in range(2):
                        i = tls[d]
                        a = nfin % 2
                        nfin += 1
                        sg_ = sgl[(2 * s + d) % NSG]
                        hv = hacc[:, i, :]
                        cx.V(lambda h: h.tensor_tensor(out=sq[:], in0=hv, in1=hv, op=ALU.mult), rd=[hacc], wr=[sq])
                        cx.V(lambda h: h.tensor_reduce(out=st4[a][:, 0:4], in_=sq[:].rearrange("p (h d) -> p h d", h=4), axis=AX.X, op=ALU.add), rd=[sq], wr=[st4[a]])
                        cx.A(lambda h: h.activation(out=st4[a][:, 4:8], in_=st4[a][:, 0:4], func=AF.Sqrt, scale=1.0 / 128, bias=eps[:, 0:1]), rd=[st4[a], eps], wr=[st4[a]])
                        cx.V(lambda h: h.reciprocal(out=st4[a][:, 8:12], in_=st4[a][:, 4:8]), rd=[st4[a]], wr=[st4[a]])
                        cx.V(lambda h: h.tensor_tensor(out=hv.rearrange("p (h d) -> p h d", h=4), in0=hv.rearrange("p (h d) -> p h d", h=4),
                                                       in1=st4[a][:, 8:12].unsqueeze(2).broadcast_to([128, 4, 128]), op=ALU.mult), rd=[hacc, st4[a]], wr=[hacc])
                        cx.V(lambda h: h.tensor_tensor(out=sq[:], in0=mg[:], in1=sg_[:], op=ALU.mult), rd=[mg, sg_], wr=[sq])
                        cx.V(lambda h: h.tensor_tensor(out=hmb[a][:], in0=hv, in1=sq[:], op=ALU.mult), rd=[hacc, sq], wr=[hmb[a]])
                        for k in range(4):
                            cx.M(lambda h: h.transpose(out=ps_h[:, k, :], in_=hmb[a][:, k * 128:(k + 1) * 128], identity=idb[:]), rd=[hmb[a], idb], wr=[ps_h])
                        cx.A(lambda h: h.activation(out=hmT[a][:], in_=ps_h[:], func=AF.Copy), rd=[ps_h], wr=[hmT[a]])
                        cx.dma("act", CATT.t[0:4, :, i * 128:(i + 1) * 128].rearrange("c f t -> f c t"), hmT[a][:], rd=[hmT[a]], wr=[CATT])
            if bg is not None:
                bg.finish()
                SC.setdefault("TABDONE", {})[0] = True
            P.barrier()
        with contextlib.ExitStack() as st4_:
            cx = Ctx(P, nc, st4_)
            cb_ = [cx.T([128, 8, 128], BF16) for _ in range(3)]
            g1, = load_mods(cx, MODS, li, [2])
            stage_f = cx.T([128, 1024], F32)

            def src(i):
                b = cb_[i % 3]
                cx.dma("pool", b[:], CATT.t[:, :, i * 128:(i + 1) * 128].rearrange("c f t -> f c t"), rd=[CATT], wr=[b])
                return b, b
            emit_outproj(cx, Xin, Xout, src, IN["ev_w_out"], g1, stage_f)
            P.barrier()
        P.flush()


W_SPECS = {
    "ada_w": [2, 1024, 6144], "ada_b": [2, 6144], "norm_mix_g": [2, 1024], "norm_ffn_g": [2, 1024],
    "ev_w_in": [1024, 2576], "ev_b_in": [1, 2576], "ev_b_inT": [128, 20], "ev_conv_wT": [128, 8, 5], "ev_conv_bT": [128, 8],
    "ev_mnorm_g": [1, 512], "ev_pool_w": [4, 128, 128], "ev_pool_scale": [1, 512], "ev_w_out": [1024, 1024],
    "od_w_in": [1024, 1536], "od_qnorm_g": [1, 128], "od_knorm_g": [1, 128], "od_w_out": [1024, 1024],
    "peer_w_q": [2, 1024, 2048], "peer_keys": [2, 2, 128, 128], "peer_u": [2, 16384, 1024], "peer_v": [2, 16384, 1024],
    "final_g": [1, 1024],
    "ident": [128, 128], "triU": [128, 128], "triL": [128, 128], "pooledge": [1, 4, 32], "ropecs": [128, 2, NT, 64], "iota16": [1, 16],
    "cT": [128, 8],
}


def host_consts():
    cn = {}
    cn["ident"] = np.eye(128, dtype=np.float32)
    s_ = np.arange(128)
    cn["triU"] = (s_[:, None] <= s_[None, :]).astype(np.float32)
    cn["triL"] = (s_[:, None] >= s_[None, :]).astype(np.float32)
    pe = np.zeros((1, 4, 32), np.float32)
    for g, w in enumerate((2, 4, 8, 16)):
        for j in range(32):
            t = j if j < 16 else S - 32 + j
            lo = max(t - w // 2, 0)
            hi = min(t + w // 2, S)
            pe[0, g, j] = w / float(hi - lo)
    cn["pooledge"] = pe
    t = np.arange(S)
    r, c = t // 64, t % 64
    freqs = (10000.0 ** (-np.arange(0, 64, 2, dtype=np.float32) / 64.0)).astype(np.float32)
    ang = np.concatenate([r[:, None].astype(np.float32) * freqs, c[:, None].astype(np.float32) * freqs], axis=-1).astype(np.float32)
    cs = np.stack([np.cos(ang), np.sin(ang)], 0).astype(np.float32)
    cn["ropecs"] = np.ascontiguousarray(cs.reshape(2, NT, 128, 64).transpose(2, 0, 1, 3))
    cn["iota16"] = np.arange(16, dtype=np.float32)[None, :]
    return cn


DEBUG = [False]


def build(first, last):
    nc = bass.Bass("TRN2", target_bir_lowering=False)
    IK = "ExternalOutput" if DEBUG[0] else "Internal"
    IN = {k: nc.dram_tensor(k, v, F32, kind="ExternalInput").ap() for k, v in W_SPECS.items()}
    xin = nc.dram_tensor("xin", [S, D], F32, kind="ExternalInput").ap()
    out = nc.dram_tensor("out", [S, D], F32, kind="ExternalOutput").ap()
    X = {}
    for k in range(0, 5):
        if k == first - 1:
            X[k] = Buf(xin)
        elif k == last:
            X[k] = Buf(out)
        elif first <= k < last:
            X[k] = Buf(nc.dram_tensor("X%d" % k, [S, D], F32, kind="Internal").ap())
    SC = {
        "CATT": Buf(nc.dram_tensor("CATT", [8, 128, S], BF16, kind=IK).ap()),
        "V1D": Buf(nc.dram_tensor("V1D", [NT, 128, 4, 130], BF16, kind=IK).ap()),
        "SIGD": Buf(nc.dram_tensor("SIGD", [NT, 128, 512], BF16, kind=IK).ap()),
        "HFD": Buf(nc.dram_tensor("HFD", [NT, 128, 512], F32, kind=IK).ap()),
        "TAB": [Buf(nc.dram_tensor("TAB%d" % i, [16384, 2048], BF16, kind="Internal").ap()) for i in range(2)],
    }
    SC["DBG1"] = Buf(nc.dram_tensor("DBG1", [128, NT, 16], F32, kind=IK).ap())
    SC["DBG2"] = Buf(nc.dram_tensor("DBG2", [128, 512], F32, kind=IK).ap())
    MODS = Buf(nc.dram_tensor("MODS", [2, 6, 128, 1024], F32, kind=IK).ap())
    with contextlib.ExitStack() as st:
        P = Prog(nc, st)
        phase_mods(P, nc, IN, MODS)
        if first <= 1 and last >= 2:
            SC["BG0"] = TableBuilder(P, nc, IN, 0, SC["TAB"][0])
        if first <= 3 and last >= 4:
            SC["BG1"] = TableBuilder(P, nc, IN, 1, SC["TAB"][1])
        for ph in range(first, last + 1):
            if ph == 1:
                phase_even(P, nc, IN, MODS, 0, X[0], X[1], SC)
            elif ph == 2:
                phase_peer(P, nc, IN, MODS, 0, X[1], X[2], SC, final=False)
            elif ph == 3:
                phase_attn(P, nc, IN, MODS, 1, X[2], X[3], SC)
            elif ph == 4:
                phase_peer(P, nc, IN, MODS, 1, X[3], X[4], SC, final=True)
    return nc


def host_inputs(inputs):
    f = lambda a: np.ascontiguousarray(np.asarray(a, dtype=np.float32))
    sh = {}
    sh["ada_w"] = f(inputs["ada_w"]); sh["ada_b"] = f(inputs["ada_b"])
    sh["norm_mix_g"] = f(inputs["norm_mix_g"]); sh["norm_ffn_g"] = f(inputs["norm_ffn_g"])
    sh["ev_w_in"] = f(inputs["ev_w_in"][0]); sh["ev_b_in"] = f(inputs["ev_b_in"])
    sh["ev_b_inT"] = f(np.asarray(inputs["ev_b_in"])[0, :2560].reshape(20, 128).T)
    cw = np.asarray(inputs["ev_conv_w"])[0, :, 0, :]
    sh["ev_conv_wT"] = f(cw.T.reshape(8, 128, 5).transpose(1, 0, 2))
    sh["ev_conv_bT"] = f(np.asarray(inputs["ev_conv_b"])[0].reshape(8, 128).T)
    sh["ev_mnorm_g"] = f(inputs["ev_mnorm_g"]); sh["ev_pool_w"] = f(inputs["ev_pool_w"][0]); sh["ev_pool_scale"] = f(inputs["ev_pool_scale"])
    sh["ev_w_out"] = f(inputs["ev_w_out"][0])
    sh["od_w_in"] = f(inputs["od_w_in"][0]); sh["od_qnorm_g"] = f(inputs["od_qnorm_g"]); sh["od_knorm_g"] = f(inputs["od_knorm_g"])
    sh["od_w_out"] = f(inputs["od_w_out"][0])
    sh["peer_w_q"] = f(inputs["peer_w_q"]); sh["peer_keys"] = f(inputs["peer_keys"])
    sh["peer_u"] = f(inputs["peer_u"]); sh["peer_v"] = f(inputs["peer_v"])
    sh["final_g"] = f(np.asarray(inputs["final_g"])[None, :])
    sh.update(host_consts())
    return sh


def run_phases(inputs, first, last, xin_list, cores):
    nc = build(first, last)
    sh = host_inputs(inputs)
    c = np.asarray(inputs["c"], dtype=np.float32)
    in_maps = []
    for j, b in enumerate(cores):
        m = dict(sh)
        m["cT"] = np.ascontiguousarray(c[b].reshape(8, 128).T)
        m["xin"] = np.ascontiguousarray(xin_list[j], dtype=np.float32)
        in_maps.append(m)
    res = run_bass_kernel_spmd(nc, in_maps, core_ids=list(range(len(cores))))
    if DEBUG[0]:
        return res.results
    return [r["out"] for r in res.results]


def kernel(**inputs):
    x = np.asarray(inputs["x"], dtype=np.float32)
    outs = run_phases(inputs, 1, 4, [x[b] for b in range(8)], list(range(8)))
    return np.stack(outs, 0).astype(np.float32)


class TableBuilder:
    def __init__(self, P, nc, IN, li, TAB):
        self.P, self.nc, self.IN, self.li, self.TAB = P, nc, IN, li, TAB
        self.blocks = [(half, blk) for half in range(2) for blk in range(64)]
        self.k = 0
        self.loaded = 0
        self.cx = None

    def attach(self, cx):
        self.cx = cx
        self.sf = [cx.T([128, 2, 1024], F32) for _ in range(2)]
        self.sb = [cx.T([128, 2, 1024], BF16) for _ in range(2)]

    def _load(self):
        if self.loaded >= len(self.blocks):
            return
        half, blk = self.blocks[self.loaded]
        f = self.sf[self.loaded % 2]
        rows = slice(blk * 256, (blk + 1) * 256)
        self.cx.dma("pool", f[:], self.IN[("peer_u", "peer_v")[half]][self.li, rows, :].rearrange("(n p) d -> p n d", p=128), wr=[f])
        self.loaded += 1

    def step(self, n=1):
        for _ in range(n):
            if self.k >= len(self.blocks):
                return
            if self.loaded == self.k:
                self._load()
            self._load()
            half, blk = self.blocks[self.k]
            f, b = self.sf[self.k % 2], self.sb[self.k % 2]
            rows = slice(blk * 256, (blk + 1) * 256)
            self.cx.G(lambda h: h.tensor_copy(out=b[:], in_=f[:]), rd=[f], wr=[b])
            self.cx.dma("pool", self.TAB.t[rows, half * 1024:(half + 1) * 1024].rearrange("(n p) d -> p n d", p=128), b[:], rd=[b], wr=[self.TAB])
            self.k += 1

    def finish(self):
        self.step(len(self.blocks))
        self.cx = None

    @property
    def done(self):
        return self.k >= len(self.blocks)


POOL_DOTS = False
PROD_DT = BF16
STT_SLOTS = (0, 1, 2, 3)


def phase_peer(P, nc, IN, MODS, li, Xin, Xout, SC, final):
    TAB = SC["TAB"][li]
    with contextlib.ExitStack() as st:
        cx = Ctx(P, nc, st)
        prebuilt = bool(SC.get("TABDONE", {}).get(li))
        sf = [cx.T([128, 4, 1024], F32) for _ in range(0 if prebuilt else 3)]
        sb = [cx.T([128, 4, 1024], BF16) for _ in range(0 if prebuilt else 3)]
        n = 0
        for half, nm in enumerate(("peer_u", "peer_v")):
            for blk in range(0 if prebuilt else 32):
                f, b = sf[n % 3], sb[n % 3]
                rows = slice(blk * 512, (blk + 1) * 512)
                cx.dma("sp", f[:], IN[nm][li, rows, :].rearrange("(n p) d -> p n d", p=128), wr=[f])
                if n % 3 == 0:
                    cx.A(lambda h: h.activation(out=b[:], in_=f[:], func=AF.Copy), rd=[f], wr=[b])
                elif n % 3 == 1:
                    cx.V(lambda h: h.tensor_copy(out=b[:], in_=f[:]), rd=[f], wr=[b])
                else:
                    cx.G(lambda h: h.tensor_copy(out=b[:], in_=f[:]), rd=[f], wr=[b])
                cx.dma("act", TAB.t[rows, half * 1024:(half + 1) * 1024].rearrange("(n p) d -> p n d", p=128), b[:], rd=[b], wr=[TAB])
                n += 1
        P.barrier()
        P.flush()
    with contextlib.ExitStack() as st:
        cx = Ctx(P, nc, st)
        idf, idb, eps = load_consts(cx, IN)
        gs2, sh2, g2 = load_mods(cx, MODS, li, [3, 4, 5])
        io16 = cx.T([128, 16], F32)
        th16 = cx.T([128, 16], F32)
        cx.dma("sp", io16[:], IN["iota16"].broadcast_to([128, 16]), wr=[io16])
        cx.V(lambda h: h.tensor_scalar(out=th16[:], in0=io16[:], scalar1=16.0, scalar2=None, op0=ALU.mult), rd=[io16], wr=[th16])
        if final:
            fg = cx.T([128, 1024], F32)
            cx.dma("sp", fg[:], IN["final_g"].broadcast_to([128, 1024]), wr=[fg])
        wq = cx.T([128, 8, 2048], BF16)
        ptr = cx.PS([128, 8, 128], BF16)
        keysT = cx.T([128, 2, 128], BF16)
        with contextlib.ExitStack() as stw:
            cw_ = Ctx(P, nc, stw)
            stg = [cw_.T([128, 2048], F32) for _ in range(2)]
            for k in range(8):
                cw_.dma("sp", stg[k % 2][:], IN["peer_w_q"][li, k * 128:(k + 1) * 128, :], wr=[stg[k % 2]])
                cw_.V(lambda h: h.tensor_copy(out=wq[:, k, :], in_=stg[k % 2][:]), rd=[stg[k % 2]], wr=[wq])
            kf = cw_.T([128, 2, 128], F32)
            kb = cw_.T([128, 2, 128], BF16)
            cw_.dma("sp", kf[:], IN["peer_keys"][li].rearrange("t n c -> n t c"), wr=[kf])
            cw_.V(lambda h: h.tensor_copy(out=kb[:], in_=kf[:]), rd=[kf], wr=[kb])
            for t in range(2):
                cw_.M(lambda h: h.transpose(out=ptr[:, t, :], in_=kb[:, t, :], identity=idb[:]), rd=[kb, idb], wr=[ptr])
            cw_.A(lambda h: h.activation(out=keysT[:], in_=ptr[:, 0:2, :], func=AF.Copy), rd=[ptr], wr=[keysT])
            P.barrier()
        pq = [cx.PS([128, 512]) for _ in range(2)]
        psc = [cx.PS([128, 4, 128]) for _ in range(2)]
        po = cx.PS([128, 1024])
        xb = [cx.T([128, 1024], F32) for _ in range(2)]
        sq = cx.T([128, 1024], F32)
        hb = cx.T([128, 1024], BF16)
        hTi = cx.T([128, 8, 128], BF16)
        st2 = cx.T([128, 4], F32)
        qb = cx.T([128, 2048], BF16)
        rs = cx.T([128, 48], F32)
        qT = cx.T([128, 16, 128], BF16)
        S1 = cx.T([128, 16, 128], F32)
        sqq = Buf(S1.t)
        sqq.k = S1.k
        sqq_ap = S1.t[:].rearrange("p g c -> p (g c)")
        wk = [cx.T([128, 128], F32) for _ in range(2)]
        m = cx.T([128, 16, 16], F32)
        ix = cx.T([128, 16, 16], U32)
        ixf = cx.T([128, 16, 16], F32)
        CS = cx.T([128, 8, 256], F32)
        wk2 = [cx.T([128, 256], F32) for _ in range(2)]
        tops = cx.T([128, 8, 16], F32)
        pos = cx.T([128, 8, 16], U32)
        posf = cx.T([128, 8, 16], F32)
        af = cx.T([128, 8, 16], F32)
        bf_ = cx.T([128, 8, 16], F32)
        oh = Buf(CS.t)
        oh.k = CS.k
        oh_ap = CS.t[:].rearrange("p h (a b) -> p h a b", a=16)
        i12 = cx.T([128, 2, 128], F32)
        idxf = cx.T([128, 128], F32)
        idx = cx.T([128, 128], I32)
        ge = cx.T([128, 8, 16], F32)
        gsum = cx.T([128, 16], F32)
        gate = cx.T([128, 128], F32)
        act = cx.T([128, 128], F32)
        gl = cx.T([128, 128], F32)
        coef = cx.T([128, 128], F32)
        NG = 5
        actT = [Tok() for _ in range(4)]
        glT = [Tok() for _ in range(4)]
        coefT = [Tok() for _ in range(4)]
        Gb = [cx.T([128, 4, 2048], BF16) for _ in range(NG)]
        Gtok = [[Tok() for _ in range(4)] for _ in range(NG)]
        diag = [cx.T([128, 4, 128], BF16) for _ in range(2)]
        junk = cx.T([128, 1024], BF16)
        NPR = 1
        junkv = cx.T([128, 1024], BF16)
        prods = [cx.T([128, 1024], PROD_DT) for _ in range(NPR)]
        yb = [cx.T([128, 1024], F32) for _ in range(1)]
        sgn = 0
        hbs = [hb, cx.T([128, 1024], BF16)]
        idxs = [idx, cx.T([128, 128], I32)]
        gates = [gate, cx.T([128, 128], F32)]
        st3 = cx.T([128, 4], F32)
        sq2 = cx.T([128, 1024], F32) if final else None
        fg_ = fg if final else None
        FSTEP = 2

        def front(i):
            x = xb[i % 2]
            hb = hbs[i % 2]
            idx = idxs[i % 2]
            gate = gates[i % 2]
            x = xb[i % 2]
            yield
            cx.dma("sp", x[:], Xin.t[i * 128:(i + 1) * 128, :], rd=[Xin], wr=[x])
            yield
            emit_norm_tile(cx, x, gs2, sh2, hb, sq, st2, idb, ptr, hTi[:], hTi)
            for nb in range(4):
                p = pq[nb % 2]
                for k in range(8):
                    yield
                    cx.M(lambda h: h.matmul(p[:], lhsT=hTi[:, k, :], rhs=wq[:, k, nb * 512:(nb + 1) * 512], start=(k == 0), stop=(k == 7)), rd=[hTi, wq], wr=[p])
                yield
                cx.A(lambda h: h.activation(out=qb[:, nb * 512:(nb + 1) * 512], in_=p[:], func=AF.Copy), rd=[p], wr=[qb])
            yield
            cx.V(lambda h: h.tensor_tensor(out=sqq_ap, in0=qb[:], in1=qb[:], op=ALU.mult), rd=[qb], wr=[sqq])
            yield
            cx.V(lambda h: h.tensor_reduce(out=rs[:, 0:16], in_=S1.t[:], axis=AX.X, op=ALU.add), rd=[sqq], wr=[rs])
            yield
            cx.A(lambda h: h.activation(out=rs[:, 16:32], in_=rs[:, 0:16], func=AF.Sqrt, scale=1.0 / 128, bias=eps[:, 0:1]), rd=[rs, eps], wr=[rs])
            yield
            cx.V(lambda h: h.reciprocal(out=rs[:, 32:48], in_=rs[:, 16:32]), rd=[rs], wr=[rs])
            for r in range(2):
                for j in range(8):
                    g_ = r * 8 + j
                    yield
                    cx.M(lambda h: h.transpose(out=ptr[:, j, :], in_=qb[:, g_ * 128:(g_ + 1) * 128], identity=idb[:]), rd=[qb, idb], wr=[ptr])
                yield
                cx.A(lambda h: h.activation(out=qT[:, r * 8:(r + 1) * 8, :], in_=ptr[:], func=AF.Copy), rd=[ptr], wr=[qT])
            for r in range(4):
                ps_ = psc[r % 2]
                for j in range(4):
                    hp = r * 4 + j
                    yield
                    cx.M(lambda h: h.matmul(ps_[:, j, :], lhsT=qT[:, hp, :], rhs=keysT[:, hp % 2, :], start=True, stop=True), rd=[qT, keysT], wr=[ps_])
                yield
                cx.V(lambda h: h.tensor_tensor(out=S1[:, r * 4:(r + 1) * 4, :], in0=ps_[:], in1=rs[:, 32 + r * 4:32 + (r + 1) * 4].unsqueeze(2).broadcast_to([128, 4, 128]), op=ALU.mult),
                     rd=[ps_, rs], wr=[S1])
            for hp in range(16):
                w_ = wk[hp % 2]
                yield
                cx.V(lambda h: h.max(out=m[:, hp, 0:8], in_=S1[:, hp, :]), rd=[S1], wr=[m])
                yield
                cx.V(lambda h: h.max_index(out=ix[:, hp, 0:8], in_max=m[:, hp, 0:8], in_values=S1[:, hp, :]), rd=[m, S1], wr=[ix])
                yield
                cx.V(lambda h: h.match_replace(out=w_[:], in_to_replace=m[:, hp, 0:8], in_values=S1[:, hp, :], imm_value=-1e30), rd=[m, S1], wr=[w_])
                yield
                cx.V(lambda h: h.max(out=m[:, hp, 8:16], in_=w_[:]), rd=[w_], wr=[m])
                yield
                cx.V(lambda h: h.max_index(out=ix[:, hp, 8:16], in_max=m[:, hp, 8:16], in_values=w_[:]), rd=[m, w_], wr=[ix])
            mv = m[:].rearrange("p (h t) k -> p h t k", t=2)
            yield
            cx.V(lambda h: h.tensor_tensor(out=CS[:].rearrange("p h (a b) -> p h a b", a=16), in0=mv[:, :, 0, :].unsqueeze(3).broadcast_to([128, 8, 16, 16]),
                                           in1=mv[:, :, 1, :].unsqueeze(2).broadcast_to([128, 8, 16, 16]), op=ALU.add), rd=[m], wr=[CS])
            for hh in range(8):
                w_ = wk2[hh % 2]
                yield
                cx.V(lambda h: h.max(out=tops[:, hh, 0:8], in_=CS[:, hh, :]), rd=[CS], wr=[tops])
                yield
                cx.V(lambda h: h.max_index(out=pos[:, hh, 0:8], in_max=tops[:, hh, 0:8], in_values=CS[:, hh, :]), rd=[tops, CS], wr=[pos])
                yield
                cx.V(lambda h: h.match_replace(out=w_[:], in_to_replace=tops[:, hh, 0:8], in_values=CS[:, hh, :], imm_value=-1e30), rd=[tops, CS], wr=[w_])
                yield
                cx.V(lambda h: h.max(out=tops[:, hh, 8:16], in_=w_[:]), rd=[w_], wr=[tops])
                yield
                cx.V(lambda h: h.max_index(out=pos[:, hh, 8:16], in_max=tops[:, hh, 8:16], in_values=w_[:]), rd=[tops, w_], wr=[pos])
            yield
            cx.V(lambda h: h.tensor_copy(out=posf[:], in_=pos[:]), rd=[pos], wr=[posf])
            yield
            cx.V(lambda h: h.tensor_copy(out=ixf[:], in_=ix[:]), rd=[ix], wr=[ixf])
            bc4 = lambda ap3: ap3.unsqueeze(3).broadcast_to([128, 8, 16, 16])
            io4 = io16[:].unsqueeze(1).unsqueeze(1).broadcast_to([128, 8, 16, 16])
            th4 = th16[:].unsqueeze(1).unsqueeze(1).broadcast_to([128, 8, 16, 16])
            yield
            cx.V(lambda h: h.tensor_tensor(out=oh_ap, in0=bc4(posf[:]), in1=th4, op=ALU.is_ge), rd=[posf, th16], wr=[oh])
            yield
            cx.V(lambda h: h.tensor_reduce(out=af[:], in_=oh_ap, axis=AX.X, op=ALU.add), rd=[oh], wr=[af])
            yield
            cx.V(lambda h: h.tensor_scalar(out=af[:], in0=af[:], scalar1=-1.0, scalar2=None, op0=ALU.add), rd=[af], wr=[af])
            yield
            cx.V(lambda h: h.scalar_tensor_tensor(out=bf_[:], in0=af[:], scalar=-16.0, in1=posf[:], op0=ALU.mult, op1=ALU.add), rd=[af, posf], wr=[bf_])
            ixv = ixf[:].rearrange("p (h t) k -> p h t k", t=2)
            for t, src in ((0, af), (1, bf_)):
                yield
                cx.V(lambda h: h.tensor_tensor(out=oh_ap, in0=bc4(src[:]), in1=io4, op=ALU.is_equal), rd=[src, io16], wr=[oh])
                yield
                cx.V(lambda h: h.tensor_tensor(out=oh_ap, in0=oh_ap, in1=ixv[:, :, t, :].unsqueeze(2).broadcast_to([128, 8, 16, 16]), op=ALU.mult), rd=[oh, ixf], wr=[oh])
                yield
                cx.V(lambda h: h.tensor_reduce(out=i12[:, t, :].rearrange("p (h k) -> p h k", h=8), in_=oh_ap, axis=AX.X, op=ALU.add), rd=[oh], wr=[i12])
            yield
            cx.V(lambda h: h.scalar_tensor_tensor(out=idxf[:], in0=i12[:, 0, :], scalar=128.0, in1=i12[:, 1, :], op0=ALU.mult, op1=ALU.add), rd=[i12], wr=[idxf])
            yield
            cx.V(lambda h: h.tensor_copy(out=idx[:], in_=idxf[:]), rd=[idxf], wr=[idx])
            yield
            cx.V(lambda h: h.tensor_tensor(out=ge[:], in0=tops[:], in1=tops[:, :, 0:1].broadcast_to([128, 8, 16]), op=ALU.subtract), rd=[tops], wr=[ge])
            yield
            cx.A(lambda h: h.activation(out=ge[:], in_=ge[:], func=AF.Exp), rd=[ge], wr=[ge])
            yield
            cx.V(lambda h: h.tensor_reduce(out=gsum[:, 0:8], in_=ge[:], axis=AX.X, op=ALU.add), rd=[ge], wr=[gsum])
            yield
            cx.V(lambda h: h.reciprocal(out=gsum[:, 8:16], in_=gsum[:, 0:8]), rd=[gsum], wr=[gsum])
            yield
            cx.V(lambda h: h.tensor_tensor(out=gate[:].rearrange("p (h k) -> p h k", h=8), in0=ge[:], in1=gsum[:, 8:16].unsqueeze(2).broadcast_to([128, 8, 16]), op=ALU.mult),
                 rd=[ge, gsum], wr=[gate])


        def back(i, fg):
            nonlocal sgn
            x = xb[i % 2]
            hb = hbs[i % 2]
            idx = idxs[i % 2]
            gate = gates[i % 2]
            cx.V(lambda h: h.memset(act[:], 0.0), wr=actT)
            PF = NG - 2
            for sgi in range(32 + PF + 1):
                if sgi < 32:
                    bq_ = (sgn + sgi) % NG
                    for jj in range(4):
                        j = sgi * 4 + jj
                        P.dma("pool", lambda h: h.indirect_dma_start(out=Gb[bq_][:, jj, :], out_offset=None, in_=TAB.t,
                                                                     in_offset=bass.IndirectOffsetOnAxis(ap=idx[:, j:j + 1], axis=0)),
                              _ks([idx, TAB]), [Gtok[bq_][jj]])
                sg = sgi - PF
                if 0 <= sg < 32:
                    b_ = (sgn + sg) % NG
                    G_ = Gb[b_]
                    for jj in range(4):
                        j = sg * 4 + jj
                        if jj in STT_SLOTS:
                            cx.V(lambda h: h.scalar_tensor_tensor(out=junkv[:], in0=G_[:, jj, 0:1024], scalar=1.0, in1=hb[:], op0=ALU.mult, op1=ALU.mult, accum_out=act[:, j:j + 1]),
                                 rd=[Gtok[b_][jj], hb], wr=[junkv, actT[sg % 4]])
                        else:
                            pr = prods[j % NPR]
                            cx.V(lambda h: h.tensor_tensor(out=pr[:], in0=G_[:, jj, 0:1024], in1=hb[:], op=ALU.mult), rd=[Gtok[b_][jj], hb], wr=[pr])
                            cx.A(lambda h: h.activation(out=junk[:], in_=pr[:], func=AF.Copy, accum_out=act[:, j:j + 1]), rd=[pr], wr=[junk, actT[sg % 4]])
                        for _ in range(FSTEP):
                            next(fg, None)
                    cs = slice(sg * 4, (sg + 1) * 4)
                    cx.A(lambda h: h.activation(out=gl[:, cs], in_=act[:, cs], func=AF.Gelu), rd=[actT[sg % 4]], wr=[glT[sg % 4]])
                sg = sgi - PF - 1
                if 0 <= sg < 32:
                    b_ = (sgn + sg) % NG
                    G_ = Gb[b_]
                    dg = diag[sg % 2]
                    cs = slice(sg * 4, (sg + 1) * 4)
                    cx.V(lambda h: h.tensor_tensor(out=coef[:, cs], in0=gl[:, cs], in1=gate[:, cs], op=ALU.mult), rd=[glT[sg % 4], gate], wr=[coefT[sg % 4]])
                    for jj in range(4):
                        j = sg * 4 + jj
                        cx.A(lambda h: h.activation(out=dg[:, jj, :], in_=idf[:], func=AF.Copy, scale=coef[:, j:j + 1]), rd=[idf, coefT[sg % 4]], wr=[dg])
                    for jj in range(4):
                        j = sg * 4 + jj
                        for hv in range(2):
                            cx.M(lambda h: h.matmul(po[:, hv * 512:(hv + 1) * 512], lhsT=dg[:, jj, :], rhs=G_[:, jj, 1024 + hv * 512:1024 + (hv + 1) * 512],
                                                    start=(j == 0), stop=(j == 127)), rd=[dg, Gtok[b_][jj]], wr=[po])
            sgn += 32
            y = yb[0]
            cx.V(lambda h: h.tensor_tensor(out=y[:], in0=po[:], in1=g2[:], op=ALU.mult), rd=[po, g2], wr=[y])
            cx.V(lambda h: h.tensor_tensor(out=y[:], in0=y[:], in1=x[:], op=ALU.add), rd=[y, x], wr=[y])
            if final:
                cx.V(lambda h: h.memset(st3[:, 0:1], 0.0), wr=[st3])
                cx.A(lambda h: h.activation(out=sq2[:], in_=y[:], func=AF.Square, accum_out=st3[:, 0:1]), rd=[y, st3], wr=[sq2, st3])
                cx.A(lambda h: h.activation(out=st3[:, 1:2], in_=st3[:, 0:1], func=AF.Sqrt, scale=1.0 / D, bias=eps[:, 0:1]), rd=[st3, eps], wr=[st3])
                cx.V(lambda h: h.reciprocal(out=st3[:, 2:3], in_=st3[:, 1:2]), rd=[st3], wr=[st3])
                cx.V(lambda h: h.scalar_tensor_tensor(out=y[:], in0=y[:], scalar=st3[:, 2:3], in1=fg_[:], op0=ALU.mult, op1=ALU.mult), rd=[y, st3, fg_], wr=[y])
            cx.dma("sp", Xout.t[i * 128:(i + 1) * 128, :], y[:], rd=[y], wr=[Xout])

        fgen = front(0)
        for _ in fgen:
            pass
        for i in range(NT):
            fgen = front(i + 1) if i + 1 < NT else iter(())
            back(i, fgen)
            for _ in fgen:
                pass
        P.barrier()
        P.flush()


def phase_attn(P, nc, IN, MODS, li, Xin, Xout, SC):
    with contextlib.ExitStack() as st0:
        c0 = Ctx(P, nc, st0)
        idf, idb, eps = load_consts(c0, IN)
        bigT = c0.T([128, 8, S], BF16)
        qT = c0.T([128, 8, S], BF16)
        kT = c0.T([128, 2, S], BF16)
        V1 = c0.T([128, NT, 2, 130], BF16)
        with contextlib.ExitStack() as st1:
            c1 = Ctx(P, nc, st1)
            gs1, sh1 = load_mods(c1, MODS, li, [0, 1])
            emit_hT(c1, Xin, gs1, sh1, idb, bigT)
            P.barrier()
        with contextlib.ExitStack() as st2:
            cx = Ctx(P, nc, st2)
            w = cx.T([128, 8, 1536], BF16)
            with contextlib.ExitStack() as stw:
                cw_ = Ctx(P, nc, stw)
                stg = [cw_.T([128, 1536], F32) for _ in range(2)]
                for k in range(8):
                    cw_.dma("sp", stg[k % 2][:], IN["od_w_in"][k * 128:(k + 1) * 128, :], wr=[stg[k % 2]])
                    cw_.V(lambda h: h.tensor_copy(out=w[:, k, :], in_=stg[k % 2][:]), rd=[stg[k % 2]], wr=[w])
                P.barrier()
            csb = [cx.T([128, 2, 64], F32) for _ in range(2)]
            gq = cx.T([128, 10, 128], F32)
            g1_ = cx.T([128, 128], F32)
            g2_ = cx.T([128, 128], F32)
            cx.dma("sp", g1_[:], IN["od_qnorm_g"].broadcast_to([128, 128]), wr=[g1_])
            cx.dma("sp", g2_[:], IN["od_knorm_g"].broadcast_to([128, 128]), wr=[g2_])
            cx.V(lambda h: h.tensor_scalar(out=g1_[:], in0=g1_[:], scalar1=float(128 ** -0.5), scalar2=None, op0=ALU.mult), rd=[g1_], wr=[g1_])
            cx.V(lambda h: h.tensor_copy(out=gq[:, 0:8, :], in_=g1_[:].unsqueeze(1).broadcast_to([128, 8, 128])), rd=[g1_], wr=[gq])
            cx.V(lambda h: h.tensor_copy(out=gq[:, 8:10, :], in_=g2_[:].unsqueeze(1).broadcast_to([128, 2, 128])), rd=[g2_], wr=[gq])
            cx.V(lambda h: h.memset(V1[:], 1.0), wr=[V1])
            pz = [cx.PS([128, 512]) for _ in range(3)]
            ptq = cx.PS([128, 8, 128], BF16)
            ptk = cx.PS([128, 2, 128], BF16)
            rs = cx.T([128, 32], F32)
            qn = cx.T([128, 10, 128], F32)
            qr = cx.T([128, 10, 128], BF16)
            t1 = cx.T([128, 10, 64], F32)
            t2 = cx.T([128, 10, 64], F32)
            for i in range(NT):
                tsl = slice(i * 128, (i + 1) * 128)
                for nb in range(3):
                    for k in range(8):
                        cx.M(lambda h: h.matmul(pz[nb][:], lhsT=bigT[:, k, tsl], rhs=w[:, k, nb * 512:(nb + 1) * 512], start=(k == 0), stop=(k == 7)), rd=[bigT, w], wr=[pz[nb]])
                cx.A(lambda h: h.activation(out=V1[:, i, :, 0:128], in_=pz[2][:, 256:512].rearrange("p (g d) -> p g d", g=2), func=AF.Copy), rd=[pz[2]], wr=[V1])
                cs = csb[i % 2]
                cx.dma("act", cs[:], IN["ropecs"][:, :, i, :], wr=[cs])
                zsrc = ((pz[0], 0, 4, 512), (pz[1], 4, 8, 512), (pz[2], 8, 10, 256))
                for (pp, g0, g1x, wd) in zsrc:
                    cx.A(lambda h: h.activation(out=qn[:, g0:g1x, :], in_=pp[:, 0:wd].rearrange("p (g d) -> p g d", d=128), func=AF.Square), rd=[pp], wr=[qn])
                cx.V(lambda h: h.tensor_reduce(out=rs[:, 0:10], in_=qn[:], axis=AX.X, op=ALU.add), rd=[qn], wr=[rs])
                cx.A(lambda h: h.activation(out=rs[:, 10:20], in_=rs[:, 0:10], func=AF.Sqrt, scale=1.0 / 128, bias=eps[:, 0:1]), rd=[rs, eps], wr=[rs])
                cx.V(lambda h: h.reciprocal(out=rs[:, 20:30], in_=rs[:, 10:20]), rd=[rs], wr=[rs])
                for (pp, g0, g1x, wd) in zsrc:
                    cx.V(lambda h: h.tensor_tensor(out=qn[:, g0:g1x, :], in0=pp[:, 0:wd].rearrange("p (g d) -> p g d", d=128),
                                                   in1=rs[:, 20 + g0:20 + g1x].unsqueeze(2).broadcast_to([128, g1x - g0, 128]), op=ALU.mult), rd=[pp, rs], wr=[qn])
                cx.V(lambda h: h.tensor_tensor(out=qn[:], in0=qn[:], in1=gq[:], op=ALU.mult), rd=[qn, gq], wr=[qn])
                qv = qn[:].rearrange("p g (d t) -> p g d t", t=2)
                qo = qr[:].rearrange("p g (d t) -> p g d t", t=2)
                cc = cs[:, 0, :].unsqueeze(1).broadcast_to([128, 10, 64])
                ss_ = cs[:, 1, :].unsqueeze(1).broadcast_to([128, 10, 64])
                cx.V(lambda h: h.tensor_tensor(out=t1[:], in0=qv[:, :, :, 0], in1=cc, op=ALU.mult), rd=[qn, cs], wr=[t1])
                cx.V(lambda h: h.tensor_tensor(out=t2[:], in0=qv[:, :, :, 1], in1=ss_, op=ALU.mult), rd=[qn, cs], wr=[t2])
                cx.V(lambda h: h.tensor_tensor(out=qo[:, :, :, 0], in0=t1[:], in1=t2[:], op=ALU.subtract), rd=[t1, t2], wr=[qr])
                cx.V(lambda h: h.tensor_tensor(out=t1[:], in0=qv[:, :, :, 0], in1=ss_, op=ALU.mult), rd=[qn, cs, qr], wr=[t1])
                cx.V(lambda h: h.tensor_tensor(out=t2[:], in0=qv[:, :, :, 1], in1=cc, op=ALU.mult), rd=[qn, cs, qr], wr=[t2])
                cx.V(lambda h: h.tensor_tensor(out=qo[:, :, :, 1], in0=t1[:], in1=t2[:], op=ALU.add), rd=[t1, t2], wr=[qr])
                for g_ in range(8):
                    cx.M(lambda h: h.transpose(out=ptq[:, g_, :], in_=qr[:, g_, :], identity=idb[:]), rd=[qr, idb], wr=[ptq])
                for g_ in range(2):
                    cx.M(lambda h: h.transpose(out=ptk[:, g_, :], in_=qr[:, 8 + g_, :], identity=idb[:]), rd=[qr, idb], wr=[ptk])
                cx.A(lambda h: h.activation(out=qT[:, :, tsl], in_=ptq[:], func=AF.Copy), rd=[ptq], wr=[qT])
                cx.A(lambda h: h.activation(out=kT[:, :, tsl], in_=ptk[:], func=AF.Copy), rd=[ptk], wr=[kT])
            P.barrier()
        import os as _os
        if _os.environ.get("ATT_STOP") == "2":
            P.barrier()
            P.flush()
            return
        with contextlib.ExitStack() as st3:
            cx = Ctx(P, nc, st3)
            pss = [cx.PS([128, 512]) for _ in range(2)]
            pacc = [cx.PS([128, 512]) for _ in range(4)]
            pto = cx.PS([128, 8, 128], BF16)
            pT = [cx.T([128, 512], BF16) for _ in range(3)]
            ao = [cx.T([128, 8, 128], BF16) for _ in range(4)]
            rinv = cx.T([128, 8], F32)
            steps = [(qb_, hd_, sj) for qb_ in range(8) for hd_ in range(8) for sj in range(NT)]
            accs = pacc

            def emit_score(n):
                qb_, hd_, sj = steps[n]
                ps_ = pss[n % 2]
                cx.M(lambda h: h.matmul(ps_[:], lhsT=kT[:, hd_ // 4, sj * 128:(sj + 1) * 128], rhs=qT[:, hd_, qb_ * 512:(qb_ + 1) * 512], start=True, stop=True), rd=[kT, qT], wr=[ps_])

            bg = SC.get("BG1")
            if bg is not None:
                bg.attach(cx)
            emit_score(0)
            for n, (qb_, hd_, sj) in enumerate(steps):
                g_ = hd_ // 4
                if bg is not None and n % 14 == 0:
                    bg.step(1)
                if n + 1 < len(steps):
                    emit_score(n + 1)
                ps_ = pss[n % 2]
                pt_ = pT[n % 3]
                cx.A(lambda h: h.activation(out=pt_[:], in_=ps_[:], func=AF.Exp), rd=[ps_], wr=[pt_])
                for qs in range(4):
                    cx.M(lambda h: h.matmul(accs[qs][:, 0:129], lhsT=pt_[:, qs * 128:(qs + 1) * 128], rhs=V1[:, sj, g_, 0:129], start=(sj == 0), stop=(sj == NT - 1)),
                         rd=[pt_, V1], wr=[accs[qs]])
                if sj == NT - 1:
                    for qs in range(4):
                        a_ = accs[qs]
                        cx.V(lambda h: h.reciprocal(out=rinv[:, qs:qs + 1], in_=a_[:, 128:129]), rd=[a_], wr=[rinv])
                        cx.V(lambda h: h.tensor_scalar(out=ao[qs][:, hd_, :], in0=a_[:, 0:128], scalar1=rinv[:, qs:qs + 1], scalar2=None, op0=ALU.mult), rd=[a_, rinv], wr=[ao[qs]])
                    if hd_ == 7:
                        for qs in range(4):
                            ti = qb_ * 4 + qs
                            for k in range(8):
                                cx.M(lambda h: h.transpose(out=pto[:, k, :], in_=ao[qs][:, k, :], identity=idb[:]), rd=[ao[qs], idb], wr=[pto])
                            cx.V(lambda h: h.tensor_copy(out=bigT[:, :, ti * 128:(ti + 1) * 128], in_=pto[:]), rd=[pto], wr=[bigT])
            if bg is not None:
                bg.finish()
                SC.setdefault("TABDONE", {})[1] = True
            P.barrier()
        if _os.environ.get("ATT_STOP") == "3":
            P.barrier()
            P.flush()
            return
        with contextlib.ExitStack() as st4:
            cx = Ctx(P, nc, st4)
            g1, = load_mods(cx, MODS, li, [2])
            stage_f = cx.T([128, 1024], F32)
            emit_outproj(cx, Xin, Xout, lambda i: (bigT[:, :, i * 128:(i + 1) * 128], bigT), IN["od_w_out"], g1, stage_f)
            P.barrier()
        P.flush()
```

```python
import contextlib
import numpy as np
import concourse.bass as bass
import concourse.mybir as mybir
from concourse.bass_utils import run_bass_kernel_spmd

F32 = mybir.dt.float32
BF16 = mybir.dt.bfloat16
I32 = mybir.dt.int32
U32 = mybir.dt.uint32
AF = mybir.ActivationFunctionType
ALU = mybir.AluOpType
AX = mybir.AxisListType

S = 4096
D = 1024
NT = S // 128
EPS = 1e-6


class Tok:
    __slots__ = ("w", "r", "name")

    def __init__(self, name=""):
        self.w = None
        self.r = {}
        self.name = name


class _Eng:
    def __init__(self, name, sem):
        self.name = name
        self.sem = sem
        self.cnt = 0
        self.seen = {}
        self.ops = []


NDMA = 12


class _Rec:
    def __getattr__(self, name):
        def f(*a, **k):
            return (name, a, k)
        return f


_REC = _Rec()


class Prog:
    ENG = ("pe", "act", "dve", "pool", "sp")

    def __init__(self, nc, stack):
        self.nc = nc
        self.stack = stack
        self.e = {}
        self.sems = {}
        for n in self.ENG:
            self.e[n] = _Eng(n, stack.enter_context(nc.semaphore("s_" + n)))
            self.sems[n] = (self.e[n].sem, 1)
        self.dslot = {}
        for q in ("sp", "pool", "act"):
            sl = []
            for j in range(NDMA):
                key = "d_%s%d" % (q, j)
                sem = stack.enter_context(nc.semaphore(key))
                self.sems[key] = (sem, 16)
                sl.append([key, 0])
            self.dslot[q] = [sl, 0]

    def _deps(self, e, rd, wr):
        deps = {}

        def need(dep, same_ok):
            if dep is None:
                return
            en, c = dep
            if en == e and (e == "pe" or not same_ok):
                return
            if deps.get(en, 0) < c:
                deps[en] = c

        for t in rd:
            need(t.w, True)
        for t in wr:
            need(t.w, False)
            for en, c in t.r.items():
                need((en, c), False)
        return deps

    def _emit_waits(self, E, deps):
        for en, c in deps.items():
            if E.seen.get(en, 0) < c:
                sem, step = self.sems[en]
                E.ops.append(lambda h, sem=sem, v=c * step: h.wait_ge(sem, v))
                E.seen[en] = c

    def op(self, e, fn, rd=(), wr=()):
        E = self.e[e]
        self._emit_waits(E, self._deps(e, rd, wr))
        E.cnt += 1
        sem = E.sem
        rec = fn(_REC)
        E.ops.append(lambda h, rec=rec, sem=sem: getattr(h, rec[0])(*rec[1], **rec[2]).then_inc(sem, 1))
        me = (e, E.cnt)
        for t in wr:
            t.w = me
            t.r = {}
        for t in rd:
            t.r[e] = E.cnt

    def dma(self, q, fn, rd=(), wr=()):
        E = self.e[q]
        slots, nxt = self.dslot[q]
        slot = slots[nxt % NDMA]
        self.dslot[q][1] = nxt + 1
        key = slot[0]
        deps = self._deps(key, rd, wr)
        if slot[1] > 0:
            deps[key] = max(deps.get(key, 0), slot[1])
        self._emit_waits(E, deps)
        slot[1] += 1
        sem = self.sems[key][0]
        rec = fn(_REC)
        E.ops.append(lambda h, rec=rec, sem=sem: getattr(h, rec[0])(*rec[1], **rec[2]).then_inc(sem, 16))
        me = (key, slot[1])
        for t in wr:
            t.w = me
            t.r = {}
        for t in rd:
            t.r[key] = slot[1]

    def barrier(self):
        tgt = {n: self.e[n].cnt for n in self.ENG}
        for q in self.dslot:
            for key, c in self.dslot[q][0]:
                tgt[key] = c
        for n in self.ENG:
            E = self.e[n]
            d = {k: v for k, v in tgt.items() if v > 0 and not (k == n and n == "pe")}
            self._emit_waits(E, d)

    def flush(self):
        nc = self.nc
        with nc.Block() as block:
            @block.tensor
            def _(h):
                for f in self.e["pe"].ops:
                    f(h)

            @block.scalar
            def _(h):
                for f in self.e["act"].ops:
                    f(h)

            @block.vector
            def _(h):
                for f in self.e["dve"].ops:
                    f(h)

            @block.gpsimd
            def _(h):
                for f in self.e["pool"].ops:
                    f(h)

            @block.sync
            def _(h):
                for f in self.e["sp"].ops:
                    f(h)
        for n in self.ENG:
            self.e[n].ops = []


class Buf:
    def __init__(self, t):
        self.t = t
        self.k = Tok()

    def __getitem__(self, i):
        return self.t[i]


def _ks(xs):
    return [x.k if isinstance(x, Buf) else x for x in xs]


_NAME = [0]


class Ctx:
    def __init__(self, P, nc, st):
        self.P, self.nc, self.st = P, nc, st
        self.n = 0

    def T(self, shape, dt, name=None):
        _NAME[0] += 1
        return Buf(self.st.enter_context(self.nc.sbuf_tensor("%s_%d" % (name or "t", _NAME[0]), list(shape), dt)))

    def PS(self, shape, dt=F32, name=None):
        _NAME[0] += 1
        return Buf(self.st.enter_context(self.nc.psum_tensor("%s_%d" % (name or "p", _NAME[0]), list(shape), dt)))

    def V(self, fn, rd=(), wr=()):
        self.P.op("dve", fn, _ks(rd), _ks(wr))

    def A(self, fn, rd=(), wr=()):
        self.P.op("act", fn, _ks(rd), _ks(wr))

    def G(self, fn, rd=(), wr=()):
        self.P.op("pool", fn, _ks(rd), _ks(wr))

    def M(self, fn, rd=(), wr=()):
        self.P.op("pe", fn, _ks(rd), _ks(wr))

    def dma(self, q, out, in_, rd=(), wr=()):
        self.P.dma(q, lambda h, out=out, in_=in_: h.dma_start(out=out, in_=in_), _ks(rd), _ks(wr))


def load_cast(cx, q, dst_bf, src_ap, stage, eng="pool"):
    cx.dma(q, stage.t[:] if not isinstance(stage, tuple) else stage[1], src_ap, wr=[stage if not isinstance(stage, tuple) else stage[0]])


def emit_norm_tile(cx, xt, gs, sh, hb, sq, st2, idb, ptr, hT_dst, hT_buf, hf=None):
    cx.V(lambda h: h.memset(st2[:, 0:1], 0.0), wr=[st2])
    cx.A(lambda h: h.activation(out=sq[:], in_=xt[:], func=AF.Square, accum_out=st2[:, 0:1]), rd=[xt, st2], wr=[sq, st2])
    cx.A(lambda h: h.activation(out=st2[:, 1:2], in_=st2[:, 0:1], func=AF.Sqrt, scale=1.0 / D, bias=EPS_AP[0][:, 0:1]), rd=[st2, EPS_AP[0]], wr=[st2])
    cx.V(lambda h: h.reciprocal(out=st2[:, 2:3], in_=st2[:, 1:2]), rd=[st2], wr=[st2])
    cx.V(lambda h: h.scalar_tensor_tensor(out=sq[:], in0=xt[:], scalar=st2[:, 2:3], in1=gs[:], op0=ALU.mult, op1=ALU.mult), rd=[xt, st2, gs], wr=[sq])
    if hf is not None:
        cx.V(lambda h: h.tensor_tensor(out=hf[:], in0=sq[:], in1=sh[:], op=ALU.add), rd=[sq, sh], wr=[hf])
        cx.G(lambda h: h.tensor_copy(out=hb[:], in_=hf[:]), rd=[hf], wr=[hb])
    else:
        cx.V(lambda h: h.tensor_tensor(out=hb[:], in0=sq[:], in1=sh[:], op=ALU.add), rd=[sq, sh], wr=[hb])
    for k in range(8):
        cx.M(lambda h, k=k: h.transpose(out=ptr[:, k, :], in_=hb[:, k * 128:(k + 1) * 128], identity=idb[:]), rd=[hb, idb], wr=[ptr])
    cx.A(lambda h: h.activation(out=hT_dst, in_=ptr[:], func=AF.Copy), rd=[ptr], wr=[hT_buf])


EPS_AP = [None]


def load_consts(cx, CN):
    idf = cx.T([128, 128], F32)
    idb = cx.T([128, 128], BF16)
    eps = cx.T([128, 1], F32)
    cx.dma("sp", idf[:], CN["ident"], wr=[idf])
    cx.V(lambda h: h.tensor_copy(out=idb[:], in_=idf[:]), rd=[idf], wr=[idb])
    cx.V(lambda h: h.memset(eps[:], EPS), wr=[eps])
    EPS_AP[0] = eps
    return idf, idb, eps


def phase_mods(P, nc, IN, MODS):
    with contextlib.ExitStack() as st:
        cx = Ctx(P, nc, st)
        cT = cx.T([128, 8], F32)
        cond = cx.T([128, 8], F32)
        crep = cx.T([128, 8, 128], F32)
        cx.dma("sp", cT[:], IN["cT"], wr=[cT])
        cx.A(lambda h: h.activation(out=cond[:], in_=cT[:], func=AF.Silu), rd=[cT], wr=[cond])
        cx.V(lambda h: h.tensor_copy(out=crep[:], in_=cond[:].unsqueeze(2).broadcast_to([128, 8, 128])), rd=[cond], wr=[crep])
        wb = [cx.T([128, 8, 512], F32) for _ in range(2)]
        ps = [cx.PS([128, 512]) for _ in range(2)]
        mod = cx.T([128, 6144], F32)
        ab = cx.T([128, 6144], F32)
        gm = cx.T([128, 1024], F32)
        gf = cx.T([128, 1024], F32)
        n = 0
        for i in range(2):
            cx.dma("act", ab[:], IN["ada_b"][i:i + 1, :].broadcast_to([128, 6144]), wr=[ab])
            cx.dma("act", gm[:], IN["norm_mix_g"][i:i + 1, :].broadcast_to([128, 1024]), wr=[gm])
            cx.dma("act", gf[:], IN["norm_ffn_g"][i:i + 1, :].broadcast_to([128, 1024]), wr=[gf])
            for nb in range(12):
                w = wb[n % 2]
                p = ps[n % 2]
                n += 1
                cx.dma("sp" if nb % 2 == 0 else "pool", w[:], IN["ada_w"][i, :, nb * 512:(nb + 1) * 512].rearrange("(k p) n -> p k n", p=128), wr=[w])
                for k in range(8):
                    cx.M(lambda h, k=k, w=w, p=p: h.matmul(p[:], lhsT=crep[:, k, :], rhs=w[:, k, :], start=(k == 0), stop=(k == 7)), rd=[crep, w], wr=[p])
                cx.V(lambda h, p=p, nb=nb: h.tensor_tensor(out=mod[:, nb * 512:(nb + 1) * 512], in0=p[:], in1=ab[:, nb * 512:(nb + 1) * 512], op=ALU.add), rd=[p, ab], wr=[mod])
            cx.V(lambda h: h.scalar_tensor_tensor(out=mod[:, 1024:2048], in0=mod[:, 1024:2048], scalar=1.0, in1=gm[:], op0=ALU.add, op1=ALU.mult), rd=[mod, gm], wr=[mod])
            cx.V(lambda h: h.scalar_tensor_tensor(out=mod[:, 4096:5120], in0=mod[:, 4096:5120], scalar=1.0, in1=gf[:], op0=ALU.add, op1=ALU.mult), rd=[mod, gf], wr=[mod])
            for j, off in enumerate([1024, 0, 2048, 4096, 3072, 5120]):
                cx.dma("sp", MODS.t[i, j], mod[:, off:off + 1024], rd=[mod], wr=[MODS])
        P.barrier()
        P.flush()


def load_mods(cx, MODS, i, js, q="act"):
    out = []
    for j in js:
        b = cx.T([128, 1024], F32)
        cx.dma(q, b[:], MODS.t[i, j], rd=[MODS], wr=[b])
        out.append(b)
    return out


def emit_hT(cx, Xin, gs, sh, idb, hT):
    xb = [cx.T([128, 1024], F32) for _ in range(2)]
    sq = cx.T([128, 1024], F32)
    hb = [cx.T([128, 1024], BF16) for _ in range(2)]
    st2 = [cx.T([128, 4], F32) for _ in range(2)]
    ptr = [cx.PS([128, 8, 128], BF16) for _ in range(2)]
    for i in range(NT):
        x = xb[i % 2]
        cx.dma("sp", x[:], Xin.t[i * 128:(i + 1) * 128, :], rd=[Xin], wr=[x])
        emit_norm_tile(cx, x, gs, sh, hb[i % 2], sq, st2[i % 2], idb, ptr[i % 2], hT[:, :, i * 128:(i + 1) * 128], hT)


def emit_outproj(cx, Xin, Xout, catT_src, w_ap, g1, stage_f, final=None):
    wob = cx.T([128, 8, 1024], BF16)
    for k in range(8):
        cx.dma("sp", stage_f[:], w_ap[k * 128:(k + 1) * 128, :], wr=[stage_f])
        cx.G(lambda h, k=k: h.tensor_copy(out=wob[:, k, :], in_=stage_f[:]), rd=[stage_f], wr=[wob])
    py = [cx.PS([128, 1024]) for _ in range(2)]
    xb = [cx.T([128, 1024], F32) for _ in range(2)]
    yb = [cx.T([128, 1024], F32) for _ in range(2)]
    for i in range(NT):
        ap, tok = catT_src(i)
        p = py[i % 2]
        x = xb[i % 2]
        y = yb[i % 2]
        cx.dma("act", x[:], Xin.t[i * 128:(i + 1) * 128, :], rd=[Xin], wr=[x])
        for nb in range(2):
            for k in range(8):
                cx.M(lambda h, k=k, nb=nb, p=p, ap=ap: h.matmul(p[:, nb * 512:(nb + 1) * 512], lhsT=ap[:, k, :], rhs=wob[:, k, nb * 512:(nb + 1) * 512],
                                                               start=(k == 0), stop=(k == 7)), rd=[tok, wob], wr=[p])
        cx.V(lambda h, p=p, y=y: h.tensor_tensor(out=y[:], in0=p[:], in1=g1[:], op=ALU.mult), rd=[p, g1], wr=[y])
        cx.G(lambda h, x=x, y=y: h.tensor_tensor(out=y[:], in0=y[:], in1=x[:], op=ALU.add), rd=[y, x], wr=[y])
        cx.dma("sp", Xout.t[i * 128:(i + 1) * 128, :], y[:], rd=[y], wr=[Xout])


def phase_even(P, nc, IN, MODS, li, Xin, Xout, SC):
    CATT, V1D, SIGD, HFD = SC["CATT"], SC["V1D"], SC["SIGD"], SC["HFD"]
    with contextlib.ExitStack() as st0:
        c0 = Ctx(P, nc, st0)
        idf, idb, eps = load_consts(c0, IN)
        QK = c0.T([128, 8, S], BF16)
        GT = c0.T([128, NT, 16], F32)
        EB = c0.T([128, NT, 8], F32)
        ES = c0.T([128, NT, 8], F32)
        EE = c0.T([128, NT, 8], F32)
        with contextlib.ExitStack() as st1:
            cx1 = Ctx(P, nc, st1)
            hT = cx1.T([128, 8, S], BF16)
            with contextlib.ExitStack() as st2:
                c2 = Ctx(P, nc, st2)
                gs1, sh1 = load_mods(c2, MODS, li, [0, 1])
                emit_hT(c2, Xin, gs1, sh1, idb, hT)
                P.barrier()
            with contextlib.ExitStack() as stf:
                cx = Ctx(P, nc, stf)
                bT = cx.T([128, 20], F32)
                cw = cx.T([128, 8, 5], F32)
                cb = cx.T([128, 8], F32)
                edge = cx.T([128, 4, 32], F32)
                cx.dma("sp", bT[:], IN["ev_b_inT"], wr=[bT])
                cx.dma("sp", cw[:], IN["ev_conv_wT"], wr=[cw])
                cx.dma("sp", cb[:], IN["ev_conv_bT"], wr=[cb])
                cx.dma("sp", edge[:], IN["pooledge"].broadcast_to([128, 4, 32]), wr=[edge])
                wst = [cx.T([128, 8, 128], F32) for _ in range(2)]
                wcb = [cx.T([128, 8, 128], BF16) for _ in range(2)]
                zc = cx.T([128, S + 16], F32)
                pa = cx.T([128, S + 16], F32)
                yb = cx.T([128, S + 16], F32)
                ybf = cx.T([128, S], BF16)
                yo = [cx.T([128, 512], BF16) for _ in range(2)]
                pw = cx.T([128, 128], F32)
                psc = cx.T([128, 128], F32)
                pwb = cx.T([128, 128], BF16)
                pz = [cx.PS([128, 512]) for _ in range(2)]
                cx.V(lambda h: h.memset(zc[:], 0.0), wr=[zc])
                nps = 0
                for c in range(12):
                    col0 = c * 128 if c < 8 else 2048 + (c - 8) * 128
                    bcol = c if c < 8 else 16 + (c - 8)
                    ws, wc = wst[c % 2], wcb[c % 2]
                    cx.dma("sp", ws[:], IN["ev_w_in"][:, col0:col0 + 128].rearrange("(k p) n -> p k n", p=128), wr=[ws])
                    cx.G(lambda h, ws=ws, wc=wc: h.tensor_copy(out=wc[:], in_=ws[:]), rd=[ws], wr=[wc])
                    for tb in range(8):
                        p = pz[nps % 2]
                        nps += 1
                        for k in range(8):
                            cx.M(lambda h, k=k, p=p, wc=wc, tb=tb: h.matmul(p[:], lhsT=wc[:, k, :], rhs=hT[:, k, tb * 512:(tb + 1) * 512], start=(k == 0), stop=(k == 7)),
                                 rd=[wc, hT], wr=[p])
                        cx.A(lambda h, p=p, tb=tb, bcol=bcol: h.activation(out=zc[:, 8 + tb * 512:8 + (tb + 1) * 512], in_=p[:], func=AF.Identity, bias=bT[:, bcol:bcol + 1]),
                             rd=[p, bT], wr=[zc])
                    if c < 8:
                        cx.V(lambda h, c=c: h.tensor_scalar(out=yb[:, 0:S], in0=zc[:, 6:6 + S], scalar1=cw[:, c, 0:1], scalar2=None, op0=ALU.mult), rd=[zc, cw], wr=[yb])
                        for j in range(1, 5):
                            cx.V(lambda h, c=c, j=j: h.scalar_tensor_tensor(out=yb[:, 0:S], in0=zc[:, 6 + j:6 + j + S], scalar=cw[:, c, j:j + 1], in1=yb[:, 0:S], op0=ALU.mult, op1=ALU.add),
                                 rd=[zc, cw, yb], wr=[yb])
                        cx.A(lambda h, c=c: h.activation(out=QK[:, c, :], in_=yb[:, 0:S], func=AF.Silu, bias=cb[:, c:c + 1]), rd=[yb, cb], wr=[QK])
                    else:
                        g = c - 8
                        win = (2, 4, 8, 16)[g]
                        half = win // 2
                        n_el = S + 15
                        cur = zc
                        bufs = [pa, yb]
                        bi = 0
                        step = 1
                        while step < win:
                            d = bufs[bi % 2]
                            bi += 1
                            cx.V(lambda h, cur=cur, d=d, step=step, n_el=n_el: h.tensor_tensor(out=d[:, 0:n_el - step + 1], in0=cur[:, 0:n_el - step + 1], in1=cur[:, step:n_el + 1], op=ALU.add),
                                 rd=[cur], wr=[d])
                            n_el = n_el - step
                            cur = d
                            step *= 2
                        o = bufs[bi % 2]
                        cx.V(lambda h, cur=cur, half=half, o=o, win=win: h.tensor_scalar(out=o[:, 0:S], in0=cur[:, 8 - half:8 - half + S], scalar1=1.0 / win, scalar2=None, op0=ALU.mult), rd=[cur], wr=[o])
                        cx.V(lambda h, o=o, g=g: h.tensor_tensor(out=o[:, 0:16], in0=o[:, 0:16], in1=edge[:, g, 0:16], op=ALU.mult), rd=[o, edge], wr=[o])
                        cx.V(lambda h, o=o, g=g: h.tensor_tensor(out=o[:, S - 16:S], in0=o[:, S - 16:S], in1=edge[:, g, 16:32], op=ALU.mult), rd=[o, edge], wr=[o])
                        cx.V(lambda h, o=o: h.tensor_tensor(out=ybf[:], in0=o[:, 0:S], in1=zc[:, 8:8 + S], op=ALU.subtract), rd=[o, zc], wr=[ybf])
                        cx.dma("sp", pw[:], IN["ev_pool_w"][g], wr=[pw])
                        cx.dma("sp", psc[:], IN["ev_pool_scale"][0:1, g * 128:(g + 1) * 128].broadcast_to([128, 128]), wr=[psc])
                        cx.V(lambda h: h.tensor_tensor(out=pwb[:], in0=pw[:], in1=psc[:], op=ALU.mult), rd=[pw, psc], wr=[pwb])
                        for tb in range(8):
                            p = pz[nps % 2]
                            y_ = yo[nps % 2]
                            nps += 1
                            cx.M(lambda h, p=p, tb=tb: h.matmul(p[:], lhsT=pwb[:], rhs=ybf[:, tb * 512:(tb + 1) * 512], start=True, stop=True), rd=[pwb, ybf], wr=[p])
                            cx.A(lambda h, p=p, y_=y_: h.activation(out=y_[:], in_=p[:], func=AF.Copy), rd=[p], wr=[y_])
                            cx.dma("sp", CATT.t[4 + g, :, tb * 512:(tb + 1) * 512], y_[:], rd=[y_], wr=[CATT])
                P.barrier()
            with contextlib.ExitStack() as stt:
                cx = Ctx(P, nc, stt)
                stage_f = cx.T([128, 1024], F32)
                wtm = cx.T([128, 8, 1040], BF16)
                for k in range(8):
                    cx.dma("sp", stage_f[:], IN["ev_w_in"][k * 128:(k + 1) * 128, 1024:2048], wr=[stage_f])
                    cx.V(lambda h, k=k: h.tensor_copy(out=wtm[:, k, 0:1024], in_=stage_f[:]), rd=[stage_f], wr=[wtm])
                gst = cx.T([128, 8, 16], F32)
                cx.dma("sp", gst[:], IN["ev_w_in"][:, 2560:2576].rearrange("(k p) n -> p k n", p=128), wr=[gst])
                cx.V(lambda h: h.tensor_copy(out=wtm[:, :, 1024:1040], in_=gst[:]), rd=[gst], wr=[wtm])
                bvo = cx.T([128, 1024], F32)
                bg = cx.T([128, 16], F32)
                cx.dma("sp", bvo[:], IN["ev_b_in"][0:1, 1024:2048].broadcast_to([128, 1024]), wr=[bvo])
                cx.dma("sp", bg[:], IN["ev_b_in"][0:1, 2560:2576].broadcast_to([128, 16]), wr=[bg])
                pv = [cx.PS([128, 512]) for _ in range(2)]
                po = [cx.PS([128, 512]) for _ in range(2)]
                pg = [cx.PS([128, 16]) for _ in range(2)]
                v1 = [cx.T([128, 4, 130], BF16) for _ in range(2)]
                of = [cx.T([128, 512], F32) for _ in range(2)]
                ob = [cx.T([128, 512], BF16) for _ in range(2)]
                for b_ in v1:
                    cx.V(lambda h, b_=b_: h.memset(b_[:], 1.0), wr=[b_])
                for i in range(NT):
                    a = i % 2
                    for (p, c0_, c1_) in ((pv[a], 0, 512), (po[a], 512, 1024), (pg[a], 1024, 1040)):
                        for k in range(8):
                            cx.M(lambda h, k=k, p=p, c0_=c0_, c1_=c1_, i=i: h.matmul(p[:], lhsT=hT[:, k, i * 128:(i + 1) * 128], rhs=wtm[:, k, c0_:c1_], start=(k == 0), stop=(k == 7)),
                                 rd=[hT, wtm], wr=[p])
                    for hh in range(4):
                        cx.V(lambda h, a=a, hh=hh: h.tensor_tensor(out=v1[a][:, hh, 0:128], in0=pv[a][:, hh * 128:(hh + 1) * 128],
                                                                   in1=bvo[:, hh * 128:(hh + 1) * 128], op=ALU.add), rd=[pv[a], bvo], wr=[v1[a]])
                    cx.dma("sp", V1D.t[i], v1[a][:], rd=[v1[a]], wr=[V1D])
                    cx.V(lambda h, a=a: h.tensor_tensor(out=of[a][:], in0=po[a][:], in1=bvo[:, 512:1024], op=ALU.add), rd=[po[a], bvo], wr=[of[a]])
                    cx.A(lambda h, a=a: h.activation(out=ob[a][:], in_=of[a][:], func=AF.Sigmoid), rd=[of[a]], wr=[ob[a]])
                    if i == 0:
                        cx.dma("sp", SC["DBG2"].t, of[a][:], rd=[of[a]], wr=[SC["DBG2"]])
                    cx.dma("sp", SIGD.t[i], ob[a][:], rd=[ob[a]], wr=[SIGD])
                    cx.V(lambda h, a=a, i=i: h.tensor_tensor(out=GT[:, i, :], in0=pg[a][:], in1=bg[:], op=ALU.add), rd=[pg[a], bg], wr=[GT])
                cx.dma("sp", SC["DBG1"].t, GT[:], rd=[GT], wr=[SC["DBG1"]])
                P.barrier()
            with contextlib.ExitStack() as stg:
                cx = Ctx(P, nc, stg)
                LF = cx.T([128, NT, 8], F32)
                t8 = cx.T([128, NT, 8], F32)
                BC = cx.T([128, NT, 16], F32)
                cx.A(lambda h: h.activation(out=t8[:], in_=GT[:, :, 8:16], func=AF.Exp, scale=-1.0), rd=[GT], wr=[t8])
                cx.V(lambda h: h.tensor_scalar(out=t8[:], in0=t8[:], scalar1=1.0, scalar2=None, op0=ALU.add), rd=[t8], wr=[t8])
                cx.A(lambda h: h.activation(out=LF[:], in_=t8[:], func=AF.Ln), rd=[t8], wr=[LF])
                cx.V(lambda h: h.tensor_scalar(out=LF[:], in0=LF[:], scalar1=-1.0, scalar2=None, op0=ALU.mult), rd=[LF], wr=[LF])
                triU = cx.T([128, 128], F32)
                triL = cx.T([128, 128], F32)
                ones = cx.T([128, 128], F32)
                cx.dma("sp", triU[:], IN["triU"], wr=[triU])
                cx.dma("sp", triL[:], IN["triL"], wr=[triL])
                cx.V(lambda h: h.memset(ones[:], 1.0), wr=[ones])
                pc = cx.PS([128, NT, 16])
                for i in range(NT):
                    cx.M(lambda h, i=i: h.matmul(pc[:, i, 0:4], lhsT=triU[:], rhs=LF[:, i, 0:4], start=True, stop=True), rd=[triU, LF], wr=[pc])
                    cx.M(lambda h, i=i: h.matmul(pc[:, i, 4:8], lhsT=triL[:], rhs=LF[:, i, 4:8], start=True, stop=True), rd=[triL, LF], wr=[pc])
                    cx.M(lambda h, i=i: h.matmul(pc[:, i, 8:16], lhsT=ones[:], rhs=LF[:, i, 0:8], start=True, stop=True), rd=[ones, LF], wr=[pc])
                cx.V(lambda h: h.tensor_copy(out=BC[:], in_=pc[:]), rd=[pc], wr=[BC])
                cx.A(lambda h: h.activation(out=EB[:], in_=BC[:, :, 0:8], func=AF.Exp), rd=[BC], wr=[EB])
                cx.A(lambda h: h.activation(out=EE[:], in_=BC[:, :, 8:16], func=AF.Exp), rd=[BC], wr=[EE])
                cx.V(lambda h: h.tensor_tensor(out=t8[:], in0=GT[:, :, 0:8], in1=BC[:, :, 0:8], op=ALU.subtract), rd=[GT, BC], wr=[t8])
                cx.V(lambda h: h.tensor_scalar(out=t8[:], in0=t8[:], scalar1=float(-0.5 * np.log(128.0)), scalar2=None, op0=ALU.add), rd=[t8], wr=[t8])
                cx.A(lambda h: h.activation(out=ES[:], in_=t8[:], func=AF.Exp), rd=[t8], wr=[ES])
                P.barrier()
        with contextlib.ExitStack() as st3:
            cx = Ctx(P, nc, st3)
            mk = []
            for nm in ("triU", "triL"):
                f = cx.T([128, 128], F32)
                cx.dma("sp", f[:], IN[nm], wr=[f])
                mk.append(f)
            Cst = cx.T([128, 8, 129], F32)
            Cb = cx.T([128, 8, 129], BF16)
            cx.V(lambda h: h.memset(Cst[:], 0.0), wr=[Cst])
            cx.V(lambda h: h.memset(Cb[:], 0.0), wr=[Cb])
            NV = 6
            v1 = [cx.T([128, 4, 130], BF16) for _ in range(NV)]
            psS = [cx.PS([128, 4, 128]) for _ in range(2)]
            psA = [cx.PS([128, 3, 129]) for _ in range(3)]
            ps_t = cx.PS([128, 8, 128], BF16)
            ps_h = cx.PS([128, 4, 128], BF16)
            AT = cx.T([128, 8, 128], BF16)
            ksb = cx.T([128, 8, 128], BF16)
            sm = cx.T([128, 8, 4], F32)
            hacc = cx.T([128, NT, 512], F32)
            NSG = 6
            sgl = [cx.T([128, 512], BF16) for _ in range(NSG)]
            sq = cx.T([128, 512], F32)
            st4 = [cx.T([128, 12], F32) for _ in range(2)]
            mg = cx.T([128, 512], F32)
            hmb = [cx.T([128, 512], BF16) for _ in range(2)]
            hmT = [cx.T([128, 4, 128], BF16) for _ in range(2)]
            cx.dma("sp", mg[:], IN["ev_mnorm_g"][0:1, :].broadcast_to([128, 512]), wr=[mg])
            bg = SC.get("BG0")
            if bg is not None:
                bg.attach(cx)
            chains = [(d, hh) for d in range(2) for hh in range(4)]

            def aslot(c):
                return psA[c // 3], c % 3

            def tile_of(s, d):
                return s if d == 0 else NT - 1 - s

            PDV = 2
            nfin = 0
            for s in range(NT + PDV):
                if s < NT:
                    for d in range(2):
                        vb = v1[(2 * s + d) % NV]
                        cx.dma("sp", vb[:], V1D.t[tile_of(s, d)], rd=[V1D], wr=[vb])
                    if s >= NT // 2:
                        for d in range(2):
                            sg_ = sgl[(2 * s + d) % NSG]
                            cx.dma("act", sg_[:], SIGD.t[tile_of(s, d)], rd=[SIGD], wr=[sg_])
                s_ = s - PDV
                if s_ < 0:
                    continue
                s = s_
                if bg is not None:
                    bg.step(4)
                vbs = [v1[(2 * s + d) % NV] for d in range(2)]
                tls = [tile_of(s, d) for d in range(2)]
                for c, (d, hh) in enumerate(chains):
                    tsl = slice(tls[d] * 128, (tls[d] + 1) * 128)
                    cx.M(lambda h: h.matmul(psS[d][:, hh, :], lhsT=QK[:, 4 + hh, tsl], rhs=QK[:, hh, tsl], start=True, stop=True), rd=[QK], wr=[psS[d]])
                for c, (d, hh) in enumerate(chains):
                    col = d * 4 + hh
                    cx.V(lambda h: h.scalar_tensor_tensor(out=AT[:, c, :], in0=psS[d][:, hh, :], scalar=ES[:, tls[d], col:col + 1], in1=mk[d][:], op0=ALU.mult, op1=ALU.mult),
                         rd=[psS[d], ES, mk[d]], wr=[AT])
                for c, (d, hh) in enumerate(chains):
                    col = d * 4 + hh
                    tsl = slice(tls[d] * 128, (tls[d] + 1) * 128)
                    pa, sl = aslot(c)
                    cx.M(lambda h: h.matmul(pa[:, sl, :], lhsT=AT[:, c, :], rhs=vbs[d][:, hh, 0:129], start=True, stop=False), rd=[AT, vbs[d]], wr=[pa])
                    cx.M(lambda h: h.matmul(pa[:, sl, :], lhsT=QK[:, hh, tsl], rhs=Cb[:, col, :], start=False, stop=True), rd=[QK, Cb], wr=[pa])
                for c, (d, hh) in enumerate(chains):
                    col = d * 4 + hh
                    pa, sl = aslot(c)
                    cx.A(lambda h: h.activation(out=sm[:, c, 2:3], in_=pa[:, sl, 128:129], func=AF.Abs, scale=EB[:, tls[d], col:col + 1]), rd=[pa, EB], wr=[sm])
                cx.V(lambda h: h.tensor_scalar(out=sm[:, :, 0:1], in0=sm[:, :, 2:3], scalar1=1.0, scalar2=None, op0=ALU.max), rd=[sm], wr=[sm])
                cx.V(lambda h: h.reciprocal(out=sm[:, :, 3:4], in_=sm[:, :, 0:1]), rd=[sm], wr=[sm])
                for d in range(2):
                    cx.V(lambda h: h.tensor_tensor(out=sm[:, d * 4:(d + 1) * 4, 1:2], in0=EB[:, tls[d], d * 4:(d + 1) * 4].unsqueeze(2), in1=sm[:, d * 4:(d + 1) * 4, 3:4], op=ALU.mult),
                         rd=[sm, EB], wr=[sm])
                for c, (d, hh) in enumerate(chains):
                    pa, sl = aslot(c)
                    dst = hacc[:, tls[d], hh * 128:(hh + 1) * 128]
                    if s < NT // 2:
                        cx.A(lambda h: h.activation(out=dst, in_=pa[:, sl, 0:128], func=AF.Copy, scale=sm[:, c, 1:2]), rd=[pa, sm], wr=[hacc])
                    else:
                        cx.V(lambda h: h.scalar_tensor_tensor(out=dst, in0=pa[:, sl, 0:128], scalar=sm[:, c, 1:2], in1=dst, op0=ALU.mult, op1=ALU.add), rd=[pa, sm, hacc], wr=[hacc])
                for c, (d, hh) in enumerate(chains):
                    tsl = slice(tls[d] * 128, (tls[d] + 1) * 128)
                    cx.M(lambda h: h.transpose(out=ps_t[:, c, :], in_=QK[:, 4 + hh, tsl], identity=idb[:]), rd=[QK, idb], wr=[ps_t])
                for c, (d, hh) in enumerate(chains):
                    col = d * 4 + hh
                    cx.A(lambda h: h.activation(out=ksb[:, c, :], in_=ps_t[:, c, :], func=AF.Copy, scale=ES[:, tls[d], col:col + 1]), rd=[ps_t, ES], wr=[ksb])
                for c, (d, hh) in enumerate(chains):
                    pa, sl = aslot(c)
                    cx.M(lambda h: h.matmul(pa[:, sl, :], lhsT=ksb[:, c, :], rhs=vbs[d][:, hh, 0:129], start=True, stop=True), rd=[ksb, vbs[d]], wr=[pa])
                for c, (d, hh) in enumerate(chains):
                    col = d * 4 + hh
                    pa, sl = aslot(c)
                    cx.V(lambda h: h.tensor_scalar(out=Cst[:, col, :], in0=Cst[:, col, :], scalar1=EE[:, tls[d], col:col + 1], scalar2=None, op0=ALU.mult), rd=[Cst, EE], wr=[Cst])
                    cx.V(lambda h: h.scalar_tensor_tensor(out=Cst[:, col, :], in0=pa[:, sl, :], scalar=EE[:, tls[d], col:col + 1], in1=Cst[:, col, :], op0=ALU.mult, op1=ALU.add),
                         rd=[pa, EE, Cst], wr=[Cst])
                cx.A(lambda h: h.activation(out=Cb[:], in_=Cst[:], func=AF.Copy), rd=[Cst], wr=[Cb])
                if s >= NT // 2:
                    for d in range(2):
                        i = tls[d]
                        a = nfin % 2
                        nfin += 1
                        sg_ = sgl[(2 * s + d) % NSG]
                        hv = hacc[:, i, :]
                        cx.V(lambda h: h.tensor_tensor(out=sq[:], in0=hv, in1=hv, op=ALU.mult), rd=[hacc], wr=[sq])
                        cx.V(lambda h: h.tensor_reduce(out=st4[a][:, 0:4], in_=sq[:].rearrange("p (h d) -> p h d", h=4), axis=AX.X, op=ALU.add), rd=[sq], wr=[st4[a]])
                        cx.A(lambda h: h.activation(out=st4[a][:, 4:8], in_=st4[a][:, 0:4], func=AF.Sqrt, scale=1.0 / 128, bias=eps[:, 0:1]), rd=[st4[a], eps], wr=[st4[a]])
                        cx.V(lambda h: h.reciprocal(out=st4[a][:, 8:12], in_=st4[a][:, 4:8]), rd=[st4[a]], wr=[st4[a]])
                        cx.V(lambda h: h.tensor_tensor(out=hv.rearrange("p (h d) -> p h d", h=4), in0=hv.rearrange("p (h d) -> p h d", h=4),
                                                       in1=st4[a][:, 8:12].unsqueeze(2).broadcast_to([128, 4, 128]), op=ALU.mult), rd=[hacc, st4[a]], wr=[hacc])
                        cx.V(lambda h: h.tensor_tensor(out=sq[:], in0=mg[:], in1=sg_[:], op=ALU.mult), rd=[mg, sg_], wr=[sq])
                        cx.V(lambda h: h.tensor_tensor(out=hmb[a][:], in0=hv, in1=sq[:], op=ALU.mult), rd=[hacc, sq], wr=[hmb[a]])
                        for k in range(4):
                            cx.M(lambda h: h.transpose(out=ps_h[:, k, :], in_=hmb[a][:, k * 128:(k + 1) * 128], identity=idb[:]), rd=[hmb[a], idb], wr=[ps_h])
                        cx.A(lambda h: h.activation(out=hmT[a][:], in_=ps_h[:], func=AF.Copy), rd=[ps_h], wr=[hmT[a]])
                        cx.dma("act", CATT.t[0:4, :, i * 128:(i + 1) * 128].rearrange("c f t -> f c t"), hmT[a][:], rd=[hmT[a]], wr=[CATT])
            if bg is not None:
                bg.finish()
                SC.setdefault("TABDONE", {})[0] = True
            P.barrier()
        with contextlib.ExitStack() as st4_:
            cx = Ctx(P, nc, st4_)
            cb_ = [cx.T([128, 8, 128], BF16) for _ in range(3)]
            g1, = load_mods(cx, MODS, li, [2])
            stage_f = cx.T([128, 1024], F32)

            def src(i):
                b = cb_[i % 3]
                cx.dma("pool", b[:], CATT.t[:, :, i * 128:(i + 1) * 128].rearrange("c f t -> f c t"), rd=[CATT], wr=[b])
                return b, b
            emit_outproj(cx, Xin, Xout, src, IN["ev_w_out"], g1, stage_f)
            P.barrier()
        P.flush()


W_SPECS = {
    "ada_w": [2, 1024, 6144], "ada_b": [2, 6144], "norm_mix_g": [2, 1024], "norm_ffn_g": [2, 1024],
    "ev_w_in": [1024, 2576], "ev_b_in": [1, 2576], "ev_b_inT": [128, 20], "ev_conv_wT": [128, 8, 5], "ev_conv_bT": [128, 8],
    "ev_mnorm_g": [1, 512], "ev_pool_w": [4, 128, 128], "ev_pool_scale": [1, 512], "ev_w_out": [1024, 1024],
    "od_w_in": [1024, 1536], "od_qnorm_g": [1, 128], "od_knorm_g": [1, 128], "od_w_out": [1024, 1024],
    "peer_w_q": [2, 1024, 2048], "peer_keys": [2, 2, 128, 128], "peer_u": [2, 16384, 1024], "peer_v": [2, 16384, 1024],
    "final_g": [1, 1024],
    "ident": [128, 128], "triU": [128, 128], "triL": [128, 128], "pooledge": [1, 4, 32], "ropecs": [128, 2, NT, 64], "iota16": [1, 16],
    "cT": [128, 8],
}


def host_consts():
    cn = {}
    cn["ident"] = np.eye(128, dtype=np.float32)
    s_ = np.arange(128)
    cn["triU"] = (s_[:, None] <= s_[None, :]).astype(np.float32)
    cn["triL"] = (s_[:, None] >= s_[None, :]).astype(np.float32)
    pe = np.zeros((1, 4, 32), np.float32)
    for g, w in enumerate((2, 4, 8, 16)):
        for j in range(32):
            t = j if j < 16 else S - 32 + j
            lo = max(t - w // 2, 0)
            hi = min(t + w // 2, S)
            pe[0, g, j] = w / float(hi - lo)
    cn["pooledge"] = pe
    t = np.arange(S)
    r, c = t // 64, t % 64
    freqs = (10000.0 ** (-np.arange(0, 64, 2, dtype=np.float32) / 64.0)).astype(np.float32)
    ang = np.concatenate([r[:, None].astype(np.float32) * freqs, c[:, None].astype(np.float32) * freqs], axis=-1).astype(np.float32)
    cs = np.stack([np.cos(ang), np.sin(ang)], 0).astype(np.float32)
    cn["ropecs"] = np.ascontiguousarray(cs.reshape(2, NT, 128, 64).transpose(2, 0, 1, 3))
    cn["iota16"] = np.arange(16, dtype=np.float32)[None, :]
    return cn


DEBUG = [False]


def build(first, last):
    nc = bass.Bass("TRN2", target_bir_lowering=False)
    IK = "ExternalOutput" if DEBUG[0] else "Internal"
    IN = {k: nc.dram_tensor(k, v, F32, kind="ExternalInput").ap() for k, v in W_SPECS.items()}
    xin = nc.dram_tensor("xin", [S, D], F32, kind="ExternalInput").ap()
    out = nc.dram_tensor("out", [S, D], F32, kind="ExternalOutput").ap()
    X = {}
    for k in range(0, 5):
        if k == first - 1:
            X[k] = Buf(xin)
        elif k == last:
            X[k] = Buf(out)
        elif first <= k < last:
            X[k] = Buf(nc.dram_tensor("X%d" % k, [S, D], F32, kind="Internal").ap())
    SC = {
        "CATT": Buf(nc.dram_tensor("CATT", [8, 128, S], BF16, kind=IK).ap()),
        "V1D": Buf(nc.dram_tensor("V1D", [NT, 128, 4, 130], BF16, kind=IK).ap()),
        "SIGD": Buf(nc.dram_tensor("SIGD", [NT, 128, 512], BF16, kind=IK).ap()),
        "HFD": Buf(nc.dram_tensor("HFD", [NT, 128, 512], F32, kind=IK).ap()),
        "TAB": [Buf(nc.dram_tensor("TAB%d" % i, [16384, 2048], BF16, kind="Internal").ap()) for i in range(2)],
    }
    SC["DBG1"] = Buf(nc.dram_tensor("DBG1", [128, NT, 16], F32, kind=IK).ap())
    SC["DBG2"] = Buf(nc.dram_tensor("DBG2", [128, 512], F32, kind=IK).ap())
    MODS = Buf(nc.dram_tensor("MODS", [2, 6, 128, 1024], F32, kind=IK).ap())
    with contextlib.ExitStack() as st:
        P = Prog(nc, st)
        phase_mods(P, nc, IN, MODS)
        if first <= 1 and last >= 2:
            SC["BG0"] = TableBuilder(P, nc, IN, 0, SC["TAB"][0])
        if first <= 3 and last >= 4:
            SC["BG1"] = TableBuilder(P, nc, IN, 1, SC["TAB"][1])
        for ph in range(first, last + 1):
            if ph == 1:
                phase_even(P, nc, IN, MODS, 0, X[0], X[1], SC)
            elif ph == 2:
                phase_peer(P, nc, IN, MODS, 0, X[1], X[2], SC, final=False)
            elif ph == 3:
                phase_attn(P, nc, IN, MODS, 1, X[2], X[3], SC)
            elif ph == 4:
                phase_peer(P, nc, IN, MODS, 1, X[3], X[4], SC, final=True)
    return nc


def host_inputs(inputs):
    f = lambda a: np.ascontiguousarray(np.asarray(a, dtype=np.float32))
    sh = {}
    sh["ada_w"] = f(inputs["ada_w"]); sh["ada_b"] = f(inputs["ada_b"])
    sh["norm_mix_g"] = f(inputs["norm_mix_g"]); sh["norm_ffn_g"] = f(inputs["norm_ffn_g"])
    sh["ev_w_in"] = f(inputs["ev_w_in"][0]); sh["ev_b_in"] = f(inputs["ev_b_in"])
    sh["ev_b_inT"] = f(np.asarray(inputs["ev_b_in"])[0, :2560].reshape(20, 128).T)
    cw = np.asarray(inputs["ev_conv_w"])[0, :, 0, :]
    sh["ev_conv_wT"] = f(cw.T.reshape(8, 128, 5).transpose(1, 0, 2))
    sh["ev_conv_bT"] = f(np.asarray(inputs["ev_conv_b"])[0].reshape(8, 128).T)
    sh["ev_mnorm_g"] = f(inputs["ev_mnorm_g"]); sh["ev_pool_w"] = f(inputs["ev_pool_w"][0]); sh["ev_pool_scale"] = f(inputs["ev_pool_scale"])
    sh["ev_w_out"] = f(inputs["ev_w_out"][0])
    sh["od_w_in"] = f(inputs["od_w_in"][0]); sh["od_qnorm_g"] = f(inputs["od_qnorm_g"]); sh["od_knorm_g"] = f(inputs["od_knorm_g"])
    sh["od_w_out"] = f(inputs["od_w_out"][0])
    sh["peer_w_q"] = f(inputs["peer_w_q"]); sh["peer_keys"] = f(inputs["peer_keys"])
    sh["peer_u"] = f(inputs["peer_u"]); sh["peer_v"] = f(inputs["peer_v"])
    sh["final_g"] = f(np.asarray(inputs["final_g"])[None, :])
    sh.update(host_consts())
    return sh


def run_phases(inputs, first, last, xin_list, cores):
    nc = build(first, last)
    sh = host_inputs(inputs)
    c = np.asarray(inputs["c"], dtype=np.float32)
    in_maps = []
    for j, b in enumerate(cores):
        m = dict(sh)
        m["cT"] = np.ascontiguousarray(c[b].reshape(8, 128).T)
        m["xin"] = np.ascontiguousarray(xin_list[j], dtype=np.float32)
        in_maps.append(m)
    res = run_bass_kernel_spmd(nc, in_maps, core_ids=list(range(len(cores))))
    if DEBUG[0]:
        return res.results
    return [r["out"] for r in res.results]


def kernel(**inputs):
    x = np.asarray(inputs["x"], dtype=np.float32)
    outs = run_phases(inputs, 1, 4, [x[b] for b in range(8)], list(range(8)))
    return np.stack(outs, 0).astype(np.float32)


class TableBuilder:
    def __init__(self, P, nc, IN, li, TAB):
        self.P, self.nc, self.IN, self.li, self.TAB = P, nc, IN, li, TAB
        self.blocks = [(half, blk) for half in range(2) for blk in range(64)]
        self.k = 0
        self.loaded = 0
        self.cx = None

    def attach(self, cx):
        self.cx = cx
        self.sf = [cx.T([128, 2, 1024], F32) for _ in range(2)]
        self.sb = [cx.T([128, 2, 1024], BF16) for _ in range(2)]

    def _load(self):
        if self.loaded >= len(self.blocks):
            return
        half, blk = self.blocks[self.loaded]
        f = self.sf[self.loaded % 2]
        rows = slice(blk * 256, (blk + 1) * 256)
        self.cx.dma("pool", f[:], self.IN[("peer_u", "peer_v")[half]][self.li, rows, :].rearrange("(n p) d -> p n d", p=128), wr=[f])
        self.loaded += 1

    def step(self, n=1):
        for _ in range(n):
            if self.k >= len(self.blocks):
                return
            if self.loaded == self.k:
                self._load()
            self._load()
            half, blk = self.blocks[self.k]
            f, b = self.sf[self.k % 2], self.sb[self.k % 2]
            rows = slice(blk * 256, (blk + 1) * 256)
            self.cx.G(lambda h: h.tensor_copy(out=b[:], in_=f[:]), rd=[f], wr=[b])
            self.cx.dma("pool", self.TAB.t[rows, half * 1024:(half + 1) * 1024].rearrange("(n p) d -> p n d", p=128), b[:], rd=[b], wr=[self.TAB])
            self.k += 1

    def finish(self):
        self.step(len(self.blocks))
        self.cx = None

    @property
    def done(self):
        return self.k >= len(self.blocks)


POOL_DOTS = False
PROD_DT = BF16
STT_SLOTS = ()
DIAG_ON_DVE = True


def phase_peer(P, nc, IN, MODS, li, Xin, Xout, SC, final):
    TAB = SC["TAB"][li]
    with contextlib.ExitStack() as st:
        cx = Ctx(P, nc, st)
        prebuilt = bool(SC.get("TABDONE", {}).get(li))
        sf = [cx.T([128, 4, 1024], F32) for _ in range(0 if prebuilt else 3)]
        sb = [cx.T([128, 4, 1024], BF16) for _ in range(0 if prebuilt else 3)]
        n = 0
        for half, nm in enumerate(("peer_u", "peer_v")):
            for blk in range(0 if prebuilt else 32):
                f, b = sf[n % 3], sb[n % 3]
                rows = slice(blk * 512, (blk + 1) * 512)
                cx.dma("sp", f[:], IN[nm][li, rows, :].rearrange("(n p) d -> p n d", p=128), wr=[f])
                if n % 3 == 0:
                    cx.A(lambda h: h.activation(out=b[:], in_=f[:], func=AF.Copy), rd=[f], wr=[b])
                elif n % 3 == 1:
                    cx.V(lambda h: h.tensor_copy(out=b[:], in_=f[:]), rd=[f], wr=[b])
                else:
                    cx.G(lambda h: h.tensor_copy(out=b[:], in_=f[:]), rd=[f], wr=[b])
                cx.dma("act", TAB.t[rows, half * 1024:(half + 1) * 1024].rearrange("(n p) d -> p n d", p=128), b[:], rd=[b], wr=[TAB])
                n += 1
        P.barrier()
        P.flush()
    with contextlib.ExitStack() as st:
        cx = Ctx(P, nc, st)
        idf, idb, eps = load_consts(cx, IN)
        gs2, sh2, g2 = load_mods(cx, MODS, li, [3, 4, 5])
        io16 = cx.T([128, 16], F32)
        th16 = cx.T([128, 16], F32)
        cx.dma("sp", io16[:], IN["iota16"].broadcast_to([128, 16]), wr=[io16])
        cx.V(lambda h: h.tensor_scalar(out=th16[:], in0=io16[:], scalar1=16.0, scalar2=None, op0=ALU.mult), rd=[io16], wr=[th16])
        if final:
            fg = cx.T([128, 1024], F32)
            cx.dma("sp", fg[:], IN["final_g"].broadcast_to([128, 1024]), wr=[fg])
        wq = cx.T([128, 8, 2048], BF16)
        ptr = cx.PS([128, 8, 128], BF16)
        keysT = cx.T([128, 2, 128], BF16)
        with contextlib.ExitStack() as stw:
            cw_ = Ctx(P, nc, stw)
            stg = [cw_.T([128, 2048], F32) for _ in range(2)]
            for k in range(8):
                cw_.dma("sp", stg[k % 2][:], IN["peer_w_q"][li, k * 128:(k + 1) * 128, :], wr=[stg[k % 2]])
                cw_.V(lambda h: h.tensor_copy(out=wq[:, k, :], in_=stg[k % 2][:]), rd=[stg[k % 2]], wr=[wq])
            kf = cw_.T([128, 2, 128], F32)
            kb = cw_.T([128, 2, 128], BF16)
            cw_.dma("sp", kf[:], IN["peer_keys"][li].rearrange("t n c -> n t c"), wr=[kf])
            cw_.V(lambda h: h.tensor_copy(out=kb[:], in_=kf[:]), rd=[kf], wr=[kb])
            for t in range(2):
                cw_.M(lambda h: h.transpose(out=ptr[:, t, :], in_=kb[:, t, :], identity=idb[:]), rd=[kb, idb], wr=[ptr])
            cw_.A(lambda h: h.activation(out=keysT[:], in_=ptr[:, 0:2, :], func=AF.Copy), rd=[ptr], wr=[keysT])
            P.barrier()
        pq = [cx.PS([128, 512]) for _ in range(2)]
        psc = [cx.PS([128, 4, 128]) for _ in range(2)]
        po = cx.PS([128, 1024])
        xb = [cx.T([128, 1024], F32) for _ in range(2)]
        sq = cx.T([128, 1024], F32)
        hb = cx.T([128, 1024], BF16)
        hTi = cx.T([128, 8, 128], BF16)
        st2 = cx.T([128, 4], F32)
        qb = cx.T([128, 2048], BF16)
        rs = cx.T([128, 48], F32)
        qT = cx.T([128, 16, 128], BF16)
        S1 = cx.T([128, 16, 128], F32)
        sqq = Buf(S1.t)
        sqq.k = S1.k
        sqq_ap = S1.t[:].rearrange("p g c -> p (g c)")
        wk = [cx.T([128, 128], F32) for _ in range(2)]
        m = cx.T([128, 16, 16], F32)
        ix = cx.T([128, 16, 16], U32)
        ixf = cx.T([128, 16, 16], F32)
        CS = cx.T([128, 8, 256], F32)
        wk2 = [cx.T([128, 256], F32) for _ in range(2)]
        tops = cx.T([128, 8, 16], F32)
        pos = cx.T([128, 8, 16], U32)
        posf = cx.T([128, 8, 16], F32)
        af = cx.T([128, 8, 16], F32)
        bf_ = cx.T([128, 8, 16], F32)
        oh = Buf(CS.t)
        oh.k = CS.k
        oh_ap = CS.t[:].rearrange("p h (a b) -> p h a b", a=16)
        i12 = cx.T([128, 2, 128], F32)
        idxf = cx.T([128, 128], F32)
        idx = cx.T([128, 128], I32)
        ge = cx.T([128, 8, 16], F32)
        gsum = cx.T([128, 16], F32)
        gate = cx.T([128, 128], F32)
        act = cx.T([128, 128], F32)
        gl = cx.T([128, 128], F32)
        coef = cx.T([128, 128], F32)
        NG = 5
        actT = [Tok() for _ in range(4)]
        glT = [Tok() for _ in range(4)]
        coefT = [Tok() for _ in range(4)]
        Gb = [cx.T([128, 4, 2048], BF16) for _ in range(NG)]
        Gtok = [[Tok() for _ in range(4)] for _ in range(NG)]
        diag = [cx.T([128, 4, 128], BF16) for _ in range(2)]
        junk = cx.T([128, 1024], BF16)
        NPR = 4
        junkv = cx.T([128, 1024], BF16) if STT_SLOTS else None
        prods = [cx.T([128, 1024], PROD_DT) for _ in range(NPR)]
        yb = [cx.T([128, 1024], F32) for _ in range(1)]
        sgn = 0
        hbs = [hb, cx.T([128, 1024], BF16)]
        idxs = [idx, cx.T([128, 128], I32)]
        gates = [gate, cx.T([128, 128], F32)]
        st3 = cx.T([128, 4], F32)
        sq2 = cx.T([128, 1024], F32) if final else None
        fg_ = fg if final else None
        FSTEP = 2

        def front(i):
            x = xb[i % 2]
            hb = hbs[i % 2]
            idx = idxs[i % 2]
            gate = gates[i % 2]
            x = xb[i % 2]
            yield
            cx.dma("sp", x[:], Xin.t[i * 128:(i + 1) * 128, :], rd=[Xin], wr=[x])
            yield
            emit_norm_tile(cx, x, gs2, sh2, hb, sq, st2, idb, ptr, hTi[:], hTi)
            for nb in range(4):
                p = pq[nb % 2]
                for k in range(8):
                    yield
                    cx.M(lambda h: h.matmul(p[:], lhsT=hTi[:, k, :], rhs=wq[:, k, nb * 512:(nb + 1) * 512], start=(k == 0), stop=(k == 7)), rd=[hTi, wq], wr=[p])
                yield
                cx.A(lambda h: h.activation(out=qb[:, nb * 512:(nb + 1) * 512], in_=p[:], func=AF.Copy), rd=[p], wr=[qb])
            yield
            cx.V(lambda h: h.tensor_tensor(out=sqq_ap, in0=qb[:], in1=qb[:], op=ALU.mult), rd=[qb], wr=[sqq])
            yield
            cx.V(lambda h: h.tensor_reduce(out=rs[:, 0:16], in_=S1.t[:], axis=AX.X, op=ALU.add), rd=[sqq], wr=[rs])
            yield
            cx.A(lambda h: h.activation(out=rs[:, 16:32], in_=rs[:, 0:16], func=AF.Sqrt, scale=1.0 / 128, bias=eps[:, 0:1]), rd=[rs, eps], wr=[rs])
            yield
            cx.V(lambda h: h.reciprocal(out=rs[:, 32:48], in_=rs[:, 16:32]), rd=[rs], wr=[rs])
            for r in range(2):
                for j in range(8):
                    g_ = r * 8 + j
                    yield
                    cx.M(lambda h: h.transpose(out=ptr[:, j, :], in_=qb[:, g_ * 128:(g_ + 1) * 128], identity=idb[:]), rd=[qb, idb], wr=[ptr])
                yield
                cx.A(lambda h: h.activation(out=qT[:, r * 8:(r + 1) * 8, :], in_=ptr[:], func=AF.Copy), rd=[ptr], wr=[qT])
            for r in range(4):
                ps_ = psc[r % 2]
                for j in range(4):
                    hp = r * 4 + j
                    yield
                    cx.M(lambda h: h.matmul(ps_[:, j, :], lhsT=qT[:, hp, :], rhs=keysT[:, hp % 2, :], start=True, stop=True), rd=[qT, keysT], wr=[ps_])
                yield
                cx.V(lambda h: h.tensor_tensor(out=S1[:, r * 4:(r + 1) * 4, :], in0=ps_[:], in1=rs[:, 32 + r * 4:32 + (r + 1) * 4].unsqueeze(2).broadcast_to([128, 4, 128]), op=ALU.mult),
                     rd=[ps_, rs], wr=[S1])
            for hp in range(16):
                w_ = wk[hp % 2]
                yield
                cx.V(lambda h: h.max(out=m[:, hp, 0:8], in_=S1[:, hp, :]), rd=[S1], wr=[m])
                yield
                cx.V(lambda h: h.max_index(out=ix[:, hp, 0:8], in_max=m[:, hp, 0:8], in_values=S1[:, hp, :]), rd=[m, S1], wr=[ix])
                yield
                cx.V(lambda h: h.match_replace(out=w_[:], in_to_replace=m[:, hp, 0:8], in_values=S1[:, hp, :], imm_value=-1e30), rd=[m, S1], wr=[w_])
                yield
                cx.V(lambda h: h.max(out=m[:, hp, 8:16], in_=w_[:]), rd=[w_], wr=[m])
                yield
                cx.V(lambda h: h.max_index(out=ix[:, hp, 8:16], in_max=m[:, hp, 8:16], in_values=w_[:]), rd=[m, w_], wr=[ix])
            mv = m[:].rearrange("p (h t) k -> p h t k", t=2)
            yield
            cx.V(lambda h: h.tensor_tensor(out=CS[:].rearrange("p h (a b) -> p h a b", a=16), in0=mv[:, :, 0, :].unsqueeze(3).broadcast_to([128, 8, 16, 16]),
                                           in1=mv[:, :, 1, :].unsqueeze(2).broadcast_to([128, 8, 16, 16]), op=ALU.add), rd=[m], wr=[CS])
            for hh in range(8):
                w_ = wk2[hh % 2]
                yield
                cx.V(lambda h: h.max(out=tops[:, hh, 0:8], in_=CS[:, hh, :]), rd=[CS], wr=[tops])
                yield
                cx.V(lambda h: h.max_index(out=pos[:, hh, 0:8], in_max=tops[:, hh, 0:8], in_values=CS[:, hh, :]), rd=[tops, CS], wr=[pos])
                yield
                cx.V(lambda h: h.match_replace(out=w_[:], in_to_replace=tops[:, hh, 0:8], in_values=CS[:, hh, :], imm_value=-1e30), rd=[tops, CS], wr=[w_])
                yield
                cx.V(lambda h: h.max(out=tops[:, hh, 8:16], in_=w_[:]), rd=[w_], wr=[tops])
                yield
                cx.V(lambda h: h.max_index(out=pos[:, hh, 8:16], in_max=tops[:, hh, 8:16], in_values=w_[:]), rd=[tops, w_], wr=[pos])
            yield
            cx.V(lambda h: h.tensor_copy(out=posf[:], in_=pos[:]), rd=[pos], wr=[posf])
            yield
            cx.V(lambda h: h.tensor_copy(out=ixf[:], in_=ix[:]), rd=[ix], wr=[ixf])
            bc4 = lambda ap3: ap3.unsqueeze(3).broadcast_to([128, 8, 16, 16])
            io4 = io16[:].unsqueeze(1).unsqueeze(1).broadcast_to([128, 8, 16, 16])
            th4 = th16[:].unsqueeze(1).unsqueeze(1).broadcast_to([128, 8, 16, 16])
            yield
            cx.V(lambda h: h.tensor_tensor(out=oh_ap, in0=bc4(posf[:]), in1=th4, op=ALU.is_ge), rd=[posf, th16], wr=[oh])
            yield
            cx.V(lambda h: h.tensor_reduce(out=af[:], in_=oh_ap, axis=AX.X, op=ALU.add), rd=[oh], wr=[af])
            yield
            cx.V(lambda h: h.tensor_scalar(out=af[:], in0=af[:], scalar1=-1.0, scalar2=None, op0=ALU.add), rd=[af], wr=[af])
            yield
            cx.V(lambda h: h.scalar_tensor_tensor(out=bf_[:], in0=af[:], scalar=-16.0, in1=posf[:], op0=ALU.mult, op1=ALU.add), rd=[af, posf], wr=[bf_])
            ixv = ixf[:].rearrange("p (h t) k -> p h t k", t=2)
            for t, src in ((0, af), (1, bf_)):
                yield
                cx.V(lambda h: h.tensor_tensor(out=oh_ap, in0=bc4(src[:]), in1=io4, op=ALU.is_equal), rd=[src, io16], wr=[oh])
                yield
                cx.V(lambda h: h.tensor_tensor(out=oh_ap, in0=oh_ap, in1=ixv[:, :, t, :].unsqueeze(2).broadcast_to([128, 8, 16, 16]), op=ALU.mult), rd=[oh, ixf], wr=[oh])
                yield
                cx.V(lambda h: h.tensor_reduce(out=i12[:, t, :].rearrange("p (h k) -> p h k", h=8), in_=oh_ap, axis=AX.X, op=ALU.add), rd=[oh], wr=[i12])
            yield
            cx.V(lambda h: h.scalar_tensor_tensor(out=idxf[:], in0=i12[:, 0, :], scalar=128.0, in1=i12[:, 1, :], op0=ALU.mult, op1=ALU.add), rd=[i12], wr=[idxf])
            yield
            cx.V(lambda h: h.tensor_copy(out=idx[:], in_=idxf[:]), rd=[idxf], wr=[idx])
            yield
            cx.V(lambda h: h.tensor_tensor(out=ge[:], in0=tops[:], in1=tops[:, :, 0:1].broadcast_to([128, 8, 16]), op=ALU.subtract), rd=[tops], wr=[ge])
            yield
            cx.A(lambda h: h.activation(out=ge[:], in_=ge[:], func=AF.Exp), rd=[ge], wr=[ge])
            yield
            cx.V(lambda h: h.tensor_reduce(out=gsum[:, 0:8], in_=ge[:], axis=AX.X, op=ALU.add), rd=[ge], wr=[gsum])
            yield
            cx.V(lambda h: h.reciprocal(out=gsum[:, 8:16], in_=gsum[:, 0:8]), rd=[gsum], wr=[gsum])
            yield
            cx.V(lambda h: h.tensor_tensor(out=gate[:].rearrange("p (h k) -> p h k", h=8), in0=ge[:], in1=gsum[:, 8:16].unsqueeze(2).broadcast_to([128, 8, 16]), op=ALU.mult),
                 rd=[ge, gsum], wr=[gate])


        def back(i, fg):
            nonlocal sgn
            x = xb[i % 2]
            hb = hbs[i % 2]
            idx = idxs[i % 2]
            gate = gates[i % 2]
            cx.V(lambda h: h.memset(act[:], 0.0), wr=actT)
            PF = NG - 2
            for sgi in range(32 + PF + 1):
                if sgi < 32:
                    bq_ = (sgn + sgi) % NG
                    for jj in range(4):
                        j = sgi * 4 + jj
                        P.dma("pool", lambda h: h.indirect_dma_start(out=Gb[bq_][:, jj, :], out_offset=None, in_=TAB.t,
                                                                     in_offset=bass.IndirectOffsetOnAxis(ap=idx[:, j:j + 1], axis=0)),
                              _ks([idx, TAB]), [Gtok[bq_][jj]])
                sg = sgi - PF
                if 0 <= sg < 32:
                    b_ = (sgn + sg) % NG
                    G_ = Gb[b_]
                    for jj in range(4):
                        j = sg * 4 + jj
                        if jj in STT_SLOTS:
                            cx.V(lambda h: h.scalar_tensor_tensor(out=junkv[:], in0=G_[:, jj, 0:1024], scalar=1.0, in1=hb[:], op0=ALU.mult, op1=ALU.mult, accum_out=act[:, j:j + 1]),
                                 rd=[Gtok[b_][jj], hb], wr=[junkv, actT[sg % 4]])
                        else:
                            pr = prods[j % NPR]
                            cx.V(lambda h: h.tensor_tensor(out=pr[:], in0=G_[:, jj, 0:1024], in1=hb[:], op=ALU.mult), rd=[Gtok[b_][jj], hb], wr=[pr])
                            cx.A(lambda h: h.activation(out=junk[:], in_=pr[:], func=AF.Copy, accum_out=act[:, j:j + 1]), rd=[pr], wr=[junk, actT[sg % 4]])
                        for _ in range(FSTEP):
                            next(fg, None)
                    cs = slice(sg * 4, (sg + 1) * 4)
                    cx.A(lambda h: h.activation(out=gl[:, cs], in_=act[:, cs], func=AF.Gelu), rd=[actT[sg % 4]], wr=[glT[sg % 4]])
                sg = sgi - PF - 1
                if 0 <= sg < 32:
                    b_ = (sgn + sg) % NG
                    G_ = Gb[b_]
                    dg = diag[sg % 2]
                    cs = slice(sg * 4, (sg + 1) * 4)
                    cx.V(lambda h: h.tensor_tensor(out=coef[:, cs], in0=gl[:, cs], in1=gate[:, cs], op=ALU.mult), rd=[glT[sg % 4], gate], wr=[coefT[sg % 4]])
                    for jj in range(4):
                        j = sg * 4 + jj
                        if DIAG_ON_DVE:
                            cx.V(lambda h: h.tensor_scalar(out=dg[:, jj, :], in0=idf[:], scalar1=coef[:, j:j + 1], scalar2=None, op0=ALU.mult), rd=[idf, coefT[sg % 4]], wr=[dg])
                        else:
                            cx.A(lambda h: h.activation(out=dg[:, jj, :], in_=idf[:], func=AF.Copy, scale=coef[:, j:j + 1]), rd=[idf, coefT[sg % 4]], wr=[dg])
                    for jj in range(4):
                        j = sg * 4 + jj
                        for hv in range(2):
                            cx.M(lambda h: h.matmul(po[:, hv * 512:(hv + 1) * 512], lhsT=dg[:, jj, :], rhs=G_[:, jj, 1024 + hv * 512:1024 + (hv + 1) * 512],
                                                    start=(j == 0), stop=(j == 127)), rd=[dg, Gtok[b_][jj]], wr=[po])
            sgn += 32
            y = yb[0]
            cx.V(lambda h: h.tensor_tensor(out=y[:], in0=po[:], in1=g2[:], op=ALU.mult), rd=[po, g2], wr=[y])
            cx.V(lambda h: h.tensor_tensor(out=y[:], in0=y[:], in1=x[:], op=ALU.add), rd=[y, x], wr=[y])
            if final:
                cx.V(lambda h: h.memset(st3[:, 0:1], 0.0), wr=[st3])
                cx.A(lambda h: h.activation(out=sq2[:], in_=y[:], func=AF.Square, accum_out=st3[:, 0:1]), rd=[y, st3], wr=[sq2, st3])
                cx.A(lambda h: h.activation(out=st3[:, 1:2], in_=st3[:, 0:1], func=AF.Sqrt, scale=1.0 / D, bias=eps[:, 0:1]), rd=[st3, eps], wr=[st3])
                cx.V(lambda h: h.reciprocal(out=st3[:, 2:3], in_=st3[:, 1:2]), rd=[st3], wr=[st3])
                cx.V(lambda h: h.scalar_tensor_tensor(out=y[:], in0=y[:], scalar=st3[:, 2:3], in1=fg_[:], op0=ALU.mult, op1=ALU.mult), rd=[y, st3, fg_], wr=[y])
            cx.dma("sp", Xout.t[i * 128:(i + 1) * 128, :], y[:], rd=[y], wr=[Xout])

        fgen = front(0)
        for _ in fgen:
            pass
        for i in range(NT):
            fgen = front(i + 1) if i + 1 < NT else iter(())
            back(i, fgen)
            for _ in fgen:
                pass
        P.barrier()
        P.flush()


def phase_attn(P, nc, IN, MODS, li, Xin, Xout, SC):
    with contextlib.ExitStack() as st0:
        c0 = Ctx(P, nc, st0)
        idf, idb, eps = load_consts(c0, IN)
        bigT = c0.T([128, 8, S], BF16)
        qT = c0.T([128, 8, S], BF16)
        kT = c0.T([128, 2, S], BF16)
        V1 = c0.T([128, NT, 2, 130], BF16)
        with contextlib.ExitStack() as st1:
            c1 = Ctx(P, nc, st1)
            gs1, sh1 = load_mods(c1, MODS, li, [0, 1])
            emit_hT(c1, Xin, gs1, sh1, idb, bigT)
            P.barrier()
        with contextlib.ExitStack() as st2:
            cx = Ctx(P, nc, st2)
            w = cx.T([128, 8, 1536], BF16)
            with contextlib.ExitStack() as stw:
                cw_ = Ctx(P, nc, stw)
                stg = [cw_.T([128, 1536], F32) for _ in range(2)]
                for k in range(8):
                    cw_.dma("sp", stg[k % 2][:], IN["od_w_in"][k * 128:(k + 1) * 128, :], wr=[stg[k % 2]])
                    cw_.V(lambda h: h.tensor_copy(out=w[:, k, :], in_=stg[k % 2][:]), rd=[stg[k % 2]], wr=[w])
                P.barrier()
            csb = [cx.T([128, 2, 64], F32) for _ in range(2)]
            gq = cx.T([128, 10, 128], F32)
            g1_ = cx.T([128, 128], F32)
            g2_ = cx.T([128, 128], F32)
            cx.dma("sp", g1_[:], IN["od_qnorm_g"].broadcast_to([128, 128]), wr=[g1_])
            cx.dma("sp", g2_[:], IN["od_knorm_g"].broadcast_to([128, 128]), wr=[g2_])
            cx.V(lambda h: h.tensor_scalar(out=g1_[:], in0=g1_[:], scalar1=float(128 ** -0.5), scalar2=None, op0=ALU.mult), rd=[g1_], wr=[g1_])
            cx.V(lambda h: h.tensor_copy(out=gq[:, 0:8, :], in_=g1_[:].unsqueeze(1).broadcast_to([128, 8, 128])), rd=[g1_], wr=[gq])
            cx.V(lambda h: h.tensor_copy(out=gq[:, 8:10, :], in_=g2_[:].unsqueeze(1).broadcast_to([128, 2, 128])), rd=[g2_], wr=[gq])
            cx.V(lambda h: h.memset(V1[:], 1.0), wr=[V1])
            pz = [cx.PS([128, 512]) for _ in range(3)]
            ptq = cx.PS([128, 8, 128], BF16)
            ptk = cx.PS([128, 2, 128], BF16)
            rs = cx.T([128, 32], F32)
            qn = cx.T([128, 10, 128], F32)
            qr = cx.T([128, 10, 128], BF16)
            t1 = cx.T([128, 10, 64], F32)
            t2 = cx.T([128, 10, 64], F32)
            for i in range(NT):
                tsl = slice(i * 128, (i + 1) * 128)
                for nb in range(3):
                    for k in range(8):
                        cx.M(lambda h: h.matmul(pz[nb][:], lhsT=bigT[:, k, tsl], rhs=w[:, k, nb * 512:(nb + 1) * 512], start=(k == 0), stop=(k == 7)), rd=[bigT, w], wr=[pz[nb]])
                cx.A(lambda h: h.activation(out=V1[:, i, :, 0:128], in_=pz[2][:, 256:512].rearrange("p (g d) -> p g d", g=2), func=AF.Copy), rd=[pz[2]], wr=[V1])
                cs = csb[i % 2]
                cx.dma("act", cs[:], IN["ropecs"][:, :, i, :], wr=[cs])
                zsrc = ((pz[0], 0, 4, 512), (pz[1], 4, 8, 512), (pz[2], 8, 10, 256))
                for (pp, g0, g1x, wd) in zsrc:
                    cx.A(lambda h: h.activation(out=qn[:, g0:g1x, :], in_=pp[:, 0:wd].rearrange("p (g d) -> p g d", d=128), func=AF.Square), rd=[pp], wr=[qn])
                cx.V(lambda h: h.tensor_reduce(out=rs[:, 0:10], in_=qn[:], axis=AX.X, op=ALU.add), rd=[qn], wr=[rs])
                cx.A(lambda h: h.activation(out=rs[:, 10:20], in_=rs[:, 0:10], func=AF.Sqrt, scale=1.0 / 128, bias=eps[:, 0:1]), rd=[rs, eps], wr=[rs])
                cx.V(lambda h: h.reciprocal(out=rs[:, 20:30], in_=rs[:, 10:20]), rd=[rs], wr=[rs])
                for (pp, g0, g1x, wd) in zsrc:
                    cx.V(lambda h: h.tensor_tensor(out=qn[:, g0:g1x, :], in0=pp[:, 0:wd].rearrange("p (g d) -> p g d", d=128),
                                                   in1=rs[:, 20 + g0:20 + g1x].unsqueeze(2).broadcast_to([128, g1x - g0, 128]), op=ALU.mult), rd=[pp, rs], wr=[qn])
                cx.V(lambda h: h.tensor_tensor(out=qn[:], in0=qn[:], in1=gq[:], op=ALU.mult), rd=[qn, gq], wr=[qn])
                qv = qn[:].rearrange("p g (d t) -> p g d t", t=2)
                qo = qr[:].rearrange("p g (d t) -> p g d t", t=2)
                cc = cs[:, 0, :].unsqueeze(1).broadcast_to([128, 10, 64])
                ss_ = cs[:, 1, :].unsqueeze(1).broadcast_to([128, 10, 64])
                cx.V(lambda h: h.tensor_tensor(out=t1[:], in0=qv[:, :, :, 0], in1=cc, op=ALU.mult), rd=[qn, cs], wr=[t1])
                cx.V(lambda h: h.tensor_tensor(out=t2[:], in0=qv[:, :, :, 1], in1=ss_, op=ALU.mult), rd=[qn, cs], wr=[t2])
                cx.V(lambda h: h.tensor_tensor(out=qo[:, :, :, 0], in0=t1[:], in1=t2[:], op=ALU.subtract), rd=[t1, t2], wr=[qr])
                cx.V(lambda h: h.tensor_tensor(out=t1[:], in0=qv[:, :, :, 0], in1=ss_, op=ALU.mult), rd=[qn, cs, qr], wr=[t1])
                cx.V(lambda h: h.tensor_tensor(out=t2[:], in0=qv[:, :, :, 1], in1=cc, op=ALU.mult), rd=[qn, cs, qr], wr=[t2])
                cx.V(lambda h: h.tensor_tensor(out=qo[:, :, :, 1], in0=t1[:], in1=t2[:], op=ALU.add), rd=[t1, t2], wr=[qr])
                for g_ in range(8):
                    cx.M(lambda h: h.transpose(out=ptq[:, g_, :], in_=qr[:, g_, :], identity=idb[:]), rd=[qr, idb], wr=[ptq])
                for g_ in range(2):
                    cx.M(lambda h: h.transpose(out=ptk[:, g_, :], in_=qr[:, 8 + g_, :], identity=idb[:]), rd=[qr, idb], wr=[ptk])
                cx.A(lambda h: h.activation(out=qT[:, :, tsl], in_=ptq[:], func=AF.Copy), rd=[ptq], wr=[qT])
                cx.A(lambda h: h.activation(out=kT[:, :, tsl], in_=ptk[:], func=AF.Copy), rd=[ptk], wr=[kT])
            P.barrier()
        import os as _os
        if _os.environ.get("ATT_STOP") == "2":
            P.barrier()
            P.flush()
            return
        with contextlib.ExitStack() as st3:
            cx = Ctx(P, nc, st3)
            pss = [cx.PS([128, 512]) for _ in range(2)]
            pacc = [cx.PS([128, 512]) for _ in range(4)]
            pto = cx.PS([128, 8, 128], BF16)
            pT = [cx.T([128, 512], BF16) for _ in range(3)]
            ao = [cx.T([128, 8, 128], BF16) for _ in range(4)]
            rinv = cx.T([128, 8], F32)
            steps = [(qb_, hd_, sj) for qb_ in range(8) for hd_ in range(8) for sj in range(NT)]
            accs = pacc

            def emit_score(n):
                qb_, hd_, sj = steps[n]
                ps_ = pss[n % 2]
                cx.M(lambda h: h.matmul(ps_[:], lhsT=kT[:, hd_ // 4, sj * 128:(sj + 1) * 128], rhs=qT[:, hd_, qb_ * 512:(qb_ + 1) * 512], start=True, stop=True), rd=[kT, qT], wr=[ps_])

            bg = SC.get("BG1")
            if bg is not None:
                bg.attach(cx)
            emit_score(0)
            for n, (qb_, hd_, sj) in enumerate(steps):
                g_ = hd_ // 4
                if bg is not None and n % 14 == 0:
                    bg.step(1)
                if n + 1 < len(steps):
                    emit_score(n + 1)
                ps_ = pss[n % 2]
                pt_ = pT[n % 3]
                cx.A(lambda h: h.activation(out=pt_[:], in_=ps_[:], func=AF.Exp), rd=[ps_], wr=[pt_])
                for qs in range(4):
                    cx.M(lambda h: h.matmul(accs[qs][:, 0:129], lhsT=pt_[:, qs * 128:(qs + 1) * 128], rhs=V1[:, sj, g_, 0:129], start=(sj == 0), stop=(sj == NT - 1)),
                         rd=[pt_, V1], wr=[accs[qs]])
                if sj == NT - 1:
                    for qs in range(4):
                        a_ = accs[qs]
                        cx.V(lambda h: h.reciprocal(out=rinv[:, qs:qs + 1], in_=a_[:, 128:129]), rd=[a_], wr=[rinv])
                        cx.V(lambda h: h.tensor_scalar(out=ao[qs][:, hd_, :], in0=a_[:, 0:128], scalar1=rinv[:, qs:qs + 1], scalar2=None, op0=ALU.mult), rd=[a_, rinv], wr=[ao[qs]])
                    if hd_ == 7:
                        for qs in range(4):
                            ti = qb_ * 4 + qs
                            for k in range(8):
                                cx.M(lambda h: h.transpose(out=pto[:, k, :], in_=ao[qs][:, k, :], identity=idb[:]), rd=[ao[qs], idb], wr=[pto])
                            cx.V(lambda h: h.tensor_copy(out=bigT[:, :, ti * 128:(ti + 1) * 128], in_=pto[:]), rd=[pto], wr=[bigT])
            if bg is not None:
                bg.finish()
                SC.setdefault("TABDONE", {})[1] = True
            P.barrier()
        if _os.environ.get("ATT_STOP") == "3":
            P.barrier()
            P.flush()
            return
        with contextlib.ExitStack() as st4:
            cx = Ctx(P, nc, st4)
            g1, = load_mods(cx, MODS, li, [2])
            stage_f = cx.T([128, 1024], F32)
            emit_outproj(cx, Xin, Xout, lambda i: (bigT[:, :, i * 128:(i + 1) * 128], bigT), IN["od_w_out"], g1, stage_f)
            P.barrier()
        P.flush()
```

```python
import contextlib
import numpy as np
import concourse.bass as bass
import concourse.mybir as mybir
from concourse.bass_utils import run_bass_kernel_spmd

F32 = mybir.dt.float32
BF16 = mybir.dt.bfloat16
I32 = mybir.dt.int32
U32 = mybir.dt.uint32
AF = mybir.ActivationFunctionType
ALU = mybir.AluOpType
AX = mybir.AxisListType

S = 4096
D = 1024
NT = S // 128
EPS = 1e-6


class Tok:
    __slots__ = ("w", "r", "name")

    def __init__(self, name=""):
        self.w = None
        self.r = {}
        self.name = name


class _Eng:
    def __init__(self, name, sem):
        self.name = name
        self.sem = sem
        self.cnt = 0
        self.seen = {}
        self.ops = []


NDMA = 12


class _Rec:
    def __getattr__(self, name):
        def f(*a, **k):
            return (name, a, k)
        return f


_REC = _Rec()


class Prog:
    ENG = ("pe", "act", "dve", "pool", "sp")

    def __init__(self, nc, stack):
        self.nc = nc
        self.stack = stack
        self.e = {}
        self.sems = {}
        for n in self.ENG:
            self.e[n] = _Eng(n, stack.enter_context(nc.semaphore("s_" + n)))
            self.sems[n] = (self.e[n].sem, 1)
        self.dslot = {}
        for q in ("sp", "pool", "act"):
            sl = []
            for j in range(NDMA):
                key = "d_%s%d" % (q, j)
                sem = stack.enter_context(nc.semaphore(key))
                self.sems[key] = (sem, 16)
                sl.append([key, 0])
            self.dslot[q] = [sl, 0]

    def _deps(self, e, rd, wr):
        deps = {}

        def need(dep, same_ok):
            if dep is None:
                return
            en, c = dep
            if en == e and (e == "pe" or not same_ok):
                return
            if deps.get(en, 0) < c:
                deps[en] = c

        for t in rd:
            need(t.w, True)
        for t in wr:
            need(t.w, False)
            for en, c in t.r.items():
                need((en, c), False)
        return deps

    def _emit_waits(self, E, deps):
        for en, c in deps.items():
            if E.seen.get(en, 0) < c:
                sem, step = self.sems[en]
                E.ops.append(lambda h, sem=sem, v=c * step: h.wait_ge(sem, v))
                E.seen[en] = c

    def op(self, e, fn, rd=(), wr=()):
        E = self.e[e]
        self._emit_waits(E, self._deps(e, rd, wr))
        E.cnt += 1
        sem = E.sem
        rec = fn(_REC)
        E.ops.append(lambda h, rec=rec, sem=sem: getattr(h, rec[0])(*rec[1], **rec[2]).then_inc(sem, 1))
        me = (e, E.cnt)
        for t in wr:
            t.w = me
            t.r = {}
        for t in rd:
            t.r[e] = E.cnt

    def dma(self, q, fn, rd=(), wr=()):
        E = self.e[q]
        slots, nxt = self.dslot[q]
        slot = slots[nxt % NDMA]
        self.dslot[q][1] = nxt + 1
        key = slot[0]
        deps = self._deps(key, rd, wr)
        if slot[1] > 0:
            deps[key] = max(deps.get(key, 0), slot[1])
        self._emit_waits(E, deps)
        slot[1] += 1
        sem = self.sems[key][0]
        rec = fn(_REC)
        E.ops.append(lambda h, rec=rec, sem=sem: getattr(h, rec[0])(*rec[1], **rec[2]).then_inc(sem, 16))
        me = (key, slot[1])
        for t in wr:
            t.w = me
            t.r = {}
        for t in rd:
            t.r[key] = slot[1]

    def barrier(self):
        tgt = {n: self.e[n].cnt for n in self.ENG}
        for q in self.dslot:
            for key, c in self.dslot[q][0]:
                tgt[key] = c
        for n in self.ENG:
            E = self.e[n]
            d = {k: v for k, v in tgt.items() if v > 0 and not (k == n and n == "pe")}
            self._emit_waits(E, d)

    def flush(self):
        nc = self.nc
        with nc.Block() as block:
            @block.tensor
            def _(h):
                for f in self.e["pe"].ops:
                    f(h)

            @block.scalar
            def _(h):
                for f in self.e["act"].ops:
                    f(h)

            @block.vector
            def _(h):
                for f in self.e["dve"].ops:
                    f(h)

            @block.gpsimd
            def _(h):
                for f in self.e["pool"].ops:
                    f(h)

            @block.sync
            def _(h):
                for f in self.e["sp"].ops:
                    f(h)
        for n in self.ENG:
            self.e[n].ops = []


class Buf:
    def __init__(self, t):
        self.t = t
        self.k = Tok()

    def __getitem__(self, i):
        return self.t[i]


def _ks(xs):
    return [x.k if isinstance(x, Buf) else x for x in xs]


_NAME = [0]


class Ctx:
    def __init__(self, P, nc, st):
        self.P, self.nc, self.st = P, nc, st
        self.n = 0

    def T(self, shape, dt, name=None):
        _NAME[0] += 1
        return Buf(self.st.enter_context(self.nc.sbuf_tensor("%s_%d" % (name or "t", _NAME[0]), list(shape), dt)))

    def PS(self, shape, dt=F32, name=None):
        _NAME[0] += 1
        return Buf(self.st.enter_context(self.nc.psum_tensor("%s_%d" % (name or "p", _NAME[0]), list(shape), dt)))

    def V(self, fn, rd=(), wr=()):
        self.P.op("dve", fn, _ks(rd), _ks(wr))

    def A(self, fn, rd=(), wr=()):
        self.P.op("act", fn, _ks(rd), _ks(wr))

    def G(self, fn, rd=(), wr=()):
        self.P.op("pool", fn, _ks(rd), _ks(wr))

    def M(self, fn, rd=(), wr=()):
        self.P.op("pe", fn, _ks(rd), _ks(wr))

    def dma(self, q, out, in_, rd=(), wr=()):
        self.P.dma(q, lambda h, out=out, in_=in_: h.dma_start(out=out, in_=in_), _ks(rd), _ks(wr))


def load_cast(cx, q, dst_bf, src_ap, stage, eng="pool"):
    cx.dma(q, stage.t[:] if not isinstance(stage, tuple) else stage[1], src_ap, wr=[stage if not isinstance(stage, tuple) else stage[0]])


def emit_norm_tile(cx, xt, gs, sh, hb, sq, st2, idb, ptr, hT_dst, hT_buf, hf=None):
    cx.V(lambda h: h.memset(st2[:, 0:1], 0.0), wr=[st2])
    cx.A(lambda h: h.activation(out=sq[:], in_=xt[:], func=AF.Square, accum_out=st2[:, 0:1]), rd=[xt, st2], wr=[sq, st2])
    cx.A(lambda h: h.activation(out=st2[:, 1:2], in_=st2[:, 0:1], func=AF.Sqrt, scale=1.0 / D, bias=EPS_AP[0][:, 0:1]), rd=[st2, EPS_AP[0]], wr=[st2])
    cx.V(lambda h: h.reciprocal(out=st2[:, 2:3], in_=st2[:, 1:2]), rd=[st2], wr=[st2])
    cx.V(lambda h: h.scalar_tensor_tensor(out=sq[:], in0=xt[:], scalar=st2[:, 2:3], in1=gs[:], op0=ALU.mult, op1=ALU.mult), rd=[xt, st2, gs], wr=[sq])
    if hf is not None:
        cx.V(lambda h: h.tensor_tensor(out=hf[:], in0=sq[:], in1=sh[:], op=ALU.add), rd=[sq, sh], wr=[hf])
        cx.G(lambda h: h.tensor_copy(out=hb[:], in_=hf[:]), rd=[hf], wr=[hb])
    else:
        cx.V(lambda h: h.tensor_tensor(out=hb[:], in0=sq[:], in1=sh[:], op=ALU.add), rd=[sq, sh], wr=[hb])
    for k in range(8):
        cx.M(lambda h, k=k: h.transpose(out=ptr[:, k, :], in_=hb[:, k * 128:(k + 1) * 128], identity=idb[:]), rd=[hb, idb], wr=[ptr])
    cx.A(lambda h: h.activation(out=hT_dst, in_=ptr[:], func=AF.Copy), rd=[ptr], wr=[hT_buf])


EPS_AP = [None]


def load_consts(cx, CN):
    idf = cx.T([128, 128], F32)
    idb = cx.T([128, 128], BF16)
    eps = cx.T([128, 1], F32)
    cx.dma("sp", idf[:], CN["ident"], wr=[idf])
    cx.V(lambda h: h.tensor_copy(out=idb[:], in_=idf[:]), rd=[idf], wr=[idb])
    cx.V(lambda h: h.memset(eps[:], EPS), wr=[eps])
    EPS_AP[0] = eps
    return idf, idb, eps


def phase_mods(P, nc, IN, MODS):
    with contextlib.ExitStack() as st:
        cx = Ctx(P, nc, st)
        cT = cx.T([128, 8], F32)
        cond = cx.T([128, 8], F32)
        crep = cx.T([128, 8, 128], F32)
        cx.dma("sp", cT[:], IN["cT"], wr=[cT])
        cx.A(lambda h: h.activation(out=cond[:], in_=cT[:], func=AF.Silu), rd=[cT], wr=[cond])
        cx.V(lambda h: h.tensor_copy(out=crep[:], in_=cond[:].unsqueeze(2).broadcast_to([128, 8, 128])), rd=[cond], wr=[crep])
        wb = [cx.T([128, 8, 512], F32) for _ in range(2)]
        ps = [cx.PS([128, 512]) for _ in range(2)]
        mod = cx.T([128, 6144], F32)
        ab = cx.T([128, 6144], F32)
        gm = cx.T([128, 1024], F32)
        gf = cx.T([128, 1024], F32)
        n = 0
        for i in range(2):
            cx.dma("act", ab[:], IN["ada_b"][i:i + 1, :].broadcast_to([128, 6144]), wr=[ab])
            cx.dma("act", gm[:], IN["norm_mix_g"][i:i + 1, :].broadcast_to([128, 1024]), wr=[gm])
            cx.dma("act", gf[:], IN["norm_ffn_g"][i:i + 1, :].broadcast_to([128, 1024]), wr=[gf])
            for nb in range(12):
                w = wb[n % 2]
                p = ps[n % 2]
                n += 1
                cx.dma("sp" if nb % 2 == 0 else "pool", w[:], IN["ada_w"][i, :, nb * 512:(nb + 1) * 512].rearrange("(k p) n -> p k n", p=128), wr=[w])
                for k in range(8):
                    cx.M(lambda h, k=k, w=w, p=p: h.matmul(p[:], lhsT=crep[:, k, :], rhs=w[:, k, :], start=(k == 0), stop=(k == 7)), rd=[crep, w], wr=[p])
                cx.V(lambda h, p=p, nb=nb: h.tensor_tensor(out=mod[:, nb * 512:(nb + 1) * 512], in0=p[:], in1=ab[:, nb * 512:(nb + 1) * 512], op=ALU.add), rd=[p, ab], wr=[mod])
            cx.V(lambda h: h.scalar_tensor_tensor(out=mod[:, 1024:2048], in0=mod[:, 1024:2048], scalar=1.0, in1=gm[:], op0=ALU.add, op1=ALU.mult), rd=[mod, gm], wr=[mod])
            cx.V(lambda h: h.scalar_tensor_tensor(out=mod[:, 4096:5120], in0=mod[:, 4096:5120], scalar=1.0, in1=gf[:], op0=ALU.add, op1=ALU.mult), rd=[mod, gf], wr=[mod])
            for j, off in enumerate([1024, 0, 2048, 4096, 3072, 5120]):
                cx.dma("sp", MODS.t[i, j], mod[:, off:off + 1024], rd=[mod], wr=[MODS])
        P.barrier()
        P.flush()


def load_mods(cx, MODS, i, js, q="act"):
    out = []
    for j in js:
        b = cx.T([128, 1024], F32)
        cx.dma(q, b[:], MODS.t[i, j], rd=[MODS], wr=[b])
        out.append(b)
    return out


def emit_hT(cx, Xin, gs, sh, idb, hT):
    xb = [cx.T([128, 1024], F32) for _ in range(2)]
    sq = cx.T([128, 1024], F32)
    hb = [cx.T([128, 1024], BF16) for _ in range(2)]
    st2 = [cx.T([128, 4], F32) for _ in range(2)]
    ptr = [cx.PS([128, 8, 128], BF16) for _ in range(2)]
    for i in range(NT):
        x = xb[i % 2]
        cx.dma("sp", x[:], Xin.t[i * 128:(i + 1) * 128, :], rd=[Xin], wr=[x])
        emit_norm_tile(cx, x, gs, sh, hb[i % 2], sq, st2[i % 2], idb, ptr[i % 2], hT[:, :, i * 128:(i + 1) * 128], hT)


def emit_outproj(cx, Xin, Xout, catT_src, w_ap, g1, stage_f, final=None):
    wob = cx.T([128, 8, 1024], BF16)
    for k in range(8):
        cx.dma("sp", stage_f[:], w_ap[k * 128:(k + 1) * 128, :], wr=[stage_f])
        cx.G(lambda h, k=k: h.tensor_copy(out=wob[:, k, :], in_=stage_f[:]), rd=[stage_f], wr=[wob])
    py = [cx.PS([128, 1024]) for _ in range(2)]
    xb = [cx.T([128, 1024], F32) for _ in range(2)]
    yb = [cx.T([128, 1024], F32) for _ in range(2)]
    for i in range(NT):
        ap, tok = catT_src(i)
        p = py[i % 2]
        x = xb[i % 2]
        y = yb[i % 2]
        cx.dma("act", x[:], Xin.t[i * 128:(i + 1) * 128, :], rd=[Xin], wr=[x])
        for nb in range(2):
            for k in range(8):
                cx.M(lambda h, k=k, nb=nb, p=p, ap=ap: h.matmul(p[:, nb * 512:(nb + 1) * 512], lhsT=ap[:, k, :], rhs=wob[:, k, nb * 512:(nb + 1) * 512],
                                                               start=(k == 0), stop=(k == 7)), rd=[tok, wob], wr=[p])
        cx.V(lambda h, p=p, y=y: h.tensor_tensor(out=y[:], in0=p[:], in1=g1[:], op=ALU.mult), rd=[p, g1], wr=[y])
        cx.G(lambda h, x=x, y=y: h.tensor_tensor(out=y[:], in0=y[:], in1=x[:], op=ALU.add), rd=[y, x], wr=[y])
        cx.dma("sp", Xout.t[i * 128:(i + 1) * 128, :], y[:], rd=[y], wr=[Xout])


def phase_even(P, nc, IN, MODS, li, Xin, Xout, SC):
    CATT, V1D, SIGD, HFD = SC["CATT"], SC["V1D"], SC["SIGD"], SC["HFD"]
    with contextlib.ExitStack() as st0:
        c0 = Ctx(P, nc, st0)
        idf, idb, eps = load_consts(c0, IN)
        QK = c0.T([128, 8, S], BF16)
        GT = c0.T([128, NT, 16], F32)
        EB = c0.T([128, NT, 8], F32)
        ES = c0.T([128, NT, 8], F32)
        EE = c0.T([128, NT, 8], F32)
        with contextlib.ExitStack() as st1:
            cx1 = Ctx(P, nc, st1)
            hT = cx1.T([128, 8, S], BF16)
            with contextlib.ExitStack() as st2:
                c2 = Ctx(P, nc, st2)
                gs1, sh1 = load_mods(c2, MODS, li, [0, 1])
                emit_hT(c2, Xin, gs1, sh1, idb, hT)
                P.barrier()
            with contextlib.ExitStack() as stf:
                cx = Ctx(P, nc, stf)
                bT = cx.T([128, 20], F32)
                cw = cx.T([128, 8, 5], F32)
                cb = cx.T([128, 8], F32)
                edge = cx.T([128, 4, 32], F32)
                cx.dma("sp", bT[:], IN["ev_b_inT"], wr=[bT])
                cx.dma("sp", cw[:], IN["ev_conv_wT"], wr=[cw])
                cx.dma("sp", cb[:], IN["ev_conv_bT"], wr=[cb])
                cx.dma("sp", edge[:], IN["pooledge"].broadcast_to([128, 4, 32]), wr=[edge])
                wst = [cx.T([128, 8, 128], F32) for _ in range(2)]
                wcb = [cx.T([128, 8, 128], BF16) for _ in range(2)]
                zc = cx.T([128, S + 16], F32)
                pa = cx.T([128, S + 16], F32)
                yb = cx.T([128, S + 16], F32)
                ybf = cx.T([128, S], BF16)
                yo = [cx.T([128, 512], BF16) for _ in range(2)]
                pw = cx.T([128, 128], F32)
                psc = cx.T([128, 128], F32)
                pwb = cx.T([128, 128], BF16)
                pz = [cx.PS([128, 512]) for _ in range(2)]
                cx.V(lambda h: h.memset(zc[:], 0.0), wr=[zc])
                nps = 0
                for c in range(12):
                    col0 = c * 128 if c < 8 else 2048 + (c - 8) * 128
                    bcol = c if c < 8 else 16 + (c - 8)
                    ws, wc = wst[c % 2], wcb[c % 2]
                    cx.dma("sp", ws[:], IN["ev_w_in"][:, col0:col0 + 128].rearrange("(k p) n -> p k n", p=128), wr=[ws])
                    cx.G(lambda h, ws=ws, wc=wc: h.tensor_copy(out=wc[:], in_=ws[:]), rd=[ws], wr=[wc])
                    for tb in range(8):
                        p = pz[nps % 2]
                        nps += 1
                        for k in range(8):
                            cx.M(lambda h, k=k, p=p, wc=wc, tb=tb: h.matmul(p[:], lhsT=wc[:, k, :], rhs=hT[:, k, tb * 512:(tb + 1) * 512], start=(k == 0), stop=(k == 7)),
                                 rd=[wc, hT], wr=[p])
                        cx.A(lambda h, p=p, tb=tb, bcol=bcol: h.activation(out=zc[:, 8 + tb * 512:8 + (tb + 1) * 512], in_=p[:], func=AF.Identity, bias=bT[:, bcol:bcol + 1]),
                             rd=[p, bT], wr=[zc])
                    if c < 8:
                        cx.V(lambda h, c=c: h.tensor_scalar(out=yb[:, 0:S], in0=zc[:, 6:6 + S], scalar1=cw[:, c, 0:1], scalar2=None, op0=ALU.mult), rd=[zc, cw], wr=[yb])
                        for j in range(1, 5):
                            cx.V(lambda h, c=c, j=j: h.scalar_tensor_tensor(out=yb[:, 0:S], in0=zc[:, 6 + j:6 + j + S], scalar=cw[:, c, j:j + 1], in1=yb[:, 0:S], op0=ALU.mult, op1=ALU.add),
                                 rd=[zc, cw, yb], wr=[yb])
                        cx.A(lambda h, c=c: h.activation(out=QK[:, c, :], in_=yb[:, 0:S], func=AF.Silu, bias=cb[:, c:c + 1]), rd=[yb, cb], wr=[QK])
                    else:
                        g = c - 8
                        win = (2, 4, 8, 16)[g]
                        half = win // 2
                        n_el = S + 15
                        cur = zc
                        bufs = [pa, yb]
                        bi = 0
                        step = 1
                        while step < win:
                            d = bufs[bi % 2]
                            bi += 1
                            cx.V(lambda h, cur=cur, d=d, step=step, n_el=n_el: h.tensor_tensor(out=d[:, 0:n_el - step + 1], in0=cur[:, 0:n_el - step + 1], in1=cur[:, step:n_el + 1], op=ALU.add),
                                 rd=[cur], wr=[d])
                            n_el = n_el - step
                            cur = d
                            step *= 2
                        o = bufs[bi % 2]
                        cx.V(lambda h, cur=cur, half=half, o=o, win=win: h.tensor_scalar(out=o[:, 0:S], in0=cur[:, 8 - half:8 - half + S], scalar1=1.0 / win, scalar2=None, op0=ALU.mult), rd=[cur], wr=[o])
                        cx.V(lambda h, o=o, g=g: h.tensor_tensor(out=o[:, 0:16], in0=o[:, 0:16], in1=edge[:, g, 0:16], op=ALU.mult), rd=[o, edge], wr=[o])
                        cx.V(lambda h, o=o, g=g: h.tensor_tensor(out=o[:, S - 16:S], in0=o[:, S - 16:S], in1=edge[:, g, 16:32], op=ALU.mult), rd=[o, edge], wr=[o])
                        cx.V(lambda h, o=o: h.tensor_tensor(out=ybf[:], in0=o[:, 0:S], in1=zc[:, 8:8 + S], op=ALU.subtract), rd=[o, zc], wr=[ybf])
                        cx.dma("sp", pw[:], IN["ev_pool_w"][g], wr=[pw])
                        cx.dma("sp", psc[:], IN["ev_pool_scale"][0:1, g * 128:(g + 1) * 128].broadcast_to([128, 128]), wr=[psc])
                        cx.V(lambda h: h.tensor_tensor(out=pwb[:], in0=pw[:], in1=psc[:], op=ALU.mult), rd=[pw, psc], wr=[pwb])
                        for tb in range(8):
                            p = pz[nps % 2]
                            y_ = yo[nps % 2]
                            nps += 1
                            cx.M(lambda h, p=p, tb=tb: h.matmul(p[:], lhsT=pwb[:], rhs=ybf[:, tb * 512:(tb + 1) * 512], start=True, stop=True), rd=[pwb, ybf], wr=[p])
                            cx.A(lambda h, p=p, y_=y_: h.activation(out=y_[:], in_=p[:], func=AF.Copy), rd=[p], wr=[y_])
                            cx.dma("sp", CATT.t[4 + g, :, tb * 512:(tb + 1) * 512], y_[:], rd=[y_], wr=[CATT])
                P.barrier()
            with contextlib.ExitStack() as stt:
                cx = Ctx(P, nc, stt)
                stage_f = cx.T([128, 1024], F32)
                wtm = cx.T([128, 8, 1040], BF16)
                for k in range(8):
                    cx.dma("sp", stage_f[:], IN["ev_w_in"][k * 128:(k + 1) * 128, 1024:2048], wr=[stage_f])
                    cx.V(lambda h, k=k: h.tensor_copy(out=wtm[:, k, 0:1024], in_=stage_f[:]), rd=[stage_f], wr=[wtm])
                gst = cx.T([128, 8, 16], F32)
                cx.dma("sp", gst[:], IN["ev_w_in"][:, 2560:2576].rearrange("(k p) n -> p k n", p=128), wr=[gst])
                cx.V(lambda h: h.tensor_copy(out=wtm[:, :, 1024:1040], in_=gst[:]), rd=[gst], wr=[wtm])
                bvo = cx.T([128, 1024], F32)
                bg = cx.T([128, 16], F32)
                cx.dma("sp", bvo[:], IN["ev_b_in"][0:1, 1024:2048].broadcast_to([128, 1024]), wr=[bvo])
                cx.dma("sp", bg[:], IN["ev_b_in"][0:1, 2560:2576].broadcast_to([128, 16]), wr=[bg])
                pv = [cx.PS([128, 512]) for _ in range(2)]
                po = [cx.PS([128, 512]) for _ in range(2)]
                pg = [cx.PS([128, 16]) for _ in range(2)]
                v1 = [cx.T([128, 4, 130], BF16) for _ in range(2)]
                of = [cx.T([128, 512], F32) for _ in range(2)]
                ob = [cx.T([128, 512], BF16) for _ in range(2)]
                for b_ in v1:
                    cx.V(lambda h, b_=b_: h.memset(b_[:], 1.0), wr=[b_])
                for i in range(NT):
                    a = i % 2
                    for (p, c0_, c1_) in ((pv[a], 0, 512), (po[a], 512, 1024), (pg[a], 1024, 1040)):
                        for k in range(8):
                            cx.M(lambda h, k=k, p=p, c0_=c0_, c1_=c1_, i=i: h.matmul(p[:], lhsT=hT[:, k, i * 128:(i + 1) * 128], rhs=wtm[:, k, c0_:c1_], start=(k == 0), stop=(k == 7)),
                                 rd=[hT, wtm], wr=[p])
                    for hh in range(4):
                        cx.V(lambda h, a=a, hh=hh: h.tensor_tensor(out=v1[a][:, hh, 0:128], in0=pv[a][:, hh * 128:(hh + 1) * 128],
                                                                   in1=bvo[:, hh * 128:(hh + 1) * 128], op=ALU.add), rd=[pv[a], bvo], wr=[v1[a]])
                    cx.dma("sp", V1D.t[i], v1[a][:], rd=[v1[a]], wr=[V1D])
                    cx.V(lambda h, a=a: h.tensor_tensor(out=of[a][:], in0=po[a][:], in1=bvo[:, 512:1024], op=ALU.add), rd=[po[a], bvo], wr=[of[a]])
                    cx.A(lambda h, a=a: h.activation(out=ob[a][:], in_=of[a][:], func=AF.Sigmoid), rd=[of[a]], wr=[ob[a]])
                    if i == 0:
                        cx.dma("sp", SC["DBG2"].t, of[a][:], rd=[of[a]], wr=[SC["DBG2"]])
                    cx.dma("sp", SIGD.t[i], ob[a][:], rd=[ob[a]], wr=[SIGD])
                    cx.V(lambda h, a=a, i=i: h.tensor_tensor(out=GT[:, i, :], in0=pg[a][:], in1=bg[:], op=ALU.add), rd=[pg[a], bg], wr=[GT])
                cx.dma("sp", SC["DBG1"].t, GT[:], rd=[GT], wr=[SC["DBG1"]])
                P.barrier()
            with contextlib.ExitStack() as stg:
                cx = Ctx(P, nc, stg)
                LF = cx.T([128, NT, 8], F32)
                t8 = cx.T([128, NT, 8], F32)
                BC = cx.T([128, NT, 16], F32)
                cx.A(lambda h: h.activation(out=t8[:], in_=GT[:, :, 8:16], func=AF.Exp, scale=-1.0), rd=[GT], wr=[t8])
                cx.V(lambda h: h.tensor_scalar(out=t8[:], in0=t8[:], scalar1=1.0, scalar2=None, op0=ALU.add), rd=[t8], wr=[t8])
                cx.A(lambda h: h.activation(out=LF[:], in_=t8[:], func=AF.Ln), rd=[t8], wr=[LF])
                cx.V(lambda h: h.tensor_scalar(out=LF[:], in0=LF[:], scalar1=-1.0, scalar2=None, op0=ALU.mult), rd=[LF], wr=[LF])
                triU = cx.T([128, 128], F32)
                triL = cx.T([128, 128], F32)
                ones = cx.T([128, 128], F32)
                cx.dma("sp", triU[:], IN["triU"], wr=[triU])
                cx.dma("sp", triL[:], IN["triL"], wr=[triL])
                cx.V(lambda h: h.memset(ones[:], 1.0), wr=[ones])
                pc = cx.PS([128, NT, 16])
                for i in range(NT):
                    cx.M(lambda h, i=i: h.matmul(pc[:, i, 0:4], lhsT=triU[:], rhs=LF[:, i, 0:4], start=True, stop=True), rd=[triU, LF], wr=[pc])
                    cx.M(lambda h, i=i: h.matmul(pc[:, i, 4:8], lhsT=triL[:], rhs=LF[:, i, 4:8], start=True, stop=True), rd=[triL, LF], wr=[pc])
                    cx.M(lambda h, i=i: h.matmul(pc[:, i, 8:16], lhsT=ones[:], rhs=LF[:, i, 0:8], start=True, stop=True), rd=[ones, LF], wr=[pc])
                cx.V(lambda h: h.tensor_copy(out=BC[:], in_=pc[:]), rd=[pc], wr=[BC])
                cx.A(lambda h: h.activation(out=EB[:], in_=BC[:, :, 0:8], func=AF.Exp), rd=[BC], wr=[EB])
                cx.A(lambda h: h.activation(out=EE[:], in_=BC[:, :, 8:16], func=AF.Exp), rd=[BC], wr=[EE])
                cx.V(lambda h: h.tensor_tensor(out=t8[:], in0=GT[:, :, 0:8], in1=BC[:, :, 0:8], op=ALU.subtract), rd=[GT, BC], wr=[t8])
                cx.V(lambda h: h.tensor_scalar(out=t8[:], in0=t8[:], scalar1=float(-0.5 * np.log(128.0)), scalar2=None, op0=ALU.add), rd=[t8], wr=[t8])
                cx.A(lambda h: h.activation(out=ES[:], in_=t8[:], func=AF.Exp), rd=[t8], wr=[ES])
                P.barrier()
        with contextlib.ExitStack() as st3:
            cx = Ctx(P, nc, st3)
            mk = []
            for nm in ("triU", "triL"):
                f = cx.T([128, 128], F32)
                cx.dma("sp", f[:], IN[nm], wr=[f])
                mk.append(f)
            Cst = cx.T([128, 8, 129], F32)
            Cb = cx.T([128, 8, 129], BF16)
            cx.V(lambda h: h.memset(Cst[:], 0.0), wr=[Cst])
            cx.V(lambda h: h.memset(Cb[:], 0.0), wr=[Cb])
            NV = 6
            v1 = [cx.T([128, 4, 130], BF16) for _ in range(NV)]
            psS = [cx.PS([128, 4, 128]) for _ in range(2)]
            psA = [cx.PS([128, 3, 129]) for _ in range(3)]
            ps_t = cx.PS([128, 8, 128], BF16)
            ps_h = cx.PS([128, 4, 128], BF16)
            AT = cx.T([128, 8, 128], BF16)
            ksb = cx.T([128, 8, 128], BF16)
            sm = cx.T([128, 8, 4], F32)
            hacc = cx.T([128, NT, 512], F32)
            NSG = 6
            sgl = [cx.T([128, 512], BF16) for _ in range(NSG)]
            sq = cx.T([128, 512], F32)
            st4 = [cx.T([128, 12], F32) for _ in range(2)]
            mg = cx.T([128, 512], F32)
            hmb = [cx.T([128, 512], BF16) for _ in range(2)]
            hmT = [cx.T([128, 4, 128], BF16) for _ in range(2)]
            cx.dma("sp", mg[:], IN["ev_mnorm_g"][0:1, :].broadcast_to([128, 512]), wr=[mg])
            bg = SC.get("BG0")
            if bg is not None:
                bg.attach(cx)
            chains = [(d, hh) for d in range(2) for hh in range(4)]

            def aslot(c):
                return psA[c // 3], c % 3

            def tile_of(s, d):
                return s if d == 0 else NT - 1 - s

            PDV = 2
            nfin = 0
            for s in range(NT + PDV):
                if s < NT:
                    for d in range(2):
                        vb = v1[(2 * s + d) % NV]
                        cx.dma("sp", vb[:], V1D.t[tile_of(s, d)], rd=[V1D], wr=[vb])
                    if s >= NT // 2:
                        for d in range(2):
                            sg_ = sgl[(2 * s + d) % NSG]
                            cx.dma("act", sg_[:], SIGD.t[tile_of(s, d)], rd=[SIGD], wr=[sg_])
                s_ = s - PDV
                if s_ < 0:
                    continue
                s = s_
                if bg is not None:
                    bg.step(4)
                vbs = [v1[(2 * s + d) % NV] for d in range(2)]
                tls = [tile_of(s, d) for d in range(2)]
                for c, (d, hh) in enumerate(chains):
                    tsl = slice(tls[d] * 128, (tls[d] + 1) * 128)
                    cx.M(lambda h: h.matmul(psS[d][:, hh, :], lhsT=QK[:, 4 + hh, tsl], rhs=QK[:, hh, tsl], start=True, stop=True), rd=[QK], wr=[psS[d]])
                for c, (d, hh) in enumerate(chains):
                    col = d * 4 + hh
                    cx.V(lambda h: h.scalar_tensor_tensor(out=AT[:, c, :], in0=psS[d][:, hh, :], scalar=ES[:, tls[d], col:col + 1], in1=mk[d][:], op0=ALU.mult, op1=ALU.mult),
                         rd=[psS[d], ES, mk[d]], wr=[AT])
                for c, (d, hh) in enumerate(chains):
                    col = d * 4 + hh
                    tsl = slice(tls[d] * 128, (tls[d] + 1) * 128)
                    pa, sl = aslot(c)
                    cx.M(lambda h: h.matmul(pa[:, sl, :], lhsT=AT[:, c, :], rhs=vbs[d][:, hh, 0:129], start=True, stop=False), rd=[AT, vbs[d]], wr=[pa])
                    cx.M(lambda h: h.matmul(pa[:, sl, :], lhsT=QK[:, hh, tsl], rhs=Cb[:, col, :], start=False, stop=True), rd=[QK, Cb], wr=[pa])
                for c, (d, hh) in enumerate(chains):
                    col = d * 4 + hh
                    pa, sl = aslot(c)
                    cx.A(lambda h: h.activation(out=sm[:, c, 2:3], in_=pa[:, sl, 128:129], func=AF.Abs, scale=EB[:, tls[d], col:col + 1]), rd=[pa, EB], wr=[sm])
                cx.V(lambda h: h.tensor_scalar(out=sm[:, :, 0:1], in0=sm[:, :, 2:3], scalar1=1.0, scalar2=None, op0=ALU.max), rd=[sm], wr=[sm])
                cx.V(lambda h: h.reciprocal(out=sm[:, :, 3:4], in_=sm[:, :, 0:1]), rd=[sm], wr=[sm])
                for d in range(2):
                    cx.V(lambda h: h.tensor_tensor(out=sm[:, d * 4:(d + 1) * 4, 1:2], in0=EB[:, tls[d], d * 4:(d + 1) * 4].unsqueeze(2), in1=sm[:, d * 4:(d + 1) * 4, 3:4], op=ALU.mult),
                         rd=[sm, EB], wr=[sm])
                for c, (d, hh) in enumerate(chains):
                    pa, sl = aslot(c)
                    dst = hacc[:, tls[d], hh * 128:(hh + 1) * 128]
                    if s < NT // 2:
                        cx.A(lambda h: h.activation(out=dst, in_=pa[:, sl, 0:128], func=AF.Copy, scale=sm[:, c, 1:2]), rd=[pa, sm], wr=[hacc])
                    else:
                        cx.V(lambda h: h.scalar_tensor_tensor(out=dst, in0=pa[:, sl, 0:128], scalar=sm[:, c, 1:2], in1=dst, op0=ALU.mult, op1=ALU.add), rd=[pa, sm, hacc], wr=[hacc])
                for c, (d, hh) in enumerate(chains):
                    tsl = slice(tls[d] * 128, (tls[d] + 1) * 128)
                    cx.M(lambda h: h.transpose(out=ps_t[:, c, :], in_=QK[:, 4 + hh, tsl], identity=idb[:]), rd=[QK, idb], wr=[ps_t])
                for c, (d, hh) in enumerate(chains):
                    col = d * 4 + hh
                    cx.A(lambda h: h.activation(out=ksb[:, c, :], in_=ps_t[:, c, :], func=AF.Copy, scale=ES[:, tls[d], col:col + 1]), rd=[ps_t, ES], wr=[ksb])
                for c, (d, hh) in enumerate(chains):
                    pa, sl = aslot(c)
                    cx.M(lambda h: h.matmul(pa[:, sl, :], lhsT=ksb[:, c, :], rhs=vbs[d][:, hh, 0:129], start=True, stop=True), rd=[ksb, vbs[d]], wr=[pa])
                for c, (d, hh) in enumerate(chains):
                    col = d * 4 + hh
                    pa, sl = aslot(c)
                    cx.V(lambda h: h.tensor_scalar(out=Cst[:, col, :], in0=Cst[:, col, :], scalar1=EE[:, tls[d], col:col + 1], scalar2=None, op0=ALU.mult), rd=[Cst, EE], wr=[Cst])
                    cx.V(lambda h: h.scalar_tensor_tensor(out=Cst[:, col, :], in0=pa[:, sl, :], scalar=EE[:, tls[d], col:col + 1], in1=Cst[:, col, :], op0=ALU.mult, op1=ALU.add),
                         rd=[pa, EE, Cst], wr=[Cst])
                cx.A(lambda h: h.activation(out=Cb[:], in_=Cst[:], func=AF.Copy), rd=[Cst], wr=[Cb])
                if s >= NT // 2:
                    for d in range(2):
                        i = tls[d]
                        a = nfin % 2
                        nfin += 1
                        sg_ = sgl[(2 * s + d) % NSG]
                        hv = hacc[:, i, :]
                        cx.V(lambda h: h.tensor_tensor(out=sq[:], in0=hv, in1=hv, op=ALU.mult), rd=[hacc], wr=[sq])
                        cx.V(lambda h: h.tensor_reduce(out=st4[a][:, 0:4], in_=sq[:].rearrange("p (h d) -> p h d", h=4), axis=AX.X, op=ALU.add), rd=[sq], wr=[st4[a]])
                        cx.A(lambda h: h.activation(out=st4[a][:, 4:8], in_=st4[a][:, 0:4], func=AF.Sqrt, scale=1.0 / 128, bias=eps[:, 0:1]), rd=[st4[a], eps], wr=[st4[a]])
                        cx.V(lambda h: h.reciprocal(out=st4[a][:, 8:12], in_=st4[a][:, 4:8]), rd=[st4[a]], wr=[st4[a]])
                        cx.V(lambda h: h.tensor_tensor(out=hv.rearrange("p (h d) -> p h d", h=4), in0=hv.rearrange("p (h d) -> p h d", h=4),
                                                       in1=st4[a][:, 8:12].unsqueeze(2).broadcast_to([128, 4, 128]), op=ALU.mult), rd=[hacc, st4[a]], wr=[hacc])
                        cx.V(lambda h: h.tensor_tensor(out=sq[:], in0=mg[:], in1=sg_[:], op=ALU.mult), rd=[mg, sg_], wr=[sq])
                        cx.V(lambda h: h.tensor_tensor(out=hmb[a][:], in0=hv, in1=sq[:], op=ALU.mult), rd=[hacc, sq], wr=[hmb[a]])
                        for k in range(4):
                            cx.M(lambda h: h.transpose(out=ps_h[:, k, :], in_=hmb[a][:, k * 128:(k + 1) * 128], identity=idb[:]), rd=[hmb[a], idb], wr=[ps_h])
                        cx.A(lambda h: h.activation(out=hmT[a][:], in_=ps_h[:], func=AF.Copy), rd=[ps_h], wr=[hmT[a]])
                        cx.dma("act", CATT.t[0:4, :, i * 128:(i + 1) * 128].rearrange("c f t -> f c t"), hmT[a][:], rd=[hmT[a]], wr=[CATT])
            if bg is not None:
                bg.finish()
                SC.setdefault("TABDONE", {})[0] = True
            P.barrier()
        with contextlib.ExitStack() as st4_:
            cx = Ctx(P, nc, st4_)
            cb_ = [cx.T([128, 8, 128], BF16) for _ in range(3)]
            g1, = load_mods(cx, MODS, li, [2])
            stage_f = cx.T([128, 1024], F32)

            def src(i):
                b = cb_[i % 3]
                cx.dma("pool", b[:], CATT.t[:, :, i * 128:(i + 1) * 128].rearrange("c f t -> f c t"), rd=[CATT], wr=[b])
                return b, b
            emit_outproj(cx, Xin, Xout, src, IN["ev_w_out"], g1, stage_f)
            P.barrier()
        P.flush()


W_SPECS = {
    "ada_w": [2, 1024, 6144], "ada_b": [2, 6144], "norm_mix_g": [2, 1024], "norm_ffn_g": [2, 1024],
    "ev_w_in": [1024, 2576], "ev_b_in": [1, 2576], "ev_b_inT": [128, 20], "ev_conv_wT": [128, 8, 5], "ev_conv_bT": [128, 8],
    "ev_mnorm_g": [1, 512], "ev_pool_w": [4, 128, 128], "ev_pool_scale": [1, 512], "ev_w_out": [1024, 1024],
    "od_w_in": [1024, 1536], "od_qnorm_g": [1, 128], "od_knorm_g": [1, 128], "od_w_out": [1024, 1024],
    "peer_w_q": [2, 1024, 2048], "peer_keys": [2, 2, 128, 128], "peer_u": [2, 16384, 1024], "peer_v": [2, 16384, 1024],
    "final_g": [1, 1024],
    "ident": [128, 128], "triU": [128, 128], "triL": [128, 128], "pooledge": [1, 4, 32], "ropecs": [128, 2, NT, 64], "iota16": [1, 16],
    "cT": [128, 8],
}


def host_consts():
    cn = {}
    cn["ident"] = np.eye(128, dtype=np.float32)
    s_ = np.arange(128)
    cn["triU"] = (s_[:, None] <= s_[None, :]).astype(np.float32)
    cn["triL"] = (s_[:, None] >= s_[None, :]).astype(np.float32)
    pe = np.zeros((1, 4, 32), np.float32)
    for g, w in enumerate((2, 4, 8, 16)):
        for j in range(32):
            t = j if j < 16 else S - 32 + j
            lo = max(t - w // 2, 0)
            hi = min(t + w // 2, S)
            pe[0, g, j] = w / float(hi - lo)
    cn["pooledge"] = pe
    t = np.arange(S)
    r, c = t // 64, t % 64
    freqs = (10000.0 ** (-np.arange(0, 64, 2, dtype=np.float32) / 64.0)).astype(np.float32)
    ang = np.concatenate([r[:, None].astype(np.float32) * freqs, c[:, None].astype(np.float32) * freqs], axis=-1).astype(np.float32)
    cs = np.stack([np.cos(ang), np.sin(ang)], 0).astype(np.float32)
    cn["ropecs"] = np.ascontiguousarray(cs.reshape(2, NT, 128, 64).transpose(2, 0, 1, 3))
    cn["iota16"] = np.arange(16, dtype=np.float32)[None, :]
    return cn


DEBUG = [False]


def build(first, last):
    nc = bass.Bass("TRN2", target_bir_lowering=False)
    IK = "ExternalOutput" if DEBUG[0] else "Internal"
    IN = {k: nc.dram_tensor(k, v, F32, kind="ExternalInput").ap() for k, v in W_SPECS.items()}
    xin = nc.dram_tensor("xin", [S, D], F32, kind="ExternalInput").ap()
    out = nc.dram_tensor("out", [S, D], F32, kind="ExternalOutput").ap()
    X = {}
    for k in range(0, 5):
        if k == first - 1:
            X[k] = Buf(xin)
        elif k == last:
            X[k] = Buf(out)
        elif first <= k < last:
            X[k] = Buf(nc.dram_tensor("X%d" % k, [S, D], F32, kind="Internal").ap())
    SC = {
        "CATT": Buf(nc.dram_tensor("CATT", [8, 128, S], BF16, kind=IK).ap()),
        "V1D": Buf(nc.dram_tensor("V1D", [NT, 128, 4, 130], BF16, kind=IK).ap()),
        "SIGD": Buf(nc.dram_tensor("SIGD", [NT, 128, 512], BF16, kind=IK).ap()),
        "HFD": Buf(nc.dram_tensor("HFD", [NT, 128, 512], F32, kind=IK).ap()),
        "TAB": [Buf(nc.dram_tensor("TAB%d" % i, [16384, 2048], BF16, kind="Internal").ap()) for i in range(2)],
    }
    SC["DBG1"] = Buf(nc.dram_tensor("DBG1", [128, NT, 16], F32, kind=IK).ap())
    SC["DBG2"] = Buf(nc.dram_tensor("DBG2", [128, 512], F32, kind=IK).ap())
    MODS = Buf(nc.dram_tensor("MODS", [2, 6, 128, 1024], F32, kind=IK).ap())
    with contextlib.ExitStack() as st:
        P = Prog(nc, st)
        phase_mods(P, nc, IN, MODS)
        if first <= 1 and last >= 2:
            SC["BG0"] = TableBuilder(P, nc, IN, 0, SC["TAB"][0])
        if first <= 3 and last >= 4:
            SC["BG1"] = TableBuilder(P, nc, IN, 1, SC["TAB"][1])
        for ph in range(first, last + 1):
            if ph == 1:
                phase_even(P, nc, IN, MODS, 0, X[0], X[1], SC)
            elif ph == 2:
                phase_peer(P, nc, IN, MODS, 0, X[1], X[2], SC, final=False)
            elif ph == 3:
                phase_attn(P, nc, IN, MODS, 1, X[2], X[3], SC)
            elif ph == 4:
                phase_peer(P, nc, IN, MODS, 1, X[3], X[4], SC, final=True)
    return nc


def host_inputs(inputs):
    f = lambda a: np.ascontiguousarray(np.asarray(a, dtype=np.float32))
    sh = {}
    sh["ada_w"] = f(inputs["ada_w"]); sh["ada_b"] = f(inputs["ada_b"])
    sh["norm_mix_g"] = f(inputs["norm_mix_g"]); sh["norm_ffn_g"] = f(inputs["norm_ffn_g"])
    sh["ev_w_in"] = f(inputs["ev_w_in"][0]); sh["ev_b_in"] = f(inputs["ev_b_in"])
    sh["ev_b_inT"] = f(np.asarray(inputs["ev_b_in"])[0, :2560].reshape(20, 128).T)
    cw = np.asarray(inputs["ev_conv_w"])[0, :, 0, :]
    sh["ev_conv_wT"] = f(cw.T.reshape(8, 128, 5).transpose(1, 0, 2))
    sh["ev_conv_bT"] = f(np.asarray(inputs["ev_conv_b"])[0].reshape(8, 128).T)
    sh["ev_mnorm_g"] = f(inputs["ev_mnorm_g"]); sh["ev_pool_w"] = f(inputs["ev_pool_w"][0]); sh["ev_pool_scale"] = f(inputs["ev_pool_scale"])
    sh["ev_w_out"] = f(inputs["ev_w_out"][0])
    sh["od_w_in"] = f(inputs["od_w_in"][0]); sh["od_qnorm_g"] = f(inputs["od_qnorm_g"]); sh["od_knorm_g"] = f(inputs["od_knorm_g"])
    sh["od_w_out"] = f(inputs["od_w_out"][0])
    sh["peer_w_q"] = f(inputs["peer_w_q"]); sh["peer_keys"] = f(inputs["peer_keys"])
    sh["peer_u"] = f(inputs["peer_u"]); sh["peer_v"] = f(inputs["peer_v"])
    sh["final_g"] = f(np.asarray(inputs["final_g"])[None, :])
    sh.update(host_consts())
    return sh


def run_phases(inputs, first, last, xin_list, cores):
    nc = build(first, last)
    sh = host_inputs(inputs)
    c = np.asarray(inputs["c"], dtype=np.float32)
    in_maps = []
    for j, b in enumerate(cores):
        m = dict(sh)
        m["cT"] = np.ascontiguousarray(c[b].reshape(8, 128).T)
        m["xin"] = np.ascontiguousarray(xin_list[j], dtype=np.float32)
        in_maps.append(m)
    res = run_bass_kernel_spmd(nc, in_maps, core_ids=list(range(len(cores))))
    if DEBUG[0]:
        return res.results
    return [r["out"] for r in res.results]


def kernel(**inputs):
    x = np.asarray(inputs["x"], dtype=np.float32)
    outs = run_phases(inputs, 1, 4, [x[b] for b in range(8)], list(range(8)))
    return np.stack(outs, 0).astype(np.float32)


class TableBuilder:
    def __init__(self, P, nc, IN, li, TAB):
        self.P, self.nc, self.IN, self.li, self.TAB = P, nc, IN, li, TAB
        self.blocks = [(half, blk) for half in range(2) for blk in range(64)]
        self.k = 0
        self.loaded = 0
        self.cx = None

    def attach(self, cx):
        self.cx = cx
        self.sf = [cx.T([128, 2, 1024], F32) for _ in range(2)]
        self.sb = [cx.T([128, 2, 1024], BF16) for _ in range(2)]

    def _load(self):
        if self.loaded >= len(self.blocks):
            return
        half, blk = self.blocks[self.loaded]
        f = self.sf[self.loaded % 2]
        rows = slice(blk * 256, (blk + 1) * 256)
        self.cx.dma("pool", f[:], self.IN[("peer_u", "peer_v")[half]][self.li, rows, :].rearrange("(n p) d -> p n d", p=128), wr=[f])
        self.loaded += 1

    def step(self, n=1):
        for _ in range(n):
            if self.k >= len(self.blocks):
                return
            if self.loaded == self.k:
                self._load()
            self._load()
            half, blk = self.blocks[self.k]
            f, b = self.sf[self.k % 2], self.sb[self.k % 2]
            rows = slice(blk * 256, (blk + 1) * 256)
            self.cx.G(lambda h: h.tensor_copy(out=b[:], in_=f[:]), rd=[f], wr=[b])
            self.cx.dma("pool", self.TAB.t[rows, half * 1024:(half + 1) * 1024].rearrange("(n p) d -> p n d", p=128), b[:], rd=[b], wr=[self.TAB])
            self.k += 1

    def finish(self):
        self.step(len(self.blocks))
        self.cx = None

    @property
    def done(self):
        return self.k >= len(self.blocks)


POOL_DOTS = False
PROD_DT = BF16
STT_SLOTS = ()
DIAG_ON_DVE = True


def phase_peer(P, nc, IN, MODS, li, Xin, Xout, SC, final):
    TAB = SC["TAB"][li]
    with contextlib.ExitStack() as st:
        cx = Ctx(P, nc, st)
        prebuilt = bool(SC.get("TABDONE", {}).get(li))
        sf = [cx.T([128, 4, 1024], F32) for _ in range(0 if prebuilt else 3)]
        sb = [cx.T([128, 4, 1024], BF16) for _ in range(0 if prebuilt else 3)]
        n = 0
        for half, nm in enumerate(("peer_u", "peer_v")):
            for blk in range(0 if prebuilt else 32):
                f, b = sf[n % 3], sb[n % 3]
                rows = slice(blk * 512, (blk + 1) * 512)
                cx.dma("sp", f[:], IN[nm][li, rows, :].rearrange("(n p) d -> p n d", p=128), wr=[f])
                if n % 3 == 0:
                    cx.A(lambda h: h.activation(out=b[:], in_=f[:], func=AF.Copy), rd=[f], wr=[b])
                elif n % 3 == 1:
                    cx.V(lambda h: h.tensor_copy(out=b[:], in_=f[:]), rd=[f], wr=[b])
                else:
                    cx.G(lambda h: h.tensor_copy(out=b[:], in_=f[:]), rd=[f], wr=[b])
                cx.dma("act", TAB.t[rows, half * 1024:(half + 1) * 1024].rearrange("(n p) d -> p n d", p=128), b[:], rd=[b], wr=[TAB])
                n += 1
        P.barrier()
        P.flush()
    with contextlib.ExitStack() as st:
        cx = Ctx(P, nc, st)
        idf, idb, eps = load_consts(cx, IN)
        gs2, sh2, g2 = load_mods(cx, MODS, li, [3, 4, 5])
        io16 = cx.T([128, 16], F32)
        th16 = cx.T([128, 16], F32)
        cx.dma("sp", io16[:], IN["iota16"].broadcast_to([128, 16]), wr=[io16])
        cx.V(lambda h: h.tensor_scalar(out=th16[:], in0=io16[:], scalar1=16.0, scalar2=None, op0=ALU.mult), rd=[io16], wr=[th16])
        if final:
            fg = cx.T([128, 1024], F32)
            cx.dma("sp", fg[:], IN["final_g"].broadcast_to([128, 1024]), wr=[fg])
        wq = cx.T([128, 8, 2048], BF16)
        ptr = cx.PS([128, 8, 128], BF16)
        keysT = cx.T([128, 2, 128], BF16)
        with contextlib.ExitStack() as stw:
            cw_ = Ctx(P, nc, stw)
            stg = [cw_.T([128, 2048], F32) for _ in range(2)]
            for k in range(8):
                cw_.dma("sp", stg[k % 2][:], IN["peer_w_q"][li, k * 128:(k + 1) * 128, :], wr=[stg[k % 2]])
                cw_.V(lambda h: h.tensor_copy(out=wq[:, k, :], in_=stg[k % 2][:]), rd=[stg[k % 2]], wr=[wq])
            kf = cw_.T([128, 2, 128], F32)
            kb = cw_.T([128, 2, 128], BF16)
            cw_.dma("sp", kf[:], IN["peer_keys"][li].rearrange("t n c -> n t c"), wr=[kf])
            cw_.V(lambda h: h.tensor_copy(out=kb[:], in_=kf[:]), rd=[kf], wr=[kb])
            for t in range(2):
                cw_.M(lambda h: h.transpose(out=ptr[:, t, :], in_=kb[:, t, :], identity=idb[:]), rd=[kb, idb], wr=[ptr])
            cw_.A(lambda h: h.activation(out=keysT[:], in_=ptr[:, 0:2, :], func=AF.Copy), rd=[ptr], wr=[keysT])
            P.barrier()
        pq = [cx.PS([128, 512]) for _ in range(2)]
        psc = [cx.PS([128, 4, 128]) for _ in range(2)]
        po = cx.PS([128, 1024])
        xb = [cx.T([128, 1024], F32) for _ in range(3)]
        sq = cx.T([128, 1024], F32)
        hb = cx.T([128, 1024], BF16)
        hTi = cx.T([128, 8, 128], BF16)
        st2 = cx.T([128, 4], F32)
        qb = cx.T([128, 2048], BF16)
        rs = cx.T([128, 48], F32)
        qT = cx.T([128, 16, 128], BF16)
        S1 = cx.T([128, 16, 128], F32)
        sqq = Buf(S1.t)
        sqq.k = S1.k
        sqq_ap = S1.t[:].rearrange("p g c -> p (g c)")
        wk = [cx.T([128, 128], F32) for _ in range(2)]
        m = cx.T([128, 16, 16], F32)
        ix = cx.T([128, 16, 16], U32)
        ixf = cx.T([128, 16, 16], F32)
        CS = cx.T([128, 8, 256], F32)
        wk2 = [cx.T([128, 256], F32) for _ in range(2)]
        tops = cx.T([128, 8, 16], F32)
        pos = cx.T([128, 8, 16], U32)
        posf = cx.T([128, 8, 16], F32)
        af = cx.T([128, 8, 16], F32)
        bf_ = cx.T([128, 8, 16], F32)
        oh = Buf(CS.t)
        oh.k = CS.k
        oh_ap = CS.t[:].rearrange("p h (a b) -> p h a b", a=16)
        i12 = cx.T([128, 2, 128], F32)
        idxf = cx.T([128, 128], F32)
        idx = cx.T([128, 128], I32)
        ge = cx.T([128, 8, 16], F32)
        gsum = cx.T([128, 16], F32)
        gate = cx.T([128, 128], F32)
        act = cx.T([128, 128], F32)
        gl = cx.T([128, 128], F32)
        coef = cx.T([128, 128], F32)
        NG = 5
        actT = [Tok() for _ in range(4)]
        glT = [Tok() for _ in range(4)]
        coefT = [Tok() for _ in range(4)]
        Gb = [cx.T([128, 4, 2048], BF16) for _ in range(NG)]
        Gtok = [[Tok() for _ in range(4)] for _ in range(NG)]
        diag = [cx.T([128, 4, 128], BF16) for _ in range(2)]
        junk = cx.T([128, 1024], BF16)
        NPR = 2
        junkv = cx.T([128, 1024], BF16) if STT_SLOTS else None
        prods = [cx.T([128, 1024], PROD_DT) for _ in range(NPR)]
        yb = [cx.T([128, 1024], F32) for _ in range(1)]
        sgn = 0
        hbs = [hb, cx.T([128, 1024], BF16)]
        idxs = [idx, cx.T([128, 128], I32)]
        gates = [gate, cx.T([128, 128], F32)]
        st3 = cx.T([128, 4], F32)
        sq2 = cx.T([128, 1024], F32) if final else None
        fg_ = fg if final else None
        FSTEP = 2

        def front(i):
            x = xb[i % 3]
            hb = hbs[i % 2]
            idx = idxs[i % 2]
            gate = gates[i % 2]
            yield
            cx.dma("sp", x[:], Xin.t[i * 128:(i + 1) * 128, :], rd=[Xin], wr=[x])
            yield
            emit_norm_tile(cx, x, gs2, sh2, hb, sq, st2, idb, ptr, hTi[:], hTi)
            for nb in range(4):
                p = pq[nb % 2]
                for k in range(8):
                    yield
                    cx.M(lambda h: h.matmul(p[:], lhsT=hTi[:, k, :], rhs=wq[:, k, nb * 512:(nb + 1) * 512], start=(k == 0), stop=(k == 7)), rd=[hTi, wq], wr=[p])
                yield
                cx.A(lambda h: h.activation(out=qb[:, nb * 512:(nb + 1) * 512], in_=p[:], func=AF.Copy), rd=[p], wr=[qb])
            yield
            cx.V(lambda h: h.tensor_tensor(out=sqq_ap, in0=qb[:], in1=qb[:], op=ALU.mult), rd=[qb], wr=[sqq])
            yield
            cx.V(lambda h: h.tensor_reduce(out=rs[:, 0:16], in_=S1.t[:], axis=AX.X, op=ALU.add), rd=[sqq], wr=[rs])
            yield
            cx.A(lambda h: h.activation(out=rs[:, 16:32], in_=rs[:, 0:16], func=AF.Sqrt, scale=1.0 / 128, bias=eps[:, 0:1]), rd=[rs, eps], wr=[rs])
            yield
            cx.V(lambda h: h.reciprocal(out=rs[:, 32:48], in_=rs[:, 16:32]), rd=[rs], wr=[rs])
            for r in range(2):
                for j in range(8):
                    g_ = r * 8 + j
                    yield
                    cx.M(lambda h: h.transpose(out=ptr[:, j, :], in_=qb[:, g_ * 128:(g_ + 1) * 128], identity=idb[:]), rd=[qb, idb], wr=[ptr])
                yield
                cx.A(lambda h: h.activation(out=qT[:, r * 8:(r + 1) * 8, :], in_=ptr[:], func=AF.Copy), rd=[ptr], wr=[qT])
            for r in range(4):
                ps_ = psc[r % 2]
                for j in range(4):
                    hp = r * 4 + j
                    yield
                    cx.M(lambda h: h.matmul(ps_[:, j, :], lhsT=qT[:, hp, :], rhs=keysT[:, hp % 2, :], start=True, stop=True), rd=[qT, keysT], wr=[ps_])
                yield
                cx.V(lambda h: h.tensor_tensor(out=S1[:, r * 4:(r + 1) * 4, :], in0=ps_[:], in1=rs[:, 32 + r * 4:32 + (r + 1) * 4].unsqueeze(2).broadcast_to([128, 4, 128]), op=ALU.mult),
                     rd=[ps_, rs], wr=[S1])
            for hp in range(16):
                w_ = wk[hp % 2]
                yield
                cx.V(lambda h: h.max(out=m[:, hp, 0:8], in_=S1[:, hp, :]), rd=[S1], wr=[m])
                yield
                cx.V(lambda h: h.max_index(out=ix[:, hp, 0:8], in_max=m[:, hp, 0:8], in_values=S1[:, hp, :]), rd=[m, S1], wr=[ix])
                yield
                cx.V(lambda h: h.match_replace(out=w_[:], in_to_replace=m[:, hp, 0:8], in_values=S1[:, hp, :], imm_value=-1e30), rd=[m, S1], wr=[w_])
                yield
                cx.V(lambda h: h.max(out=m[:, hp, 8:16], in_=w_[:]), rd=[w_], wr=[m])
                yield
                cx.V(lambda h: h.max_index(out=ix[:, hp, 8:16], in_max=m[:, hp, 8:16], in_values=w_[:]), rd=[m, w_], wr=[ix])
            mv = m[:].rearrange("p (h t) k -> p h t k", t=2)
            yield
            cx.V(lambda h: h.tensor_tensor(out=CS[:].rearrange("p h (a b) -> p h a b", a=16), in0=mv[:, :, 0, :].unsqueeze(3).broadcast_to([128, 8, 16, 16]),
                                           in1=mv[:, :, 1, :].unsqueeze(2).broadcast_to([128, 8, 16, 16]), op=ALU.add), rd=[m], wr=[CS])
            for hh in range(8):
                w_ = wk2[hh % 2]
                yield
                cx.V(lambda h: h.max(out=tops[:, hh, 0:8], in_=CS[:, hh, :]), rd=[CS], wr=[tops])
                yield
                cx.V(lambda h: h.max_index(out=pos[:, hh, 0:8], in_max=tops[:, hh, 0:8], in_values=CS[:, hh, :]), rd=[tops, CS], wr=[pos])
                yield
                cx.V(lambda h: h.match_replace(out=w_[:], in_to_replace=tops[:, hh, 0:8], in_values=CS[:, hh, :], imm_value=-1e30), rd=[tops, CS], wr=[w_])
                yield
                cx.V(lambda h: h.max(out=tops[:, hh, 8:16], in_=w_[:]), rd=[w_], wr=[tops])
                yield
                cx.V(lambda h: h.max_index(out=pos[:, hh, 8:16], in_max=tops[:, hh, 8:16], in_values=w_[:]), rd=[tops, w_], wr=[pos])
            yield
            cx.V(lambda h: h.tensor_copy(out=posf[:], in_=pos[:]), rd=[pos], wr=[posf])
            yield
            cx.V(lambda h: h.tensor_copy(out=ixf[:], in_=ix[:]), rd=[ix], wr=[ixf])
            bc4 = lambda ap3: ap3.unsqueeze(3).broadcast_to([128, 8, 16, 16])
            io4 = io16[:].unsqueeze(1).unsqueeze(1).broadcast_to([128, 8, 16, 16])
            th4 = th16[:].unsqueeze(1).unsqueeze(1).broadcast_to([128, 8, 16, 16])
            yield
            cx.V(lambda h: h.tensor_tensor(out=oh_ap, in0=bc4(posf[:]), in1=th4, op=ALU.is_ge), rd=[posf, th16], wr=[oh])
            yield
            cx.V(lambda h: h.tensor_reduce(out=af[:], in_=oh_ap, axis=AX.X, op=ALU.add), rd=[oh], wr=[af])
            yield
            cx.V(lambda h: h.tensor_scalar(out=af[:], in0=af[:], scalar1=-1.0, scalar2=None, op0=ALU.add), rd=[af], wr=[af])
            yield
            cx.V(lambda h: h.scalar_tensor_tensor(out=bf_[:], in0=af[:], scalar=-16.0, in1=posf[:], op0=ALU.mult, op1=ALU.add), rd=[af, posf], wr=[bf_])
            ixv = ixf[:].rearrange("p (h t) k -> p h t k", t=2)
            for t, src in ((0, af), (1, bf_)):
                yield
                cx.V(lambda h: h.tensor_tensor(out=oh_ap, in0=bc4(src[:]), in1=io4, op=ALU.is_equal), rd=[src, io16], wr=[oh])
                yield
                cx.V(lambda h: h.tensor_tensor(out=oh_ap, in0=oh_ap, in1=ixv[:, :, t, :].unsqueeze(2).broadcast_to([128, 8, 16, 16]), op=ALU.mult), rd=[oh, ixf], wr=[oh])
                yield
                cx.V(lambda h: h.tensor_reduce(out=i12[:, t, :].rearrange("p (h k) -> p h k", h=8), in_=oh_ap, axis=AX.X, op=ALU.add), rd=[oh], wr=[i12])
            yield
            cx.V(lambda h: h.scalar_tensor_tensor(out=idxf[:], in0=i12[:, 0, :], scalar=128.0, in1=i12[:, 1, :], op0=ALU.mult, op1=ALU.add), rd=[i12], wr=[idxf])
            yield
            cx.V(lambda h: h.tensor_copy(out=idx[:], in_=idxf[:]), rd=[idxf], wr=[idx])
            yield
            cx.V(lambda h: h.tensor_tensor(out=ge[:], in0=tops[:], in1=tops[:, :, 0:1].broadcast_to([128, 8, 16]), op=ALU.subtract), rd=[tops], wr=[ge])
            yield
            cx.A(lambda h: h.activation(out=ge[:], in_=ge[:], func=AF.Exp), rd=[ge], wr=[ge])
            yield
            cx.V(lambda h: h.tensor_reduce(out=gsum[:, 0:8], in_=ge[:], axis=AX.X, op=ALU.add), rd=[ge], wr=[gsum])
            yield
            cx.V(lambda h: h.reciprocal(out=gsum[:, 8:16], in_=gsum[:, 0:8]), rd=[gsum], wr=[gsum])
            yield
            cx.V(lambda h: h.tensor_tensor(out=gate[:].rearrange("p (h k) -> p h k", h=8), in0=ge[:], in1=gsum[:, 8:16].unsqueeze(2).broadcast_to([128, 8, 16]), op=ALU.mult),
                 rd=[ge, gsum], wr=[gate])


        PF = NG - 2
        NGRP = NT * 32
        fgens = {}

        def drain(i):
            g = fgens.pop(i, None)
            if g is not None:
                for _ in g:
                    pass

        fgens[0] = front(0)
        drain(0)
        for gn in range(NGRP + PF + 1):
            if gn < NGRP:
                i, sg = divmod(gn, 32)
                if sg == 0:
                    drain(i)
                idx = idxs[i % 2]
                bq_ = gn % NG
                for jj in range(4):
                    j = sg * 4 + jj
                    P.dma("pool", lambda h: h.indirect_dma_start(out=Gb[bq_][:, jj, :], out_offset=None, in_=TAB.t,
                                                                 in_offset=bass.IndirectOffsetOnAxis(ap=idx[:, j:j + 1], axis=0)),
                          _ks([idx, TAB]), [Gtok[bq_][jj]])
            gm = gn - PF
            if 0 <= gm < NGRP:
                i, sg = divmod(gm, 32)
                hb = hbs[i % 2]
                if sg == 0:
                    cx.V(lambda h: h.memset(act[:], 0.0), wr=actT)
                    if i + 1 < NT:
                        fgens[i + 1] = front(i + 1)
                fg = fgens.get(i + 1)
                b_ = gm % NG
                G_ = Gb[b_]
                for jj in range(4):
                    j = sg * 4 + jj
                    if jj in STT_SLOTS:
                        cx.V(lambda h: h.scalar_tensor_tensor(out=junkv[:], in0=G_[:, jj, 0:1024], scalar=1.0, in1=hb[:], op0=ALU.mult, op1=ALU.mult, accum_out=act[:, j:j + 1]),
                             rd=[Gtok[b_][jj], hb], wr=[junkv, actT[sg % 4]])
                    else:
                        pr = prods[j % NPR]
                        cx.V(lambda h: h.tensor_tensor(out=pr[:], in0=G_[:, jj, 0:1024], in1=hb[:], op=ALU.mult), rd=[Gtok[b_][jj], hb], wr=[pr])
                        cx.A(lambda h: h.activation(out=junk[:], in_=pr[:], func=AF.Copy, accum_out=act[:, j:j + 1]), rd=[pr], wr=[junk, actT[sg % 4]])
                    if fg is not None:
                        for _ in range(FSTEP):
                            next(fg, None)
                cs = slice(sg * 4, (sg + 1) * 4)
                cx.A(lambda h: h.activation(out=gl[:, cs], in_=act[:, cs], func=AF.Gelu), rd=[actT[sg % 4]], wr=[glT[sg % 4]])
            gc = gn - PF - 1
            if 0 <= gc < NGRP:
                i, sg = divmod(gc, 32)
                gate = gates[i % 2]
                x = xb[i % 3]
                b_ = gc % NG
                G_ = Gb[b_]
                dg = diag[sg % 2]
                cs = slice(sg * 4, (sg + 1) * 4)
                cx.V(lambda h: h.tensor_tensor(out=coef[:, cs], in0=gl[:, cs], in1=gate[:, cs], op=ALU.mult), rd=[glT[sg % 4], gate], wr=[coefT[sg % 4]])
                for jj in range(4):
                    j = sg * 4 + jj
                    if DIAG_ON_DVE:
                        cx.V(lambda h: h.tensor_scalar(out=dg[:, jj, :], in0=idf[:], scalar1=coef[:, j:j + 1], scalar2=None, op0=ALU.mult), rd=[idf, coefT[sg % 4]], wr=[dg])
                    else:
                        cx.A(lambda h: h.activation(out=dg[:, jj, :], in_=idf[:], func=AF.Copy, scale=coef[:, j:j + 1]), rd=[idf, coefT[sg % 4]], wr=[dg])
                for jj in range(4):
                    j = sg * 4 + jj
                    for hv in range(2):
                        cx.M(lambda h: h.matmul(po[:, hv * 512:(hv + 1) * 512], lhsT=dg[:, jj, :], rhs=G_[:, jj, 1024 + hv * 512:1024 + (hv + 1) * 512],
                                                start=(j == 0), stop=(j == 127)), rd=[dg, Gtok[b_][jj]], wr=[po])
                if sg == 31:
                    y = yb[0]
                    cx.V(lambda h: h.tensor_tensor(out=y[:], in0=po[:], in1=g2[:], op=ALU.mult), rd=[po, g2], wr=[y])
                    cx.V(lambda h: h.tensor_tensor(out=y[:], in0=y[:], in1=x[:], op=ALU.add), rd=[y, x], wr=[y])
                    if final:
                        cx.V(lambda h: h.memset(st3[:, 0:1], 0.0), wr=[st3])
                        cx.A(lambda h: h.activation(out=sq2[:], in_=y[:], func=AF.Square, accum_out=st3[:, 0:1]), rd=[y, st3], wr=[sq2, st3])
                        cx.A(lambda h: h.activation(out=st3[:, 1:2], in_=st3[:, 0:1], func=AF.Sqrt, scale=1.0 / D, bias=eps[:, 0:1]), rd=[st3, eps], wr=[st3])
                        cx.V(lambda h: h.reciprocal(out=st3[:, 2:3], in_=st3[:, 1:2]), rd=[st3], wr=[st3])
                        cx.V(lambda h: h.scalar_tensor_tensor(out=y[:], in0=y[:], scalar=st3[:, 2:3], in1=fg_[:], op0=ALU.mult, op1=ALU.mult), rd=[y, st3, fg_], wr=[y])
                    cx.dma("sp", Xout.t[i * 128:(i + 1) * 128, :], y[:], rd=[y], wr=[Xout])
        P.barrier()
        P.flush()


def phase_attn(P, nc, IN, MODS, li, Xin, Xout, SC):
    with contextlib.ExitStack() as st0:
        c0 = Ctx(P, nc, st0)
        idf, idb, eps = load_consts(c0, IN)
        bigT = c0.T([128, 8, S], BF16)
        qT = c0.T([128, 8, S], BF16)
        kT = c0.T([128, 2, S], BF16)
        V1 = c0.T([128, NT, 2, 130], BF16)
        with contextlib.ExitStack() as st1:
            c1 = Ctx(P, nc, st1)
            gs1, sh1 = load_mods(c1, MODS, li, [0, 1])
            emit_hT(c1, Xin, gs1, sh1, idb, bigT)
            P.barrier()
        with contextlib.ExitStack() as st2:
            cx = Ctx(P, nc, st2)
            w = cx.T([128, 8, 1536], BF16)
            with contextlib.ExitStack() as stw:
                cw_ = Ctx(P, nc, stw)
                stg = [cw_.T([128, 1536], F32) for _ in range(2)]
                for k in range(8):
                    cw_.dma("sp", stg[k % 2][:], IN["od_w_in"][k * 128:(k + 1) * 128, :], wr=[stg[k % 2]])
                    cw_.V(lambda h: h.tensor_copy(out=w[:, k, :], in_=stg[k % 2][:]), rd=[stg[k % 2]], wr=[w])
                P.barrier()
            csb = [cx.T([128, 2, 64], F32) for _ in range(2)]
            gq = cx.T([128, 10, 128], F32)
            g1_ = cx.T([128, 128], F32)
            g2_ = cx.T([128, 128], F32)
            cx.dma("sp", g1_[:], IN["od_qnorm_g"].broadcast_to([128, 128]), wr=[g1_])
            cx.dma("sp", g2_[:], IN["od_knorm_g"].broadcast_to([128, 128]), wr=[g2_])
            cx.V(lambda h: h.tensor_scalar(out=g1_[:], in0=g1_[:], scalar1=float(128 ** -0.5), scalar2=None, op0=ALU.mult), rd=[g1_], wr=[g1_])
            cx.V(lambda h: h.tensor_copy(out=gq[:, 0:8, :], in_=g1_[:].unsqueeze(1).broadcast_to([128, 8, 128])), rd=[g1_], wr=[gq])
            cx.V(lambda h: h.tensor_copy(out=gq[:, 8:10, :], in_=g2_[:].unsqueeze(1).broadcast_to([128, 2, 128])), rd=[g2_], wr=[gq])
            cx.V(lambda h: h.memset(V1[:], 1.0), wr=[V1])
            pz = [cx.PS([128, 512]) for _ in range(3)]
            ptq = cx.PS([128, 8, 128], BF16)
            ptk = cx.PS([128, 2, 128], BF16)
            rs = cx.T([128, 32], F32)
            qn = cx.T([128, 10, 128], F32)
            qr = cx.T([128, 10, 128], BF16)
            t1 = cx.T([128, 10, 64], F32)
            t2 = cx.T([128, 10, 64], F32)
            for i in range(NT):
                tsl = slice(i * 128, (i + 1) * 128)
                for nb in range(3):
                    for k in range(8):
                        cx.M(lambda h: h.matmul(pz[nb][:], lhsT=bigT[:, k, tsl], rhs=w[:, k, nb * 512:(nb + 1) * 512], start=(k == 0), stop=(k == 7)), rd=[bigT, w], wr=[pz[nb]])
                cx.A(lambda h: h.activation(out=V1[:, i, :, 0:128], in_=pz[2][:, 256:512].rearrange("p (g d) -> p g d", g=2), func=AF.Copy), rd=[pz[2]], wr=[V1])
                cs = csb[i % 2]
                cx.dma("act", cs[:], IN["ropecs"][:, :, i, :], wr=[cs])
                zsrc = ((pz[0], 0, 4, 512), (pz[1], 4, 8, 512), (pz[2], 8, 10, 256))
                for (pp, g0, g1x, wd) in zsrc:
                    cx.A(lambda h: h.activation(out=qn[:, g0:g1x, :], in_=pp[:, 0:wd].rearrange("p (g d) -> p g d", d=128), func=AF.Square), rd=[pp], wr=[qn])
                cx.V(lambda h: h.tensor_reduce(out=rs[:, 0:10], in_=qn[:], axis=AX.X, op=ALU.add), rd=[qn], wr=[rs])
                cx.A(lambda h: h.activation(out=rs[:, 10:20], in_=rs[:, 0:10], func=AF.Sqrt, scale=1.0 / 128, bias=eps[:, 0:1]), rd=[rs, eps], wr=[rs])
                cx.V(lambda h: h.reciprocal(out=rs[:, 20:30], in_=rs[:, 10:20]), rd=[rs], wr=[rs])
                for (pp, g0, g1x, wd) in zsrc:
                    cx.V(lambda h: h.tensor_tensor(out=qn[:, g0:g1x, :], in0=pp[:, 0:wd].rearrange("p (g d) -> p g d", d=128),
                                                   in1=rs[:, 20 + g0:20 + g1x].unsqueeze(2).broadcast_to([128, g1x - g0, 128]), op=ALU.mult), rd=[pp, rs], wr=[qn])
                cx.V(lambda h: h.tensor_tensor(out=qn[:], in0=qn[:], in1=gq[:], op=ALU.mult), rd=[qn, gq], wr=[qn])
                qv = qn[:].rearrange("p g (d t) -> p g d t", t=2)
                qo = qr[:].rearrange("p g (d t) -> p g d t", t=2)
                cc = cs[:, 0, :].unsqueeze(1).broadcast_to([128, 10, 64])
                ss_ = cs[:, 1, :].unsqueeze(1).broadcast_to([128, 10, 64])
                cx.V(lambda h: h.tensor_tensor(out=t1[:], in0=qv[:, :, :, 0], in1=cc, op=ALU.mult), rd=[qn, cs], wr=[t1])
                cx.V(lambda h: h.tensor_tensor(out=t2[:], in0=qv[:, :, :, 1], in1=ss_, op=ALU.mult), rd=[qn, cs], wr=[t2])
                cx.V(lambda h: h.tensor_tensor(out=qo[:, :, :, 0], in0=t1[:], in1=t2[:], op=ALU.subtract), rd=[t1, t2], wr=[qr])
                cx.V(lambda h: h.tensor_tensor(out=t1[:], in0=qv[:, :, :, 0], in1=ss_, op=ALU.mult), rd=[qn, cs, qr], wr=[t1])
                cx.V(lambda h: h.tensor_tensor(out=t2[:], in0=qv[:, :, :, 1], in1=cc, op=ALU.mult), rd=[qn, cs, qr], wr=[t2])
                cx.V(lambda h: h.tensor_tensor(out=qo[:, :, :, 1], in0=t1[:], in1=t2[:], op=ALU.add), rd=[t1, t2], wr=[qr])
                for g_ in range(8):
                    cx.M(lambda h: h.transpose(out=ptq[:, g_, :], in_=qr[:, g_, :], identity=idb[:]), rd=[qr, idb], wr=[ptq])
                for g_ in range(2):
                    cx.M(lambda h: h.transpose(out=ptk[:, g_, :], in_=qr[:, 8 + g_, :], identity=idb[:]), rd=[qr, idb], wr=[ptk])
                cx.A(lambda h: h.activation(out=qT[:, :, tsl], in_=ptq[:], func=AF.Copy), rd=[ptq], wr=[qT])
                cx.A(lambda h: h.activation(out=kT[:, :, tsl], in_=ptk[:], func=AF.Copy), rd=[ptk], wr=[kT])
            P.barrier()
        import os as _os
        if _os.environ.get("ATT_STOP") == "2":
            P.barrier()
            P.flush()
            return
        with contextlib.ExitStack() as st3:
            cx = Ctx(P, nc, st3)
            pss = [cx.PS([128, 512]) for _ in range(2)]
            pacc = [cx.PS([128, 512]) for _ in range(4)]
            pto = cx.PS([128, 8, 128], BF16)
            pT = [cx.T([128, 512], BF16) for _ in range(3)]
            ao = [cx.T([128, 8, 128], BF16) for _ in range(4)]
            rinv = cx.T([128, 8], F32)
            steps = [(qb_, hd_, sj) for qb_ in range(8) for hd_ in range(8) for sj in range(NT)]
            accs = pacc

            def emit_score(n):
                qb_, hd_, sj = steps[n]
                ps_ = pss[n % 2]
                cx.M(lambda h: h.matmul(ps_[:], lhsT=kT[:, hd_ // 4, sj * 128:(sj + 1) * 128], rhs=qT[:, hd_, qb_ * 512:(qb_ + 1) * 512], start=True, stop=True), rd=[kT, qT], wr=[ps_])

            bg = SC.get("BG1")
            if bg is not None:
                bg.attach(cx)
            emit_score(0)
            for n, (qb_, hd_, sj) in enumerate(steps):
                g_ = hd_ // 4
                if bg is not None and n % 14 == 0:
                    bg.step(1)
                if n + 1 < len(steps):
                    emit_score(n + 1)
                ps_ = pss[n % 2]
                pt_ = pT[n % 3]
                cx.A(lambda h: h.activation(out=pt_[:], in_=ps_[:], func=AF.Exp), rd=[ps_], wr=[pt_])
                for qs in range(4):
                    cx.M(lambda h: h.matmul(accs[qs][:, 0:129], lhsT=pt_[:, qs * 128:(qs + 1) * 128], rhs=V1[:, sj, g_, 0:129], start=(sj == 0), stop=(sj == NT - 1)),
                         rd=[pt_, V1], wr=[accs[qs]])
                if sj == NT - 1:
                    for qs in range(4):
                        a_ = accs[qs]
                        cx.V(lambda h: h.reciprocal(out=rinv[:, qs:qs + 1], in_=a_[:, 128:129]), rd=[a_], wr=[rinv])
                        cx.V(lambda h: h.tensor_scalar(out=ao[qs][:, hd_, :], in0=a_[:, 0:128], scalar1=rinv[:, qs:qs + 1], scalar2=None, op0=ALU.mult), rd=[a_, rinv], wr=[ao[qs]])
                    if hd_ == 7:
                        for qs in range(4):
                            ti = qb_ * 4 + qs
                            for k in range(8):
                                cx.M(lambda h: h.transpose(out=pto[:, k, :], in_=ao[qs][:, k, :], identity=idb[:]), rd=[ao[qs], idb], wr=[pto])
                            cx.V(lambda h: h.tensor_copy(out=bigT[:, :, ti * 128:(ti + 1) * 128], in_=pto[:]), rd=[pto], wr=[bigT])
            if bg is not None:
                bg.finish()
                SC.setdefault("TABDONE", {})[1] = True
            P.barrier()
        if _os.environ.get("ATT_STOP") == "3":
            P.barrier()
            P.flush()
            return
        with contextlib.ExitStack() as st4:
            cx = Ctx(P, nc, st4)
            g1, = load_mods(cx, MODS, li, [2])
            stage_f = cx.T([128, 1024], F32)
            emit_outproj(cx, Xin, Xout, lambda i: (bigT[:, :, i * 128:(i + 1) * 128], bigT), IN["od_w_out"], g1, stage_f)
            P.barrier()
        P.flush()
```

```python
import contextlib
import numpy as np
import concourse.bass as bass
import concourse.mybir as mybir
from concourse.bass_utils import run_bass_kernel_spmd

F32 = mybir.dt.float32
BF16 = mybir.dt.bfloat16
I32 = mybir.dt.int32
U32 = mybir.dt.uint32
AF = mybir.ActivationFunctionType
ALU = mybir.AluOpType
AX = mybir.AxisListType

S = 4096
D = 1024
NT = S // 128
EPS = 1e-6


class Tok:
    __slots__ = ("w", "r", "name")

    def __init__(self, name=""):
        self.w = None
        self.r = {}
        self.name = name


class _Eng:
    def __init__(self, name, sem):
        self.name = name
        self.sem = sem
        self.cnt = 0
        self.seen = {}
        self.ops = []


NDMA = 12


class _Rec:
    def __getattr__(self, name):
        def f(*a, **k):
            return (name, a, k)
        return f


_REC = _Rec()


class Prog:
    ENG = ("pe", "act", "dve", "pool", "sp")

    def __init__(self, nc, stack):
        self.nc = nc
        self.stack = stack
        self.e = {}
        self.sems = {}
        for n in self.ENG:
            self.e[n] = _Eng(n, stack.enter_context(nc.semaphore("s_" + n)))
            self.sems[n] = (self.e[n].sem, 1)
        self.dslot = {}
        for q in ("sp", "pool", "act"):
            sl = []
            for j in range(NDMA):
                key = "d_%s%d" % (q, j)
                sem = stack.enter_context(nc.semaphore(key))
                self.sems[key] = (sem, 16)
                sl.append([key, 0])
            self.dslot[q] = [sl, 0]

    def _deps(self, e, rd, wr):
        deps = {}

        def need(dep, same_ok):
            if dep is None:
                return
            en, c = dep
            if en == e and (e == "pe" or not same_ok):
                return
            if deps.get(en, 0) < c:
                deps[en] = c

        for t in rd:
            need(t.w, True)
        for t in wr:
            need(t.w, False)
            for en, c in t.r.items():
                need((en, c), False)
        return deps

    def _emit_waits(self, E, deps):
        for en, c in deps.items():
            if E.seen.get(en, 0) < c:
                sem, step = self.sems[en]
                E.ops.append(lambda h, sem=sem, v=c * step: h.wait_ge(sem, v))
                E.seen[en] = c

    def op(self, e, fn, rd=(), wr=()):
        E = self.e[e]
        self._emit_waits(E, self._deps(e, rd, wr))
        E.cnt += 1
        sem = E.sem
        rec = fn(_REC)
        E.ops.append(lambda h, rec=rec, sem=sem: getattr(h, rec[0])(*rec[1], **rec[2]).then_inc(sem, 1))
        me = (e, E.cnt)
        for t in wr:
            t.w = me
            t.r = {}
        for t in rd:
            t.r[e] = E.cnt

    def dma(self, q, fn, rd=(), wr=()):
        E = self.e[q]
        slots, nxt = self.dslot[q]
        slot = slots[nxt % NDMA]
        self.dslot[q][1] = nxt + 1
        key = slot[0]
        deps = self._deps(key, rd, wr)
        if slot[1] > 0:
            deps[key] = max(deps.get(key, 0), slot[1])
        self._emit_waits(E, deps)
        slot[1] += 1
        sem = self.sems[key][0]
        rec = fn(_REC)
        E.ops.append(lambda h, rec=rec, sem=sem: getattr(h, rec[0])(*rec[1], **rec[2]).then_inc(sem, 16))
        me = (key, slot[1])
        for t in wr:
            t.w = me
            t.r = {}
        for t in rd:
            t.r[key] = slot[1]

    def barrier(self):
        tgt = {n: self.e[n].cnt for n in self.ENG}
        for q in self.dslot:
            for key, c in self.dslot[q][0]:
                tgt[key] = c
        for n in self.ENG:
            E = self.e[n]
            d = {k: v for k, v in tgt.items() if v > 0 and not (k == n and n == "pe")}
            self._emit_waits(E, d)

    def flush(self):
        nc = self.nc
        with nc.Block() as block:
            @block.tensor
            def _(h):
                for f in self.e["pe"].ops:
                    f(h)

            @block.scalar
            def _(h):
                for f in self.e["act"].ops:
                    f(h)

            @block.vector
            def _(h):
                for f in self.e["dve"].ops:
                    f(h)

            @block.gpsimd
            def _(h):
                for f in self.e["pool"].ops:
                    f(h)

            @block.sync
            def _(h):
                for f in self.e["sp"].ops:
                    f(h)
        for n in self.ENG:
            self.e[n].ops = []


class Buf:
    def __init__(self, t):
        self.t = t
        self.k = Tok()

    def __getitem__(self, i):
        return self.t[i]


def _ks(xs):
    return [x.k if isinstance(x, Buf) else x for x in xs]


_NAME = [0]


class Ctx:
    def __init__(self, P, nc, st):
        self.P, self.nc, self.st = P, nc, st
        self.n = 0

    def T(self, shape, dt, name=None):
        _NAME[0] += 1
        return Buf(self.st.enter_context(self.nc.sbuf_tensor("%s_%d" % (name or "t", _NAME[0]), list(shape), dt)))

    def PS(self, shape, dt=F32, name=None):
        _NAME[0] += 1
        return Buf(self.st.enter_context(self.nc.psum_tensor("%s_%d" % (name or "p", _NAME[0]), list(shape), dt)))

    def V(self, fn, rd=(), wr=()):
        self.P.op("dve", fn, _ks(rd), _ks(wr))

    def A(self, fn, rd=(), wr=()):
        self.P.op("act", fn, _ks(rd), _ks(wr))

    def G(self, fn, rd=(), wr=()):
        self.P.op("pool", fn, _ks(rd), _ks(wr))

    def M(self, fn, rd=(), wr=()):
        self.P.op("pe", fn, _ks(rd), _ks(wr))

    def dma(self, q, out, in_, rd=(), wr=()):
        self.P.dma(q, lambda h, out=out, in_=in_: h.dma_start(out=out, in_=in_), _ks(rd), _ks(wr))


def load_cast(cx, q, dst_bf, src_ap, stage, eng="pool"):
    cx.dma(q, stage.t[:] if not isinstance(stage, tuple) else stage[1], src_ap, wr=[stage if not isinstance(stage, tuple) else stage[0]])


def emit_norm_tile(cx, xt, gs, sh, hb, sq, st2, idb, ptr, hT_dst, hT_buf, hf=None):
    cx.V(lambda h: h.memset(st2[:, 0:1], 0.0), wr=[st2])
    cx.A(lambda h: h.activation(out=sq[:], in_=xt[:], func=AF.Square, accum_out=st2[:, 0:1]), rd=[xt, st2], wr=[sq, st2])
    cx.A(lambda h: h.activation(out=st2[:, 1:2], in_=st2[:, 0:1], func=AF.Sqrt, scale=1.0 / D, bias=EPS_AP[0][:, 0:1]), rd=[st2, EPS_AP[0]], wr=[st2])
    cx.V(lambda h: h.reciprocal(out=st2[:, 2:3], in_=st2[:, 1:2]), rd=[st2], wr=[st2])
    cx.V(lambda h: h.scalar_tensor_tensor(out=sq[:], in0=xt[:], scalar=st2[:, 2:3], in1=gs[:], op0=ALU.mult, op1=ALU.mult), rd=[xt, st2, gs], wr=[sq])
    if hf is not None:
        cx.V(lambda h: h.tensor_tensor(out=hf[:], in0=sq[:], in1=sh[:], op=ALU.add), rd=[sq, sh], wr=[hf])
        cx.G(lambda h: h.tensor_copy(out=hb[:], in_=hf[:]), rd=[hf], wr=[hb])
    else:
        cx.V(lambda h: h.tensor_tensor(out=hb[:], in0=sq[:], in1=sh[:], op=ALU.add), rd=[sq, sh], wr=[hb])
    for k in range(8):
        cx.M(lambda h, k=k: h.transpose(out=ptr[:, k, :], in_=hb[:, k * 128:(k + 1) * 128], identity=idb[:]), rd=[hb, idb], wr=[ptr])
    cx.A(lambda h: h.activation(out=hT_dst, in_=ptr[:], func=AF.Copy), rd=[ptr], wr=[hT_buf])


EPS_AP = [None]


def load_consts(cx, CN):
    idf = cx.T([128, 128], F32)
    idb = cx.T([128, 128], BF16)
    eps = cx.T([128, 1], F32)
    cx.dma("sp", idf[:], CN["ident"], wr=[idf])
    cx.V(lambda h: h.tensor_copy(out=idb[:], in_=idf[:]), rd=[idf], wr=[idb])
    cx.V(lambda h: h.memset(eps[:], EPS), wr=[eps])
    EPS_AP[0] = eps
    return idf, idb, eps


def phase_mods(P, nc, IN, MODS):
    with contextlib.ExitStack() as st:
        cx = Ctx(P, nc, st)
        cT = cx.T([128, 8], F32)
        cond = cx.T([128, 8], F32)
        crep = cx.T([128, 8, 128], F32)
        cx.dma("sp", cT[:], IN["cT"], wr=[cT])
        cx.A(lambda h: h.activation(out=cond[:], in_=cT[:], func=AF.Silu), rd=[cT], wr=[cond])
        cx.V(lambda h: h.tensor_copy(out=crep[:], in_=cond[:].unsqueeze(2).broadcast_to([128, 8, 128])), rd=[cond], wr=[crep])
        wb = [cx.T([128, 8, 512], F32) for _ in range(2)]
        ps = [cx.PS([128, 512]) for _ in range(2)]
        mod = cx.T([128, 6144], F32)
        ab = cx.T([128, 6144], F32)
        gm = cx.T([128, 1024], F32)
        gf = cx.T([128, 1024], F32)
        n = 0
        for i in range(2):
            cx.dma("act", ab[:], IN["ada_b"][i:i + 1, :].broadcast_to([128, 6144]), wr=[ab])
            cx.dma("act", gm[:], IN["norm_mix_g"][i:i + 1, :].broadcast_to([128, 1024]), wr=[gm])
            cx.dma("act", gf[:], IN["norm_ffn_g"][i:i + 1, :].broadcast_to([128, 1024]), wr=[gf])
            for nb in range(12):
                w = wb[n % 2]
                p = ps[n % 2]
                n += 1
                cx.dma("sp" if nb % 2 == 0 else "pool", w[:], IN["ada_w"][i, :, nb * 512:(nb + 1) * 512].rearrange("(k p) n -> p k n", p=128), wr=[w])
                for k in range(8):
                    cx.M(lambda h, k=k, w=w, p=p: h.matmul(p[:], lhsT=crep[:, k, :], rhs=w[:, k, :], start=(k == 0), stop=(k == 7)), rd=[crep, w], wr=[p])
                cx.V(lambda h, p=p, nb=nb: h.tensor_tensor(out=mod[:, nb * 512:(nb + 1) * 512], in0=p[:], in1=ab[:, nb * 512:(nb + 1) * 512], op=ALU.add), rd=[p, ab], wr=[mod])
            cx.V(lambda h: h.scalar_tensor_tensor(out=mod[:, 1024:2048], in0=mod[:, 1024:2048], scalar=1.0, in1=gm[:], op0=ALU.add, op1=ALU.mult), rd=[mod, gm], wr=[mod])
            cx.V(lambda h: h.scalar_tensor_tensor(out=mod[:, 4096:5120], in0=mod[:, 4096:5120], scalar=1.0, in1=gf[:], op0=ALU.add, op1=ALU.mult), rd=[mod, gf], wr=[mod])
            for j, off in enumerate([1024, 0, 2048, 4096, 3072, 5120]):
                cx.dma("sp", MODS.t[i, j], mod[:, off:off + 1024], rd=[mod], wr=[MODS])
        P.barrier()
        P.flush()


def load_mods(cx, MODS, i, js, q="act"):
    out = []
    for j in js:
        b = cx.T([128, 1024], F32)
        cx.dma(q, b[:], MODS.t[i, j], rd=[MODS], wr=[b])
        out.append(b)
    return out


def emit_hT(cx, Xin, gs, sh, idb, hT):
    xb = [cx.T([128, 1024], F32) for _ in range(2)]
    sq = cx.T([128, 1024], F32)
    hb = [cx.T([128, 1024], BF16) for _ in range(2)]
    st2 = [cx.T([128, 4], F32) for _ in range(2)]
    ptr = [cx.PS([128, 8, 128], BF16) for _ in range(2)]
    for i in range(NT):
        x = xb[i % 2]
        cx.dma("sp", x[:], Xin.t[i * 128:(i + 1) * 128, :], rd=[Xin], wr=[x])
        emit_norm_tile(cx, x, gs, sh, hb[i % 2], sq, st2[i % 2], idb, ptr[i % 2], hT[:, :, i * 128:(i + 1) * 128], hT)


def emit_outproj(cx, Xin, Xout, catT_src, w_ap, g1, stage_f, final=None, nx=4):
    wob = cx.T([128, 8, 1024], BF16)
    for k in range(8):
        cx.dma("sp", stage_f[:], w_ap[k * 128:(k + 1) * 128, :], wr=[stage_f])
        cx.G(lambda h, k=k: h.tensor_copy(out=wob[:, k, :], in_=stage_f[:]), rd=[stage_f], wr=[wob])
    py = [cx.PS([128, 1024]) for _ in range(2)]
    NX = nx
    xb = [cx.T([128, 1024], F32) for _ in range(NX)]
    yb = [cx.T([128, 1024], F32) for _ in range(2)]
    PD = 2
    srcs = {}
    for it in range(NT + PD):
        if it < NT:
            srcs[it] = catT_src(it)
            cx.dma("act", xb[it % NX][:], Xin.t[it * 128:(it + 1) * 128, :], rd=[Xin], wr=[xb[it % NX]])
        i = it - PD
        if i < 0:
            continue
        ap, tok = srcs.pop(i)
        p = py[i % 2]
        x = xb[i % NX]
        y = yb[i % 2]
        for nb in range(2):
            for k in range(8):
                cx.M(lambda h, k=k, nb=nb, p=p, ap=ap: h.matmul(p[:, nb * 512:(nb + 1) * 512], lhsT=ap[:, k, :], rhs=wob[:, k, nb * 512:(nb + 1) * 512],
                                                               start=(k == 0), stop=(k == 7)), rd=[tok, wob], wr=[p])
        cx.V(lambda h, p=p, y=y: h.tensor_tensor(out=y[:], in0=p[:], in1=g1[:], op=ALU.mult), rd=[p, g1], wr=[y])
        cx.G(lambda h, x=x, y=y: h.tensor_tensor(out=y[:], in0=y[:], in1=x[:], op=ALU.add), rd=[y, x], wr=[y])
        cx.dma("sp", Xout.t[i * 128:(i + 1) * 128, :], y[:], rd=[y], wr=[Xout])


def phase_even(P, nc, IN, MODS, li, Xin, Xout, SC):
    CATT, V1D, SIGD, HFD = SC["CATT"], SC["V1D"], SC["SIGD"], SC["HFD"]
    with contextlib.ExitStack() as st0:
        c0 = Ctx(P, nc, st0)
        idf, idb, eps = load_consts(c0, IN)
        QK = c0.T([128, 8, S], BF16)
        GT = c0.T([128, NT, 16], F32)
        EB = c0.T([128, NT, 8], F32)
        ES = c0.T([128, NT, 8], F32)
        EE = c0.T([128, NT, 8], F32)
        with contextlib.ExitStack() as st1:
            cx1 = Ctx(P, nc, st1)
            hT = cx1.T([128, 8, S], BF16)
            with contextlib.ExitStack() as st2:
                c2 = Ctx(P, nc, st2)
                gs1, sh1 = load_mods(c2, MODS, li, [0, 1])
                emit_hT(c2, Xin, gs1, sh1, idb, hT)
                P.barrier()
            with contextlib.ExitStack() as stf:
                cx = Ctx(P, nc, stf)
                bT = cx.T([128, 20], F32)
                cw = cx.T([128, 8, 5], F32)
                cb = cx.T([128, 8], F32)
                edge = cx.T([128, 4, 32], F32)
                cx.dma("sp", bT[:], IN["ev_b_inT"], wr=[bT])
                cx.dma("sp", cw[:], IN["ev_conv_wT"], wr=[cw])
                cx.dma("sp", cb[:], IN["ev_conv_bT"], wr=[cb])
                cx.dma("sp", edge[:], IN["pooledge"].broadcast_to([128, 4, 32]), wr=[edge])
                wst = [cx.T([128, 8, 128], F32) for _ in range(2)]
                wcb = [cx.T([128, 8, 128], BF16) for _ in range(2)]
                zc = cx.T([128, S + 16], F32)
                pa = cx.T([128, S + 16], F32)
                yb = cx.T([128, S + 16], F32)
                ybf = cx.T([128, S], BF16)
                yo = [cx.T([128, 512], BF16) for _ in range(2)]
                pw = cx.T([128, 128], F32)
                psc = cx.T([128, 128], F32)
                pwb = cx.T([128, 128], BF16)
                pz = [cx.PS([128, 512]) for _ in range(2)]
                cx.V(lambda h: h.memset(zc[:], 0.0), wr=[zc])
                nps = 0
                for c in range(12):
                    col0 = c * 128 if c < 8 else 2048 + (c - 8) * 128
                    bcol = c if c < 8 else 16 + (c - 8)
                    ws, wc = wst[c % 2], wcb[c % 2]
                    cx.dma("sp", ws[:], IN["ev_w_in"][:, col0:col0 + 128].rearrange("(k p) n -> p k n", p=128), wr=[ws])
                    cx.G(lambda h, ws=ws, wc=wc: h.tensor_copy(out=wc[:], in_=ws[:]), rd=[ws], wr=[wc])
                    for tb in range(8):
                        p = pz[nps % 2]
                        nps += 1
                        for k in range(8):
                            cx.M(lambda h, k=k, p=p, wc=wc, tb=tb: h.matmul(p[:], lhsT=wc[:, k, :], rhs=hT[:, k, tb * 512:(tb + 1) * 512], start=(k == 0), stop=(k == 7)),
                                 rd=[wc, hT], wr=[p])
                        cx.A(lambda h, p=p, tb=tb, bcol=bcol: h.activation(out=zc[:, 8 + tb * 512:8 + (tb + 1) * 512], in_=p[:], func=AF.Identity, bias=bT[:, bcol:bcol + 1]),
                             rd=[p, bT], wr=[zc])
                    if c < 8:
                        cx.V(lambda h, c=c: h.tensor_scalar(out=yb[:, 0:S], in0=zc[:, 6:6 + S], scalar1=cw[:, c, 0:1], scalar2=None, op0=ALU.mult), rd=[zc, cw], wr=[yb])
                        for j in range(1, 5):
                            cx.V(lambda h, c=c, j=j: h.scalar_tensor_tensor(out=yb[:, 0:S], in0=zc[:, 6 + j:6 + j + S], scalar=cw[:, c, j:j + 1], in1=yb[:, 0:S], op0=ALU.mult, op1=ALU.add),
                                 rd=[zc, cw, yb], wr=[yb])
                        cx.A(lambda h, c=c: h.activation(out=QK[:, c, :], in_=yb[:, 0:S], func=AF.Silu, bias=cb[:, c:c + 1]), rd=[yb, cb], wr=[QK])
                    else:
                        g = c - 8
                        win = (2, 4, 8, 16)[g]
                        half = win // 2
                        n_el = S + 15
                        cur = zc
                        bufs = [pa, yb]
                        bi = 0
                        step = 1
                        while step < win:
                            d = bufs[bi % 2]
                            bi += 1
                            cx.V(lambda h, cur=cur, d=d, step=step, n_el=n_el: h.tensor_tensor(out=d[:, 0:n_el - step + 1], in0=cur[:, 0:n_el - step + 1], in1=cur[:, step:n_el + 1], op=ALU.add),
                                 rd=[cur], wr=[d])
                            n_el = n_el - step
                            cur = d
                            step *= 2
                        o = bufs[bi % 2]
                        cx.V(lambda h, cur=cur, half=half, o=o, win=win: h.tensor_scalar(out=o[:, 0:S], in0=cur[:, 8 - half:8 - half + S], scalar1=1.0 / win, scalar2=None, op0=ALU.mult), rd=[cur], wr=[o])
                        cx.V(lambda h, o=o, g=g: h.tensor_tensor(out=o[:, 0:16], in0=o[:, 0:16], in1=edge[:, g, 0:16], op=ALU.mult), rd=[o, edge], wr=[o])
                        cx.V(lambda h, o=o, g=g: h.tensor_tensor(out=o[:, S - 16:S], in0=o[:, S - 16:S], in1=edge[:, g, 16:32], op=ALU.mult), rd=[o, edge], wr=[o])
                        cx.V(lambda h, o=o: h.tensor_tensor(out=ybf[:], in0=o[:, 0:S], in1=zc[:, 8:8 + S], op=ALU.subtract), rd=[o, zc], wr=[ybf])
                        cx.dma("sp", pw[:], IN["ev_pool_w"][g], wr=[pw])
                        cx.dma("sp", psc[:], IN["ev_pool_scale"][0:1, g * 128:(g + 1) * 128].broadcast_to([128, 128]), wr=[psc])
                        cx.V(lambda h: h.tensor_tensor(out=pwb[:], in0=pw[:], in1=psc[:], op=ALU.mult), rd=[pw, psc], wr=[pwb])
                        for tb in range(8):
                            p = pz[nps % 2]
                            y_ = yo[nps % 2]
                            nps += 1
                            cx.M(lambda h, p=p, tb=tb: h.matmul(p[:], lhsT=pwb[:], rhs=ybf[:, tb * 512:(tb + 1) * 512], start=True, stop=True), rd=[pwb, ybf], wr=[p])
                            cx.A(lambda h, p=p, y_=y_: h.activation(out=y_[:], in_=p[:], func=AF.Copy), rd=[p], wr=[y_])
                            cx.dma("sp", CATT.t[tb * 4:(tb + 1) * 4, :, 4 + g, :].rearrange("n f t -> f n t"), y_[:].rearrange("f (n t) -> f n t", n=4), rd=[y_], wr=[CATT])
                P.barrier()
            with contextlib.ExitStack() as stt:
                cx = Ctx(P, nc, stt)
                stage_f = cx.T([128, 1024], F32)
                wtm = cx.T([128, 8, 1040], BF16)
                for k in range(8):
                    cx.dma("sp", stage_f[:], IN["ev_w_in"][k * 128:(k + 1) * 128, 1024:2048], wr=[stage_f])
                    cx.V(lambda h, k=k: h.tensor_copy(out=wtm[:, k, 0:1024], in_=stage_f[:]), rd=[stage_f], wr=[wtm])
                gst = cx.T([128, 8, 16], F32)
                cx.dma("sp", gst[:], IN["ev_w_in"][:, 2560:2576].rearrange("(k p) n -> p k n", p=128), wr=[gst])
                cx.V(lambda h: h.tensor_copy(out=wtm[:, :, 1024:1040], in_=gst[:]), rd=[gst], wr=[wtm])
                bvo = cx.T([128, 1024], F32)
                bg = cx.T([128, 16], F32)
                cx.dma("sp", bvo[:], IN["ev_b_in"][0:1, 1024:2048].broadcast_to([128, 1024]), wr=[bvo])
                cx.dma("sp", bg[:], IN["ev_b_in"][0:1, 2560:2576].broadcast_to([128, 16]), wr=[bg])
                pv = [cx.PS([128, 512]) for _ in range(2)]
                po = [cx.PS([128, 512]) for _ in range(2)]
                pg = [cx.PS([128, 16]) for _ in range(2)]
                v1 = [cx.T([128, 4, 130], BF16) for _ in range(2)]
                of = [cx.T([128, 512], F32) for _ in range(2)]
                ob = [cx.T([128, 512], BF16) for _ in range(2)]
                for b_ in v1:
                    cx.V(lambda h, b_=b_: h.memset(b_[:], 1.0), wr=[b_])
                for i in range(NT):
                    a = i % 2
                    for (p, c0_, c1_) in ((pv[a], 0, 512), (po[a], 512, 1024), (pg[a], 1024, 1040)):
                        for k in range(8):
                            cx.M(lambda h, k=k, p=p, c0_=c0_, c1_=c1_, i=i: h.matmul(p[:], lhsT=hT[:, k, i * 128:(i + 1) * 128], rhs=wtm[:, k, c0_:c1_], start=(k == 0), stop=(k == 7)),
                                 rd=[hT, wtm], wr=[p])
                    for hh in range(4):
                        cx.V(lambda h, a=a, hh=hh: h.tensor_tensor(out=v1[a][:, hh, 0:128], in0=pv[a][:, hh * 128:(hh + 1) * 128],
                                                                   in1=bvo[:, hh * 128:(hh + 1) * 128], op=ALU.add), rd=[pv[a], bvo], wr=[v1[a]])
                    cx.dma("sp", V1D.t[i], v1[a][:], rd=[v1[a]], wr=[V1D])
                    cx.V(lambda h, a=a: h.tensor_tensor(out=of[a][:], in0=po[a][:], in1=bvo[:, 512:1024], op=ALU.add), rd=[po[a], bvo], wr=[of[a]])
                    cx.A(lambda h, a=a: h.activation(out=ob[a][:], in_=of[a][:], func=AF.Sigmoid), rd=[of[a]], wr=[ob[a]])
                    if i == 0:
                        cx.dma("sp", SC["DBG2"].t, of[a][:], rd=[of[a]], wr=[SC["DBG2"]])
                    cx.dma("sp", SIGD.t[i], ob[a][:], rd=[ob[a]], wr=[SIGD])
                    cx.V(lambda h, a=a, i=i: h.tensor_tensor(out=GT[:, i, :], in0=pg[a][:], in1=bg[:], op=ALU.add), rd=[pg[a], bg], wr=[GT])
                cx.dma("sp", SC["DBG1"].t, GT[:], rd=[GT], wr=[SC["DBG1"]])
                P.barrier()
            with contextlib.ExitStack() as stg:
                cx = Ctx(P, nc, stg)
                LF = cx.T([128, NT, 8], F32)
                t8 = cx.T([128, NT, 8], F32)
                BC = cx.T([128, NT, 16], F32)
                cx.A(lambda h: h.activation(out=t8[:], in_=GT[:, :, 8:16], func=AF.Exp, scale=-1.0), rd=[GT], wr=[t8])
                cx.V(lambda h: h.tensor_scalar(out=t8[:], in0=t8[:], scalar1=1.0, scalar2=None, op0=ALU.add), rd=[t8], wr=[t8])
                cx.A(lambda h: h.activation(out=LF[:], in_=t8[:], func=AF.Ln), rd=[t8], wr=[LF])
                cx.V(lambda h: h.tensor_scalar(out=LF[:], in0=LF[:], scalar1=-1.0, scalar2=None, op0=ALU.mult), rd=[LF], wr=[LF])
                triU = cx.T([128, 128], F32)
                triL = cx.T([128, 128], F32)
                ones = cx.T([128, 128], F32)
                cx.dma("sp", triU[:], IN["triU"], wr=[triU])
                cx.dma("sp", triL[:], IN["triL"], wr=[triL])
                cx.V(lambda h: h.memset(ones[:], 1.0), wr=[ones])
                pc = cx.PS([128, NT, 16])
                for i in range(NT):
                    cx.M(lambda h, i=i: h.matmul(pc[:, i, 0:4], lhsT=triU[:], rhs=LF[:, i, 0:4], start=True, stop=True), rd=[triU, LF], wr=[pc])
                    cx.M(lambda h, i=i: h.matmul(pc[:, i, 4:8], lhsT=triL[:], rhs=LF[:, i, 4:8], start=True, stop=True), rd=[triL, LF], wr=[pc])
                    cx.M(lambda h, i=i: h.matmul(pc[:, i, 8:16], lhsT=ones[:], rhs=LF[:, i, 0:8], start=True, stop=True), rd=[ones, LF], wr=[pc])
                cx.V(lambda h: h.tensor_copy(out=BC[:], in_=pc[:]), rd=[pc], wr=[BC])
                cx.A(lambda h: h.activation(out=EB[:], in_=BC[:, :, 0:8], func=AF.Exp), rd=[BC], wr=[EB])
                cx.A(lambda h: h.activation(out=EE[:], in_=BC[:, :, 8:16], func=AF.Exp), rd=[BC], wr=[EE])
                cx.V(lambda h: h.tensor_tensor(out=t8[:], in0=GT[:, :, 0:8], in1=BC[:, :, 0:8], op=ALU.subtract), rd=[GT, BC], wr=[t8])
                cx.V(lambda h: h.tensor_scalar(out=t8[:], in0=t8[:], scalar1=float(-0.5 * np.log(128.0)), scalar2=None, op0=ALU.add), rd=[t8], wr=[t8])
                cx.A(lambda h: h.activation(out=ES[:], in_=t8[:], func=AF.Exp), rd=[t8], wr=[ES])
                P.barrier()
        with contextlib.ExitStack() as st3:
            cx = Ctx(P, nc, st3)
            mk = []
            for nm in ("triU", "triL"):
                f = cx.T([128, 128], F32)
                cx.dma("sp", f[:], IN[nm], wr=[f])
                mk.append(f)
            Cst = cx.T([128, 8, 129], F32)
            Cb = cx.T([128, 8, 129], BF16)
            cx.V(lambda h: h.memset(Cst[:], 0.0), wr=[Cst])
            cx.V(lambda h: h.memset(Cb[:], 0.0), wr=[Cb])
            NV = 6
            v1 = [cx.T([128, 4, 130], BF16) for _ in range(NV)]
            psS = [cx.PS([128, 4, 128]) for _ in range(2)]
            psA = [cx.PS([128, 3, 129]) for _ in range(3)]
            ps_t = cx.PS([128, 8, 128], BF16)
            ps_h = cx.PS([128, 4, 128], BF16)
            AT = cx.T([128, 8, 128], BF16)
            ksb = cx.T([128, 8, 128], BF16)
            sm = cx.T([128, 8, 4], F32)
            hacc = cx.T([128, NT, 512], F32)
            NSG = 6
            sgl = [cx.T([128, 512], BF16) for _ in range(NSG)]
            sq = cx.T([128, 512], F32)
            st4 = [cx.T([128, 12], F32) for _ in range(2)]
            mg = cx.T([128, 512], F32)
            hmb = [cx.T([128, 512], BF16) for _ in range(2)]
            hmT = [cx.T([128, 4, 128], BF16) for _ in range(2)]
            cx.dma("sp", mg[:], IN["ev_mnorm_g"][0:1, :].broadcast_to([128, 512]), wr=[mg])
            bg = SC.get("BG0")
            if bg is not None:
                bg.attach(cx)
            chains = [(d, hh) for d in range(2) for hh in range(4)]

            def aslot(c):
                return psA[c // 3], c % 3

            def tile_of(s, d):
                return s if d == 0 else NT - 1 - s

            PDV = 2
            nfin = 0
            for s in range(NT + PDV):
                if s < NT:
                    for d in range(2):
                        vb = v1[(2 * s + d) % NV]
                        cx.dma("sp", vb[:], V1D.t[tile_of(s, d)], rd=[V1D], wr=[vb])
                    if s >= NT // 2:
                        for d in range(2):
                            sg_ = sgl[(2 * s + d) % NSG]
                            cx.dma("act", sg_[:], SIGD.t[tile_of(s, d)], rd=[SIGD], wr=[sg_])
                s_ = s - PDV
                if s_ < 0:
                    continue
                s = s_
                if bg is not None:
                    bg.step(4)
                vbs = [v1[(2 * s + d) % NV] for d in range(2)]
                tls = [tile_of(s, d) for d in range(2)]
                for c, (d, hh) in enumerate(chains):
                    tsl = slice(tls[d] * 128, (tls[d] + 1) * 128)
                    cx.M(lambda h: h.matmul(psS[d][:, hh, :], lhsT=QK[:, 4 + hh, tsl], rhs=QK[:, hh, tsl], start=True, stop=True), rd=[QK], wr=[psS[d]])
                for c, (d, hh) in enumerate(chains):
                    col = d * 4 + hh
                    cx.V(lambda h: h.scalar_tensor_tensor(out=AT[:, c, :], in0=psS[d][:, hh, :], scalar=ES[:, tls[d], col:col + 1], in1=mk[d][:], op0=ALU.mult, op1=ALU.mult),
                         rd=[psS[d], ES, mk[d]], wr=[AT])
                for c, (d, hh) in enumerate(chains):
                    col = d * 4 + hh
                    tsl = slice(tls[d] * 128, (tls[d] + 1) * 128)
                    pa, sl = aslot(c)
                    cx.M(lambda h: h.matmul(pa[:, sl, :], lhsT=AT[:, c, :], rhs=vbs[d][:, hh, 0:129], start=True, stop=False), rd=[AT, vbs[d]], wr=[pa])
                    cx.M(lambda h: h.matmul(pa[:, sl, :], lhsT=QK[:, hh, tsl], rhs=Cb[:, col, :], start=False, stop=True), rd=[QK, Cb], wr=[pa])
                for c, (d, hh) in enumerate(chains):
                    col = d * 4 + hh
                    pa, sl = aslot(c)
                    cx.A(lambda h: h.activation(out=sm[:, c, 2:3], in_=pa[:, sl, 128:129], func=AF.Abs, scale=EB[:, tls[d], col:col + 1]), rd=[pa, EB], wr=[sm])
                cx.V(lambda h: h.tensor_scalar(out=sm[:, :, 0:1], in0=sm[:, :, 2:3], scalar1=1.0, scalar2=None, op0=ALU.max), rd=[sm], wr=[sm])
                cx.V(lambda h: h.reciprocal(out=sm[:, :, 3:4], in_=sm[:, :, 0:1]), rd=[sm], wr=[sm])
                for d in range(2):
                    cx.V(lambda h: h.tensor_tensor(out=sm[:, d * 4:(d + 1) * 4, 1:2], in0=EB[:, tls[d], d * 4:(d + 1) * 4].unsqueeze(2), in1=sm[:, d * 4:(d + 1) * 4, 3:4], op=ALU.mult),
                         rd=[sm, EB], wr=[sm])
                for c, (d, hh) in enumerate(chains):
                    pa, sl = aslot(c)
                    dst = hacc[:, tls[d], hh * 128:(hh + 1) * 128]
                    if s < NT // 2:
                        cx.A(lambda h: h.activation(out=dst, in_=pa[:, sl, 0:128], func=AF.Copy, scale=sm[:, c, 1:2]), rd=[pa, sm], wr=[hacc])
                    else:
                        cx.V(lambda h: h.scalar_tensor_tensor(out=dst, in0=pa[:, sl, 0:128], scalar=sm[:, c, 1:2], in1=dst, op0=ALU.mult, op1=ALU.add), rd=[pa, sm, hacc], wr=[hacc])
                for c, (d, hh) in enumerate(chains):
                    tsl = slice(tls[d] * 128, (tls[d] + 1) * 128)
                    cx.M(lambda h: h.transpose(out=ps_t[:, c, :], in_=QK[:, 4 + hh, tsl], identity=idb[:]), rd=[QK, idb], wr=[ps_t])
                for c, (d, hh) in enumerate(chains):
                    col = d * 4 + hh
                    cx.A(lambda h: h.activation(out=ksb[:, c, :], in_=ps_t[:, c, :], func=AF.Copy, scale=ES[:, tls[d], col:col + 1]), rd=[ps_t, ES], wr=[ksb])
                for c, (d, hh) in enumerate(chains):
                    pa, sl = aslot(c)
                    cx.M(lambda h: h.matmul(pa[:, sl, :], lhsT=ksb[:, c, :], rhs=vbs[d][:, hh, 0:129], start=True, stop=True), rd=[ksb, vbs[d]], wr=[pa])
                for c, (d, hh) in enumerate(chains):
                    col = d * 4 + hh
                    pa, sl = aslot(c)
                    cx.V(lambda h: h.tensor_scalar(out=Cst[:, col, :], in0=Cst[:, col, :], scalar1=EE[:, tls[d], col:col + 1], scalar2=None, op0=ALU.mult), rd=[Cst, EE], wr=[Cst])
                    cx.V(lambda h: h.scalar_tensor_tensor(out=Cst[:, col, :], in0=pa[:, sl, :], scalar=EE[:, tls[d], col:col + 1], in1=Cst[:, col, :], op0=ALU.mult, op1=ALU.add),
                         rd=[pa, EE, Cst], wr=[Cst])
                cx.A(lambda h: h.activation(out=Cb[:], in_=Cst[:], func=AF.Copy), rd=[Cst], wr=[Cb])
                if s >= NT // 2:
                    for d in range(2):
                        i = tls[d]
                        a = nfin % 2
                        nfin += 1
                        sg_ = sgl[(2 * s + d) % NSG]
                        hv = hacc[:, i, :]
                        cx.V(lambda h: h.tensor_tensor(out=sq[:], in0=hv, in1=hv, op=ALU.mult), rd=[hacc], wr=[sq])
                        cx.V(lambda h: h.tensor_reduce(out=st4[a][:, 0:4], in_=sq[:].rearrange("p (h d) -> p h d", h=4), axis=AX.X, op=ALU.add), rd=[sq], wr=[st4[a]])
                        cx.A(lambda h: h.activation(out=st4[a][:, 4:8], in_=st4[a][:, 0:4], func=AF.Sqrt, scale=1.0 / 128, bias=eps[:, 0:1]), rd=[st4[a], eps], wr=[st4[a]])
                        cx.V(lambda h: h.reciprocal(out=st4[a][:, 8:12], in_=st4[a][:, 4:8]), rd=[st4[a]], wr=[st4[a]])
                        cx.V(lambda h: h.tensor_tensor(out=hv.rearrange("p (h d) -> p h d", h=4), in0=hv.rearrange("p (h d) -> p h d", h=4),
                                                       in1=st4[a][:, 8:12].unsqueeze(2).broadcast_to([128, 4, 128]), op=ALU.mult), rd=[hacc, st4[a]], wr=[hacc])
                        cx.V(lambda h: h.tensor_tensor(out=sq[:], in0=mg[:], in1=sg_[:], op=ALU.mult), rd=[mg, sg_], wr=[sq])
                        cx.V(lambda h: h.tensor_tensor(out=hmb[a][:], in0=hv, in1=sq[:], op=ALU.mult), rd=[hacc, sq], wr=[hmb[a]])
                        for k in range(4):
                            cx.M(lambda h: h.transpose(out=ps_h[:, k, :], in_=hmb[a][:, k * 128:(k + 1) * 128], identity=idb[:]), rd=[hmb[a], idb], wr=[ps_h])
                        cx.A(lambda h: h.activation(out=hmT[a][:], in_=ps_h[:], func=AF.Copy), rd=[ps_h], wr=[hmT[a]])
                        cx.dma("act", CATT.t[i, :, 0:4, :], hmT[a][:], rd=[hmT[a]], wr=[CATT])
            if bg is not None:
                bg.finish()
                SC.setdefault("TABDONE", {})[0] = True
            P.barrier()
        with contextlib.ExitStack() as st4_:
            cx = Ctx(P, nc, st4_)
            cb_ = [cx.T([128, 8, 128], BF16) for _ in range(4)]
            g1, = load_mods(cx, MODS, li, [2])
            stage_f = cx.T([128, 1024], F32)

            def src(i):
                b = cb_[i % 4]
                cx.dma("pool", b[:], CATT.t[i], rd=[CATT], wr=[b])
                return b, b
            emit_outproj(cx, Xin, Xout, src, IN["ev_w_out"], g1, stage_f)
            P.barrier()
        P.flush()


W_SPECS = {
    "ada_w": [2, 1024, 6144], "ada_b": [2, 6144], "norm_mix_g": [2, 1024], "norm_ffn_g": [2, 1024],
    "ev_w_in": [1024, 2576], "ev_b_in": [1, 2576], "ev_b_inT": [128, 20], "ev_conv_wT": [128, 8, 5], "ev_conv_bT": [128, 8],
    "ev_mnorm_g": [1, 512], "ev_pool_w": [4, 128, 128], "ev_pool_scale": [1, 512], "ev_w_out": [1024, 1024],
    "od_w_in": [1024, 1536], "od_qnorm_g": [1, 128], "od_knorm_g": [1, 128], "od_w_out": [1024, 1024],
    "peer_w_q": [2, 1024, 2048], "peer_keys": [2, 2, 128, 128], "peer_u": [2, 16384, 1024], "peer_v": [2, 16384, 1024],
    "final_g": [1, 1024],
    "ident": [128, 128], "triU": [128, 128], "triL": [128, 128], "pooledge": [1, 4, 32], "ropecs": [128, 2, NT, 64], "iota16": [1, 16],
    "cT": [128, 8],
}


def host_consts():
    cn = {}
    cn["ident"] = np.eye(128, dtype=np.float32)
    s_ = np.arange(128)
    cn["triU"] = (s_[:, None] <= s_[None, :]).astype(np.float32)
    cn["triL"] = (s_[:, None] >= s_[None, :]).astype(np.float32)
    pe = np.zeros((1, 4, 32), np.float32)
    for g, w in enumerate((2, 4, 8, 16)):
        for j in range(32):
            t = j if j < 16 else S - 32 + j
            lo = max(t - w // 2, 0)
            hi = min(t + w // 2, S)
            pe[0, g, j] = w / float(hi - lo)
    cn["pooledge"] = pe
    t = np.arange(S)
    r, c = t // 64, t % 64
    freqs = (10000.0 ** (-np.arange(0, 64, 2, dtype=np.float32) / 64.0)).astype(np.float32)
    ang = np.concatenate([r[:, None].astype(np.float32) * freqs, c[:, None].astype(np.float32) * freqs], axis=-1).astype(np.float32)
    cs = np.stack([np.cos(ang), np.sin(ang)], 0).astype(np.float32)
    cn["ropecs"] = np.ascontiguousarray(cs.reshape(2, NT, 128, 64).transpose(2, 0, 1, 3))
    cn["iota16"] = np.arange(16, dtype=np.float32)[None, :]
    return cn


DEBUG = [False]


def build(first, last):
    nc = bass.Bass("TRN2", target_bir_lowering=False)
    IK = "ExternalOutput" if DEBUG[0] else "Internal"
    IN = {k: nc.dram_tensor(k, v, F32, kind="ExternalInput").ap() for k, v in W_SPECS.items()}
    xin = nc.dram_tensor("xin", [S, D], F32, kind="ExternalInput").ap()
    out = nc.dram_tensor("out", [S, D], F32, kind="ExternalOutput").ap()
    X = {}
    for k in range(0, 5):
        if k == first - 1:
            X[k] = Buf(xin)
        elif k == last:
            X[k] = Buf(out)
        elif first <= k < last:
            X[k] = Buf(nc.dram_tensor("X%d" % k, [S, D], F32, kind="Internal").ap())
    SC = {
        "CATT": Buf(nc.dram_tensor("CATT", [NT, 128, 8, 128], BF16, kind=IK).ap()),
        "V1D": Buf(nc.dram_tensor("V1D", [NT, 128, 4, 130], BF16, kind=IK).ap()),
        "SIGD": Buf(nc.dram_tensor("SIGD", [NT, 128, 512], BF16, kind=IK).ap()),
        "HFD": Buf(nc.dram_tensor("HFD", [NT, 128, 512], F32, kind=IK).ap()),
        "TAB": [Buf(nc.dram_tensor("TAB%d" % i, [16384, 2048], BF16, kind="Internal").ap()) for i in range(2)],
    }
    SC["DBG1"] = Buf(nc.dram_tensor("DBG1", [128, NT, 16], F32, kind=IK).ap())
    SC["DBG2"] = Buf(nc.dram_tensor("DBG2", [128, 512], F32, kind=IK).ap())
    MODS = Buf(nc.dram_tensor("MODS", [2, 6, 128, 1024], F32, kind=IK).ap())
    with contextlib.ExitStack() as st:
        P = Prog(nc, st)
        phase_mods(P, nc, IN, MODS)
        if first <= 1 and last >= 2:
            SC["BG0"] = TableBuilder(P, nc, IN, 0, SC["TAB"][0])
        if first <= 3 and last >= 4:
            SC["BG1"] = TableBuilder(P, nc, IN, 1, SC["TAB"][1])
        for ph in range(first, last + 1):
            if ph == 1:
                phase_even(P, nc, IN, MODS, 0, X[0], X[1], SC)
            elif ph == 2:
                phase_peer(P, nc, IN, MODS, 0, X[1], X[2], SC, final=False)
            elif ph == 3:
                phase_attn(P, nc, IN, MODS, 1, X[2], X[3], SC)
            elif ph == 4:
                phase_peer(P, nc, IN, MODS, 1, X[3], X[4], SC, final=True)
    return nc


def host_inputs(inputs):
    f = lambda a: np.ascontiguousarray(np.asarray(a, dtype=np.float32))
    sh = {}
    sh["ada_w"] = f(inputs["ada_w"]); sh["ada_b"] = f(inputs["ada_b"])
    sh["norm_mix_g"] = f(inputs["norm_mix_g"]); sh["norm_ffn_g"] = f(inputs["norm_ffn_g"])
    sh["ev_w_in"] = f(inputs["ev_w_in"][0]); sh["ev_b_in"] = f(inputs["ev_b_in"])
    sh["ev_b_inT"] = f(np.asarray(inputs["ev_b_in"])[0, :2560].reshape(20, 128).T)
    cw = np.asarray(inputs["ev_conv_w"])[0, :, 0, :]
    sh["ev_conv_wT"] = f(cw.T.reshape(8, 128, 5).transpose(1, 0, 2))
    sh["ev_conv_bT"] = f(np.asarray(inputs["ev_conv_b"])[0].reshape(8, 128).T)
    sh["ev_mnorm_g"] = f(inputs["ev_mnorm_g"]); sh["ev_pool_w"] = f(inputs["ev_pool_w"][0]); sh["ev_pool_scale"] = f(inputs["ev_pool_scale"])
    sh["ev_w_out"] = f(inputs["ev_w_out"][0])
    sh["od_w_in"] = f(inputs["od_w_in"][0]); sh["od_qnorm_g"] = f(inputs["od_qnorm_g"]); sh["od_knorm_g"] = f(inputs["od_knorm_g"])
    sh["od_w_out"] = f(inputs["od_w_out"][0])
    sh["peer_w_q"] = f(inputs["peer_w_q"]); sh["peer_keys"] = f(inputs["peer_keys"])
    sh["peer_u"] = f(inputs["peer_u"]); sh["peer_v"] = f(inputs["peer_v"])
    sh["final_g"] = f(np.asarray(inputs["final_g"])[None, :])
    sh.update(host_consts())
    return sh


def run_phases(inputs, first, last, xin_list, cores):
    nc = build(first, last)
    sh = host_inputs(inputs)
    c = np.asarray(inputs["c"], dtype=np.float32)
    in_maps = []
    for j, b in enumerate(cores):
        m = dict(sh)
        m["cT"] = np.ascontiguousarray(c[b].reshape(8, 128).T)
        m["xin"] = np.ascontiguousarray(xin_list[j], dtype=np.float32)
        in_maps.append(m)
    res = run_bass_kernel_spmd(nc, in_maps, core_ids=list(range(len(cores))))
    if DEBUG[0]:
        return res.results
    return [r["out"] for r in res.results]


def kernel(**inputs):
    x = np.asarray(inputs["x"], dtype=np.float32)
    outs = run_phases(inputs, 1, 4, [x[b] for b in range(8)], list(range(8)))
    return np.stack(outs, 0).astype(np.float32)


class TableBuilder:
    def __init__(self, P, nc, IN, li, TAB):
        self.P, self.nc, self.IN, self.li, self.TAB = P, nc, IN, li, TAB
        self.blocks = [(half, blk) for half in range(2) for blk in range(64)]
        self.k = 0
        self.loaded = 0
        self.cx = None

    def attach(self, cx):
        self.cx = cx
        self.sf = [cx.T([128, 2, 1024], F32) for _ in range(2)]
        self.sb = [cx.T([128, 2, 1024], BF16) for _ in range(2)]

    def _load(self):
        if self.loaded >= len(self.blocks):
            return
        half, blk = self.blocks[self.loaded]
        f = self.sf[self.loaded % 2]
        rows = slice(blk * 256, (blk + 1) * 256)
        self.cx.dma("pool", f[:], self.IN[("peer_u", "peer_v")[half]][self.li, rows, :].rearrange("(n p) d -> p n d", p=128), wr=[f])
        self.loaded += 1

    def step(self, n=1):
        for _ in range(n):
            if self.k >= len(self.blocks):
                return
            if self.loaded == self.k:
                self._load()
            self._load()
            half, blk = self.blocks[self.k]
            f, b = self.sf[self.k % 2], self.sb[self.k % 2]
            rows = slice(blk * 256, (blk + 1) * 256)
            self.cx.G(lambda h: h.tensor_copy(out=b[:], in_=f[:]), rd=[f], wr=[b])
            self.cx.dma("pool", self.TAB.t[rows, half * 1024:(half + 1) * 1024].rearrange("(n p) d -> p n d", p=128), b[:], rd=[b], wr=[self.TAB])
            self.k += 1

    def finish(self):
        self.step(len(self.blocks))
        self.cx = None

    @property
    def done(self):
        return self.k >= len(self.blocks)


POOL_DOTS = False
PROD_DT = BF16
STT_SLOTS = ()
DIAG_ON_DVE = True


def phase_peer(P, nc, IN, MODS, li, Xin, Xout, SC, final):
    TAB = SC["TAB"][li]
    with contextlib.ExitStack() as st:
        cx = Ctx(P, nc, st)
        prebuilt = bool(SC.get("TABDONE", {}).get(li))
        sf = [cx.T([128, 4, 1024], F32) for _ in range(0 if prebuilt else 3)]
        sb = [cx.T([128, 4, 1024], BF16) for _ in range(0 if prebuilt else 3)]
        n = 0
        for half, nm in enumerate(("peer_u", "peer_v")):
            for blk in range(0 if prebuilt else 32):
                f, b = sf[n % 3], sb[n % 3]
                rows = slice(blk * 512, (blk + 1) * 512)
                cx.dma("sp", f[:], IN[nm][li, rows, :].rearrange("(n p) d -> p n d", p=128), wr=[f])
                if n % 3 == 0:
                    cx.A(lambda h: h.activation(out=b[:], in_=f[:], func=AF.Copy), rd=[f], wr=[b])
                elif n % 3 == 1:
                    cx.V(lambda h: h.tensor_copy(out=b[:], in_=f[:]), rd=[f], wr=[b])
                else:
                    cx.G(lambda h: h.tensor_copy(out=b[:], in_=f[:]), rd=[f], wr=[b])
                cx.dma("act", TAB.t[rows, half * 1024:(half + 1) * 1024].rearrange("(n p) d -> p n d", p=128), b[:], rd=[b], wr=[TAB])
                n += 1
        P.barrier()
        P.flush()
    with contextlib.ExitStack() as st:
        cx = Ctx(P, nc, st)
        idf, idb, eps = load_consts(cx, IN)
        gs2, sh2, g2 = load_mods(cx, MODS, li, [3, 4, 5])
        io16 = cx.T([128, 16], F32)
        th16 = cx.T([128, 16], F32)
        cx.dma("sp", io16[:], IN["iota16"].broadcast_to([128, 16]), wr=[io16])
        cx.V(lambda h: h.tensor_scalar(out=th16[:], in0=io16[:], scalar1=16.0, scalar2=None, op0=ALU.mult), rd=[io16], wr=[th16])
        if final:
            fg = cx.T([128, 1024], F32)
            cx.dma("sp", fg[:], IN["final_g"].broadcast_to([128, 1024]), wr=[fg])
        wq = cx.T([128, 8, 2048], BF16)
        ptr = cx.PS([128, 8, 128], BF16)
        keysT = cx.T([128, 2, 128], BF16)
        with contextlib.ExitStack() as stw:
            cw_ = Ctx(P, nc, stw)
            stg = [cw_.T([128, 2048], F32) for _ in range(2)]
            for k in range(8):
                cw_.dma("sp", stg[k % 2][:], IN["peer_w_q"][li, k * 128:(k + 1) * 128, :], wr=[stg[k % 2]])
                cw_.V(lambda h: h.tensor_copy(out=wq[:, k, :], in_=stg[k % 2][:]), rd=[stg[k % 2]], wr=[wq])
            kf = cw_.T([128, 2, 128], F32)
            kb = cw_.T([128, 2, 128], BF16)
            cw_.dma("sp", kf[:], IN["peer_keys"][li].rearrange("t n c -> n t c"), wr=[kf])
            cw_.V(lambda h: h.tensor_copy(out=kb[:], in_=kf[:]), rd=[kf], wr=[kb])
            for t in range(2):
                cw_.M(lambda h: h.transpose(out=ptr[:, t, :], in_=kb[:, t, :], identity=idb[:]), rd=[kb, idb], wr=[ptr])
            cw_.A(lambda h: h.activation(out=keysT[:], in_=ptr[:, 0:2, :], func=AF.Copy), rd=[ptr], wr=[keysT])
            P.barrier()
        pq = [cx.PS([128, 512]) for _ in range(2)]
        psc = [cx.PS([128, 4, 128]) for _ in range(2)]
        po = cx.PS([128, 1024])
        xb = [cx.T([128, 1024], F32) for _ in range(3)]
        sq = cx.T([128, 1024], F32)
        hb = cx.T([128, 1024], BF16)
        hTi = cx.T([128, 8, 128], BF16)
        st2 = cx.T([128, 4], F32)
        qb = cx.T([128, 2048], BF16)
        rs = cx.T([128, 48], F32)
        qT = cx.T([128, 16, 128], BF16)
        S1 = cx.T([128, 16, 128], F32)
        sqq = Buf(S1.t)
        sqq.k = S1.k
        sqq_ap = S1.t[:].rearrange("p g c -> p (g c)")
        wk = [cx.T([128, 128], F32) for _ in range(2)]
        m = cx.T([128, 16, 16], F32)
        ix = cx.T([128, 16, 16], U32)
        ixf = cx.T([128, 16, 16], F32)
        CS = cx.T([128, 8, 256], F32)
        wk2 = [cx.T([128, 256], F32) for _ in range(2)]
        tops = cx.T([128, 8, 16], F32)
        pos = cx.T([128, 8, 16], U32)
        posf = cx.T([128, 8, 16], F32)
        af = cx.T([128, 8, 16], F32)
        bf_ = cx.T([128, 8, 16], F32)
        oh = Buf(CS.t)
        oh.k = CS.k
        oh_ap = CS.t[:].rearrange("p h (a b) -> p h a b", a=16)
        i12 = cx.T([128, 2, 128], F32)
        idxf = cx.T([128, 128], F32)
        idx = cx.T([128, 128], I32)
        ge = cx.T([128, 8, 16], F32)
        gsum = cx.T([128, 16], F32)
        gate = cx.T([128, 128], F32)
        act = cx.T([128, 128], F32)
        gl = cx.T([128, 128], F32)
        coef = cx.T([128, 128], F32)
        NG = 5
        actT = [Tok() for _ in range(4)]
        glT = [Tok() for _ in range(4)]
        coefT = [Tok() for _ in range(4)]
        Gb = [cx.T([128, 4, 2048], BF16) for _ in range(NG)]
        Gtok = [[Tok() for _ in range(4)] for _ in range(NG)]
        diag = [cx.T([128, 4, 128], BF16) for _ in range(2)]
        junk = cx.T([128, 1024], BF16)
        NPR = 2
        junkv = cx.T([128, 1024], BF16) if STT_SLOTS else None
        prods = [cx.T([128, 1024], PROD_DT) for _ in range(NPR)]
        yb = [cx.T([128, 1024], F32) for _ in range(1)]
        sgn = 0
        hbs = [hb, cx.T([128, 1024], BF16)]
        idxs = [idx, cx.T([128, 128], I32)]
        gates = [gate, cx.T([128, 128], F32)]
        st3 = cx.T([128, 4], F32)
        sq2 = cx.T([128, 1024], F32) if final else None
        fg_ = fg if final else None
        FSTEP = 2

        def front(i):
            x = xb[i % 3]
            hb = hbs[i % 2]
            idx = idxs[i % 2]
            gate = gates[i % 2]
            yield
            cx.dma("sp", x[:], Xin.t[i * 128:(i + 1) * 128, :], rd=[Xin], wr=[x])
            yield
            emit_norm_tile(cx, x, gs2, sh2, hb, sq, st2, idb, ptr, hTi[:], hTi)
            for nb in range(4):
                p = pq[nb % 2]
                for k in range(8):
                    yield
                    cx.M(lambda h: h.matmul(p[:], lhsT=hTi[:, k, :], rhs=wq[:, k, nb * 512:(nb + 1) * 512], start=(k == 0), stop=(k == 7)), rd=[hTi, wq], wr=[p])
                yield
                cx.A(lambda h: h.activation(out=qb[:, nb * 512:(nb + 1) * 512], in_=p[:], func=AF.Copy), rd=[p], wr=[qb])
            yield
            cx.V(lambda h: h.tensor_tensor(out=sqq_ap, in0=qb[:], in1=qb[:], op=ALU.mult), rd=[qb], wr=[sqq])
            yield
            cx.V(lambda h: h.tensor_reduce(out=rs[:, 0:16], in_=S1.t[:], axis=AX.X, op=ALU.add), rd=[sqq], wr=[rs])
            yield
            cx.A(lambda h: h.activation(out=rs[:, 16:32], in_=rs[:, 0:16], func=AF.Sqrt, scale=1.0 / 128, bias=eps[:, 0:1]), rd=[rs, eps], wr=[rs])
            yield
            cx.V(lambda h: h.reciprocal(out=rs[:, 32:48], in_=rs[:, 16:32]), rd=[rs], wr=[rs])
            for r in range(2):
                for j in range(8):
                    g_ = r * 8 + j
                    yield
                    cx.M(lambda h: h.transpose(out=ptr[:, j, :], in_=qb[:, g_ * 128:(g_ + 1) * 128], identity=idb[:]), rd=[qb, idb], wr=[ptr])
                yield
                cx.A(lambda h: h.activation(out=qT[:, r * 8:(r + 1) * 8, :], in_=ptr[:], func=AF.Copy), rd=[ptr], wr=[qT])
            for r in range(4):
                ps_ = psc[r % 2]
                for j in range(4):
                    hp = r * 4 + j
                    yield
                    cx.M(lambda h: h.matmul(ps_[:, j, :], lhsT=qT[:, hp, :], rhs=keysT[:, hp % 2, :], start=True, stop=True), rd=[qT, keysT], wr=[ps_])
                yield
                cx.V(lambda h: h.tensor_tensor(out=S1[:, r * 4:(r + 1) * 4, :], in0=ps_[:], in1=rs[:, 32 + r * 4:32 + (r + 1) * 4].unsqueeze(2).broadcast_to([128, 4, 128]), op=ALU.mult),
                     rd=[ps_, rs], wr=[S1])
            for hp in range(16):
                w_ = wk[hp % 2]
                yield
                cx.V(lambda h: h.max(out=m[:, hp, 0:8], in_=S1[:, hp, :]), rd=[S1], wr=[m])
                yield
                cx.V(lambda h: h.max_index(out=ix[:, hp, 0:8], in_max=m[:, hp, 0:8], in_values=S1[:, hp, :]), rd=[m, S1], wr=[ix])
                yield
                cx.V(lambda h: h.match_replace(out=w_[:], in_to_replace=m[:, hp, 0:8], in_values=S1[:, hp, :], imm_value=-1e30), rd=[m, S1], wr=[w_])
                yield
                cx.V(lambda h: h.max(out=m[:, hp, 8:16], in_=w_[:]), rd=[w_], wr=[m])
                yield
                cx.V(lambda h: h.max_index(out=ix[:, hp, 8:16], in_max=m[:, hp, 8:16], in_values=w_[:]), rd=[m, w_], wr=[ix])
            mv = m[:].rearrange("p (h t) k -> p h t k", t=2)
            yield
            cx.V(lambda h: h.tensor_tensor(out=CS[:].rearrange("p h (a b) -> p h a b", a=16), in0=mv[:, :, 0, :].unsqueeze(3).broadcast_to([128, 8, 16, 16]),
                                           in1=mv[:, :, 1, :].unsqueeze(2).broadcast_to([128, 8, 16, 16]), op=ALU.add), rd=[m], wr=[CS])
            for hh in range(8):
                w_ = wk2[hh % 2]
                yield
                cx.V(lambda h: h.max(out=tops[:, hh, 0:8], in_=CS[:, hh, :]), rd=[CS], wr=[tops])
                yield
                cx.V(lambda h: h.max_index(out=pos[:, hh, 0:8], in_max=tops[:, hh, 0:8], in_values=CS[:, hh, :]), rd=[tops, CS], wr=[pos])
                yield
                cx.V(lambda h: h.match_replace(out=w_[:], in_to_replace=tops[:, hh, 0:8], in_values=CS[:, hh, :], imm_value=-1e30), rd=[tops, CS], wr=[w_])
                yield
                cx.V(lambda h: h.max(out=tops[:, hh, 8:16], in_=w_[:]), rd=[w_], wr=[tops])
                yield
                cx.V(lambda h: h.max_index(out=pos[:, hh, 8:16], in_max=tops[:, hh, 8:16], in_values=w_[:]), rd=[tops, w_], wr=[pos])
            yield
            cx.V(lambda h: h.tensor_copy(out=posf[:], in_=pos[:]), rd=[pos], wr=[posf])
            yield
            cx.V(lambda h: h.tensor_copy(out=ixf[:], in_=ix[:]), rd=[ix], wr=[ixf])
            bc4 = lambda ap3: ap3.unsqueeze(3).broadcast_to([128, 8, 16, 16])
            io4 = io16[:].unsqueeze(1).unsqueeze(1).broadcast_to([128, 8, 16, 16])
            th4 = th16[:].unsqueeze(1).unsqueeze(1).broadcast_to([128, 8, 16, 16])
            yield
            cx.V(lambda h: h.tensor_tensor(out=oh_ap, in0=bc4(posf[:]), in1=th4, op=ALU.is_ge), rd=[posf, th16], wr=[oh])
            yield
            cx.V(lambda h: h.tensor_reduce(out=af[:], in_=oh_ap, axis=AX.X, op=ALU.add), rd=[oh], wr=[af])
            yield
            cx.V(lambda h: h.tensor_scalar(out=af[:], in0=af[:], scalar1=-1.0, scalar2=None, op0=ALU.add), rd=[af], wr=[af])
            yield
            cx.V(lambda h: h.scalar_tensor_tensor(out=bf_[:], in0=af[:], scalar=-16.0, in1=posf[:], op0=ALU.mult, op1=ALU.add), rd=[af, posf], wr=[bf_])
            ixv = ixf[:].rearrange("p (h t) k -> p h t k", t=2)
            for t, src in ((0, af), (1, bf_)):
                yield
                cx.V(lambda h: h.tensor_tensor(out=oh_ap, in0=bc4(src[:]), in1=io4, op=ALU.is_equal), rd=[src, io16], wr=[oh])
                yield
                cx.V(lambda h: h.tensor_tensor(out=oh_ap, in0=oh_ap, in1=ixv[:, :, t, :].unsqueeze(2).broadcast_to([128, 8, 16, 16]), op=ALU.mult), rd=[oh, ixf], wr=[oh])
                yield
                cx.V(lambda h: h.tensor_reduce(out=i12[:, t, :].rearrange("p (h k) -> p h k", h=8), in_=oh_ap, axis=AX.X, op=ALU.add), rd=[oh], wr=[i12])
            yield
            cx.V(lambda h: h.scalar_tensor_tensor(out=idxf[:], in0=i12[:, 0, :], scalar=128.0, in1=i12[:, 1, :], op0=ALU.mult, op1=ALU.add), rd=[i12], wr=[idxf])
            yield
            cx.V(lambda h: h.tensor_copy(out=idx[:], in_=idxf[:]), rd=[idxf], wr=[idx])
            yield
            cx.V(lambda h: h.tensor_tensor(out=ge[:], in0=tops[:], in1=tops[:, :, 0:1].broadcast_to([128, 8, 16]), op=ALU.subtract), rd=[tops], wr=[ge])
            yield
            cx.A(lambda h: h.activation(out=ge[:], in_=ge[:], func=AF.Exp), rd=[ge], wr=[ge])
            yield
            cx.V(lambda h: h.tensor_reduce(out=gsum[:, 0:8], in_=ge[:], axis=AX.X, op=ALU.add), rd=[ge], wr=[gsum])
            yield
            cx.V(lambda h: h.reciprocal(out=gsum[:, 8:16], in_=gsum[:, 0:8]), rd=[gsum], wr=[gsum])
            yield
            cx.V(lambda h: h.tensor_tensor(out=gate[:].rearrange("p (h k) -> p h k", h=8), in0=ge[:], in1=gsum[:, 8:16].unsqueeze(2).broadcast_to([128, 8, 16]), op=ALU.mult),
                 rd=[ge, gsum], wr=[gate])


        PF = NG - 2
        NGRP = NT * 32
        fgens = {}

        def drain(i):
            g = fgens.pop(i, None)
            if g is not None:
                for _ in g:
                    pass

        fgens[0] = front(0)
        drain(0)
        for gn in range(NGRP + PF + 1):
            if gn < NGRP:
                i, sg = divmod(gn, 32)
                if sg == 0:
                    drain(i)
                idx = idxs[i % 2]
                bq_ = gn % NG
                for jj in range(4):
                    j = sg * 4 + jj
                    P.dma("pool", lambda h: h.indirect_dma_start(out=Gb[bq_][:, jj, :], out_offset=None, in_=TAB.t,
                                                                 in_offset=bass.IndirectOffsetOnAxis(ap=idx[:, j:j + 1], axis=0)),
                          _ks([idx, TAB]), [Gtok[bq_][jj]])
            gm = gn - PF
            if 0 <= gm < NGRP:
                i, sg = divmod(gm, 32)
                hb = hbs[i % 2]
                if sg == 0:
                    cx.V(lambda h: h.memset(act[:], 0.0), wr=actT)
                    if i + 1 < NT:
                        fgens[i + 1] = front(i + 1)
                fg = fgens.get(i + 1)
                b_ = gm % NG
                G_ = Gb[b_]
                for jj in range(4):
                    j = sg * 4 + jj
                    if jj in STT_SLOTS:
                        cx.V(lambda h: h.scalar_tensor_tensor(out=junkv[:], in0=G_[:, jj, 0:1024], scalar=1.0, in1=hb[:], op0=ALU.mult, op1=ALU.mult, accum_out=act[:, j:j + 1]),
                             rd=[Gtok[b_][jj], hb], wr=[junkv, actT[sg % 4]])
                    else:
                        pr = prods[j % NPR]
                        cx.V(lambda h: h.tensor_tensor(out=pr[:], in0=G_[:, jj, 0:1024], in1=hb[:], op=ALU.mult), rd=[Gtok[b_][jj], hb], wr=[pr])
                        cx.A(lambda h: h.activation(out=junk[:], in_=pr[:], func=AF.Copy, accum_out=act[:, j:j + 1]), rd=[pr], wr=[junk, actT[sg % 4]])
                    if fg is not None:
                        for _ in range(FSTEP):
                            next(fg, None)
                cs = slice(sg * 4, (sg + 1) * 4)
                cx.A(lambda h: h.activation(out=gl[:, cs], in_=act[:, cs], func=AF.Gelu), rd=[actT[sg % 4]], wr=[glT[sg % 4]])
            gc = gn - PF - 1
            if 0 <= gc < NGRP:
                i, sg = divmod(gc, 32)
                gate = gates[i % 2]
                x = xb[i % 3]
                b_ = gc % NG
                G_ = Gb[b_]
                dg = diag[sg % 2]
                cs = slice(sg * 4, (sg + 1) * 4)
                cx.V(lambda h: h.tensor_tensor(out=coef[:, cs], in0=gl[:, cs], in1=gate[:, cs], op=ALU.mult), rd=[glT[sg % 4], gate], wr=[coefT[sg % 4]])
                for jj in range(4):
                    j = sg * 4 + jj
                    if DIAG_ON_DVE:
                        cx.V(lambda h: h.tensor_scalar(out=dg[:, jj, :], in0=idf[:], scalar1=coef[:, j:j + 1], scalar2=None, op0=ALU.mult), rd=[idf, coefT[sg % 4]], wr=[dg])
                    else:
                        cx.A(lambda h: h.activation(out=dg[:, jj, :], in_=idf[:], func=AF.Copy, scale=coef[:, j:j + 1]), rd=[idf, coefT[sg % 4]], wr=[dg])
                for jj in range(4):
                    j = sg * 4 + jj
                    for hv in range(2):
                        cx.M(lambda h: h.matmul(po[:, hv * 512:(hv + 1) * 512], lhsT=dg[:, jj, :], rhs=G_[:, jj, 1024 + hv * 512:1024 + (hv + 1) * 512],
                                                start=(j == 0), stop=(j == 127)), rd=[dg, Gtok[b_][jj]], wr=[po])
                if sg == 31:
                    y = yb[0]
                    cx.V(lambda h: h.tensor_tensor(out=y[:], in0=po[:], in1=g2[:], op=ALU.mult), rd=[po, g2], wr=[y])
                    cx.V(lambda h: h.tensor_tensor(out=y[:], in0=y[:], in1=x[:], op=ALU.add), rd=[y, x], wr=[y])
                    if final:
                        cx.V(lambda h: h.memset(st3[:, 0:1], 0.0), wr=[st3])
                        cx.A(lambda h: h.activation(out=sq2[:], in_=y[:], func=AF.Square, accum_out=st3[:, 0:1]), rd=[y, st3], wr=[sq2, st3])
                        cx.A(lambda h: h.activation(out=st3[:, 1:2], in_=st3[:, 0:1], func=AF.Sqrt, scale=1.0 / D, bias=eps[:, 0:1]), rd=[st3, eps], wr=[st3])
                        cx.V(lambda h: h.reciprocal(out=st3[:, 2:3], in_=st3[:, 1:2]), rd=[st3], wr=[st3])
                        cx.V(lambda h: h.scalar_tensor_tensor(out=y[:], in0=y[:], scalar=st3[:, 2:3], in1=fg_[:], op0=ALU.mult, op1=ALU.mult), rd=[y, st3, fg_], wr=[y])
                    cx.dma("sp", Xout.t[i * 128:(i + 1) * 128, :], y[:], rd=[y], wr=[Xout])
        P.barrier()
        P.flush()


def phase_attn(P, nc, IN, MODS, li, Xin, Xout, SC):
    with contextlib.ExitStack() as st0:
        c0 = Ctx(P, nc, st0)
        idf, idb, eps = load_consts(c0, IN)
        bigT = c0.T([128, 8, S], BF16)
        qT = c0.T([128, 8, S], BF16)
        kT = c0.T([128, 2, S], BF16)
        V1 = c0.T([128, NT, 2, 130], BF16)
        with contextlib.ExitStack() as st1:
            c1 = Ctx(P, nc, st1)
            gs1, sh1 = load_mods(c1, MODS, li, [0, 1])
            emit_hT(c1, Xin, gs1, sh1, idb, bigT)
            P.barrier()
        with contextlib.ExitStack() as st2:
            cx = Ctx(P, nc, st2)
            w = cx.T([128, 8, 1536], BF16)
            with contextlib.ExitStack() as stw:
                cw_ = Ctx(P, nc, stw)
                stg = [cw_.T([128, 1536], F32) for _ in range(2)]
                for k in range(8):
                    cw_.dma("sp", stg[k % 2][:], IN["od_w_in"][k * 128:(k + 1) * 128, :], wr=[stg[k % 2]])
                    cw_.V(lambda h: h.tensor_copy(out=w[:, k, :], in_=stg[k % 2][:]), rd=[stg[k % 2]], wr=[w])
                P.barrier()
            csb = [cx.T([128, 2, 64], F32) for _ in range(2)]
            gq = cx.T([128, 10, 128], F32)
            g1_ = cx.T([128, 128], F32)
            g2_ = cx.T([128, 128], F32)
            cx.dma("sp", g1_[:], IN["od_qnorm_g"].broadcast_to([128, 128]), wr=[g1_])
            cx.dma("sp", g2_[:], IN["od_knorm_g"].broadcast_to([128, 128]), wr=[g2_])
            cx.V(lambda h: h.tensor_scalar(out=g1_[:], in0=g1_[:], scalar1=float(128 ** -0.5), scalar2=None, op0=ALU.mult), rd=[g1_], wr=[g1_])
            cx.V(lambda h: h.tensor_copy(out=gq[:, 0:8, :], in_=g1_[:].unsqueeze(1).broadcast_to([128, 8, 128])), rd=[g1_], wr=[gq])
            cx.V(lambda h: h.tensor_copy(out=gq[:, 8:10, :], in_=g2_[:].unsqueeze(1).broadcast_to([128, 2, 128])), rd=[g2_], wr=[gq])
            cx.V(lambda h: h.memset(V1[:], 1.0), wr=[V1])
            pz = [cx.PS([128, 512]) for _ in range(3)]
            ptq = cx.PS([128, 8, 128], BF16)
            ptk = cx.PS([128, 2, 128], BF16)
            rs = cx.T([128, 32], F32)
            qn = cx.T([128, 10, 128], F32)
            qr = cx.T([128, 10, 128], BF16)
            t1 = cx.T([128, 10, 64], F32)
            t2 = cx.T([128, 10, 64], F32)
            for i in range(NT):
                tsl = slice(i * 128, (i + 1) * 128)
                for nb in range(3):
                    for k in range(8):
                        cx.M(lambda h: h.matmul(pz[nb][:], lhsT=bigT[:, k, tsl], rhs=w[:, k, nb * 512:(nb + 1) * 512], start=(k == 0), stop=(k == 7)), rd=[bigT, w], wr=[pz[nb]])
                cx.A(lambda h: h.activation(out=V1[:, i, :, 0:128], in_=pz[2][:, 256:512].rearrange("p (g d) -> p g d", g=2), func=AF.Copy), rd=[pz[2]], wr=[V1])
                cs = csb[i % 2]
                cx.dma("act", cs[:], IN["ropecs"][:, :, i, :], wr=[cs])
                zsrc = ((pz[0], 0, 4, 512), (pz[1], 4, 8, 512), (pz[2], 8, 10, 256))
                for (pp, g0, g1x, wd) in zsrc:
                    cx.A(lambda h: h.activation(out=qn[:, g0:g1x, :], in_=pp[:, 0:wd].rearrange("p (g d) -> p g d", d=128), func=AF.Square), rd=[pp], wr=[qn])
                cx.V(lambda h: h.tensor_reduce(out=rs[:, 0:10], in_=qn[:], axis=AX.X, op=ALU.add), rd=[qn], wr=[rs])
                cx.A(lambda h: h.activation(out=rs[:, 10:20], in_=rs[:, 0:10], func=AF.Sqrt, scale=1.0 / 128, bias=eps[:, 0:1]), rd=[rs, eps], wr=[rs])
                cx.V(lambda h: h.reciprocal(out=rs[:, 20:30], in_=rs[:, 10:20]), rd=[rs], wr=[rs])
                for (pp, g0, g1x, wd) in zsrc:
                    cx.V(lambda h: h.tensor_tensor(out=qn[:, g0:g1x, :], in0=pp[:, 0:wd].rearrange("p (g d) -> p g d", d=128),
                                                   in1=rs[:, 20 + g0:20 + g1x].unsqueeze(2).broadcast_to([128, g1x - g0, 128]), op=ALU.mult), rd=[pp, rs], wr=[qn])
                cx.V(lambda h: h.tensor_tensor(out=qn[:], in0=qn[:], in1=gq[:], op=ALU.mult), rd=[qn, gq], wr=[qn])
                qv = qn[:].rearrange("p g (d t) -> p g d t", t=2)
                qo = qr[:].rearrange("p g (d t) -> p g d t", t=2)
                cc = cs[:, 0, :].unsqueeze(1).broadcast_to([128, 10, 64])
                ss_ = cs[:, 1, :].unsqueeze(1).broadcast_to([128, 10, 64])
                cx.V(lambda h: h.tensor_tensor(out=t1[:], in0=qv[:, :, :, 0], in1=cc, op=ALU.mult), rd=[qn, cs], wr=[t1])
                cx.V(lambda h: h.tensor_tensor(out=t2[:], in0=qv[:, :, :, 1], in1=ss_, op=ALU.mult), rd=[qn, cs], wr=[t2])
                cx.V(lambda h: h.tensor_tensor(out=qo[:, :, :, 0], in0=t1[:], in1=t2[:], op=ALU.subtract), rd=[t1, t2], wr=[qr])
                cx.V(lambda h: h.tensor_tensor(out=t1[:], in0=qv[:, :, :, 0], in1=ss_, op=ALU.mult), rd=[qn, cs, qr], wr=[t1])
                cx.V(lambda h: h.tensor_tensor(out=t2[:], in0=qv[:, :, :, 1], in1=cc, op=ALU.mult), rd=[qn, cs, qr], wr=[t2])
                cx.V(lambda h: h.tensor_tensor(out=qo[:, :, :, 1], in0=t1[:], in1=t2[:], op=ALU.add), rd=[t1, t2], wr=[qr])
                for g_ in range(8):
                    cx.M(lambda h: h.transpose(out=ptq[:, g_, :], in_=qr[:, g_, :], identity=idb[:]), rd=[qr, idb], wr=[ptq])
                for g_ in range(2):
                    cx.M(lambda h: h.transpose(out=ptk[:, g_, :], in_=qr[:, 8 + g_, :], identity=idb[:]), rd=[qr, idb], wr=[ptk])
                cx.A(lambda h: h.activation(out=qT[:, :, tsl], in_=ptq[:], func=AF.Copy), rd=[ptq], wr=[qT])
                cx.A(lambda h: h.activation(out=kT[:, :, tsl], in_=ptk[:], func=AF.Copy), rd=[ptk], wr=[kT])
            P.barrier()
        import os as _os
        if _os.environ.get("ATT_STOP") == "2":
            P.barrier()
            P.flush()
            return
        with contextlib.ExitStack() as st3:
            cx = Ctx(P, nc, st3)
            pss = [cx.PS([128, 512]) for _ in range(2)]
            pacc = [cx.PS([128, 512]) for _ in range(4)]
            pto = cx.PS([128, 8, 128], BF16)
            pT = [cx.T([128, 512], BF16) for _ in range(3)]
            ao = [cx.T([128, 8, 128], BF16) for _ in range(4)]
            rinv = cx.T([128, 8], F32)
            steps = [(qb_, hd_, sj) for qb_ in range(8) for hd_ in range(8) for sj in range(NT)]
            accs = pacc

            def emit_score(n):
                qb_, hd_, sj = steps[n]
                ps_ = pss[n % 2]
                cx.M(lambda h: h.matmul(ps_[:], lhsT=kT[:, hd_ // 4, sj * 128:(sj + 1) * 128], rhs=qT[:, hd_, qb_ * 512:(qb_ + 1) * 512], start=True, stop=True), rd=[kT, qT], wr=[ps_])

            bg = SC.get("BG1")
            if bg is not None:
                bg.attach(cx)
            emit_score(0)
            for n, (qb_, hd_, sj) in enumerate(steps):
                g_ = hd_ // 4
                if bg is not None and n % 14 == 0:
                    bg.step(1)
                if n + 1 < len(steps):
                    emit_score(n + 1)
                ps_ = pss[n % 2]
                pt_ = pT[n % 3]
                cx.A(lambda h: h.activation(out=pt_[:], in_=ps_[:], func=AF.Exp), rd=[ps_], wr=[pt_])
                for qs in range(4):
                    cx.M(lambda h: h.matmul(accs[qs][:, 0:129], lhsT=pt_[:, qs * 128:(qs + 1) * 128], rhs=V1[:, sj, g_, 0:129], start=(sj == 0), stop=(sj == NT - 1)),
                         rd=[pt_, V1], wr=[accs[qs]])
                if sj == NT - 1:
                    for qs in range(4):
                        a_ = accs[qs]
                        cx.V(lambda h: h.reciprocal(out=rinv[:, qs:qs + 1], in_=a_[:, 128:129]), rd=[a_], wr=[rinv])
                        cx.V(lambda h: h.tensor_scalar(out=ao[qs][:, hd_, :], in0=a_[:, 0:128], scalar1=rinv[:, qs:qs + 1], scalar2=None, op0=ALU.mult), rd=[a_, rinv], wr=[ao[qs]])
                    if hd_ == 7:
                        for qs in range(4):
                            ti = qb_ * 4 + qs
                            for k in range(8):
                                cx.M(lambda h: h.transpose(out=pto[:, k, :], in_=ao[qs][:, k, :], identity=idb[:]), rd=[ao[qs], idb], wr=[pto])
                            cx.V(lambda h: h.tensor_copy(out=bigT[:, :, ti * 128:(ti + 1) * 128], in_=pto[:]), rd=[pto], wr=[bigT])
            if bg is not None:
                bg.finish()
                SC.setdefault("TABDONE", {})[1] = True
            P.barrier()
        if _os.environ.get("ATT_STOP") == "3":
            P.barrier()
            P.flush()
            return
        with contextlib.ExitStack() as st4:
            cx = Ctx(P, nc, st4)
            g1, = load_mods(cx, MODS, li, [2])
            stage_f = cx.T([128, 1024], F32)
            emit_outproj(cx, Xin, Xout, lambda i: (bigT[:, :, i * 128:(i + 1) * 128], bigT), IN["od_w_out"], g1, stage_f, nx=3)
            P.barrier()
        P.flush()
```

```python
import contextlib
import numpy as np
import concourse.bass as bass
import concourse.mybir as mybir
from concourse.bass_utils import run_bass_kernel_spmd

F32 = mybir.dt.float32
BF16 = mybir.dt.bfloat16
I32 = mybir.dt.int32
U32 = mybir.dt.uint32
AF = mybir.ActivationFunctionType
ALU = mybir.AluOpType
AX = mybir.AxisListType

S = 4096
D = 1024
NT = S // 128
EPS = 1e-6


class Tok:
    __slots__ = ("w", "r", "name")

    def __init__(self, name=""):
        self.w = None
        self.r = {}
        self.name = name


class _Eng:
    def __init__(self, name, sem):
        self.name = name
        self.sem = sem
        self.cnt = 0
        self.seen = {}
        self.ops = []


NDMA = 12


class _Rec:
    def __getattr__(self, name):
        def f(*a, **k):
            return (name, a, k)
        return f


_REC = _Rec()


class Prog:
    ENG = ("pe", "act", "dve", "pool", "sp")

    def __init__(self, nc, stack):
        self.nc = nc
        self.stack = stack
        self.e = {}
        self.sems = {}
        for n in self.ENG:
            self.e[n] = _Eng(n, stack.enter_context(nc.semaphore("s_" + n)))
            self.sems[n] = (self.e[n].sem, 1)
        self.dslot = {}
        for q in ("sp", "pool", "act"):
            sl = []
            for j in range(NDMA):
                key = "d_%s%d" % (q, j)
                sem = stack.enter_context(nc.semaphore(key))
                self.sems[key] = (sem, 16)
                sl.append([key, 0])
            self.dslot[q] = [sl, 0]

    def _deps(self, e, rd, wr):
        deps = {}

        def need(dep, same_ok):
            if dep is None:
                return
            en, c = dep
            if en == e and (e == "pe" or not same_ok):
                return
            if deps.get(en, 0) < c:
                deps[en] = c

        for t in rd:
            need(t.w, True)
        for t in wr:
            need(t.w, False)
            for en, c in t.r.items():
                need((en, c), False)
        return deps

    def _emit_waits(self, E, deps):
        for en, c in deps.items():
            if E.seen.get(en, 0) < c:
                sem, step = self.sems[en]
                E.ops.append(lambda h, sem=sem, v=c * step: h.wait_ge(sem, v))
                E.seen[en] = c

    def op(self, e, fn, rd=(), wr=()):
        E = self.e[e]
        self._emit_waits(E, self._deps(e, rd, wr))
        E.cnt += 1
        sem = E.sem
        rec = fn(_REC)
        E.ops.append(lambda h, rec=rec, sem=sem: getattr(h, rec[0])(*rec[1], **rec[2]).then_inc(sem, 1))
        me = (e, E.cnt)
        for t in wr:
            t.w = me
            t.r = {}
        for t in rd:
            t.r[e] = E.cnt

    def dma(self, q, fn, rd=(), wr=()):
        E = self.e[q]
        slots, nxt = self.dslot[q]
        slot = slots[nxt % NDMA]
        self.dslot[q][1] = nxt + 1
        key = slot[0]
        deps = self._deps(key, rd, wr)
        if slot[1] > 0:
            deps[key] = max(deps.get(key, 0), slot[1])
        self._emit_waits(E, deps)
        slot[1] += 1
        sem = self.sems[key][0]
        rec = fn(_REC)
        E.ops.append(lambda h, rec=rec, sem=sem: getattr(h, rec[0])(*rec[1], **rec[2]).then_inc(sem, 16))
        me = (key, slot[1])
        for t in wr:
            t.w = me
            t.r = {}
        for t in rd:
            t.r[key] = slot[1]

    def barrier(self):
        tgt = {n: self.e[n].cnt for n in self.ENG}
        for q in self.dslot:
            for key, c in self.dslot[q][0]:
                tgt[key] = c
        for n in self.ENG:
            E = self.e[n]
            d = {k: v for k, v in tgt.items() if v > 0 and not (k == n and n == "pe")}
            self._emit_waits(E, d)

    def flush(self):
        nc = self.nc
        with nc.Block() as block:
            @block.tensor
            def _(h):
                for f in self.e["pe"].ops:
                    f(h)

            @block.scalar
            def _(h):
                for f in self.e["act"].ops:
                    f(h)

            @block.vector
            def _(h):
                for f in self.e["dve"].ops:
                    f(h)

            @block.gpsimd
            def _(h):
                for f in self.e["pool"].ops:
                    f(h)

            @block.sync
            def _(h):
                for f in self.e["sp"].ops:
                    f(h)
        for n in self.ENG:
            self.e[n].ops = []


class Buf:
    def __init__(self, t):
        self.t = t
        self.k = Tok()

    def __getitem__(self, i):
        return self.t[i]


def _ks(xs):
    return [x.k if isinstance(x, Buf) else x for x in xs]


_NAME = [0]


class Ctx:
    def __init__(self, P, nc, st):
        self.P, self.nc, self.st = P, nc, st
        self.n = 0

    def T(self, shape, dt, name=None):
        _NAME[0] += 1
        return Buf(self.st.enter_context(self.nc.sbuf_tensor("%s_%d" % (name or "t", _NAME[0]), list(shape), dt)))

    def PS(self, shape, dt=F32, name=None):
        _NAME[0] += 1
        return Buf(self.st.enter_context(self.nc.psum_tensor("%s_%d" % (name or "p", _NAME[0]), list(shape), dt)))

    def V(self, fn, rd=(), wr=()):
        self.P.op("dve", fn, _ks(rd), _ks(wr))

    def A(self, fn, rd=(), wr=()):
        self.P.op("act", fn, _ks(rd), _ks(wr))

    def G(self, fn, rd=(), wr=()):
        self.P.op("pool", fn, _ks(rd), _ks(wr))

    def M(self, fn, rd=(), wr=()):
        self.P.op("pe", fn, _ks(rd), _ks(wr))

    def dma(self, q, out, in_, rd=(), wr=()):
        self.P.dma(q, lambda h, out=out, in_=in_: h.dma_start(out=out, in_=in_), _ks(rd), _ks(wr))


def load_cast(cx, q, dst_bf, src_ap, stage, eng="pool"):
    cx.dma(q, stage.t[:] if not isinstance(stage, tuple) else stage[1], src_ap, wr=[stage if not isinstance(stage, tuple) else stage[0]])


def emit_norm_tile(cx, xt, gs, sh, hb, sq, st2, idb, ptr, hT_dst, hT_buf, hf=None):
    cx.V(lambda h: h.memset(st2[:, 0:1], 0.0), wr=[st2])
    cx.A(lambda h: h.activation(out=sq[:], in_=xt[:], func=AF.Square, accum_out=st2[:, 0:1]), rd=[xt, st2], wr=[sq, st2])
    cx.A(lambda h: h.activation(out=st2[:, 1:2], in_=st2[:, 0:1], func=AF.Sqrt, scale=1.0 / D, bias=EPS_AP[0][:, 0:1]), rd=[st2, EPS_AP[0]], wr=[st2])
    cx.V(lambda h: h.reciprocal(out=st2[:, 2:3], in_=st2[:, 1:2]), rd=[st2], wr=[st2])
    cx.V(lambda h: h.scalar_tensor_tensor(out=sq[:], in0=xt[:], scalar=st2[:, 2:3], in1=gs[:], op0=ALU.mult, op1=ALU.mult), rd=[xt, st2, gs], wr=[sq])
    if hf is not None:
        cx.V(lambda h: h.tensor_tensor(out=hf[:], in0=sq[:], in1=sh[:], op=ALU.add), rd=[sq, sh], wr=[hf])
        cx.G(lambda h: h.tensor_copy(out=hb[:], in_=hf[:]), rd=[hf], wr=[hb])
    else:
        cx.V(lambda h: h.tensor_tensor(out=hb[:], in0=sq[:], in1=sh[:], op=ALU.add), rd=[sq, sh], wr=[hb])
    for k in range(8):
        cx.M(lambda h, k=k: h.transpose(out=ptr[:, k, :], in_=hb[:, k * 128:(k + 1) * 128], identity=idb[:]), rd=[hb, idb], wr=[ptr])
    cx.A(lambda h: h.activation(out=hT_dst, in_=ptr[:], func=AF.Copy), rd=[ptr], wr=[hT_buf])


EPS_AP = [None]


def load_consts(cx, CN):
    idf = cx.T([128, 128], F32)
    idb = cx.T([128, 128], BF16)
    eps = cx.T([128, 1], F32)
    cx.dma("sp", idf[:], CN["ident"], wr=[idf])
    cx.V(lambda h: h.tensor_copy(out=idb[:], in_=idf[:]), rd=[idf], wr=[idb])
    cx.V(lambda h: h.memset(eps[:], EPS), wr=[eps])
    EPS_AP[0] = eps
    return idf, idb, eps


def phase_mods(P, nc, IN, MODS):
    with contextlib.ExitStack() as st:
        cx = Ctx(P, nc, st)
        cT = cx.T([128, 8], F32)
        cond = cx.T([128, 8], F32)
        crep = cx.T([128, 8, 128], F32)
        cx.dma("sp", cT[:], IN["cT"], wr=[cT])
        cx.A(lambda h: h.activation(out=cond[:], in_=cT[:], func=AF.Silu), rd=[cT], wr=[cond])
        cx.V(lambda h: h.tensor_copy(out=crep[:], in_=cond[:].unsqueeze(2).broadcast_to([128, 8, 128])), rd=[cond], wr=[crep])
        wb = [cx.T([128, 8, 512], F32) for _ in range(2)]
        ps = [cx.PS([128, 512]) for _ in range(2)]
        mod = cx.T([128, 6144], F32)
        ab = cx.T([128, 6144], F32)
        gm = cx.T([128, 1024], F32)
        gf = cx.T([128, 1024], F32)
        n = 0
        for i in range(2):
            cx.dma("act", ab[:], IN["ada_b"][i:i + 1, :].broadcast_to([128, 6144]), wr=[ab])
            cx.dma("act", gm[:], IN["norm_mix_g"][i:i + 1, :].broadcast_to([128, 1024]), wr=[gm])
            cx.dma("act", gf[:], IN["norm_ffn_g"][i:i + 1, :].broadcast_to([128, 1024]), wr=[gf])
            for nb in range(12):
                w = wb[n % 2]
                p = ps[n % 2]
                n += 1
                cx.dma("sp" if nb % 2 == 0 else "pool", w[:], IN["ada_w"][i, :, nb * 512:(nb + 1) * 512].rearrange("(k p) n -> p k n", p=128), wr=[w])
                for k in range(8):
                    cx.M(lambda h, k=k, w=w, p=p: h.matmul(p[:], lhsT=crep[:, k, :], rhs=w[:, k, :], start=(k == 0), stop=(k == 7)), rd=[crep, w], wr=[p])
                cx.V(lambda h, p=p, nb=nb: h.tensor_tensor(out=mod[:, nb * 512:(nb + 1) * 512], in0=p[:], in1=ab[:, nb * 512:(nb + 1) * 512], op=ALU.add), rd=[p, ab], wr=[mod])
            cx.V(lambda h: h.scalar_tensor_tensor(out=mod[:, 1024:2048], in0=mod[:, 1024:2048], scalar=1.0, in1=gm[:], op0=ALU.add, op1=ALU.mult), rd=[mod, gm], wr=[mod])
            cx.V(lambda h: h.scalar_tensor_tensor(out=mod[:, 4096:5120], in0=mod[:, 4096:5120], scalar=1.0, in1=gf[:], op0=ALU.add, op1=ALU.mult), rd=[mod, gf], wr=[mod])
            for j, off in enumerate([1024, 0, 2048, 4096, 3072, 5120]):
                cx.dma("sp", MODS.t[i, j], mod[:, off:off + 1024], rd=[mod], wr=[MODS])
        P.barrier()
        P.flush()


def load_mods(cx, MODS, i, js, q="act"):
    out = []
    for j in js:
        b = cx.T([128, 1024], F32)
        cx.dma(q, b[:], MODS.t[i, j], rd=[MODS], wr=[b])
        out.append(b)
    return out


def emit_hT(cx, Xin, gs, sh, idb, hT):
    xb = [cx.T([128, 1024], F32) for _ in range(2)]
    sq = cx.T([128, 1024], F32)
    hb = [cx.T([128, 1024], BF16) for _ in range(2)]
    st2 = [cx.T([128, 4], F32) for _ in range(2)]
    ptr = [cx.PS([128, 8, 128], BF16) for _ in range(2)]
    for i in range(NT):
        x = xb[i % 2]
        cx.dma("sp", x[:], Xin.t[i * 128:(i + 1) * 128, :], rd=[Xin], wr=[x])
        emit_norm_tile(cx, x, gs, sh, hb[i % 2], sq, st2[i % 2], idb, ptr[i % 2], hT[:, :, i * 128:(i + 1) * 128], hT)


def emit_outproj(cx, Xin, Xout, catT_src, w_ap, g1, stage_f, final=None, nx=4):
    wob = cx.T([128, 8, 1024], BF16)
    for k in range(8):
        cx.dma("sp", stage_f[:], w_ap[k * 128:(k + 1) * 128, :], wr=[stage_f])
        cx.G(lambda h, k=k: h.tensor_copy(out=wob[:, k, :], in_=stage_f[:]), rd=[stage_f], wr=[wob])
    py = [cx.PS([128, 1024]) for _ in range(2)]
    NX = nx
    xb = [cx.T([128, 1024], F32) for _ in range(NX)]
    yb = [cx.T([128, 1024], F32) for _ in range(2)]
    PD = 2
    srcs = {}
    for it in range(NT + PD):
        if it < NT:
            srcs[it] = catT_src(it)
            cx.dma("act", xb[it % NX][:], Xin.t[it * 128:(it + 1) * 128, :], rd=[Xin], wr=[xb[it % NX]])
        i = it - PD
        if i < 0:
            continue
        ap, tok = srcs.pop(i)
        p = py[i % 2]
        x = xb[i % NX]
        y = yb[i % 2]
        for nb in range(2):
            for k in range(8):
                cx.M(lambda h, k=k, nb=nb, p=p, ap=ap: h.matmul(p[:, nb * 512:(nb + 1) * 512], lhsT=ap[:, k, :], rhs=wob[:, k, nb * 512:(nb + 1) * 512],
                                                               start=(k == 0), stop=(k == 7)), rd=[tok, wob], wr=[p])
        cx.V(lambda h, p=p, y=y: h.tensor_tensor(out=y[:], in0=p[:], in1=g1[:], op=ALU.mult), rd=[p, g1], wr=[y])
        cx.G(lambda h, x=x, y=y: h.tensor_tensor(out=y[:], in0=y[:], in1=x[:], op=ALU.add), rd=[y, x], wr=[y])
        cx.dma("sp", Xout.t[i * 128:(i + 1) * 128, :], y[:], rd=[y], wr=[Xout])


def phase_even(P, nc, IN, MODS, li, Xin, Xout, SC):
    CATT, V1D, SIGD, HFD = SC["CATT"], SC["V1D"], SC["SIGD"], SC["HFD"]
    with contextlib.ExitStack() as st0:
        c0 = Ctx(P, nc, st0)
        idf, idb, eps = load_consts(c0, IN)
        QK = c0.T([128, 8, S], BF16)
        GT = c0.T([128, NT, 16], F32)
        EB = c0.T([128, NT, 8], F32)
        ES = c0.T([128, NT, 8], F32)
        EE = c0.T([128, NT, 8], F32)
        with contextlib.ExitStack() as st1:
            cx1 = Ctx(P, nc, st1)
            hT = cx1.T([128, 8, S], BF16)
            with contextlib.ExitStack() as st2:
                c2 = Ctx(P, nc, st2)
                gs1, sh1 = load_mods(c2, MODS, li, [0, 1])
                emit_hT(c2, Xin, gs1, sh1, idb, hT)
                P.barrier()
            with contextlib.ExitStack() as stf:
                cx = Ctx(P, nc, stf)
                bT = cx.T([128, 20], F32)
                cw = cx.T([128, 8, 5], F32)
                cb = cx.T([128, 8], F32)
                edge = cx.T([128, 4, 32], F32)
                cx.dma("sp", bT[:], IN["ev_b_inT"], wr=[bT])
                cx.dma("sp", cw[:], IN["ev_conv_wT"], wr=[cw])
                cx.dma("sp", cb[:], IN["ev_conv_bT"], wr=[cb])
                cx.dma("sp", edge[:], IN["pooledge"].broadcast_to([128, 4, 32]), wr=[edge])
                wst = [cx.T([128, 8, 128], F32) for _ in range(2)]
                wcb = [cx.T([128, 8, 128], BF16) for _ in range(2)]
                zc = cx.T([128, S + 16], F32)
                pa = cx.T([128, S + 16], F32)
                yb = cx.T([128, S + 16], F32)
                ybf = cx.T([128, S], BF16)
                yo = [cx.T([128, 512], BF16) for _ in range(2)]
                pw = cx.T([128, 128], F32)
                psc = cx.T([128, 128], F32)
                pwb = cx.T([128, 128], BF16)
                pz = [cx.PS([128, 512]) for _ in range(2)]
                cx.V(lambda h: h.memset(zc[:], 0.0), wr=[zc])
                nps = 0
                for c in range(12):
                    col0 = c * 128 if c < 8 else 2048 + (c - 8) * 128
                    bcol = c if c < 8 else 16 + (c - 8)
                    ws, wc = wst[c % 2], wcb[c % 2]
                    cx.dma("sp", ws[:], IN["ev_w_in"][:, col0:col0 + 128].rearrange("(k p) n -> p k n", p=128), wr=[ws])
                    cx.G(lambda h, ws=ws, wc=wc: h.tensor_copy(out=wc[:], in_=ws[:]), rd=[ws], wr=[wc])
                    for tb in range(8):
                        p = pz[nps % 2]
                        nps += 1
                        for k in range(8):
                            cx.M(lambda h, k=k, p=p, wc=wc, tb=tb: h.matmul(p[:], lhsT=wc[:, k, :], rhs=hT[:, k, tb * 512:(tb + 1) * 512], start=(k == 0), stop=(k == 7)),
                                 rd=[wc, hT], wr=[p])
                        cx.A(lambda h, p=p, tb=tb, bcol=bcol: h.activation(out=zc[:, 8 + tb * 512:8 + (tb + 1) * 512], in_=p[:], func=AF.Identity, bias=bT[:, bcol:bcol + 1]),
                             rd=[p, bT], wr=[zc])
                    if c < 8:
                        cx.V(lambda h, c=c: h.tensor_scalar(out=yb[:, 0:S], in0=zc[:, 6:6 + S], scalar1=cw[:, c, 0:1], scalar2=None, op0=ALU.mult), rd=[zc, cw], wr=[yb])
                        for j in range(1, 5):
                            cx.V(lambda h, c=c, j=j: h.scalar_tensor_tensor(out=yb[:, 0:S], in0=zc[:, 6 + j:6 + j + S], scalar=cw[:, c, j:j + 1], in1=yb[:, 0:S], op0=ALU.mult, op1=ALU.add),
                                 rd=[zc, cw, yb], wr=[yb])
                        cx.A(lambda h, c=c: h.activation(out=QK[:, c, :], in_=yb[:, 0:S], func=AF.Silu, bias=cb[:, c:c + 1]), rd=[yb, cb], wr=[QK])
                    else:
                        g = c - 8
                        win = (2, 4, 8, 16)[g]
                        half = win // 2
                        n_el = S + 15
                        cur = zc
                        bufs = [pa, yb]
                        bi = 0
                        step = 1
                        while step < win:
                            d = bufs[bi % 2]
                            bi += 1
                            cx.V(lambda h, cur=cur, d=d, step=step, n_el=n_el: h.tensor_tensor(out=d[:, 0:n_el - step + 1], in0=cur[:, 0:n_el - step + 1], in1=cur[:, step:n_el + 1], op=ALU.add),
                                 rd=[cur], wr=[d])
                            n_el = n_el - step
                            cur = d
                            step *= 2
                        o = bufs[bi % 2]
                        cx.V(lambda h, cur=cur, half=half, o=o, win=win: h.tensor_scalar(out=o[:, 0:S], in0=cur[:, 8 - half:8 - half + S], scalar1=1.0 / win, scalar2=None, op0=ALU.mult), rd=[cur], wr=[o])
                        cx.V(lambda h, o=o, g=g: h.tensor_tensor(out=o[:, 0:16], in0=o[:, 0:16], in1=edge[:, g, 0:16], op=ALU.mult), rd=[o, edge], wr=[o])
                        cx.V(lambda h, o=o, g=g: h.tensor_tensor(out=o[:, S - 16:S], in0=o[:, S - 16:S], in1=edge[:, g, 16:32], op=ALU.mult), rd=[o, edge], wr=[o])
                        cx.V(lambda h, o=o: h.tensor_tensor(out=ybf[:], in0=o[:, 0:S], in1=zc[:, 8:8 + S], op=ALU.subtract), rd=[o, zc], wr=[ybf])
                        cx.dma("sp", pw[:], IN["ev_pool_w"][g], wr=[pw])
                        cx.dma("sp", psc[:], IN["ev_pool_scale"][0:1, g * 128:(g + 1) * 128].broadcast_to([128, 128]), wr=[psc])
                        cx.V(lambda h: h.tensor_tensor(out=pwb[:], in0=pw[:], in1=psc[:], op=ALU.mult), rd=[pw, psc], wr=[pwb])
                        for tb in range(8):
                            p = pz[nps % 2]
                            y_ = yo[nps % 2]
                            nps += 1
                            cx.M(lambda h, p=p, tb=tb: h.matmul(p[:], lhsT=pwb[:], rhs=ybf[:, tb * 512:(tb + 1) * 512], start=True, stop=True), rd=[pwb, ybf], wr=[p])
                            cx.A(lambda h, p=p, y_=y_: h.activation(out=y_[:], in_=p[:], func=AF.Copy), rd=[p], wr=[y_])
                            cx.dma("sp", CATT.t[tb * 4:(tb + 1) * 4, :, 4 + g, :].rearrange("n f t -> f n t"), y_[:].rearrange("f (n t) -> f n t", n=4), rd=[y_], wr=[CATT])
                P.barrier()
            with contextlib.ExitStack() as stt:
                cx = Ctx(P, nc, stt)
                stage_f = cx.T([128, 1024], F32)
                wtm = cx.T([128, 8, 1040], BF16)
                for k in range(8):
                    cx.dma("sp", stage_f[:], IN["ev_w_in"][k * 128:(k + 1) * 128, 1024:2048], wr=[stage_f])
                    cx.V(lambda h, k=k: h.tensor_copy(out=wtm[:, k, 0:1024], in_=stage_f[:]), rd=[stage_f], wr=[wtm])
                gst = cx.T([128, 8, 16], F32)
                cx.dma("sp", gst[:], IN["ev_w_in"][:, 2560:2576].rearrange("(k p) n -> p k n", p=128), wr=[gst])
                cx.V(lambda h: h.tensor_copy(out=wtm[:, :, 1024:1040], in_=gst[:]), rd=[gst], wr=[wtm])
                bvo = cx.T([128, 1024], F32)
                bg = cx.T([128, 16], F32)
                cx.dma("sp", bvo[:], IN["ev_b_in"][0:1, 1024:2048].broadcast_to([128, 1024]), wr=[bvo])
                cx.dma("sp", bg[:], IN["ev_b_in"][0:1, 2560:2576].broadcast_to([128, 16]), wr=[bg])
                pv = [cx.PS([128, 512]) for _ in range(2)]
                po = [cx.PS([128, 512]) for _ in range(2)]
                pg = [cx.PS([128, 16]) for _ in range(2)]
                v1 = [cx.T([128, 4, 130], BF16) for _ in range(2)]
                of = [cx.T([128, 512], F32) for _ in range(2)]
                ob = [cx.T([128, 512], BF16) for _ in range(2)]
                for b_ in v1:
                    cx.V(lambda h, b_=b_: h.memset(b_[:], 1.0), wr=[b_])
                for i in range(NT):
                    a = i % 2
                    for (p, c0_, c1_) in ((pv[a], 0, 512), (po[a], 512, 1024), (pg[a], 1024, 1040)):
                        for k in range(8):
                            cx.M(lambda h, k=k, p=p, c0_=c0_, c1_=c1_, i=i: h.matmul(p[:], lhsT=hT[:, k, i * 128:(i + 1) * 128], rhs=wtm[:, k, c0_:c1_], start=(k == 0), stop=(k == 7)),
                                 rd=[hT, wtm], wr=[p])
                    for hh in range(4):
                        cx.V(lambda h, a=a, hh=hh: h.tensor_tensor(out=v1[a][:, hh, 0:128], in0=pv[a][:, hh * 128:(hh + 1) * 128],
                                                                   in1=bvo[:, hh * 128:(hh + 1) * 128], op=ALU.add), rd=[pv[a], bvo], wr=[v1[a]])
                    cx.dma("sp", V1D.t[i], v1[a][:], rd=[v1[a]], wr=[V1D])
                    cx.V(lambda h, a=a: h.tensor_tensor(out=of[a][:], in0=po[a][:], in1=bvo[:, 512:1024], op=ALU.add), rd=[po[a], bvo], wr=[of[a]])
                    cx.A(lambda h, a=a: h.activation(out=ob[a][:], in_=of[a][:], func=AF.Sigmoid), rd=[of[a]], wr=[ob[a]])
                    if i == 0:
                        cx.dma("sp", SC["DBG2"].t, of[a][:], rd=[of[a]], wr=[SC["DBG2"]])
                    cx.dma("sp", SIGD.t[i], ob[a][:], rd=[ob[a]], wr=[SIGD])
                    cx.V(lambda h, a=a, i=i: h.tensor_tensor(out=GT[:, i, :], in0=pg[a][:], in1=bg[:], op=ALU.add), rd=[pg[a], bg], wr=[GT])
                cx.dma("sp", SC["DBG1"].t, GT[:], rd=[GT], wr=[SC["DBG1"]])
                P.barrier()
            with contextlib.ExitStack() as stg:
                cx = Ctx(P, nc, stg)
                LF = cx.T([128, NT, 8], F32)
                t8 = cx.T([128, NT, 8], F32)
                BC = cx.T([128, NT, 16], F32)
                cx.A(lambda h: h.activation(out=t8[:], in_=GT[:, :, 8:16], func=AF.Exp, scale=-1.0), rd=[GT], wr=[t8])
                cx.V(lambda h: h.tensor_scalar(out=t8[:], in0=t8[:], scalar1=1.0, scalar2=None, op0=ALU.add), rd=[t8], wr=[t8])
                cx.A(lambda h: h.activation(out=LF[:], in_=t8[:], func=AF.Ln), rd=[t8], wr=[LF])
                cx.V(lambda h: h.tensor_scalar(out=LF[:], in0=LF[:], scalar1=-1.0, scalar2=None, op0=ALU.mult), rd=[LF], wr=[LF])
                triU = cx.T([128, 128], F32)
                triL = cx.T([128, 128], F32)
                ones = cx.T([128, 128], F32)
                cx.dma("sp", triU[:], IN["triU"], wr=[triU])
                cx.dma("sp", triL[:], IN["triL"], wr=[triL])
                cx.V(lambda h: h.memset(ones[:], 1.0), wr=[ones])
                pc = cx.PS([128, NT, 16])
                for i in range(NT):
                    cx.M(lambda h, i=i: h.matmul(pc[:, i, 0:4], lhsT=triU[:], rhs=LF[:, i, 0:4], start=True, stop=True), rd=[triU, LF], wr=[pc])
                    cx.M(lambda h, i=i: h.matmul(pc[:, i, 4:8], lhsT=triL[:], rhs=LF[:, i, 4:8], start=True, stop=True), rd=[triL, LF], wr=[pc])
                    cx.M(lambda h, i=i: h.matmul(pc[:, i, 8:16], lhsT=ones[:], rhs=LF[:, i, 0:8], start=True, stop=True), rd=[ones, LF], wr=[pc])
                cx.V(lambda h: h.tensor_copy(out=BC[:], in_=pc[:]), rd=[pc], wr=[BC])
                cx.A(lambda h: h.activation(out=EB[:], in_=BC[:, :, 0:8], func=AF.Exp), rd=[BC], wr=[EB])
                cx.A(lambda h: h.activation(out=EE[:], in_=BC[:, :, 8:16], func=AF.Exp), rd=[BC], wr=[EE])
                cx.V(lambda h: h.tensor_tensor(out=t8[:], in0=GT[:, :, 0:8], in1=BC[:, :, 0:8], op=ALU.subtract), rd=[GT, BC], wr=[t8])
                cx.V(lambda h: h.tensor_scalar(out=t8[:], in0=t8[:], scalar1=float(-0.5 * np.log(128.0)), scalar2=None, op0=ALU.add), rd=[t8], wr=[t8])
                cx.A(lambda h: h.activation(out=ES[:], in_=t8[:], func=AF.Exp), rd=[t8], wr=[ES])
                P.barrier()
        with contextlib.ExitStack() as st3:
            cx = Ctx(P, nc, st3)
            mk = []
            for nm in ("triU", "triL"):
                f = cx.T([128, 128], F32)
                cx.dma("sp", f[:], IN[nm], wr=[f])
                mk.append(f)
            Cst = cx.T([128, 8, 129], F32)
            Cb = cx.T([128, 8, 129], BF16)
            cx.V(lambda h: h.memset(Cst[:], 0.0), wr=[Cst])
            cx.V(lambda h: h.memset(Cb[:], 0.0), wr=[Cb])
            NV = 6
            v1 = [cx.T([128, 4, 130], BF16) for _ in range(NV)]
            psS = [cx.PS([128, 4, 128]) for _ in range(2)]
            psA = [cx.PS([128, 3, 129]) for _ in range(3)]
            ps_t = cx.PS([128, 8, 128], BF16)
            ps_h = cx.PS([128, 4, 128], BF16)
            AT = cx.T([128, 8, 128], BF16)
            ksb = cx.T([128, 8, 128], BF16)
            sm = cx.T([128, 8, 4], F32)
            hacc = cx.T([128, NT, 512], F32)
            NSG = 6
            sgl = [cx.T([128, 512], BF16) for _ in range(NSG)]
            sq = cx.T([128, 512], F32)
            st4 = [cx.T([128, 12], F32) for _ in range(2)]
            mg = cx.T([128, 512], F32)
            hmb = [cx.T([128, 512], BF16) for _ in range(2)]
            hmT = [cx.T([128, 4, 128], BF16) for _ in range(2)]
            cx.dma("sp", mg[:], IN["ev_mnorm_g"][0:1, :].broadcast_to([128, 512]), wr=[mg])
            bg = SC.get("BG0")
            if bg is not None:
                bg.attach(cx)
            chains = [(d, hh) for d in range(2) for hh in range(4)]

            def aslot(c):
                return psA[c // 3], c % 3

            def tile_of(s, d):
                return s if d == 0 else NT - 1 - s

            PDV = 2
            nfin = 0
            for s in range(NT + PDV):
                if s < NT:
                    for d in range(2):
                        vb = v1[(2 * s + d) % NV]
                        cx.dma("sp", vb[:], V1D.t[tile_of(s, d)], rd=[V1D], wr=[vb])
                    if s >= NT // 2:
                        for d in range(2):
                            sg_ = sgl[(2 * s + d) % NSG]
                            cx.dma("act", sg_[:], SIGD.t[tile_of(s, d)], rd=[SIGD], wr=[sg_])
                s_ = s - PDV
                if s_ < 0:
                    continue
                s = s_
                if bg is not None:
                    bg.step(2)
                vbs = [v1[(2 * s + d) % NV] for d in range(2)]
                tls = [tile_of(s, d) for d in range(2)]
                for c, (d, hh) in enumerate(chains):
                    tsl = slice(tls[d] * 128, (tls[d] + 1) * 128)
                    cx.M(lambda h: h.matmul(psS[d][:, hh, :], lhsT=QK[:, 4 + hh, tsl], rhs=QK[:, hh, tsl], start=True, stop=True), rd=[QK], wr=[psS[d]])
                for c, (d, hh) in enumerate(chains):
                    col = d * 4 + hh
                    cx.V(lambda h: h.scalar_tensor_tensor(out=AT[:, c, :], in0=psS[d][:, hh, :], scalar=ES[:, tls[d], col:col + 1], in1=mk[d][:], op0=ALU.mult, op1=ALU.mult),
                         rd=[psS[d], ES, mk[d]], wr=[AT])
                for c, (d, hh) in enumerate(chains):
                    col = d * 4 + hh
                    tsl = slice(tls[d] * 128, (tls[d] + 1) * 128)
                    pa, sl = aslot(c)
                    cx.M(lambda h: h.matmul(pa[:, sl, :], lhsT=AT[:, c, :], rhs=vbs[d][:, hh, 0:129], start=True, stop=False), rd=[AT, vbs[d]], wr=[pa])
                    cx.M(lambda h: h.matmul(pa[:, sl, :], lhsT=QK[:, hh, tsl], rhs=Cb[:, col, :], start=False, stop=True), rd=[QK, Cb], wr=[pa])
                for c, (d, hh) in enumerate(chains):
                    col = d * 4 + hh
                    pa, sl = aslot(c)
                    cx.A(lambda h: h.activation(out=sm[:, c, 2:3], in_=pa[:, sl, 128:129], func=AF.Abs, scale=EB[:, tls[d], col:col + 1]), rd=[pa, EB], wr=[sm])
                cx.V(lambda h: h.tensor_scalar(out=sm[:, :, 0:1], in0=sm[:, :, 2:3], scalar1=1.0, scalar2=None, op0=ALU.max), rd=[sm], wr=[sm])
                cx.V(lambda h: h.reciprocal(out=sm[:, :, 3:4], in_=sm[:, :, 0:1]), rd=[sm], wr=[sm])
                for d in range(2):
                    cx.V(lambda h: h.tensor_tensor(out=sm[:, d * 4:(d + 1) * 4, 1:2], in0=EB[:, tls[d], d * 4:(d + 1) * 4].unsqueeze(2), in1=sm[:, d * 4:(d + 1) * 4, 3:4], op=ALU.mult),
                         rd=[sm, EB], wr=[sm])
                for c, (d, hh) in enumerate(chains):
                    pa, sl = aslot(c)
                    dst = hacc[:, tls[d], hh * 128:(hh + 1) * 128]
                    if s < NT // 2:
                        cx.A(lambda h: h.activation(out=dst, in_=pa[:, sl, 0:128], func=AF.Copy, scale=sm[:, c, 1:2]), rd=[pa, sm], wr=[hacc])
                    else:
                        cx.V(lambda h: h.scalar_tensor_tensor(out=dst, in0=pa[:, sl, 0:128], scalar=sm[:, c, 1:2], in1=dst, op0=ALU.mult, op1=ALU.add), rd=[pa, sm, hacc], wr=[hacc])
                for c, (d, hh) in enumerate(chains):
                    tsl = slice(tls[d] * 128, (tls[d] + 1) * 128)
                    cx.M(lambda h: h.transpose(out=ps_t[:, c, :], in_=QK[:, 4 + hh, tsl], identity=idb[:]), rd=[QK, idb], wr=[ps_t])
                for c, (d, hh) in enumerate(chains):
                    col = d * 4 + hh
                    cx.A(lambda h: h.activation(out=ksb[:, c, :], in_=ps_t[:, c, :], func=AF.Copy, scale=ES[:, tls[d], col:col + 1]), rd=[ps_t, ES], wr=[ksb])
                for c, (d, hh) in enumerate(chains):
                    pa, sl = aslot(c)
                    cx.M(lambda h: h.matmul(pa[:, sl, :], lhsT=ksb[:, c, :], rhs=vbs[d][:, hh, 0:129], start=True, stop=True), rd=[ksb, vbs[d]], wr=[pa])
                for c, (d, hh) in enumerate(chains):
                    col = d * 4 + hh
                    pa, sl = aslot(c)
                    cx.V(lambda h: h.tensor_scalar(out=Cst[:, col, :], in0=Cst[:, col, :], scalar1=EE[:, tls[d], col:col + 1], scalar2=None, op0=ALU.mult), rd=[Cst, EE], wr=[Cst])
                    cx.V(lambda h: h.scalar_tensor_tensor(out=Cst[:, col, :], in0=pa[:, sl, :], scalar=EE[:, tls[d], col:col + 1], in1=Cst[:, col, :], op0=ALU.mult, op1=ALU.add),
                         rd=[pa, EE, Cst], wr=[Cst])
                cx.A(lambda h: h.activation(out=Cb[:], in_=Cst[:], func=AF.Copy), rd=[Cst], wr=[Cb])
                if s >= NT // 2:
                    for d in range(2):
                        i = tls[d]
                        a = nfin % 2
                        nfin += 1
                        sg_ = sgl[(2 * s + d) % NSG]
                        hv = hacc[:, i, :]
                        cx.V(lambda h: h.tensor_tensor(out=sq[:], in0=hv, in1=hv, op=ALU.mult), rd=[hacc], wr=[sq])
                        cx.V(lambda h: h.tensor_reduce(out=st4[a][:, 0:4], in_=sq[:].rearrange("p (h d) -> p h d", h=4), axis=AX.X, op=ALU.add), rd=[sq], wr=[st4[a]])
                        cx.A(lambda h: h.activation(out=st4[a][:, 4:8], in_=st4[a][:, 0:4], func=AF.Sqrt, scale=1.0 / 128, bias=eps[:, 0:1]), rd=[st4[a], eps], wr=[st4[a]])
                        cx.V(lambda h: h.reciprocal(out=st4[a][:, 8:12], in_=st4[a][:, 4:8]), rd=[st4[a]], wr=[st4[a]])
                        cx.V(lambda h: h.tensor_tensor(out=hv.rearrange("p (h d) -> p h d", h=4), in0=hv.rearrange("p (h d) -> p h d", h=4),
                                                       in1=st4[a][:, 8:12].unsqueeze(2).broadcast_to([128, 4, 128]), op=ALU.mult), rd=[hacc, st4[a]], wr=[hacc])
                        cx.V(lambda h: h.tensor_tensor(out=sq[:], in0=mg[:], in1=sg_[:], op=ALU.mult), rd=[mg, sg_], wr=[sq])
                        cx.V(lambda h: h.tensor_tensor(out=hmb[a][:], in0=hv, in1=sq[:], op=ALU.mult), rd=[hacc, sq], wr=[hmb[a]])
                        for k in range(4):
                            cx.M(lambda h: h.transpose(out=ps_h[:, k, :], in_=hmb[a][:, k * 128:(k + 1) * 128], identity=idb[:]), rd=[hmb[a], idb], wr=[ps_h])
                        cx.A(lambda h: h.activation(out=hmT[a][:], in_=ps_h[:], func=AF.Copy), rd=[ps_h], wr=[hmT[a]])
                        cx.dma("act", CATT.t[i, :, 0:4, :], hmT[a][:], rd=[hmT[a]], wr=[CATT])
            if bg is not None:
                bg.finish()
                SC.setdefault("TABDONE", {})[0] = True
            P.barrier()
        with contextlib.ExitStack() as st4_:
            cx = Ctx(P, nc, st4_)
            cb_ = [cx.T([128, 8, 128], BF16) for _ in range(4)]
            g1, = load_mods(cx, MODS, li, [2])
            stage_f = cx.T([128, 1024], F32)

            def src(i):
                b = cb_[i % 4]
                cx.dma("pool", b[:], CATT.t[i], rd=[CATT], wr=[b])
                return b, b
            emit_outproj(cx, Xin, Xout, src, IN["ev_w_out"], g1, stage_f)
            P.barrier()
        P.flush()


W_SPECS = {
    "ada_w": [2, 1024, 6144], "ada_b": [2, 6144], "norm_mix_g": [2, 1024], "norm_ffn_g": [2, 1024],
    "ev_w_in": [1024, 2576], "ev_b_in": [1, 2576], "ev_b_inT": [128, 20], "ev_conv_wT": [128, 8, 5], "ev_conv_bT": [128, 8],
    "ev_mnorm_g": [1, 512], "ev_pool_w": [4, 128, 128], "ev_pool_scale": [1, 512], "ev_w_out": [1024, 1024],
    "od_w_in": [1024, 1536], "od_qnorm_g": [1, 128], "od_knorm_g": [1, 128], "od_w_out": [1024, 1024],
    "peer_w_q": [2, 1024, 2048], "peer_keys": [2, 2, 128, 128], "peer_u": [2, 16384, 1024], "peer_v": [2, 16384, 1024],
    "final_g": [1, 1024],
    "ident": [128, 128], "triU": [128, 128], "triL": [128, 128], "pooledge": [1, 4, 32], "ropecs": [128, 2, NT, 64], "iota16": [1, 16],
    "cT": [128, 8],
}


def host_consts():
    cn = {}
    cn["ident"] = np.eye(128, dtype=np.float32)
    s_ = np.arange(128)
    cn["triU"] = (s_[:, None] <= s_[None, :]).astype(np.float32)
    cn["triL"] = (s_[:, None] >= s_[None, :]).astype(np.float32)
    pe = np.zeros((1, 4, 32), np.float32)
    for g, w in enumerate((2, 4, 8, 16)):
        for j in range(32):
            t = j if j < 16 else S - 32 + j
            lo = max(t - w // 2, 0)
            hi = min(t + w // 2, S)
            pe[0, g, j] = w / float(hi - lo)
    cn["pooledge"] = pe
    t = np.arange(S)
    r, c = t // 64, t % 64
    freqs = (10000.0 ** (-np.arange(0, 64, 2, dtype=np.float32) / 64.0)).astype(np.float32)
    ang = np.concatenate([r[:, None].astype(np.float32) * freqs, c[:, None].astype(np.float32) * freqs], axis=-1).astype(np.float32)
    cs = np.stack([np.cos(ang), np.sin(ang)], 0).astype(np.float32)
    cn["ropecs"] = np.ascontiguousarray(cs.reshape(2, NT, 128, 64).transpose(2, 0, 1, 3))
    cn["iota16"] = np.arange(16, dtype=np.float32)[None, :]
    return cn


DEBUG = [False]


def build(first, last):
    nc = bass.Bass("TRN2", target_bir_lowering=False)
    IK = "ExternalOutput" if DEBUG[0] else "Internal"
    IN = {k: nc.dram_tensor(k, v, F32, kind="ExternalInput").ap() for k, v in W_SPECS.items()}
    xin = nc.dram_tensor("xin", [S, D], F32, kind="ExternalInput").ap()
    out = nc.dram_tensor("out", [S, D], F32, kind="ExternalOutput").ap()
    X = {}
    for k in range(0, 5):
        if k == first - 1:
            X[k] = Buf(xin)
        elif k == last:
            X[k] = Buf(out)
        elif first <= k < last:
            X[k] = Buf(nc.dram_tensor("X%d" % k, [S, D], F32, kind="Internal").ap())
    SC = {
        "CATT": Buf(nc.dram_tensor("CATT", [NT, 128, 8, 128], BF16, kind=IK).ap()),
        "V1D": Buf(nc.dram_tensor("V1D", [NT, 128, 4, 130], BF16, kind=IK).ap()),
        "SIGD": Buf(nc.dram_tensor("SIGD", [NT, 128, 512], BF16, kind=IK).ap()),
        "HFD": Buf(nc.dram_tensor("HFD", [NT, 128, 512], F32, kind=IK).ap()),
        "TAB": [Buf(nc.dram_tensor("TAB%d" % i, [16384, 2048], BF16, kind="Internal").ap()) for i in range(2)],
    }
    SC["DBG1"] = Buf(nc.dram_tensor("DBG1", [128, NT, 16], F32, kind=IK).ap())
    SC["DBG2"] = Buf(nc.dram_tensor("DBG2", [128, 512], F32, kind=IK).ap())
    MODS = Buf(nc.dram_tensor("MODS", [2, 6, 128, 1024], F32, kind=IK).ap())
    with contextlib.ExitStack() as st:
        P = Prog(nc, st)
        phase_mods(P, nc, IN, MODS)
        if first <= 1 and last >= 2:
            SC["BG0"] = TableBuilder(P, nc, IN, 0, SC["TAB"][0])
        if first <= 3 and last >= 4:
            SC["BG1"] = TableBuilder(P, nc, IN, 1, SC["TAB"][1])
        for ph in range(first, last + 1):
            if ph == 1:
                phase_even(P, nc, IN, MODS, 0, X[0], X[1], SC)
            elif ph == 2:
                phase_peer(P, nc, IN, MODS, 0, X[1], X[2], SC, final=False)
            elif ph == 3:
                phase_attn(P, nc, IN, MODS, 1, X[2], X[3], SC)
            elif ph == 4:
                phase_peer(P, nc, IN, MODS, 1, X[3], X[4], SC, final=True)
    return nc


def host_inputs(inputs):
    f = lambda a: np.ascontiguousarray(np.asarray(a, dtype=np.float32))
    sh = {}
    sh["ada_w"] = f(inputs["ada_w"]); sh["ada_b"] = f(inputs["ada_b"])
    sh["norm_mix_g"] = f(inputs["norm_mix_g"]); sh["norm_ffn_g"] = f(inputs["norm_ffn_g"])
    sh["ev_w_in"] = f(inputs["ev_w_in"][0]); sh["ev_b_in"] = f(inputs["ev_b_in"])
    sh["ev_b_inT"] = f(np.asarray(inputs["ev_b_in"])[0, :2560].reshape(20, 128).T)
    cw = np.asarray(inputs["ev_conv_w"])[0, :, 0, :]
    sh["ev_conv_wT"] = f(cw.T.reshape(8, 128, 5).transpose(1, 0, 2))
    sh["ev_conv_bT"] = f(np.asarray(inputs["ev_conv_b"])[0].reshape(8, 128).T)
    sh["ev_mnorm_g"] = f(inputs["ev_mnorm_g"]); sh["ev_pool_w"] = f(inputs["ev_pool_w"][0]); sh["ev_pool_scale"] = f(inputs["ev_pool_scale"])
    sh["ev_w_out"] = f(inputs["ev_w_out"][0])
    sh["od_w_in"] = f(inputs["od_w_in"][0]); sh["od_qnorm_g"] = f(inputs["od_qnorm_g"]); sh["od_knorm_g"] = f(inputs["od_knorm_g"])
    sh["od_w_out"] = f(inputs["od_w_out"][0])
    sh["peer_w_q"] = f(inputs["peer_w_q"]); sh["peer_keys"] = f(inputs["peer_keys"])
    sh["peer_u"] = f(inputs["peer_u"]); sh["peer_v"] = f(inputs["peer_v"])
    sh["final_g"] = f(np.asarray(inputs["final_g"])[None, :])
    sh.update(host_consts())
    return sh


def run_phases(inputs, first, last, xin_list, cores):
    nc = build(first, last)
    sh = host_inputs(inputs)
    c = np.asarray(inputs["c"], dtype=np.float32)
    in_maps = []
    for j, b in enumerate(cores):
        m = dict(sh)
        m["cT"] = np.ascontiguousarray(c[b].reshape(8, 128).T)
        m["xin"] = np.ascontiguousarray(xin_list[j], dtype=np.float32)
        in_maps.append(m)
    res = run_bass_kernel_spmd(nc, in_maps, core_ids=list(range(len(cores))))
    if DEBUG[0]:
        return res.results
    return [r["out"] for r in res.results]


def kernel(**inputs):
    x = np.asarray(inputs["x"], dtype=np.float32)
    outs = run_phases(inputs, 1, 4, [x[b] for b in range(8)], list(range(8)))
    return np.stack(outs, 0).astype(np.float32)


class TableBuilder:
    ROWS = 512

    def __init__(self, P, nc, IN, li, TAB):
        self.P, self.nc, self.IN, self.li, self.TAB = P, nc, IN, li, TAB
        nb = 16384 // self.ROWS
        self.blocks = [(half, blk) for blk in range(nb) for half in range(2)]
        self.k = 0
        self.cx = None

    def attach(self, cx):
        self.cx = cx

    def step(self, n=1):
        for _ in range(n):
            if self.k >= len(self.blocks):
                return
            half, blk = self.blocks[self.k]
            rows = slice(blk * self.ROWS, (blk + 1) * self.ROWS)
            self.cx.dma("pool", self.TAB.t[rows, half * 1024:(half + 1) * 1024], self.IN[("peer_u", "peer_v")[half]][self.li, rows, :], wr=[Tok()])
            self.k += 1
            self.last = True

    def finish(self):
        self.step(len(self.blocks))
        self.cx = None

    @property
    def done(self):
        return self.k >= len(self.blocks)


POOL_DOTS = False
PROD_DT = BF16
STT_SLOTS = ()
DIAG_ON_DVE = True


def phase_peer(P, nc, IN, MODS, li, Xin, Xout, SC, final):
    TAB = SC["TAB"][li]
    with contextlib.ExitStack() as st:
        cx = Ctx(P, nc, st)
        prebuilt = bool(SC.get("TABDONE", {}).get(li))
        sf = [cx.T([128, 4, 1024], F32) for _ in range(0 if prebuilt else 3)]
        sb = [cx.T([128, 4, 1024], BF16) for _ in range(0 if prebuilt else 3)]
        n = 0
        for half, nm in enumerate(("peer_u", "peer_v")):
            for blk in range(0 if prebuilt else 32):
                f, b = sf[n % 3], sb[n % 3]
                rows = slice(blk * 512, (blk + 1) * 512)
                cx.dma("sp", f[:], IN[nm][li, rows, :].rearrange("(n p) d -> p n d", p=128), wr=[f])
                if n % 3 == 0:
                    cx.A(lambda h: h.activation(out=b[:], in_=f[:], func=AF.Copy), rd=[f], wr=[b])
                elif n % 3 == 1:
                    cx.V(lambda h: h.tensor_copy(out=b[:], in_=f[:]), rd=[f], wr=[b])
                else:
                    cx.G(lambda h: h.tensor_copy(out=b[:], in_=f[:]), rd=[f], wr=[b])
                cx.dma("act", TAB.t[rows, half * 1024:(half + 1) * 1024].rearrange("(n p) d -> p n d", p=128), b[:], rd=[b], wr=[TAB])
                n += 1
        P.barrier()
        P.flush()
    with contextlib.ExitStack() as st:
        cx = Ctx(P, nc, st)
        idf, idb, eps = load_consts(cx, IN)
        gs2, sh2, g2 = load_mods(cx, MODS, li, [3, 4, 5])
        io16 = cx.T([128, 16], F32)
        th16 = cx.T([128, 16], F32)
        cx.dma("sp", io16[:], IN["iota16"].broadcast_to([128, 16]), wr=[io16])
        cx.V(lambda h: h.tensor_scalar(out=th16[:], in0=io16[:], scalar1=16.0, scalar2=None, op0=ALU.mult), rd=[io16], wr=[th16])
        if final:
            fg = cx.T([128, 1024], F32)
            cx.dma("sp", fg[:], IN["final_g"].broadcast_to([128, 1024]), wr=[fg])
        wq = cx.T([128, 8, 2048], BF16)
        ptr = cx.PS([128, 8, 128], BF16)
        keysT = cx.T([128, 2, 128], BF16)
        with contextlib.ExitStack() as stw:
            cw_ = Ctx(P, nc, stw)
            stg = [cw_.T([128, 2048], F32) for _ in range(2)]
            for k in range(8):
                cw_.dma("sp", stg[k % 2][:], IN["peer_w_q"][li, k * 128:(k + 1) * 128, :], wr=[stg[k % 2]])
                cw_.V(lambda h: h.tensor_copy(out=wq[:, k, :], in_=stg[k % 2][:]), rd=[stg[k % 2]], wr=[wq])
            kf = cw_.T([128, 2, 128], F32)
            kb = cw_.T([128, 2, 128], BF16)
            cw_.dma("sp", kf[:], IN["peer_keys"][li].rearrange("t n c -> n t c"), wr=[kf])
            cw_.V(lambda h: h.tensor_copy(out=kb[:], in_=kf[:]), rd=[kf], wr=[kb])
            for t in range(2):
                cw_.M(lambda h: h.transpose(out=ptr[:, t, :], in_=kb[:, t, :], identity=idb[:]), rd=[kb, idb], wr=[ptr])
            cw_.A(lambda h: h.activation(out=keysT[:], in_=ptr[:, 0:2, :], func=AF.Copy), rd=[ptr], wr=[keysT])
            P.barrier()
        pq = [cx.PS([128, 512]) for _ in range(2)]
        psc = [cx.PS([128, 4, 128]) for _ in range(2)]
        po = cx.PS([128, 1024])
        xb = [cx.T([128, 1024], F32) for _ in range(3)]
        sq = cx.T([128, 1024], F32)
        hb = cx.T([128, 1024], BF16)
        hTi = cx.T([128, 8, 128], BF16)
        st2 = cx.T([128, 4], F32)
        qb = cx.T([128, 2048], BF16)
        rs = cx.T([128, 48], F32)
        qT = cx.T([128, 16, 128], BF16)
        S1 = cx.T([128, 16, 128], F32)
        sqq = Buf(S1.t)
        sqq.k = S1.k
        sqq_ap = S1.t[:].rearrange("p g c -> p (g c)")
        wk = [cx.T([128, 128], F32) for _ in range(2)]
        m = cx.T([128, 16, 16], F32)
        ix = cx.T([128, 16, 16], U32)
        ixf = cx.T([128, 16, 16], F32)
        CS = cx.T([128, 8, 256], F32)
        wk2 = [cx.T([128, 256], F32) for _ in range(2)]
        tops = cx.T([128, 8, 16], F32)
        pos = cx.T([128, 8, 16], U32)
        posf = cx.T([128, 8, 16], F32)
        af = cx.T([128, 8, 16], F32)
        bf_ = cx.T([128, 8, 16], F32)
        oh = Buf(CS.t)
        oh.k = CS.k
        oh_ap = CS.t[:].rearrange("p h (a b) -> p h a b", a=16)
        i12 = cx.T([128, 2, 128], F32)
        idxf = cx.T([128, 128], F32)
        idx = cx.T([128, 128], I32)
        ge = cx.T([128, 8, 16], F32)
        gsum = cx.T([128, 16], F32)
        gate = cx.T([128, 128], F32)
        act = cx.T([128, 128], F32)
        gl = cx.T([128, 128], F32)
        coef = cx.T([128, 128], F32)
        NG = 5
        actT = [Tok() for _ in range(4)]
        glT = [Tok() for _ in range(4)]
        coefT = [Tok() for _ in range(4)]
        Gb = [cx.T([128, 4, 2048], BF16) for _ in range(NG)]
        Gtok = [[Tok() for _ in range(4)] for _ in range(NG)]
        diag = [cx.T([128, 4, 128], BF16) for _ in range(2)]
        junk = cx.T([128, 1024], BF16)
        NPR = 2
        junkv = cx.T([128, 1024], BF16) if STT_SLOTS else None
        prods = [cx.T([128, 1024], PROD_DT) for _ in range(NPR)]
        yb = [cx.T([128, 1024], F32) for _ in range(1)]
        sgn = 0
        hbs = [hb, cx.T([128, 1024], BF16)]
        idxs = [idx, cx.T([128, 128], I32)]
        gates = [gate, cx.T([128, 128], F32)]
        st3 = cx.T([128, 4], F32)
        sq2 = cx.T([128, 1024], F32) if final else None
        fg_ = fg if final else None
        FSTEP = 2

        def front(i):
            x = xb[i % 3]
            hb = hbs[i % 2]
            idx = idxs[i % 2]
            gate = gates[i % 2]
            yield
            cx.dma("sp", x[:], Xin.t[i * 128:(i + 1) * 128, :], rd=[Xin], wr=[x])
            yield
            emit_norm_tile(cx, x, gs2, sh2, hb, sq, st2, idb, ptr, hTi[:], hTi)
            for nb in range(4):
                p = pq[nb % 2]
                for k in range(8):
                    yield
                    cx.M(lambda h: h.matmul(p[:], lhsT=hTi[:, k, :], rhs=wq[:, k, nb * 512:(nb + 1) * 512], start=(k == 0), stop=(k == 7)), rd=[hTi, wq], wr=[p])
                yield
                cx.A(lambda h: h.activation(out=qb[:, nb * 512:(nb + 1) * 512], in_=p[:], func=AF.Copy), rd=[p], wr=[qb])
            yield
            cx.V(lambda h: h.tensor_tensor(out=sqq_ap, in0=qb[:], in1=qb[:], op=ALU.mult), rd=[qb], wr=[sqq])
            yield
            cx.V(lambda h: h.tensor_reduce(out=rs[:, 0:16], in_=S1.t[:], axis=AX.X, op=ALU.add), rd=[sqq], wr=[rs])
            yield
            cx.A(lambda h: h.activation(out=rs[:, 16:32], in_=rs[:, 0:16], func=AF.Sqrt, scale=1.0 / 128, bias=eps[:, 0:1]), rd=[rs, eps], wr=[rs])
            yield
            cx.V(lambda h: h.reciprocal(out=rs[:, 32:48], in_=rs[:, 16:32]), rd=[rs], wr=[rs])
            for r in range(2):
                for j in range(8):
                    g_ = r * 8 + j
                    yield
                    cx.M(lambda h: h.transpose(out=ptr[:, j, :], in_=qb[:, g_ * 128:(g_ + 1) * 128], identity=idb[:]), rd=[qb, idb], wr=[ptr])
                yield
                cx.A(lambda h: h.activation(out=qT[:, r * 8:(r + 1) * 8, :], in_=ptr[:], func=AF.Copy), rd=[ptr], wr=[qT])
            for r in range(4):
                ps_ = psc[r % 2]
                for j in range(4):
                    hp = r * 4 + j
                    yield
                    cx.M(lambda h: h.matmul(ps_[:, j, :], lhsT=qT[:, hp, :], rhs=keysT[:, hp % 2, :], start=True, stop=True), rd=[qT, keysT], wr=[ps_])
                yield
                cx.V(lambda h: h.tensor_tensor(out=S1[:, r * 4:(r + 1) * 4, :], in0=ps_[:], in1=rs[:, 32 + r * 4:32 + (r + 1) * 4].unsqueeze(2).broadcast_to([128, 4, 128]), op=ALU.mult),
                     rd=[ps_, rs], wr=[S1])
            for hp in range(16):
                w_ = wk[hp % 2]
                yield
                cx.V(lambda h: h.max(out=m[:, hp, 0:8], in_=S1[:, hp, :]), rd=[S1], wr=[m])
                yield
                cx.V(lambda h: h.max_index(out=ix[:, hp, 0:8], in_max=m[:, hp, 0:8], in_values=S1[:, hp, :]), rd=[m, S1], wr=[ix])
                yield
                cx.V(lambda h: h.match_replace(out=w_[:], in_to_replace=m[:, hp, 0:8], in_values=S1[:, hp, :], imm_value=-1e30), rd=[m, S1], wr=[w_])
                yield
                cx.V(lambda h: h.max(out=m[:, hp, 8:16], in_=w_[:]), rd=[w_], wr=[m])
                yield
                cx.V(lambda h: h.max_index(out=ix[:, hp, 8:16], in_max=m[:, hp, 8:16], in_values=w_[:]), rd=[m, w_], wr=[ix])
            mv = m[:].rearrange("p (h t) k -> p h t k", t=2)
            yield
            cx.V(lambda h: h.tensor_tensor(out=CS[:].rearrange("p h (a b) -> p h a b", a=16), in0=mv[:, :, 0, :].unsqueeze(3).broadcast_to([128, 8, 16, 16]),
                                           in1=mv[:, :, 1, :].unsqueeze(2).broadcast_to([128, 8, 16, 16]), op=ALU.add), rd=[m], wr=[CS])
            for hh in range(8):
                w_ = wk2[hh % 2]
                yield
                cx.V(lambda h: h.max(out=tops[:, hh, 0:8], in_=CS[:, hh, :]), rd=[CS], wr=[tops])
                yield
                cx.V(lambda h: h.max_index(out=pos[:, hh, 0:8], in_max=tops[:, hh, 0:8], in_values=CS[:, hh, :]), rd=[tops, CS], wr=[pos])
                yield
                cx.V(lambda h: h.match_replace(out=w_[:], in_to_replace=tops[:, hh, 0:8], in_values=CS[:, hh, :], imm_value=-1e30), rd=[tops, CS], wr=[w_])
                yield
                cx.V(lambda h: h.max(out=tops[:, hh, 8:16], in_=w_[:]), rd=[w_], wr=[tops])
                yield
                cx.V(lambda h: h.max_index(out=pos[:, hh, 8:16], in_max=tops[:, hh, 8:16], in_values=w_[:]), rd=[tops, w_], wr=[pos])
            yield
            cx.V(lambda h: h.tensor_copy(out=posf[:], in_=pos[:]), rd=[pos], wr=[posf])
            yield
            cx.V(lambda h: h.tensor_copy(out=ixf[:], in_=ix[:]), rd=[ix], wr=[ixf])
            bc4 = lambda ap3: ap3.unsqueeze(3).broadcast_to([128, 8, 16, 16])
            io4 = io16[:].unsqueeze(1).unsqueeze(1).broadcast_to([128, 8, 16, 16])
            th4 = th16[:].unsqueeze(1).unsqueeze(1).broadcast_to([128, 8, 16, 16])
            yield
            cx.V(lambda h: h.tensor_tensor(out=oh_ap, in0=bc4(posf[:]), in1=th4, op=ALU.is_ge), rd=[posf, th16], wr=[oh])
            yield
            cx.V(lambda h: h.tensor_reduce(out=af[:], in_=oh_ap, axis=AX.X, op=ALU.add), rd=[oh], wr=[af])
            yield
            cx.V(lambda h: h.tensor_scalar(out=af[:], in0=af[:], scalar1=-1.0, scalar2=None, op0=ALU.add), rd=[af], wr=[af])
            yield
            cx.V(lambda h: h.scalar_tensor_tensor(out=bf_[:], in0=af[:], scalar=-16.0, in1=posf[:], op0=ALU.mult, op1=ALU.add), rd=[af, posf], wr=[bf_])
            ixv = ixf[:].rearrange("p (h t) k -> p h t k", t=2)
            for t, src in ((0, af), (1, bf_)):
                yield
                cx.V(lambda h: h.tensor_tensor(out=oh_ap, in0=bc4(src[:]), in1=io4, op=ALU.is_equal), rd=[src, io16], wr=[oh])
                yield
                cx.V(lambda h: h.tensor_tensor(out=oh_ap, in0=oh_ap, in1=ixv[:, :, t, :].unsqueeze(2).broadcast_to([128, 8, 16, 16]), op=ALU.mult), rd=[oh, ixf], wr=[oh])
                yield
                cx.V(lambda h: h.tensor_reduce(out=i12[:, t, :].rearrange("p (h k) -> p h k", h=8), in_=oh_ap, axis=AX.X, op=ALU.add), rd=[oh], wr=[i12])
            yield
            cx.V(lambda h: h.scalar_tensor_tensor(out=idxf[:], in0=i12[:, 0, :], scalar=128.0, in1=i12[:, 1, :], op0=ALU.mult, op1=ALU.add), rd=[i12], wr=[idxf])
            yield
            cx.V(lambda h: h.tensor_copy(out=idx[:], in_=idxf[:]), rd=[idxf], wr=[idx])
            yield
            cx.V(lambda h: h.tensor_tensor(out=ge[:], in0=tops[:], in1=tops[:, :, 0:1].broadcast_to([128, 8, 16]), op=ALU.subtract), rd=[tops], wr=[ge])
            yield
            cx.A(lambda h: h.activation(out=ge[:], in_=ge[:], func=AF.Exp), rd=[ge], wr=[ge])
            yield
            cx.V(lambda h: h.tensor_reduce(out=gsum[:, 0:8], in_=ge[:], axis=AX.X, op=ALU.add), rd=[ge], wr=[gsum])
            yield
            cx.V(lambda h: h.reciprocal(out=gsum[:, 8:16], in_=gsum[:, 0:8]), rd=[gsum], wr=[gsum])
            yield
            cx.V(lambda h: h.tensor_tensor(out=gate[:].rearrange("p (h k) -> p h k", h=8), in0=ge[:], in1=gsum[:, 8:16].unsqueeze(2).broadcast_to([128, 8, 16]), op=ALU.mult),
                 rd=[ge, gsum], wr=[gate])


        PF = NG - 2
        NGRP = NT * 32
        fgens = {}

        def drain(i):
            g = fgens.pop(i, None)
            if g is not None:
                for _ in g:
                    pass

        fgens[0] = front(0)
        drain(0)
        for gn in range(NGRP + PF + 1):
            if gn < NGRP:
                i, sg = divmod(gn, 32)
                if sg == 0:
                    drain(i)
                idx = idxs[i % 2]
                bq_ = gn % NG
                for jj in range(4):
                    j = sg * 4 + jj
                    P.dma("pool", lambda h: h.indirect_dma_start(out=Gb[bq_][:, jj, :], out_offset=None, in_=TAB.t,
                                                                 in_offset=bass.IndirectOffsetOnAxis(ap=idx[:, j:j + 1], axis=0)),
                          _ks([idx, TAB]), [Gtok[bq_][jj]])
            gm = gn - PF
            if 0 <= gm < NGRP:
                i, sg = divmod(gm, 32)
                hb = hbs[i % 2]
                if sg == 0:
                    cx.V(lambda h: h.memset(act[:], 0.0), wr=actT)
                    if i + 1 < NT:
                        fgens[i + 1] = front(i + 1)
                fg = fgens.get(i + 1)
                b_ = gm % NG
                G_ = Gb[b_]
                for jj in range(4):
                    j = sg * 4 + jj
                    if jj in STT_SLOTS:
                        cx.V(lambda h: h.scalar_tensor_tensor(out=junkv[:], in0=G_[:, jj, 0:1024], scalar=1.0, in1=hb[:], op0=ALU.mult, op1=ALU.mult, accum_out=act[:, j:j + 1]),
                             rd=[Gtok[b_][jj], hb], wr=[junkv, actT[sg % 4]])
                    else:
                        pr = prods[j % NPR]
                        cx.V(lambda h: h.tensor_tensor(out=pr[:], in0=G_[:, jj, 0:1024], in1=hb[:], op=ALU.mult), rd=[Gtok[b_][jj], hb], wr=[pr])
                        cx.A(lambda h: h.activation(out=junk[:], in_=pr[:], func=AF.Copy, accum_out=act[:, j:j + 1]), rd=[pr], wr=[junk, actT[sg % 4]])
                    if fg is not None:
                        for _ in range(FSTEP):
                            next(fg, None)
                cs = slice(sg * 4, (sg + 1) * 4)
                cx.A(lambda h: h.activation(out=gl[:, cs], in_=act[:, cs], func=AF.Gelu), rd=[actT[sg % 4]], wr=[glT[sg % 4]])
            gc = gn - PF - 1
            if 0 <= gc < NGRP:
                i, sg = divmod(gc, 32)
                gate = gates[i % 2]
                x = xb[i % 3]
                b_ = gc % NG
                G_ = Gb[b_]
                dg = diag[sg % 2]
                cs = slice(sg * 4, (sg + 1) * 4)
                cx.V(lambda h: h.tensor_tensor(out=coef[:, cs], in0=gl[:, cs], in1=gate[:, cs], op=ALU.mult), rd=[glT[sg % 4], gate], wr=[coefT[sg % 4]])
                for jj in range(4):
                    j = sg * 4 + jj
                    if DIAG_ON_DVE:
                        cx.V(lambda h: h.tensor_scalar(out=dg[:, jj, :], in0=idf[:], scalar1=coef[:, j:j + 1], scalar2=None, op0=ALU.mult), rd=[idf, coefT[sg % 4]], wr=[dg])
                    else:
                        cx.A(lambda h: h.activation(out=dg[:, jj, :], in_=idf[:], func=AF.Copy, scale=coef[:, j:j + 1]), rd=[idf, coefT[sg % 4]], wr=[dg])
                for jj in range(4):
                    j = sg * 4 + jj
                    for hv in range(2):
                        cx.M(lambda h: h.matmul(po[:, hv * 512:(hv + 1) * 512], lhsT=dg[:, jj, :], rhs=G_[:, jj, 1024 + hv * 512:1024 + (hv + 1) * 512],
                                                start=(j == 0), stop=(j == 127)), rd=[dg, Gtok[b_][jj]], wr=[po])
                if sg == 31:
                    y = yb[0]
                    cx.V(lambda h: h.tensor_tensor(out=y[:], in0=po[:], in1=g2[:], op=ALU.mult), rd=[po, g2], wr=[y])
                    cx.V(lambda h: h.tensor_tensor(out=y[:], in0=y[:], in1=x[:], op=ALU.add), rd=[y, x], wr=[y])
                    if final:
                        cx.V(lambda h: h.memset(st3[:, 0:1], 0.0), wr=[st3])
                        cx.A(lambda h: h.activation(out=sq2[:], in_=y[:], func=AF.Square, accum_out=st3[:, 0:1]), rd=[y, st3], wr=[sq2, st3])
                        cx.A(lambda h: h.activation(out=st3[:, 1:2], in_=st3[:, 0:1], func=AF.Sqrt, scale=1.0 / D, bias=eps[:, 0:1]), rd=[st3, eps], wr=[st3])
                        cx.V(lambda h: h.reciprocal(out=st3[:, 2:3], in_=st3[:, 1:2]), rd=[st3], wr=[st3])
                        cx.V(lambda h: h.scalar_tensor_tensor(out=y[:], in0=y[:], scalar=st3[:, 2:3], in1=fg_[:], op0=ALU.mult, op1=ALU.mult), rd=[y, st3, fg_], wr=[y])
                    cx.dma("sp", Xout.t[i * 128:(i + 1) * 128, :], y[:], rd=[y], wr=[Xout])
        P.barrier()
        P.flush()


def phase_attn(P, nc, IN, MODS, li, Xin, Xout, SC):
    with contextlib.ExitStack() as st0:
        c0 = Ctx(P, nc, st0)
        idf, idb, eps = load_consts(c0, IN)
        bigT = c0.T([128, 8, S], BF16)
        qT = c0.T([128, 8, S], BF16)
        kT = c0.T([128, 2, S], BF16)
        V1 = c0.T([128, NT, 2, 130], BF16)
        with contextlib.ExitStack() as st1:
            c1 = Ctx(P, nc, st1)
            gs1, sh1 = load_mods(c1, MODS, li, [0, 1])
            emit_hT(c1, Xin, gs1, sh1, idb, bigT)
            P.barrier()
        with contextlib.ExitStack() as st2:
            cx = Ctx(P, nc, st2)
            w = cx.T([128, 8, 1536], BF16)
            with contextlib.ExitStack() as stw:
                cw_ = Ctx(P, nc, stw)
                stg = [cw_.T([128, 1536], F32) for _ in range(2)]
                for k in range(8):
                    cw_.dma("sp", stg[k % 2][:], IN["od_w_in"][k * 128:(k + 1) * 128, :], wr=[stg[k % 2]])
                    cw_.V(lambda h: h.tensor_copy(out=w[:, k, :], in_=stg[k % 2][:]), rd=[stg[k % 2]], wr=[w])
                P.barrier()
            csb = [cx.T([128, 2, 64], F32) for _ in range(2)]
            gq = cx.T([128, 10, 128], F32)
            g1_ = cx.T([128, 128], F32)
            g2_ = cx.T([128, 128], F32)
            cx.dma("sp", g1_[:], IN["od_qnorm_g"].broadcast_to([128, 128]), wr=[g1_])
            cx.dma("sp", g2_[:], IN["od_knorm_g"].broadcast_to([128, 128]), wr=[g2_])
            cx.V(lambda h: h.tensor_scalar(out=g1_[:], in0=g1_[:], scalar1=float(128 ** -0.5), scalar2=None, op0=ALU.mult), rd=[g1_], wr=[g1_])
            cx.V(lambda h: h.tensor_copy(out=gq[:, 0:8, :], in_=g1_[:].unsqueeze(1).broadcast_to([128, 8, 128])), rd=[g1_], wr=[gq])
            cx.V(lambda h: h.tensor_copy(out=gq[:, 8:10, :], in_=g2_[:].unsqueeze(1).broadcast_to([128, 2, 128])), rd=[g2_], wr=[gq])
            cx.V(lambda h: h.memset(V1[:], 1.0), wr=[V1])
            pz = [cx.PS([128, 512]) for _ in range(3)]
            ptq = cx.PS([128, 8, 128], BF16)
            ptk = cx.PS([128, 2, 128], BF16)
            rs = cx.T([128, 32], F32)
            qn = cx.T([128, 10, 128], F32)
            qr = cx.T([128, 10, 128], BF16)
            t1 = cx.T([128, 10, 64], F32)
            t2 = cx.T([128, 10, 64], F32)
            for i in range(NT):
                tsl = slice(i * 128, (i + 1) * 128)
                for nb in range(3):
                    for k in range(8):
                        cx.M(lambda h: h.matmul(pz[nb][:], lhsT=bigT[:, k, tsl], rhs=w[:, k, nb * 512:(nb + 1) * 512], start=(k == 0), stop=(k == 7)), rd=[bigT, w], wr=[pz[nb]])
                cx.A(lambda h: h.activation(out=V1[:, i, :, 0:128], in_=pz[2][:, 256:512].rearrange("p (g d) -> p g d", g=2), func=AF.Copy), rd=[pz[2]], wr=[V1])
                cs = csb[i % 2]
                cx.dma("act", cs[:], IN["ropecs"][:, :, i, :], wr=[cs])
                zsrc = ((pz[0], 0, 4, 512), (pz[1], 4, 8, 512), (pz[2], 8, 10, 256))
                for (pp, g0, g1x, wd) in zsrc:
                    cx.A(lambda h: h.activation(out=qn[:, g0:g1x, :], in_=pp[:, 0:wd].rearrange("p (g d) -> p g d", d=128), func=AF.Square), rd=[pp], wr=[qn])
                cx.V(lambda h: h.tensor_reduce(out=rs[:, 0:10], in_=qn[:], axis=AX.X, op=ALU.add), rd=[qn], wr=[rs])
                cx.A(lambda h: h.activation(out=rs[:, 10:20], in_=rs[:, 0:10], func=AF.Sqrt, scale=1.0 / 128, bias=eps[:, 0:1]), rd=[rs, eps], wr=[rs])
                cx.V(lambda h: h.reciprocal(out=rs[:, 20:30], in_=rs[:, 10:20]), rd=[rs], wr=[rs])
                for (pp, g0, g1x, wd) in zsrc:
                    cx.V(lambda h: h.tensor_tensor(out=qn[:, g0:g1x, :], in0=pp[:, 0:wd].rearrange("p (g d) -> p g d", d=128),
                                                   in1=rs[:, 20 + g0:20 + g1x].unsqueeze(2).broadcast_to([128, g1x - g0, 128]), op=ALU.mult), rd=[pp, rs], wr=[qn])
                cx.V(lambda h: h.tensor_tensor(out=qn[:], in0=qn[:], in1=gq[:], op=ALU.mult), rd=[qn, gq], wr=[qn])
                qv = qn[:].rearrange("p g (d t) -> p g d t", t=2)
                qo = qr[:].rearrange("p g (d t) -> p g d t", t=2)
                cc = cs[:, 0, :].unsqueeze(1).broadcast_to([128, 10, 64])
                ss_ = cs[:, 1, :].unsqueeze(1).broadcast_to([128, 10, 64])
                cx.V(lambda h: h.tensor_tensor(out=t1[:], in0=qv[:, :, :, 0], in1=cc, op=ALU.mult), rd=[qn, cs], wr=[t1])
                cx.V(lambda h: h.tensor_tensor(out=t2[:], in0=qv[:, :, :, 1], in1=ss_, op=ALU.mult), rd=[qn, cs], wr=[t2])
                cx.V(lambda h: h.tensor_tensor(out=qo[:, :, :, 0], in0=t1[:], in1=t2[:], op=ALU.subtract), rd=[t1, t2], wr=[qr])
                cx.V(lambda h: h.tensor_tensor(out=t1[:], in0=qv[:, :, :, 0], in1=ss_, op=ALU.mult), rd=[qn, cs, qr], wr=[t1])
                cx.V(lambda h: h.tensor_tensor(out=t2[:], in0=qv[:, :, :, 1], in1=cc, op=ALU.mult), rd=[qn, cs, qr], wr=[t2])
                cx.V(lambda h: h.tensor_tensor(out=qo[:, :, :, 1], in0=t1[:], in1=t2[:], op=ALU.add), rd=[t1, t2], wr=[qr])
                for g_ in range(8):
                    cx.M(lambda h: h.transpose(out=ptq[:, g_, :], in_=qr[:, g_, :], identity=idb[:]), rd=[qr, idb], wr=[ptq])
                for g_ in range(2):
                    cx.M(lambda h: h.transpose(out=ptk[:, g_, :], in_=qr[:, 8 + g_, :], identity=idb[:]), rd=[qr, idb], wr=[ptk])
                cx.A(lambda h: h.activation(out=qT[:, :, tsl], in_=ptq[:], func=AF.Copy), rd=[ptq], wr=[qT])
                cx.A(lambda h: h.activation(out=kT[:, :, tsl], in_=ptk[:], func=AF.Copy), rd=[ptk], wr=[kT])
            P.barrier()
        import os as _os
        if _os.environ.get("ATT_STOP") == "2":
            P.barrier()
            P.flush()
            return
        with contextlib.ExitStack() as st3:
            cx = Ctx(P, nc, st3)
            pss = [cx.PS([128, 512]) for _ in range(2)]
            pacc = [cx.PS([128, 512]) for _ in range(4)]
            pto = cx.PS([128, 8, 128], BF16)
            pT = [cx.T([128, 512], BF16) for _ in range(3)]
            ao = [cx.T([128, 8, 128], BF16) for _ in range(4)]
            rinv = cx.T([128, 8], F32)
            steps = [(qb_, hd_, sj) for qb_ in range(8) for hd_ in range(8) for sj in range(NT)]
            accs = pacc

            def emit_score(n):
                qb_, hd_, sj = steps[n]
                ps_ = pss[n % 2]
                cx.M(lambda h: h.matmul(ps_[:], lhsT=kT[:, hd_ // 4, sj * 128:(sj + 1) * 128], rhs=qT[:, hd_, qb_ * 512:(qb_ + 1) * 512], start=True, stop=True), rd=[kT, qT], wr=[ps_])

            bg = SC.get("BG1")
            if bg is not None:
                bg.attach(cx)
            emit_score(0)
            for n, (qb_, hd_, sj) in enumerate(steps):
                g_ = hd_ // 4
                if bg is not None and n % 32 == 0:
                    bg.step(1)
                if n + 1 < len(steps):
                    emit_score(n + 1)
                ps_ = pss[n % 2]
                pt_ = pT[n % 3]
                cx.A(lambda h: h.activation(out=pt_[:], in_=ps_[:], func=AF.Exp), rd=[ps_], wr=[pt_])
                for qs in range(4):
                    cx.M(lambda h: h.matmul(accs[qs][:, 0:129], lhsT=pt_[:, qs * 128:(qs + 1) * 128], rhs=V1[:, sj, g_, 0:129], start=(sj == 0), stop=(sj == NT - 1)),
                         rd=[pt_, V1], wr=[accs[qs]])
                if sj == NT - 1:
                    for qs in range(4):
                        a_ = accs[qs]
                        cx.V(lambda h: h.reciprocal(out=rinv[:, qs:qs + 1], in_=a_[:, 128:129]), rd=[a_], wr=[rinv])
                        cx.V(lambda h: h.tensor_scalar(out=ao[qs][:, hd_, :], in0=a_[:, 0:128], scalar1=rinv[:, qs:qs + 1], scalar2=None, op0=ALU.mult), rd=[a_, rinv], wr=[ao[qs]])
                    if hd_ == 7:
                        for qs in range(4):
                            ti = qb_ * 4 + qs
                            for k in range(8):
                                cx.M(lambda h: h.transpose(out=pto[:, k, :], in_=ao[qs][:, k, :], identity=idb[:]), rd=[ao[qs], idb], wr=[pto])
                            cx.V(lambda h: h.tensor_copy(out=bigT[:, :, ti * 128:(ti + 1) * 128], in_=pto[:]), rd=[pto], wr=[bigT])
            if bg is not None:
                bg.finish()
                SC.setdefault("TABDONE", {})[1] = True
            P.barrier()
        if _os.environ.get("ATT_STOP") == "3":
            P.barrier()
            P.flush()
            return
        with contextlib.ExitStack() as st4:
            cx = Ctx(P, nc, st4)
            g1, = load_mods(cx, MODS, li, [2])
            stage_f = cx.T([128, 1024], F32)
            emit_outproj(cx, Xin, Xout, lambda i: (bigT[:, :, i * 128:(i + 1) * 128], bigT), IN["od_w_out"], g1, stage_f, nx=3)
            P.barrier()
        P.flush()
```

```python
import contextlib
import numpy as np
import concourse.bass as bass
import concourse.mybir as mybir
from concourse.bass_utils import run_bass_kernel_spmd

F32 = mybir.dt.float32
BF16 = mybir.dt.bfloat16
I32 = mybir.dt.int32
U32 = mybir.dt.uint32
AF = mybir.ActivationFunctionType
ALU = mybir.AluOpType
AX = mybir.AxisListType

S = 4096
D = 1024
NT = S // 128
EPS = 1e-6


class Tok:
    __slots__ = ("w", "r", "name")

    def __init__(self, name=""):
        self.w = None
        self.r = {}
        self.name = name


class _Eng:
    def __init__(self, name, sem):
        self.name = name
        self.sem = sem
        self.cnt = 0
        self.seen = {}
        self.ops = []


NDMA = 12


class _Rec:
    def __getattr__(self, name):
        def f(*a, **k):
            return (name, a, k)
        return f


_REC = _Rec()


class Prog:
    ENG = ("pe", "act", "dve", "pool", "sp")

    def __init__(self, nc, stack):
        self.nc = nc
        self.stack = stack
        self.e = {}
        self.sems = {}
        for n in self.ENG:
            self.e[n] = _Eng(n, stack.enter_context(nc.semaphore("s_" + n)))
            self.sems[n] = (self.e[n].sem, 1)
        self.dslot = {}
        for q in ("sp", "pool", "act"):
            sl = []
            for j in range(NDMA):
                key = "d_%s%d" % (q, j)
                sem = stack.enter_context(nc.semaphore(key))
                self.sems[key] = (sem, 16)
                sl.append([key, 0])
            self.dslot[q] = [sl, 0]

    def _deps(self, e, rd, wr):
        deps = {}

        def need(dep, same_ok):
            if dep is None:
                return
            en, c = dep
            if en == e and (e == "pe" or not same_ok):
                return
            if deps.get(en, 0) < c:
                deps[en] = c

        for t in rd:
            need(t.w, True)
        for t in wr:
            need(t.w, False)
            for en, c in t.r.items():
                need((en, c), False)
        return deps

    def _emit_waits(self, E, deps):
        for en, c in deps.items():
            if E.seen.get(en, 0) < c:
                sem, step = self.sems[en]
                E.ops.append(lambda h, sem=sem, v=c * step: h.wait_ge(sem, v))
                E.seen[en] = c

    def op(self, e, fn, rd=(), wr=()):
        E = self.e[e]
        self._emit_waits(E, self._deps(e, rd, wr))
        E.cnt += 1
        sem = E.sem
        rec = fn(_REC)
        E.ops.append(lambda h, rec=rec, sem=sem: getattr(h, rec[0])(*rec[1], **rec[2]).then_inc(sem, 1))
        me = (e, E.cnt)
        for t in wr:
            t.w = me
            t.r = {}
        for t in rd:
            t.r[e] = E.cnt

    def dma(self, q, fn, rd=(), wr=()):
        E = self.e[q]
        slots, nxt = self.dslot[q]
        slot = slots[nxt % NDMA]
        self.dslot[q][1] = nxt + 1
        key = slot[0]
        deps = self._deps(key, rd, wr)
        if slot[1] > 0:
            deps[key] = max(deps.get(key, 0), slot[1])
        self._emit_waits(E, deps)
        slot[1] += 1
        sem = self.sems[key][0]
        rec = fn(_REC)
        E.ops.append(lambda h, rec=rec, sem=sem: getattr(h, rec[0])(*rec[1], **rec[2]).then_inc(sem, 16))
        me = (key, slot[1])
        for t in wr:
            t.w = me
            t.r = {}
        for t in rd:
            t.r[key] = slot[1]

    def barrier(self):
        tgt = {n: self.e[n].cnt for n in self.ENG}
        for q in self.dslot:
            for key, c in self.dslot[q][0]:
                tgt[key] = c
        for n in self.ENG:
            E = self.e[n]
            d = {k: v for k, v in tgt.items() if v > 0 and not (k == n and n == "pe")}
            self._emit_waits(E, d)

    def flush(self):
        nc = self.nc
        with nc.Block() as block:
            @block.tensor
            def _(h):
                for f in self.e["pe"].ops:
                    f(h)

            @block.scalar
            def _(h):
                for f in self.e["act"].ops:
                    f(h)

            @block.vector
            def _(h):
                for f in self.e["dve"].ops:
                    f(h)

            @block.gpsimd
            def _(h):
                for f in self.e["pool"].ops:
                    f(h)

            @block.sync
            def _(h):
                for f in self.e["sp"].ops:
                    f(h)
        for n in self.ENG:
            self.e[n].ops = []


class Buf:
    def __init__(self, t):
        self.t = t
        self.k = Tok()

    def __getitem__(self, i):
        return self.t[i]


def _ks(xs):
    return [x.k if isinstance(x, Buf) else x for x in xs]


_NAME = [0]


class Ctx:
    def __init__(self, P, nc, st):
        self.P, self.nc, self.st = P, nc, st
        self.n = 0

    def T(self, shape, dt, name=None):
        _NAME[0] += 1
        return Buf(self.st.enter_context(self.nc.sbuf_tensor("%s_%d" % (name or "t", _NAME[0]), list(shape), dt)))

    def PS(self, shape, dt=F32, name=None):
        _NAME[0] += 1
        return Buf(self.st.enter_context(self.nc.psum_tensor("%s_%d" % (name or "p", _NAME[0]), list(shape), dt)))

    def V(self, fn, rd=(), wr=()):
        self.P.op("dve", fn, _ks(rd), _ks(wr))

    def A(self, fn, rd=(), wr=()):
        self.P.op("act", fn, _ks(rd), _ks(wr))

    def G(self, fn, rd=(), wr=()):
        self.P.op("pool", fn, _ks(rd), _ks(wr))

    def M(self, fn, rd=(), wr=()):
        self.P.op("pe", fn, _ks(rd), _ks(wr))

    def dma(self, q, out, in_, rd=(), wr=()):
        self.P.dma(q, lambda h, out=out, in_=in_: h.dma_start(out=out, in_=in_), _ks(rd), _ks(wr))


def load_cast(cx, q, dst_bf, src_ap, stage, eng="pool"):
    cx.dma(q, stage.t[:] if not isinstance(stage, tuple) else stage[1], src_ap, wr=[stage if not isinstance(stage, tuple) else stage[0]])


def emit_norm_tile(cx, xt, gs, sh, hb, sq, st2, idb, ptr, hT_dst, hT_buf, hf=None):
    cx.V(lambda h: h.memset(st2[:, 0:1], 0.0), wr=[st2])
    cx.A(lambda h: h.activation(out=sq[:], in_=xt[:], func=AF.Square, accum_out=st2[:, 0:1]), rd=[xt, st2], wr=[sq, st2])
    cx.A(lambda h: h.activation(out=st2[:, 1:2], in_=st2[:, 0:1], func=AF.Sqrt, scale=1.0 / D, bias=EPS_AP[0][:, 0:1]), rd=[st2, EPS_AP[0]], wr=[st2])
    cx.V(lambda h: h.reciprocal(out=st2[:, 2:3], in_=st2[:, 1:2]), rd=[st2], wr=[st2])
    cx.V(lambda h: h.scalar_tensor_tensor(out=sq[:], in0=xt[:], scalar=st2[:, 2:3], in1=gs[:], op0=ALU.mult, op1=ALU.mult), rd=[xt, st2, gs], wr=[sq])
    if hf is not None:
        cx.V(lambda h: h.tensor_tensor(out=hf[:], in0=sq[:], in1=sh[:], op=ALU.add), rd=[sq, sh], wr=[hf])
        cx.G(lambda h: h.tensor_copy(out=hb[:], in_=hf[:]), rd=[hf], wr=[hb])
    else:
        cx.V(lambda h: h.tensor_tensor(out=hb[:], in0=sq[:], in1=sh[:], op=ALU.add), rd=[sq, sh], wr=[hb])
    for k in range(8):
        cx.M(lambda h, k=k: h.transpose(out=ptr[:, k, :], in_=hb[:, k * 128:(k + 1) * 128], identity=idb[:]), rd=[hb, idb], wr=[ptr])
    cx.A(lambda h: h.activation(out=hT_dst, in_=ptr[:], func=AF.Copy), rd=[ptr], wr=[hT_buf])


EPS_AP = [None]


def load_consts(cx, CN):
    idf = cx.T([128, 128], F32)
    idb = cx.T([128, 128], BF16)
    eps = cx.T([128, 1], F32)
    cx.dma("sp", idf[:], CN["ident"], wr=[idf])
    cx.V(lambda h: h.tensor_copy(out=idb[:], in_=idf[:]), rd=[idf], wr=[idb])
    cx.V(lambda h: h.memset(eps[:], EPS), wr=[eps])
    EPS_AP[0] = eps
    return idf, idb, eps


def phase_mods(P, nc, IN, MODS):
    with contextlib.ExitStack() as st:
        cx = Ctx(P, nc, st)
        cT = cx.T([128, 8], F32)
        cond = cx.T([128, 8], F32)
        crep = cx.T([128, 8, 128], F32)
        cx.dma("sp", cT[:], IN["cT"], wr=[cT])
        cx.A(lambda h: h.activation(out=cond[:], in_=cT[:], func=AF.Silu), rd=[cT], wr=[cond])
        cx.V(lambda h: h.tensor_copy(out=crep[:], in_=cond[:].unsqueeze(2).broadcast_to([128, 8, 128])), rd=[cond], wr=[crep])
        wb = [cx.T([128, 8, 512], F32) for _ in range(2)]
        ps = [cx.PS([128, 512]) for _ in range(2)]
        mod = cx.T([128, 6144], F32)
        ab = cx.T([128, 6144], F32)
        gm = cx.T([128, 1024], F32)
        gf = cx.T([128, 1024], F32)
        n = 0
        for i in range(2):
            cx.dma("act", ab[:], IN["ada_b"][i:i + 1, :].broadcast_to([128, 6144]), wr=[ab])
            cx.dma("act", gm[:], IN["norm_mix_g"][i:i + 1, :].broadcast_to([128, 1024]), wr=[gm])
            cx.dma("act", gf[:], IN["norm_ffn_g"][i:i + 1, :].broadcast_to([128, 1024]), wr=[gf])
            for nb in range(12):
                w = wb[n % 2]
                p = ps[n % 2]
                n += 1
                cx.dma("sp" if nb % 2 == 0 else "pool", w[:], IN["ada_w"][i, :, nb * 512:(nb + 1) * 512].rearrange("(k p) n -> p k n", p=128), wr=[w])
                for k in range(8):
                    cx.M(lambda h, k=k, w=w, p=p: h.matmul(p[:], lhsT=crep[:, k, :], rhs=w[:, k, :], start=(k == 0), stop=(k == 7)), rd=[crep, w], wr=[p])
                cx.V(lambda h, p=p, nb=nb: h.tensor_tensor(out=mod[:, nb * 512:(nb + 1) * 512], in0=p[:], in1=ab[:, nb * 512:(nb + 1) * 512], op=ALU.add), rd=[p, ab], wr=[mod])
            cx.V(lambda h: h.scalar_tensor_tensor(out=mod[:, 1024:2048], in0=mod[:, 1024:2048], scalar=1.0, in1=gm[:], op0=ALU.add, op1=ALU.mult), rd=[mod, gm], wr=[mod])
            cx.V(lambda h: h.scalar_tensor_tensor(out=mod[:, 4096:5120], in0=mod[:, 4096:5120], scalar=1.0, in1=gf[:], op0=ALU.add, op1=ALU.mult), rd=[mod, gf], wr=[mod])
            for j, off in enumerate([1024, 0, 2048, 4096, 3072, 5120]):
                cx.dma("sp", MODS.t[i, j], mod[:, off:off + 1024], rd=[mod], wr=[MODS])
        P.barrier()
        P.flush()


def load_mods(cx, MODS, i, js, q="act"):
    out = []
    for j in js:
        b = cx.T([128, 1024], F32)
        cx.dma(q, b[:], MODS.t[i, j], rd=[MODS], wr=[b])
        out.append(b)
    return out


def emit_hT(cx, Xin, gs, sh, idb, hT):
    xb = [cx.T([128, 1024], F32) for _ in range(2)]
    sq = cx.T([128, 1024], F32)
    hb = [cx.T([128, 1024], BF16) for _ in range(2)]
    st2 = [cx.T([128, 4], F32) for _ in range(2)]
    ptr = [cx.PS([128, 8, 128], BF16) for _ in range(2)]
    for i in range(NT):
        x = xb[i % 2]
        cx.dma("sp", x[:], Xin.t[i * 128:(i + 1) * 128, :], rd=[Xin], wr=[x])
        emit_norm_tile(cx, x, gs, sh, hb[i % 2], sq, st2[i % 2], idb, ptr[i % 2], hT[:, :, i * 128:(i + 1) * 128], hT)


def emit_outproj(cx, Xin, Xout, catT_src, w_ap, g1, stage_f, final=None, nx=4):
    wob = cx.T([128, 8, 1024], BF16)
    for k in range(8):
        cx.dma("pool", wob[:, k, :], w_ap[k * 128:(k + 1) * 128, :], wr=[wob])
    py = [cx.PS([128, 1024]) for _ in range(2)]
    NX = nx
    xb = [cx.T([128, 1024], F32) for _ in range(NX)]
    yb = [cx.T([128, 1024], F32) for _ in range(2)]
    PD = 2
    srcs = {}
    for it in range(NT + PD):
        if it < NT:
            srcs[it] = catT_src(it)
            cx.dma("act", xb[it % NX][:], Xin.t[it * 128:(it + 1) * 128, :], rd=[Xin], wr=[xb[it % NX]])
        i = it - PD
        if i < 0:
            continue
        ap, tok = srcs.pop(i)
        p = py[i % 2]
        x = xb[i % NX]
        y = yb[i % 2]
        for nb in range(2):
            for k in range(8):
                cx.M(lambda h, k=k, nb=nb, p=p, ap=ap: h.matmul(p[:, nb * 512:(nb + 1) * 512], lhsT=ap[:, k, :], rhs=wob[:, k, nb * 512:(nb + 1) * 512],
                                                               start=(k == 0), stop=(k == 7)), rd=[tok, wob], wr=[p])
        cx.V(lambda h, p=p, y=y: h.tensor_tensor(out=y[:], in0=p[:], in1=g1[:], op=ALU.mult), rd=[p, g1], wr=[y])
        cx.G(lambda h, x=x, y=y: h.tensor_tensor(out=y[:], in0=y[:], in1=x[:], op=ALU.add), rd=[y, x], wr=[y])
        cx.dma("sp", Xout.t[i * 128:(i + 1) * 128, :], y[:], rd=[y], wr=[Xout])


def phase_even(P, nc, IN, MODS, li, Xin, Xout, SC):
    CATT, V1D, SIGD, HFD = SC["CATT"], SC["V1D"], SC["SIGD"], SC["HFD"]
    with contextlib.ExitStack() as st0:
        c0 = Ctx(P, nc, st0)
        idf, idb, eps = load_consts(c0, IN)
        QK = c0.T([128, 8, S], BF16)
        GT = c0.T([128, NT, 16], F32)
        EB = c0.T([128, NT, 8], F32)
        ES = c0.T([128, NT, 8], F32)
        EE = c0.T([128, NT, 8], F32)
        with contextlib.ExitStack() as st1:
            cx1 = Ctx(P, nc, st1)
            hT = cx1.T([128, 8, S], BF16)
            with contextlib.ExitStack() as st2:
                c2 = Ctx(P, nc, st2)
                gs1, sh1 = load_mods(c2, MODS, li, [0, 1])
                emit_hT(c2, Xin, gs1, sh1, idb, hT)
                P.barrier()
            with contextlib.ExitStack() as stf:
                cx = Ctx(P, nc, stf)
                bT = cx.T([128, 20], F32)
                cw = cx.T([128, 8, 5], F32)
                cb = cx.T([128, 8], F32)
                edge = cx.T([128, 4, 32], F32)
                cx.dma("sp", bT[:], IN["ev_b_inT"], wr=[bT])
                cx.dma("sp", cw[:], IN["ev_conv_wT"], wr=[cw])
                cx.dma("sp", cb[:], IN["ev_conv_bT"], wr=[cb])
                cx.dma("sp", edge[:], IN["pooledge"].broadcast_to([128, 4, 32]), wr=[edge])
                wst = [cx.T([128, 8, 128], F32) for _ in range(2)]
                wcb = [cx.T([128, 8, 128], BF16) for _ in range(2)]
                zc = cx.T([128, S + 16], F32)
                pa = cx.T([128, S + 16], F32)
                yb = cx.T([128, S + 16], F32)
                ybf = cx.T([128, S], BF16)
                yo = [cx.T([128, 512], BF16) for _ in range(2)]
                pw = cx.T([128, 128], F32)
                psc = cx.T([128, 128], F32)
                pwb = cx.T([128, 128], BF16)
                pz = [cx.PS([128, 512]) for _ in range(2)]
                cx.V(lambda h: h.memset(zc[:], 0.0), wr=[zc])
                nps = 0
                for c in range(12):
                    col0 = c * 128 if c < 8 else 2048 + (c - 8) * 128
                    bcol = c if c < 8 else 16 + (c - 8)
                    ws, wc = wst[c % 2], wcb[c % 2]
                    cx.dma("pool", wc[:], IN["ev_w_in"][:, col0:col0 + 128].rearrange("(k p) n -> p k n", p=128), wr=[wc])
                    for tb in range(8):
                        p = pz[nps % 2]
                        nps += 1
                        for k in range(8):
                            cx.M(lambda h, k=k, p=p, wc=wc, tb=tb: h.matmul(p[:], lhsT=wc[:, k, :], rhs=hT[:, k, tb * 512:(tb + 1) * 512], start=(k == 0), stop=(k == 7)),
                                 rd=[wc, hT], wr=[p])
                        cx.A(lambda h, p=p, tb=tb, bcol=bcol: h.activation(out=zc[:, 8 + tb * 512:8 + (tb + 1) * 512], in_=p[:], func=AF.Identity, bias=bT[:, bcol:bcol + 1]),
                             rd=[p, bT], wr=[zc])
                    if c < 8:
                        cx.V(lambda h, c=c: h.tensor_scalar(out=yb[:, 0:S], in0=zc[:, 6:6 + S], scalar1=cw[:, c, 0:1], scalar2=None, op0=ALU.mult), rd=[zc, cw], wr=[yb])
                        for j in range(1, 5):
                            cx.V(lambda h, c=c, j=j: h.scalar_tensor_tensor(out=yb[:, 0:S], in0=zc[:, 6 + j:6 + j + S], scalar=cw[:, c, j:j + 1], in1=yb[:, 0:S], op0=ALU.mult, op1=ALU.add),
                                 rd=[zc, cw, yb], wr=[yb])
                        cx.A(lambda h, c=c: h.activation(out=QK[:, c, :], in_=yb[:, 0:S], func=AF.Silu, bias=cb[:, c:c + 1]), rd=[yb, cb], wr=[QK])
                    else:
                        g = c - 8
                        win = (2, 4, 8, 16)[g]
                        half = win // 2
                        n_el = S + 15
                        cur = zc
                        bufs = [pa, yb]
                        bi = 0
                        step = 1
                        while step < win:
                            d = bufs[bi % 2]
                            bi += 1
                            cx.V(lambda h, cur=cur, d=d, step=step, n_el=n_el: h.tensor_tensor(out=d[:, 0:n_el - step + 1], in0=cur[:, 0:n_el - step + 1], in1=cur[:, step:n_el + 1], op=ALU.add),
                                 rd=[cur], wr=[d])
                            n_el = n_el - step
                            cur = d
                            step *= 2
                        o = bufs[bi % 2]
                        cx.V(lambda h, cur=cur, half=half, o=o, win=win: h.tensor_scalar(out=o[:, 0:S], in0=cur[:, 8 - half:8 - half + S], scalar1=1.0 / win, scalar2=None, op0=ALU.mult), rd=[cur], wr=[o])
                        cx.V(lambda h, o=o, g=g: h.tensor_tensor(out=o[:, 0:16], in0=o[:, 0:16], in1=edge[:, g, 0:16], op=ALU.mult), rd=[o, edge], wr=[o])
                        cx.V(lambda h, o=o, g=g: h.tensor_tensor(out=o[:, S - 16:S], in0=o[:, S - 16:S], in1=edge[:, g, 16:32], op=ALU.mult), rd=[o, edge], wr=[o])
                        cx.V(lambda h, o=o: h.tensor_tensor(out=ybf[:], in0=o[:, 0:S], in1=zc[:, 8:8 + S], op=ALU.subtract), rd=[o, zc], wr=[ybf])
                        cx.dma("sp", pw[:], IN["ev_pool_w"][g], wr=[pw])
                        cx.dma("sp", psc[:], IN["ev_pool_scale"][0:1, g * 128:(g + 1) * 128].broadcast_to([128, 128]), wr=[psc])
                        cx.V(lambda h: h.tensor_tensor(out=pwb[:], in0=pw[:], in1=psc[:], op=ALU.mult), rd=[pw, psc], wr=[pwb])
                        for tb in range(8):
                            p = pz[nps % 2]
                            y_ = yo[nps % 2]
                            nps += 1
                            cx.M(lambda h, p=p, tb=tb: h.matmul(p[:], lhsT=pwb[:], rhs=ybf[:, tb * 512:(tb + 1) * 512], start=True, stop=True), rd=[pwb, ybf], wr=[p])
                            cx.A(lambda h, p=p, y_=y_: h.activation(out=y_[:], in_=p[:], func=AF.Copy), rd=[p], wr=[y_])
                            cx.dma("sp", CATT.t[tb * 4:(tb + 1) * 4, :, 4 + g, :].rearrange("n f t -> f n t"), y_[:].rearrange("f (n t) -> f n t", n=4), rd=[y_], wr=[CATT])
                P.barrier()
            with contextlib.ExitStack() as stt:
                cx = Ctx(P, nc, stt)
                stage_f = cx.T([128, 1024], F32)
                wtm = cx.T([128, 8, 1040], BF16)
                for k in range(8):
                    cx.dma("pool", wtm[:, k, 0:1024], IN["ev_w_in"][k * 128:(k + 1) * 128, 1024:2048], wr=[wtm])
                cx.dma("pool", wtm[:, :, 1024:1040], IN["ev_w_in"][:, 2560:2576].rearrange("(k p) n -> p k n", p=128), wr=[wtm])
                bvo = cx.T([128, 1024], F32)
                bg = cx.T([128, 16], F32)
                cx.dma("sp", bvo[:], IN["ev_b_in"][0:1, 1024:2048].broadcast_to([128, 1024]), wr=[bvo])
                cx.dma("sp", bg[:], IN["ev_b_in"][0:1, 2560:2576].broadcast_to([128, 16]), wr=[bg])
                pv = [cx.PS([128, 512]) for _ in range(2)]
                po = [cx.PS([128, 512]) for _ in range(2)]
                pg = [cx.PS([128, 16]) for _ in range(2)]
                v1 = [cx.T([128, 4, 130], BF16) for _ in range(2)]
                of = [cx.T([128, 512], F32) for _ in range(2)]
                ob = [cx.T([128, 512], BF16) for _ in range(2)]
                for b_ in v1:
                    cx.V(lambda h, b_=b_: h.memset(b_[:], 1.0), wr=[b_])
                for i in range(NT):
                    a = i % 2
                    for (p, c0_, c1_) in ((pv[a], 0, 512), (po[a], 512, 1024), (pg[a], 1024, 1040)):
                        for k in range(8):
                            cx.M(lambda h, k=k, p=p, c0_=c0_, c1_=c1_, i=i: h.matmul(p[:], lhsT=hT[:, k, i * 128:(i + 1) * 128], rhs=wtm[:, k, c0_:c1_], start=(k == 0), stop=(k == 7)),
                                 rd=[hT, wtm], wr=[p])
                    for hh in range(4):
                        cx.V(lambda h, a=a, hh=hh: h.tensor_tensor(out=v1[a][:, hh, 0:128], in0=pv[a][:, hh * 128:(hh + 1) * 128],
                                                                   in1=bvo[:, hh * 128:(hh + 1) * 128], op=ALU.add), rd=[pv[a], bvo], wr=[v1[a]])
                    cx.dma("sp", V1D.t[i], v1[a][:], rd=[v1[a]], wr=[V1D])
                    cx.V(lambda h, a=a: h.tensor_tensor(out=of[a][:], in0=po[a][:], in1=bvo[:, 512:1024], op=ALU.add), rd=[po[a], bvo], wr=[of[a]])
                    cx.A(lambda h, a=a: h.activation(out=ob[a][:], in_=of[a][:], func=AF.Sigmoid), rd=[of[a]], wr=[ob[a]])
                    if i == 0:
                        cx.dma("sp", SC["DBG2"].t, of[a][:], rd=[of[a]], wr=[SC["DBG2"]])
                    cx.dma("sp", SIGD.t[i], ob[a][:], rd=[ob[a]], wr=[SIGD])
                    cx.V(lambda h, a=a, i=i: h.tensor_tensor(out=GT[:, i, :], in0=pg[a][:], in1=bg[:], op=ALU.add), rd=[pg[a], bg], wr=[GT])
                cx.dma("sp", SC["DBG1"].t, GT[:], rd=[GT], wr=[SC["DBG1"]])
                P.barrier()
            with contextlib.ExitStack() as stg:
                cx = Ctx(P, nc, stg)
                LF = cx.T([128, NT, 8], F32)
                t8 = cx.T([128, NT, 8], F32)
                BC = cx.T([128, NT, 16], F32)
                cx.A(lambda h: h.activation(out=t8[:], in_=GT[:, :, 8:16], func=AF.Exp, scale=-1.0), rd=[GT], wr=[t8])
                cx.V(lambda h: h.tensor_scalar(out=t8[:], in0=t8[:], scalar1=1.0, scalar2=None, op0=ALU.add), rd=[t8], wr=[t8])
                cx.A(lambda h: h.activation(out=LF[:], in_=t8[:], func=AF.Ln), rd=[t8], wr=[LF])
                cx.V(lambda h: h.tensor_scalar(out=LF[:], in0=LF[:], scalar1=-1.0, scalar2=None, op0=ALU.mult), rd=[LF], wr=[LF])
                triU = cx.T([128, 128], F32)
                triL = cx.T([128, 128], F32)
                ones = cx.T([128, 128], F32)
                cx.dma("sp", triU[:], IN["triU"], wr=[triU])
                cx.dma("sp", triL[:], IN["triL"], wr=[triL])
                cx.V(lambda h: h.memset(ones[:], 1.0), wr=[ones])
                pc = cx.PS([128, NT, 16])
                for i in range(NT):
                    cx.M(lambda h, i=i: h.matmul(pc[:, i, 0:4], lhsT=triU[:], rhs=LF[:, i, 0:4], start=True, stop=True), rd=[triU, LF], wr=[pc])
                    cx.M(lambda h, i=i: h.matmul(pc[:, i, 4:8], lhsT=triL[:], rhs=LF[:, i, 4:8], start=True, stop=True), rd=[triL, LF], wr=[pc])
                    cx.M(lambda h, i=i: h.matmul(pc[:, i, 8:16], lhsT=ones[:], rhs=LF[:, i, 0:8], start=True, stop=True), rd=[ones, LF], wr=[pc])
                cx.V(lambda h: h.tensor_copy(out=BC[:], in_=pc[:]), rd=[pc], wr=[BC])
                cx.A(lambda h: h.activation(out=EB[:], in_=BC[:, :, 0:8], func=AF.Exp), rd=[BC], wr=[EB])
                cx.A(lambda h: h.activation(out=EE[:], in_=BC[:, :, 8:16], func=AF.Exp), rd=[BC], wr=[EE])
                cx.V(lambda h: h.tensor_tensor(out=t8[:], in0=GT[:, :, 0:8], in1=BC[:, :, 0:8], op=ALU.subtract), rd=[GT, BC], wr=[t8])
                cx.V(lambda h: h.tensor_scalar(out=t8[:], in0=t8[:], scalar1=float(-0.5 * np.log(128.0)), scalar2=None, op0=ALU.add), rd=[t8], wr=[t8])
                cx.A(lambda h: h.activation(out=ES[:], in_=t8[:], func=AF.Exp), rd=[t8], wr=[ES])
                P.barrier()
        with contextlib.ExitStack() as st3:
            cx = Ctx(P, nc, st3)
            mk = []
            for nm in ("triU", "triL"):
                f = cx.T([128, 128], F32)
                cx.dma("sp", f[:], IN[nm], wr=[f])
                mk.append(f)
            Cst = cx.T([128, 8, 129], F32)
            Cb = cx.T([128, 8, 129], BF16)
            cx.V(lambda h: h.memset(Cst[:], 0.0), wr=[Cst])
            cx.V(lambda h: h.memset(Cb[:], 0.0), wr=[Cb])
            NV = 6
            v1 = [cx.T([128, 4, 130], BF16) for _ in range(NV)]
            psS = [cx.PS([128, 4, 128]) for _ in range(2)]
            psA = [cx.PS([128, 3, 129]) for _ in range(3)]
            ps_t = cx.PS([128, 8, 128], BF16)
            ps_h = cx.PS([128, 4, 128], BF16)
            AT = cx.T([128, 8, 128], BF16)
            ksb = cx.T([128, 8, 128], BF16)
            sm = cx.T([128, 8, 4], F32)
            hacc = cx.T([128, NT, 512], F32)
            NSG = 6
            sgl = [cx.T([128, 512], BF16) for _ in range(NSG)]
            sq = cx.T([128, 512], F32)
            st4 = [cx.T([128, 12], F32) for _ in range(2)]
            mg = cx.T([128, 512], F32)
            hmb = [cx.T([128, 512], BF16) for _ in range(2)]
            hmT = [cx.T([128, 4, 128], BF16) for _ in range(2)]
            cx.dma("sp", mg[:], IN["ev_mnorm_g"][0:1, :].broadcast_to([128, 512]), wr=[mg])
            bg = SC.get("BG0")
            if bg is not None:
                bg.attach(cx)
            chains = [(d, hh) for d in range(2) for hh in range(4)]

            def aslot(c):
                return psA[c // 3], c % 3

            def tile_of(s, d):
                return s if d == 0 else NT - 1 - s

            PDV = 2
            nfin = 0
            for s in range(NT + PDV):
                if s < NT:
                    for d in range(2):
                        vb = v1[(2 * s + d) % NV]
                        cx.dma("sp", vb[:], V1D.t[tile_of(s, d)], rd=[V1D], wr=[vb])
                    if s >= NT // 2:
                        for d in range(2):
                            sg_ = sgl[(2 * s + d) % NSG]
                            cx.dma("act", sg_[:], SIGD.t[tile_of(s, d)], rd=[SIGD], wr=[sg_])
                s_ = s - PDV
                if s_ < 0:
                    continue
                s = s_
                if bg is not None:
                    bg.step(2)
                vbs = [v1[(2 * s + d) % NV] for d in range(2)]
                tls = [tile_of(s, d) for d in range(2)]
                for c, (d, hh) in enumerate(chains):
                    tsl = slice(tls[d] * 128, (tls[d] + 1) * 128)
                    cx.M(lambda h: h.matmul(psS[d][:, hh, :], lhsT=QK[:, 4 + hh, tsl], rhs=QK[:, hh, tsl], start=True, stop=True), rd=[QK], wr=[psS[d]])
                for c, (d, hh) in enumerate(chains):
                    col = d * 4 + hh
                    cx.V(lambda h: h.scalar_tensor_tensor(out=AT[:, c, :], in0=psS[d][:, hh, :], scalar=ES[:, tls[d], col:col + 1], in1=mk[d][:], op0=ALU.mult, op1=ALU.mult),
                         rd=[psS[d], ES, mk[d]], wr=[AT])
                for c, (d, hh) in enumerate(chains):
                    col = d * 4 + hh
                    tsl = slice(tls[d] * 128, (tls[d] + 1) * 128)
                    pa, sl = aslot(c)
                    cx.M(lambda h: h.matmul(pa[:, sl, :], lhsT=AT[:, c, :], rhs=vbs[d][:, hh, 0:129], start=True, stop=False), rd=[AT, vbs[d]], wr=[pa])
                    cx.M(lambda h: h.matmul(pa[:, sl, :], lhsT=QK[:, hh, tsl], rhs=Cb[:, col, :], start=False, stop=True), rd=[QK, Cb], wr=[pa])
                for c, (d, hh) in enumerate(chains):
                    col = d * 4 + hh
                    pa, sl = aslot(c)
                    cx.A(lambda h: h.activation(out=sm[:, c, 2:3], in_=pa[:, sl, 128:129], func=AF.Abs, scale=EB[:, tls[d], col:col + 1]), rd=[pa, EB], wr=[sm])
                cx.V(lambda h: h.tensor_scalar(out=sm[:, :, 0:1], in0=sm[:, :, 2:3], scalar1=1.0, scalar2=None, op0=ALU.max), rd=[sm], wr=[sm])
                cx.V(lambda h: h.reciprocal(out=sm[:, :, 3:4], in_=sm[:, :, 0:1]), rd=[sm], wr=[sm])
                for d in range(2):
                    cx.V(lambda h: h.tensor_tensor(out=sm[:, d * 4:(d + 1) * 4, 1:2], in0=EB[:, tls[d], d * 4:(d + 1) * 4].unsqueeze(2), in1=sm[:, d * 4:(d + 1) * 4, 3:4], op=ALU.mult),
                         rd=[sm, EB], wr=[sm])
                for c, (d, hh) in enumerate(chains):
                    pa, sl = aslot(c)
                    dst = hacc[:, tls[d], hh * 128:(hh + 1) * 128]
                    if s < NT // 2:
                        cx.A(lambda h: h.activation(out=dst, in_=pa[:, sl, 0:128], func=AF.Copy, scale=sm[:, c, 1:2]), rd=[pa, sm], wr=[hacc])
                    else:
                        cx.V(lambda h: h.scalar_tensor_tensor(out=dst, in0=pa[:, sl, 0:128], scalar=sm[:, c, 1:2], in1=dst, op0=ALU.mult, op1=ALU.add), rd=[pa, sm, hacc], wr=[hacc])
                for c, (d, hh) in enumerate(chains):
                    tsl = slice(tls[d] * 128, (tls[d] + 1) * 128)
                    cx.M(lambda h: h.transpose(out=ps_t[:, c, :], in_=QK[:, 4 + hh, tsl], identity=idb[:]), rd=[QK, idb], wr=[ps_t])
                for c, (d, hh) in enumerate(chains):
                    col = d * 4 + hh
                    cx.A(lambda h: h.activation(out=ksb[:, c, :], in_=ps_t[:, c, :], func=AF.Copy, scale=ES[:, tls[d], col:col + 1]), rd=[ps_t, ES], wr=[ksb])
                for c, (d, hh) in enumerate(chains):
                    pa, sl = aslot(c)
                    cx.M(lambda h: h.matmul(pa[:, sl, :], lhsT=ksb[:, c, :], rhs=vbs[d][:, hh, 0:129], start=True, stop=True), rd=[ksb, vbs[d]], wr=[pa])
                for c, (d, hh) in enumerate(chains):
                    col = d * 4 + hh
                    pa, sl = aslot(c)
                    cx.V(lambda h: h.tensor_scalar(out=Cst[:, col, :], in0=Cst[:, col, :], scalar1=EE[:, tls[d], col:col + 1], scalar2=None, op0=ALU.mult), rd=[Cst, EE], wr=[Cst])
                    cx.V(lambda h: h.scalar_tensor_tensor(out=Cst[:, col, :], in0=pa[:, sl, :], scalar=EE[:, tls[d], col:col + 1], in1=Cst[:, col, :], op0=ALU.mult, op1=ALU.add),
                         rd=[pa, EE, Cst], wr=[Cst])
                cx.A(lambda h: h.activation(out=Cb[:], in_=Cst[:], func=AF.Copy), rd=[Cst], wr=[Cb])
                if s >= NT // 2:
                    for d in range(2):
                        i = tls[d]
                        a = nfin % 2
                        nfin += 1
                        sg_ = sgl[(2 * s + d) % NSG]
                        hv = hacc[:, i, :]
                        cx.V(lambda h: h.tensor_tensor(out=sq[:], in0=hv, in1=hv, op=ALU.mult), rd=[hacc], wr=[sq])
                        cx.V(lambda h: h.tensor_reduce(out=st4[a][:, 0:4], in_=sq[:].rearrange("p (h d) -> p h d", h=4), axis=AX.X, op=ALU.add), rd=[sq], wr=[st4[a]])
                        cx.A(lambda h: h.activation(out=st4[a][:, 4:8], in_=st4[a][:, 0:4], func=AF.Sqrt, scale=1.0 / 128, bias=eps[:, 0:1]), rd=[st4[a], eps], wr=[st4[a]])
                        cx.V(lambda h: h.reciprocal(out=st4[a][:, 8:12], in_=st4[a][:, 4:8]), rd=[st4[a]], wr=[st4[a]])
                        cx.V(lambda h: h.tensor_tensor(out=hv.rearrange("p (h d) -> p h d", h=4), in0=hv.rearrange("p (h d) -> p h d", h=4),
                                                       in1=st4[a][:, 8:12].unsqueeze(2).broadcast_to([128, 4, 128]), op=ALU.mult), rd=[hacc, st4[a]], wr=[hacc])
                        cx.V(lambda h: h.tensor_tensor(out=sq[:], in0=mg[:], in1=sg_[:], op=ALU.mult), rd=[mg, sg_], wr=[sq])
                        cx.V(lambda h: h.tensor_tensor(out=hmb[a][:], in0=hv, in1=sq[:], op=ALU.mult), rd=[hacc, sq], wr=[hmb[a]])
                        for k in range(4):
                            cx.M(lambda h: h.transpose(out=ps_h[:, k, :], in_=hmb[a][:, k * 128:(k + 1) * 128], identity=idb[:]), rd=[hmb[a], idb], wr=[ps_h])
                        cx.A(lambda h: h.activation(out=hmT[a][:], in_=ps_h[:], func=AF.Copy), rd=[ps_h], wr=[hmT[a]])
                        cx.dma("act", CATT.t[i, :, 0:4, :], hmT[a][:], rd=[hmT[a]], wr=[CATT])
            if bg is not None:
                bg.finish()
                SC.setdefault("TABDONE", {})[0] = True
            P.barrier()
        with contextlib.ExitStack() as st4_:
            cx = Ctx(P, nc, st4_)
            cb_ = [cx.T([128, 8, 128], BF16) for _ in range(4)]
            g1, = load_mods(cx, MODS, li, [2])
            stage_f = cx.T([128, 1024], F32)

            def src(i):
                b = cb_[i % 4]
                cx.dma("pool", b[:], CATT.t[i], rd=[CATT], wr=[b])
                return b, b
            emit_outproj(cx, Xin, Xout, src, IN["ev_w_out"], g1, stage_f)
            P.barrier()
        P.flush()


W_SPECS = {
    "ada_w": [2, 1024, 6144], "ada_b": [2, 6144], "norm_mix_g": [2, 1024], "norm_ffn_g": [2, 1024],
    "ev_w_in": [1024, 2576], "ev_b_in": [1, 2576], "ev_b_inT": [128, 20], "ev_conv_wT": [128, 8, 5], "ev_conv_bT": [128, 8],
    "ev_mnorm_g": [1, 512], "ev_pool_w": [4, 128, 128], "ev_pool_scale": [1, 512], "ev_w_out": [1024, 1024],
    "od_w_in": [1024, 1536], "od_qnorm_g": [1, 128], "od_knorm_g": [1, 128], "od_w_out": [1024, 1024],
    "peer_w_q": [2, 1024, 2048], "peer_keys": [2, 2, 128, 128], "peer_u": [2, 16384, 1024], "peer_v": [2, 16384, 1024],
    "final_g": [1, 1024],
    "ident": [128, 128], "triU": [128, 128], "triL": [128, 128], "pooledge": [1, 4, 32], "ropecs": [128, 2, NT, 64], "iota16": [1, 16],
    "cT": [128, 8],
}


def host_consts():
    cn = {}
    cn["ident"] = np.eye(128, dtype=np.float32)
    s_ = np.arange(128)
    cn["triU"] = (s_[:, None] <= s_[None, :]).astype(np.float32)
    cn["triL"] = (s_[:, None] >= s_[None, :]).astype(np.float32)
    pe = np.zeros((1, 4, 32), np.float32)
    for g, w in enumerate((2, 4, 8, 16)):
        for j in range(32):
            t = j if j < 16 else S - 32 + j
            lo = max(t - w // 2, 0)
            hi = min(t + w // 2, S)
            pe[0, g, j] = w / float(hi - lo)
    cn["pooledge"] = pe
    t = np.arange(S)
    r, c = t // 64, t % 64
    freqs = (10000.0 ** (-np.arange(0, 64, 2, dtype=np.float32) / 64.0)).astype(np.float32)
    ang = np.concatenate([r[:, None].astype(np.float32) * freqs, c[:, None].astype(np.float32) * freqs], axis=-1).astype(np.float32)
    cs = np.stack([np.cos(ang), np.sin(ang)], 0).astype(np.float32)
    cn["ropecs"] = np.ascontiguousarray(cs.reshape(2, NT, 128, 64).transpose(2, 0, 1, 3))
    cn["iota16"] = np.arange(16, dtype=np.float32)[None, :]
    return cn


DEBUG = [False]


def build(first, last):
    nc = bass.Bass("TRN2", target_bir_lowering=False)
    IK = "ExternalOutput" if DEBUG[0] else "Internal"
    IN = {k: nc.dram_tensor(k, v, F32, kind="ExternalInput").ap() for k, v in W_SPECS.items()}
    xin = nc.dram_tensor("xin", [S, D], F32, kind="ExternalInput").ap()
    out = nc.dram_tensor("out", [S, D], F32, kind="ExternalOutput").ap()
    X = {}
    for k in range(0, 5):
        if k == first - 1:
            X[k] = Buf(xin)
        elif k == last:
            X[k] = Buf(out)
        elif first <= k < last:
            X[k] = Buf(nc.dram_tensor("X%d" % k, [S, D], F32, kind="Internal").ap())
    SC = {
        "CATT": Buf(nc.dram_tensor("CATT", [NT, 128, 8, 128], BF16, kind=IK).ap()),
        "V1D": Buf(nc.dram_tensor("V1D", [NT, 128, 4, 130], BF16, kind=IK).ap()),
        "SIGD": Buf(nc.dram_tensor("SIGD", [NT, 128, 512], BF16, kind=IK).ap()),
        "HFD": Buf(nc.dram_tensor("HFD", [NT, 128, 512], F32, kind=IK).ap()),
        "TAB": [Buf(nc.dram_tensor("TAB%d" % i, [16384, 2048], BF16, kind="Internal").ap()) for i in range(2)],
    }
    SC["DBG1"] = Buf(nc.dram_tensor("DBG1", [128, NT, 16], F32, kind=IK).ap())
    SC["DBG2"] = Buf(nc.dram_tensor("DBG2", [128, 512], F32, kind=IK).ap())
    MODS = Buf(nc.dram_tensor("MODS", [2, 6, 128, 1024], F32, kind=IK).ap())
    with contextlib.ExitStack() as st:
        P = Prog(nc, st)
        phase_mods(P, nc, IN, MODS)
        if first <= 1 and last >= 2:
            SC["BG0"] = TableBuilder(P, nc, IN, 0, SC["TAB"][0])
        if first <= 3 and last >= 4:
            SC["BG1"] = TableBuilder(P, nc, IN, 1, SC["TAB"][1])
        for ph in range(first, last + 1):
            if ph == 1:
                phase_even(P, nc, IN, MODS, 0, X[0], X[1], SC)
            elif ph == 2:
                phase_peer(P, nc, IN, MODS, 0, X[1], X[2], SC, final=False)
            elif ph == 3:
                phase_attn(P, nc, IN, MODS, 1, X[2], X[3], SC)
            elif ph == 4:
                phase_peer(P, nc, IN, MODS, 1, X[3], X[4], SC, final=True)
    return nc


def host_inputs(inputs):
    f = lambda a: np.ascontiguousarray(np.asarray(a, dtype=np.float32))
    sh = {}
    sh["ada_w"] = f(inputs["ada_w"]); sh["ada_b"] = f(inputs["ada_b"])
    sh["norm_mix_g"] = f(inputs["norm_mix_g"]); sh["norm_ffn_g"] = f(inputs["norm_ffn_g"])
    sh["ev_w_in"] = f(inputs["ev_w_in"][0]); sh["ev_b_in"] = f(inputs["ev_b_in"])
    sh["ev_b_inT"] = f(np.asarray(inputs["ev_b_in"])[0, :2560].reshape(20, 128).T)
    cw = np.asarray(inputs["ev_conv_w"])[0, :, 0, :]
    sh["ev_conv_wT"] = f(cw.T.reshape(8, 128, 5).transpose(1, 0, 2))
    sh["ev_conv_bT"] = f(np.asarray(inputs["ev_conv_b"])[0].reshape(8, 128).T)
    sh["ev_mnorm_g"] = f(inputs["ev_mnorm_g"]); sh["ev_pool_w"] = f(inputs["ev_pool_w"][0]); sh["ev_pool_scale"] = f(inputs["ev_pool_scale"])
    sh["ev_w_out"] = f(inputs["ev_w_out"][0])
    sh["od_w_in"] = f(inputs["od_w_in"][0]); sh["od_qnorm_g"] = f(inputs["od_qnorm_g"]); sh["od_knorm_g"] = f(inputs["od_knorm_g"])
    sh["od_w_out"] = f(inputs["od_w_out"][0])
    sh["peer_w_q"] = f(inputs["peer_w_q"]); sh["peer_keys"] = f(inputs["peer_keys"])
    sh["peer_u"] = f(inputs["peer_u"]); sh["peer_v"] = f(inputs["peer_v"])
    sh["final_g"] = f(np.asarray(inputs["final_g"])[None, :])
    sh.update(host_consts())
    return sh


def run_phases(inputs, first, last, xin_list, cores):
    nc = build(first, last)
    sh = host_inputs(inputs)
    c = np.asarray(inputs["c"], dtype=np.float32)
    in_maps = []
    for j, b in enumerate(cores):
        m = dict(sh)
        m["cT"] = np.ascontiguousarray(c[b].reshape(8, 128).T)
        m["xin"] = np.ascontiguousarray(xin_list[j], dtype=np.float32)
        in_maps.append(m)
    res = run_bass_kernel_spmd(nc, in_maps, core_ids=list(range(len(cores))))
    if DEBUG[0]:
        return res.results
    return [r["out"] for r in res.results]


def kernel(**inputs):
    x = np.asarray(inputs["x"], dtype=np.float32)
    outs = run_phases(inputs, 1, 4, [x[b] for b in range(8)], list(range(8)))
    return np.stack(outs, 0).astype(np.float32)


class TableBuilder:
    ROWS = 512

    def __init__(self, P, nc, IN, li, TAB):
        self.P, self.nc, self.IN, self.li, self.TAB = P, nc, IN, li, TAB
        nb = 16384 // self.ROWS
        self.blocks = [(half, blk) for blk in range(nb) for half in range(2)]
        self.k = 0
        self.cx = None

    def attach(self, cx):
        self.cx = cx

    def step(self, n=1):
        for _ in range(n):
            if self.k >= len(self.blocks):
                return
            half, blk = self.blocks[self.k]
            rows = slice(blk * self.ROWS, (blk + 1) * self.ROWS)
            self.cx.dma("pool", self.TAB.t[rows, half * 1024:(half + 1) * 1024], self.IN[("peer_u", "peer_v")[half]][self.li, rows, :], wr=[Tok()])
            self.k += 1
            self.last = True

    def finish(self):
        self.step(len(self.blocks))
        self.cx = None

    @property
    def done(self):
        return self.k >= len(self.blocks)


POOL_DOTS = False
PROD_DT = BF16
STT_SLOTS = ()
DIAG_ON_DVE = True


def phase_peer(P, nc, IN, MODS, li, Xin, Xout, SC, final):
    TAB = SC["TAB"][li]
    with contextlib.ExitStack() as st:
        cx = Ctx(P, nc, st)
        prebuilt = bool(SC.get("TABDONE", {}).get(li))
        sf = [cx.T([128, 4, 1024], F32) for _ in range(0 if prebuilt else 3)]
        sb = [cx.T([128, 4, 1024], BF16) for _ in range(0 if prebuilt else 3)]
        n = 0
        for half, nm in enumerate(("peer_u", "peer_v")):
            for blk in range(0 if prebuilt else 32):
                f, b = sf[n % 3], sb[n % 3]
                rows = slice(blk * 512, (blk + 1) * 512)
                cx.dma("sp", f[:], IN[nm][li, rows, :].rearrange("(n p) d -> p n d", p=128), wr=[f])
                if n % 3 == 0:
                    cx.A(lambda h: h.activation(out=b[:], in_=f[:], func=AF.Copy), rd=[f], wr=[b])
                elif n % 3 == 1:
                    cx.V(lambda h: h.tensor_copy(out=b[:], in_=f[:]), rd=[f], wr=[b])
                else:
                    cx.G(lambda h: h.tensor_copy(out=b[:], in_=f[:]), rd=[f], wr=[b])
                cx.dma("act", TAB.t[rows, half * 1024:(half + 1) * 1024].rearrange("(n p) d -> p n d", p=128), b[:], rd=[b], wr=[TAB])
                n += 1
        P.barrier()
        P.flush()
    with contextlib.ExitStack() as st:
        cx = Ctx(P, nc, st)
        idf, idb, eps = load_consts(cx, IN)
        gs2, sh2, g2 = load_mods(cx, MODS, li, [3, 4, 5])
        io16 = cx.T([128, 16], F32)
        th16 = cx.T([128, 16], F32)
        cx.dma("sp", io16[:], IN["iota16"].broadcast_to([128, 16]), wr=[io16])
        cx.V(lambda h: h.tensor_scalar(out=th16[:], in0=io16[:], scalar1=16.0, scalar2=None, op0=ALU.mult), rd=[io16], wr=[th16])
        if final:
            fg = cx.T([128, 1024], F32)
            cx.dma("sp", fg[:], IN["final_g"].broadcast_to([128, 1024]), wr=[fg])
        wq = cx.T([128, 8, 2048], BF16)
        ptr = cx.PS([128, 8, 128], BF16)
        keysT = cx.T([128, 2, 128], BF16)
        with contextlib.ExitStack() as stw:
            cw_ = Ctx(P, nc, stw)
            for k in range(8):
                cw_.dma("pool", wq[:, k, :], IN["peer_w_q"][li, k * 128:(k + 1) * 128, :], wr=[wq])
            kf = cw_.T([128, 2, 128], F32)
            kb = cw_.T([128, 2, 128], BF16)
            cw_.dma("sp", kf[:], IN["peer_keys"][li].rearrange("t n c -> n t c"), wr=[kf])
            cw_.V(lambda h: h.tensor_copy(out=kb[:], in_=kf[:]), rd=[kf], wr=[kb])
            for t in range(2):
                cw_.M(lambda h: h.transpose(out=ptr[:, t, :], in_=kb[:, t, :], identity=idb[:]), rd=[kb, idb], wr=[ptr])
            cw_.A(lambda h: h.activation(out=keysT[:], in_=ptr[:, 0:2, :], func=AF.Copy), rd=[ptr], wr=[keysT])
            P.barrier()
        pq = [cx.PS([128, 512]) for _ in range(2)]
        psc = [cx.PS([128, 4, 128]) for _ in range(2)]
        po = cx.PS([128, 1024])
        xb = [cx.T([128, 1024], F32) for _ in range(3)]
        sq = cx.T([128, 1024], F32)
        hb = cx.T([128, 1024], BF16)
        hTi = cx.T([128, 8, 128], BF16)
        st2 = cx.T([128, 4], F32)
        qb = cx.T([128, 2048], BF16)
        rs = cx.T([128, 48], F32)
        qT = cx.T([128, 16, 128], BF16)
        S1 = cx.T([128, 16, 128], F32)
        sqq = Buf(S1.t)
        sqq.k = S1.k
        sqq_ap = S1.t[:].rearrange("p g c -> p (g c)")
        wk = [cx.T([128, 128], F32) for _ in range(2)]
        m = cx.T([128, 16, 16], F32)
        ix = cx.T([128, 16, 16], U32)
        ixf = cx.T([128, 16, 16], F32)
        CS = cx.T([128, 8, 256], F32)
        wk2 = [cx.T([128, 256], F32) for _ in range(2)]
        tops = cx.T([128, 8, 16], F32)
        pos = cx.T([128, 8, 16], U32)
        posf = cx.T([128, 8, 16], F32)
        af = cx.T([128, 8, 16], F32)
        bf_ = cx.T([128, 8, 16], F32)
        oh = Buf(CS.t)
        oh.k = CS.k
        oh_ap = CS.t[:].rearrange("p h (a b) -> p h a b", a=16)
        i12 = cx.T([128, 2, 128], F32)
        idxf = cx.T([128, 128], F32)
        idx = cx.T([128, 128], I32)
        ge = cx.T([128, 8, 16], F32)
        gsum = cx.T([128, 16], F32)
        gate = cx.T([128, 128], F32)
        act = cx.T([128, 128], F32)
        gl = cx.T([128, 128], F32)
        coef = cx.T([128, 128], F32)
        NG = 5
        actT = [Tok() for _ in range(4)]
        glT = [Tok() for _ in range(4)]
        coefT = [Tok() for _ in range(4)]
        Gb = [cx.T([128, 4, 2048], BF16) for _ in range(NG)]
        Gtok = [[Tok() for _ in range(4)] for _ in range(NG)]
        diag = [cx.T([128, 4, 128], BF16) for _ in range(2)]
        junk = cx.T([128, 1024], BF16)
        NPR = 2
        junkv = cx.T([128, 1024], BF16) if STT_SLOTS else None
        prods = [cx.T([128, 1024], PROD_DT) for _ in range(NPR)]
        yb = [cx.T([128, 1024], F32) for _ in range(1)]
        sgn = 0
        hbs = [hb, cx.T([128, 1024], BF16)]
        idxs = [idx, cx.T([128, 128], I32)]
        gates = [gate, cx.T([128, 128], F32)]
        st3 = cx.T([128, 4], F32)
        sq2 = cx.T([128, 1024], F32) if final else None
        fg_ = fg if final else None
        FSTEP = 2

        def front(i):
            x = xb[i % 3]
            hb = hbs[i % 2]
            idx = idxs[i % 2]
            gate = gates[i % 2]
            yield
            cx.dma("sp", x[:], Xin.t[i * 128:(i + 1) * 128, :], rd=[Xin], wr=[x])
            yield
            emit_norm_tile(cx, x, gs2, sh2, hb, sq, st2, idb, ptr, hTi[:], hTi)
            for nb in range(4):
                p = pq[nb % 2]
                for k in range(8):
                    yield
                    cx.M(lambda h: h.matmul(p[:], lhsT=hTi[:, k, :], rhs=wq[:, k, nb * 512:(nb + 1) * 512], start=(k == 0), stop=(k == 7)), rd=[hTi, wq], wr=[p])
                yield
                cx.A(lambda h: h.activation(out=qb[:, nb * 512:(nb + 1) * 512], in_=p[:], func=AF.Copy), rd=[p], wr=[qb])
            yield
            cx.V(lambda h: h.tensor_tensor(out=sqq_ap, in0=qb[:], in1=qb[:], op=ALU.mult), rd=[qb], wr=[sqq])
            yield
            cx.V(lambda h: h.tensor_reduce(out=rs[:, 0:16], in_=S1.t[:], axis=AX.X, op=ALU.add), rd=[sqq], wr=[rs])
            yield
            cx.A(lambda h: h.activation(out=rs[:, 16:32], in_=rs[:, 0:16], func=AF.Sqrt, scale=1.0 / 128, bias=eps[:, 0:1]), rd=[rs, eps], wr=[rs])
            yield
            cx.V(lambda h: h.reciprocal(out=rs[:, 32:48], in_=rs[:, 16:32]), rd=[rs], wr=[rs])
            for r in range(2):
                for j in range(8):
                    g_ = r * 8 + j
                    yield
                    cx.M(lambda h: h.transpose(out=ptr[:, j, :], in_=qb[:, g_ * 128:(g_ + 1) * 128], identity=idb[:]), rd=[qb, idb], wr=[ptr])
                yield
                cx.A(lambda h: h.activation(out=qT[:, r * 8:(r + 1) * 8, :], in_=ptr[:], func=AF.Copy), rd=[ptr], wr=[qT])
            for r in range(4):
                ps_ = psc[r % 2]
                for j in range(4):
                    hp = r * 4 + j
                    yield
                    cx.M(lambda h: h.matmul(ps_[:, j, :], lhsT=qT[:, hp, :], rhs=keysT[:, hp % 2, :], start=True, stop=True), rd=[qT, keysT], wr=[ps_])
                yield
                cx.V(lambda h: h.tensor_tensor(out=S1[:, r * 4:(r + 1) * 4, :], in0=ps_[:], in1=rs[:, 32 + r * 4:32 + (r + 1) * 4].unsqueeze(2).broadcast_to([128, 4, 128]), op=ALU.mult),
                     rd=[ps_, rs], wr=[S1])
            for hp in range(16):
                w_ = wk[hp % 2]
                yield
                cx.V(lambda h: h.max(out=m[:, hp, 0:8], in_=S1[:, hp, :]), rd=[S1], wr=[m])
                yield
                cx.V(lambda h: h.max_index(out=ix[:, hp, 0:8], in_max=m[:, hp, 0:8], in_values=S1[:, hp, :]), rd=[m, S1], wr=[ix])
                yield
                cx.V(lambda h: h.match_replace(out=w_[:], in_to_replace=m[:, hp, 0:8], in_values=S1[:, hp, :], imm_value=-1e30), rd=[m, S1], wr=[w_])
                yield
                cx.V(lambda h: h.max(out=m[:, hp, 8:16], in_=w_[:]), rd=[w_], wr=[m])
                yield
                cx.V(lambda h: h.max_index(out=ix[:, hp, 8:16], in_max=m[:, hp, 8:16], in_values=w_[:]), rd=[m, w_], wr=[ix])
            mv = m[:].rearrange("p (h t) k -> p h t k", t=2)
            yield
            cx.V(lambda h: h.tensor_tensor(out=CS[:].rearrange("p h (a b) -> p h a b", a=16), in0=mv[:, :, 0, :].unsqueeze(3).broadcast_to([128, 8, 16, 16]),
                                           in1=mv[:, :, 1, :].unsqueeze(2).broadcast_to([128, 8, 16, 16]), op=ALU.add), rd=[m], wr=[CS])
            for hh in range(8):
                w_ = wk2[hh % 2]
                yield
                cx.V(lambda h: h.max(out=tops[:, hh, 0:8], in_=CS[:, hh, :]), rd=[CS], wr=[tops])
                yield
                cx.V(lambda h: h.max_index(out=pos[:, hh, 0:8], in_max=tops[:, hh, 0:8], in_values=CS[:, hh, :]), rd=[tops, CS], wr=[pos])
                yield
                cx.V(lambda h: h.match_replace(out=w_[:], in_to_replace=tops[:, hh, 0:8], in_values=CS[:, hh, :], imm_value=-1e30), rd=[tops, CS], wr=[w_])
                yield
                cx.V(lambda h: h.max(out=tops[:, hh, 8:16], in_=w_[:]), rd=[w_], wr=[tops])
                yield
                cx.V(lambda h: h.max_index(out=pos[:, hh, 8:16], in_max=tops[:, hh, 8:16], in_values=w_[:]), rd=[tops, w_], wr=[pos])
            yield
            cx.V(lambda h: h.tensor_copy(out=posf[:], in_=pos[:]), rd=[pos], wr=[posf])
            yield
            cx.V(lambda h: h.tensor_copy(out=ixf[:], in_=ix[:]), rd=[ix], wr=[ixf])
            bc4 = lambda ap3: ap3.unsqueeze(3).broadcast_to([128, 8, 16, 16])
            io4 = io16[:].unsqueeze(1).unsqueeze(1).broadcast_to([128, 8, 16, 16])
            th4 = th16[:].unsqueeze(1).unsqueeze(1).broadcast_to([128, 8, 16, 16])
            yield
            cx.V(lambda h: h.tensor_tensor(out=oh_ap, in0=bc4(posf[:]), in1=th4, op=ALU.is_ge), rd=[posf, th16], wr=[oh])
            yield
            cx.V(lambda h: h.tensor_reduce(out=af[:], in_=oh_ap, axis=AX.X, op=ALU.add), rd=[oh], wr=[af])
            yield
            cx.V(lambda h: h.tensor_scalar(out=af[:], in0=af[:], scalar1=-1.0, scalar2=None, op0=ALU.add), rd=[af], wr=[af])
            yield
            cx.V(lambda h: h.scalar_tensor_tensor(out=bf_[:], in0=af[:], scalar=-16.0, in1=posf[:], op0=ALU.mult, op1=ALU.add), rd=[af, posf], wr=[bf_])
            ixv = ixf[:].rearrange("p (h t) k -> p h t k", t=2)
            for t, src in ((0, af), (1, bf_)):
                yield
                cx.V(lambda h: h.tensor_tensor(out=oh_ap, in0=bc4(src[:]), in1=io4, op=ALU.is_equal), rd=[src, io16], wr=[oh])
                yield
                cx.V(lambda h: h.tensor_tensor(out=oh_ap, in0=oh_ap, in1=ixv[:, :, t, :].unsqueeze(2).broadcast_to([128, 8, 16, 16]), op=ALU.mult), rd=[oh, ixf], wr=[oh])
                yield
                cx.V(lambda h: h.tensor_reduce(out=i12[:, t, :].rearrange("p (h k) -> p h k", h=8), in_=oh_ap, axis=AX.X, op=ALU.add), rd=[oh], wr=[i12])
            yield
            cx.V(lambda h: h.scalar_tensor_tensor(out=idxf[:], in0=i12[:, 0, :], scalar=128.0, in1=i12[:, 1, :], op0=ALU.mult, op1=ALU.add), rd=[i12], wr=[idxf])
            yield
            cx.V(lambda h: h.tensor_copy(out=idx[:], in_=idxf[:]), rd=[idxf], wr=[idx])
            yield
            cx.V(lambda h: h.tensor_tensor(out=ge[:], in0=tops[:], in1=tops[:, :, 0:1].broadcast_to([128, 8, 16]), op=ALU.subtract), rd=[tops], wr=[ge])
            yield
            cx.A(lambda h: h.activation(out=ge[:], in_=ge[:], func=AF.Exp), rd=[ge], wr=[ge])
            yield
            cx.V(lambda h: h.tensor_reduce(out=gsum[:, 0:8], in_=ge[:], axis=AX.X, op=ALU.add), rd=[ge], wr=[gsum])
            yield
            cx.V(lambda h: h.reciprocal(out=gsum[:, 8:16], in_=gsum[:, 0:8]), rd=[gsum], wr=[gsum])
            yield
            cx.V(lambda h: h.tensor_tensor(out=gate[:].rearrange("p (h k) -> p h k", h=8), in0=ge[:], in1=gsum[:, 8:16].unsqueeze(2).broadcast_to([128, 8, 16]), op=ALU.mult),
                 rd=[ge, gsum], wr=[gate])


        PF = NG - 2
        NGRP = NT * 32
        fgens = {}

        def drain(i):
            g = fgens.pop(i, None)
            if g is not None:
                for _ in g:
                    pass

        fgens[0] = front(0)
        drain(0)
        for gn in range(NGRP + PF + 1):
            if gn < NGRP:
                i, sg = divmod(gn, 32)
                if sg == 0:
                    drain(i)
                idx = idxs[i % 2]
                bq_ = gn % NG
                for jj in range(4):
                    j = sg * 4 + jj
                    P.dma("pool", lambda h: h.indirect_dma_start(out=Gb[bq_][:, jj, :], out_offset=None, in_=TAB.t,
                                                                 in_offset=bass.IndirectOffsetOnAxis(ap=idx[:, j:j + 1], axis=0)),
                          _ks([idx, TAB]), [Gtok[bq_][jj]])
            gm = gn - PF
            if 0 <= gm < NGRP:
                i, sg = divmod(gm, 32)
                hb = hbs[i % 2]
                if sg == 0:
                    cx.V(lambda h: h.memset(act[:], 0.0), wr=actT)
                    if i + 1 < NT:
                        fgens[i + 1] = front(i + 1)
                fg = fgens.get(i + 1)
                b_ = gm % NG
                G_ = Gb[b_]
                for jj in range(4):
                    j = sg * 4 + jj
                    if jj in STT_SLOTS:
                        cx.V(lambda h: h.scalar_tensor_tensor(out=junkv[:], in0=G_[:, jj, 0:1024], scalar=1.0, in1=hb[:], op0=ALU.mult, op1=ALU.mult, accum_out=act[:, j:j + 1]),
                             rd=[Gtok[b_][jj], hb], wr=[junkv, actT[sg % 4]])
                    else:
                        pr = prods[j % NPR]
                        cx.V(lambda h: h.tensor_tensor(out=pr[:], in0=G_[:, jj, 0:1024], in1=hb[:], op=ALU.mult), rd=[Gtok[b_][jj], hb], wr=[pr])
                        cx.A(lambda h: h.activation(out=junk[:], in_=pr[:], func=AF.Copy, accum_out=act[:, j:j + 1]), rd=[pr], wr=[junk, actT[sg % 4]])
                    if fg is not None:
                        for _ in range(FSTEP):
                            next(fg, None)
                cs = slice(sg * 4, (sg + 1) * 4)
                cx.A(lambda h: h.activation(out=gl[:, cs], in_=act[:, cs], func=AF.Gelu), rd=[actT[sg % 4]], wr=[glT[sg % 4]])
            gc = gn - PF - 1
            if 0 <= gc < NGRP:
                i, sg = divmod(gc, 32)
                gate = gates[i % 2]
                x = xb[i % 3]
                b_ = gc % NG
                G_ = Gb[b_]
                dg = diag[sg % 2]
                cs = slice(sg * 4, (sg + 1) * 4)
                cx.V(lambda h: h.tensor_tensor(out=coef[:, cs], in0=gl[:, cs], in1=gate[:, cs], op=ALU.mult), rd=[glT[sg % 4], gate], wr=[coefT[sg % 4]])
                for jj in range(4):
                    j = sg * 4 + jj
                    if DIAG_ON_DVE:
                        cx.V(lambda h: h.tensor_scalar(out=dg[:, jj, :], in0=idf[:], scalar1=coef[:, j:j + 1], scalar2=None, op0=ALU.mult), rd=[idf, coefT[sg % 4]], wr=[dg])
                    else:
                        cx.A(lambda h: h.activation(out=dg[:, jj, :], in_=idf[:], func=AF.Copy, scale=coef[:, j:j + 1]), rd=[idf, coefT[sg % 4]], wr=[dg])
                for jj in range(4):
                    j = sg * 4 + jj
                    for hv in range(2):
                        cx.M(lambda h: h.matmul(po[:, hv * 512:(hv + 1) * 512], lhsT=dg[:, jj, :], rhs=G_[:, jj, 1024 + hv * 512:1024 + (hv + 1) * 512],
                                                start=(j == 0), stop=(j == 127)), rd=[dg, Gtok[b_][jj]], wr=[po])
                if sg == 31:
                    y = yb[0]
                    cx.V(lambda h: h.tensor_tensor(out=y[:], in0=po[:], in1=g2[:], op=ALU.mult), rd=[po, g2], wr=[y])
                    cx.V(lambda h: h.tensor_tensor(out=y[:], in0=y[:], in1=x[:], op=ALU.add), rd=[y, x], wr=[y])
                    if final:
                        cx.V(lambda h: h.memset(st3[:, 0:1], 0.0), wr=[st3])
                        cx.A(lambda h: h.activation(out=sq2[:], in_=y[:], func=AF.Square, accum_out=st3[:, 0:1]), rd=[y, st3], wr=[sq2, st3])
                        cx.A(lambda h: h.activation(out=st3[:, 1:2], in_=st3[:, 0:1], func=AF.Sqrt, scale=1.0 / D, bias=eps[:, 0:1]), rd=[st3, eps], wr=[st3])
                        cx.V(lambda h: h.reciprocal(out=st3[:, 2:3], in_=st3[:, 1:2]), rd=[st3], wr=[st3])
                        cx.V(lambda h: h.scalar_tensor_tensor(out=y[:], in0=y[:], scalar=st3[:, 2:3], in1=fg_[:], op0=ALU.mult, op1=ALU.mult), rd=[y, st3, fg_], wr=[y])
                    cx.dma("sp", Xout.t[i * 128:(i + 1) * 128, :], y[:], rd=[y], wr=[Xout])
        P.barrier()
        P.flush()


def phase_attn(P, nc, IN, MODS, li, Xin, Xout, SC):
    with contextlib.ExitStack() as st0:
        c0 = Ctx(P, nc, st0)
        idf, idb, eps = load_consts(c0, IN)
        bigT = c0.T([128, 8, S], BF16)
        qT = c0.T([128, 8, S], BF16)
        kT = c0.T([128, 2, S], BF16)
        V1 = c0.T([128, NT, 2, 130], BF16)
        with contextlib.ExitStack() as st1:
            c1 = Ctx(P, nc, st1)
            gs1, sh1 = load_mods(c1, MODS, li, [0, 1])
            emit_hT(c1, Xin, gs1, sh1, idb, bigT)
            P.barrier()
        with contextlib.ExitStack() as st2:
            cx = Ctx(P, nc, st2)
            w = cx.T([128, 8, 1536], BF16)
            with contextlib.ExitStack() as stw:
                cw_ = Ctx(P, nc, stw)
                for k in range(8):
                    cw_.dma("pool", w[:, k, :], IN["od_w_in"][k * 128:(k + 1) * 128, :], wr=[w])
                P.barrier()
            csb = [cx.T([128, 2, 64], F32) for _ in range(2)]
            gq = cx.T([128, 10, 128], F32)
            g1_ = cx.T([128, 128], F32)
            g2_ = cx.T([128, 128], F32)
            cx.dma("sp", g1_[:], IN["od_qnorm_g"].broadcast_to([128, 128]), wr=[g1_])
            cx.dma("sp", g2_[:], IN["od_knorm_g"].broadcast_to([128, 128]), wr=[g2_])
            cx.V(lambda h: h.tensor_scalar(out=g1_[:], in0=g1_[:], scalar1=float(128 ** -0.5), scalar2=None, op0=ALU.mult), rd=[g1_], wr=[g1_])
            cx.V(lambda h: h.tensor_copy(out=gq[:, 0:8, :], in_=g1_[:].unsqueeze(1).broadcast_to([128, 8, 128])), rd=[g1_], wr=[gq])
            cx.V(lambda h: h.tensor_copy(out=gq[:, 8:10, :], in_=g2_[:].unsqueeze(1).broadcast_to([128, 2, 128])), rd=[g2_], wr=[gq])
            cx.V(lambda h: h.memset(V1[:], 1.0), wr=[V1])
            pz = [cx.PS([128, 512]) for _ in range(3)]
            ptq = cx.PS([128, 8, 128], BF16)
            ptk = cx.PS([128, 2, 128], BF16)
            rs = cx.T([128, 32], F32)
            qn = cx.T([128, 10, 128], F32)
            qr = cx.T([128, 10, 128], BF16)
            t1 = cx.T([128, 10, 64], F32)
            t2 = cx.T([128, 10, 64], F32)
            for i in range(NT):
                tsl = slice(i * 128, (i + 1) * 128)
                for nb in range(3):
                    for k in range(8):
                        cx.M(lambda h: h.matmul(pz[nb][:], lhsT=bigT[:, k, tsl], rhs=w[:, k, nb * 512:(nb + 1) * 512], start=(k == 0), stop=(k == 7)), rd=[bigT, w], wr=[pz[nb]])
                cx.A(lambda h: h.activation(out=V1[:, i, :, 0:128], in_=pz[2][:, 256:512].rearrange("p (g d) -> p g d", g=2), func=AF.Copy), rd=[pz[2]], wr=[V1])
                cs = csb[i % 2]
                cx.dma("act", cs[:], IN["ropecs"][:, :, i, :], wr=[cs])
                zsrc = ((pz[0], 0, 4, 512), (pz[1], 4, 8, 512), (pz[2], 8, 10, 256))
                for (pp, g0, g1x, wd) in zsrc:
                    cx.A(lambda h: h.activation(out=qn[:, g0:g1x, :], in_=pp[:, 0:wd].rearrange("p (g d) -> p g d", d=128), func=AF.Square), rd=[pp], wr=[qn])
                cx.V(lambda h: h.tensor_reduce(out=rs[:, 0:10], in_=qn[:], axis=AX.X, op=ALU.add), rd=[qn], wr=[rs])
                cx.A(lambda h: h.activation(out=rs[:, 10:20], in_=rs[:, 0:10], func=AF.Sqrt, scale=1.0 / 128, bias=eps[:, 0:1]), rd=[rs, eps], wr=[rs])
                cx.V(lambda h: h.reciprocal(out=rs[:, 20:30], in_=rs[:, 10:20]), rd=[rs], wr=[rs])
                for (pp, g0, g1x, wd) in zsrc:
                    cx.V(lambda h: h.tensor_tensor(out=qn[:, g0:g1x, :], in0=pp[:, 0:wd].rearrange("p (g d) -> p g d", d=128),
                                                   in1=rs[:, 20 + g0:20 + g1x].unsqueeze(2).broadcast_to([128, g1x - g0, 128]), op=ALU.mult), rd=[pp, rs], wr=[qn])
                cx.V(lambda h: h.tensor_tensor(out=qn[:], in0=qn[:], in1=gq[:], op=ALU.mult), rd=[qn, gq], wr=[qn])
                qv = qn[:].rearrange("p g (d t) -> p g d t", t=2)
                qo = qr[:].rearrange("p g (d t) -> p g d t", t=2)
                cc = cs[:, 0, :].unsqueeze(1).broadcast_to([128, 10, 64])
                ss_ = cs[:, 1, :].unsqueeze(1).broadcast_to([128, 10, 64])
                cx.V(lambda h: h.tensor_tensor(out=t1[:], in0=qv[:, :, :, 0], in1=cc, op=ALU.mult), rd=[qn, cs], wr=[t1])
                cx.V(lambda h: h.tensor_tensor(out=t2[:], in0=qv[:, :, :, 1], in1=ss_, op=ALU.mult), rd=[qn, cs], wr=[t2])
                cx.V(lambda h: h.tensor_tensor(out=qo[:, :, :, 0], in0=t1[:], in1=t2[:], op=ALU.subtract), rd=[t1, t2], wr=[qr])
                cx.V(lambda h: h.tensor_tensor(out=t1[:], in0=qv[:, :, :, 0], in1=ss_, op=ALU.mult), rd=[qn, cs, qr], wr=[t1])
                cx.V(lambda h: h.tensor_tensor(out=t2[:], in0=qv[:, :, :, 1], in1=cc, op=ALU.mult), rd=[qn, cs, qr], wr=[t2])
                cx.V(lambda h: h.tensor_tensor(out=qo[:, :, :, 1], in0=t1[:], in1=t2[:], op=ALU.add), rd=[t1, t2], wr=[qr])
                for g_ in range(8):
                    cx.M(lambda h: h.transpose(out=ptq[:, g_, :], in_=qr[:, g_, :], identity=idb[:]), rd=[qr, idb], wr=[ptq])
                for g_ in range(2):
                    cx.M(lambda h: h.transpose(out=ptk[:, g_, :], in_=qr[:, 8 + g_, :], identity=idb[:]), rd=[qr, idb], wr=[ptk])
                cx.A(lambda h: h.activation(out=qT[:, :, tsl], in_=ptq[:], func=AF.Copy), rd=[ptq], wr=[qT])
                cx.A(lambda h: h.activation(out=kT[:, :, tsl], in_=ptk[:], func=AF.Copy), rd=[ptk], wr=[kT])
            P.barrier()
        import os as _os
        if _os.environ.get("ATT_STOP") == "2":
            P.barrier()
            P.flush()
            return
        with contextlib.ExitStack() as st3:
            cx = Ctx(P, nc, st3)
            pss = [cx.PS([128, 512]) for _ in range(2)]
            pacc = [cx.PS([128, 512]) for _ in range(4)]
            pto = cx.PS([128, 8, 128], BF16)
            pT = [cx.T([128, 512], BF16) for _ in range(3)]
            ao = [cx.T([128, 8, 128], BF16) for _ in range(4)]
            rinv = cx.T([128, 8], F32)
            steps = [(qb_, hd_, sj) for qb_ in range(8) for hd_ in range(8) for sj in range(NT)]
            accs = pacc

            def emit_score(n):
                qb_, hd_, sj = steps[n]
                ps_ = pss[n % 2]
                cx.M(lambda h: h.matmul(ps_[:], lhsT=kT[:, hd_ // 4, sj * 128:(sj + 1) * 128], rhs=qT[:, hd_, qb_ * 512:(qb_ + 1) * 512], start=True, stop=True), rd=[kT, qT], wr=[ps_])

            bg = SC.get("BG1")
            if bg is not None:
                bg.attach(cx)
            emit_score(0)
            for n, (qb_, hd_, sj) in enumerate(steps):
                g_ = hd_ // 4
                if bg is not None and n % 32 == 0:
                    bg.step(1)
                if n + 1 < len(steps):
                    emit_score(n + 1)
                ps_ = pss[n % 2]
                pt_ = pT[n % 3]
                cx.A(lambda h: h.activation(out=pt_[:], in_=ps_[:], func=AF.Exp), rd=[ps_], wr=[pt_])
                for qs in range(4):
                    cx.M(lambda h: h.matmul(accs[qs][:, 0:129], lhsT=pt_[:, qs * 128:(qs + 1) * 128], rhs=V1[:, sj, g_, 0:129], start=(sj == 0), stop=(sj == NT - 1)),
                         rd=[pt_, V1], wr=[accs[qs]])
                if sj == NT - 1:
                    for qs in range(4):
                        a_ = accs[qs]
                        cx.V(lambda h: h.reciprocal(out=rinv[:, qs:qs + 1], in_=a_[:, 128:129]), rd=[a_], wr=[rinv])
                        cx.V(lambda h: h.tensor_scalar(out=ao[qs][:, hd_, :], in0=a_[:, 0:128], scalar1=rinv[:, qs:qs + 1], scalar2=None, op0=ALU.mult), rd=[a_, rinv], wr=[ao[qs]])
                    if hd_ == 7:
                        for qs in range(4):
                            ti = qb_ * 4 + qs
                            for k in range(8):
                                cx.M(lambda h: h.transpose(out=pto[:, k, :], in_=ao[qs][:, k, :], identity=idb[:]), rd=[ao[qs], idb], wr=[pto])
                            cx.V(lambda h: h.tensor_copy(out=bigT[:, :, ti * 128:(ti + 1) * 128], in_=pto[:]), rd=[pto], wr=[bigT])
            if bg is not None:
                bg.finish()
                SC.setdefault("TABDONE", {})[1] = True
            P.barrier()
        if _os.environ.get("ATT_STOP") == "3":
            P.barrier()
            P.flush()
            return
        with contextlib.ExitStack() as st4:
            cx = Ctx(P, nc, st4)
            g1, = load_mods(cx, MODS, li, [2])
            stage_f = cx.T([128, 1024], F32)
            emit_outproj(cx, Xin, Xout, lambda i: (bigT[:, :, i * 128:(i + 1) * 128], bigT), IN["od_w_out"], g1, stage_f, nx=3)
            P.barrier()
        P.flush()
```

```python
import contextlib
import numpy as np
import concourse.bass as bass
import concourse.mybir as mybir
from concourse.bass_utils import run_bass_kernel_spmd

F32 = mybir.dt.float32
BF16 = mybir.dt.bfloat16
I32 = mybir.dt.int32
U32 = mybir.dt.uint32
AF = mybir.ActivationFunctionType
ALU = mybir.AluOpType
AX = mybir.AxisListType

S = 4096
D = 1024
NT = S // 128
EPS = 1e-6


class Tok:
    __slots__ = ("w", "r", "name")

    def __init__(self, name=""):
        self.w = None
        self.r = {}
        self.name = name


class _Eng:
    def __init__(self, name, sem):
        self.name = name
        self.sem = sem
        self.cnt = 0
        self.seen = {}
        self.ops = []


NDMA = 12


class _Rec:
    def __getattr__(self, name):
        def f(*a, **k):
            return (name, a, k)
        return f


_REC = _Rec()


class Prog:
    ENG = ("pe", "act", "dve", "pool", "sp")

    def __init__(self, nc, stack):
        self.nc = nc
        self.stack = stack
        self.e = {}
        self.sems = {}
        for n in self.ENG:
            self.e[n] = _Eng(n, stack.enter_context(nc.semaphore("s_" + n)))
            self.sems[n] = (self.e[n].sem, 1)
        self.dslot = {}
        for q in ("sp", "pool", "act"):
            sl = []
            for j in range(NDMA):
                key = "d_%s%d" % (q, j)
                sem = stack.enter_context(nc.semaphore(key))
                self.sems[key] = (sem, 16)
                sl.append([key, 0])
            self.dslot[q] = [sl, 0]

    def _deps(self, e, rd, wr):
        deps = {}

        def need(dep, same_ok):
            if dep is None:
                return
            en, c = dep
            if en == e and (e == "pe" or not same_ok):
                return
            if deps.get(en, 0) < c:
                deps[en] = c

        for t in rd:
            need(t.w, True)
        for t in wr:
            need(t.w, False)
            for en, c in t.r.items():
                need((en, c), False)
        return deps

    def _emit_waits(self, E, deps):
        for en, c in deps.items():
            if E.seen.get(en, 0) < c:
                sem, step = self.sems[en]
                E.ops.append(lambda h, sem=sem, v=c * step: h.wait_ge(sem, v))
                E.seen[en] = c

    def op(self, e, fn, rd=(), wr=()):
        E = self.e[e]
        self._emit_waits(E, self._deps(e, rd, wr))
        E.cnt += 1
        sem = E.sem
        rec = fn(_REC)
        E.ops.append(lambda h, rec=rec, sem=sem: getattr(h, rec[0])(*rec[1], **rec[2]).then_inc(sem, 1))
        me = (e, E.cnt)
        for t in wr:
            t.w = me
            t.r = {}
        for t in rd:
            t.r[e] = E.cnt

    def dma(self, q, fn, rd=(), wr=()):
        E = self.e[q]
        slots, nxt = self.dslot[q]
        slot = slots[nxt % NDMA]
        self.dslot[q][1] = nxt + 1
        key = slot[0]
        deps = self._deps(key, rd, wr)
        if slot[1] > 0:
            deps[key] = max(deps.get(key, 0), slot[1])
        self._emit_waits(E, deps)
        slot[1] += 1
        sem = self.sems[key][0]
        rec = fn(_REC)
        E.ops.append(lambda h, rec=rec, sem=sem: getattr(h, rec[0])(*rec[1], **rec[2]).then_inc(sem, 16))
        me = (key, slot[1])
        for t in wr:
            t.w = me
            t.r = {}
        for t in rd:
            t.r[key] = slot[1]

    def barrier(self):
        tgt = {n: self.e[n].cnt for n in self.ENG}
        for q in self.dslot:
            for key, c in self.dslot[q][0]:
                tgt[key] = c
        for n in self.ENG:
            E = self.e[n]
            d = {k: v for k, v in tgt.items() if v > 0 and not (k == n and n == "pe")}
            self._emit_waits(E, d)

    def flush(self):
        nc = self.nc
        with nc.Block() as block:
            @block.tensor
            def _(h):
                for f in self.e["pe"].ops:
                    f(h)

            @block.scalar
            def _(h):
                for f in self.e["act"].ops:
                    f(h)

            @block.vector
            def _(h):
                for f in self.e["dve"].ops:
                    f(h)

            @block.gpsimd
            def _(h):
                for f in self.e["pool"].ops:
                    f(h)

            @block.sync
            def _(h):
                for f in self.e["sp"].ops:
                    f(h)
        for n in self.ENG:
            self.e[n].ops = []


class Buf:
    def __init__(self, t):
        self.t = t
        self.k = Tok()

    def __getitem__(self, i):
        return self.t[i]


def _ks(xs):
    return [x.k if isinstance(x, Buf) else x for x in xs]


_NAME = [0]


class Ctx:
    def __init__(self, P, nc, st):
        self.P, self.nc, self.st = P, nc, st
        self.n = 0

    def T(self, shape, dt, name=None):
        _NAME[0] += 1
        return Buf(self.st.enter_context(self.nc.sbuf_tensor("%s_%d" % (name or "t", _NAME[0]), list(shape), dt)))

    def PS(self, shape, dt=F32, name=None):
        _NAME[0] += 1
        return Buf(self.st.enter_context(self.nc.psum_tensor("%s_%d" % (name or "p", _NAME[0]), list(shape), dt)))

    def V(self, fn, rd=(), wr=()):
        self.P.op("dve", fn, _ks(rd), _ks(wr))

    def A(self, fn, rd=(), wr=()):
        self.P.op("act", fn, _ks(rd), _ks(wr))

    def G(self, fn, rd=(), wr=()):
        self.P.op("pool", fn, _ks(rd), _ks(wr))

    def M(self, fn, rd=(), wr=()):
        self.P.op("pe", fn, _ks(rd), _ks(wr))

    def dma(self, q, out, in_, rd=(), wr=()):
        self.P.dma(q, lambda h, out=out, in_=in_: h.dma_start(out=out, in_=in_), _ks(rd), _ks(wr))


def load_cast(cx, q, dst_bf, src_ap, stage, eng="pool"):
    cx.dma(q, stage.t[:] if not isinstance(stage, tuple) else stage[1], src_ap, wr=[stage if not isinstance(stage, tuple) else stage[0]])


def emit_norm_tile(cx, xt, gs, sh, hb, sq, st2, idb, ptr, hT_dst, hT_buf, hf=None):
    cx.V(lambda h: h.memset(st2[:, 0:1], 0.0), wr=[st2])
    cx.A(lambda h: h.activation(out=sq[:], in_=xt[:], func=AF.Square, accum_out=st2[:, 0:1]), rd=[xt, st2], wr=[sq, st2])
    cx.A(lambda h: h.activation(out=st2[:, 1:2], in_=st2[:, 0:1], func=AF.Sqrt, scale=1.0 / D, bias=EPS_AP[0][:, 0:1]), rd=[st2, EPS_AP[0]], wr=[st2])
    cx.V(lambda h: h.reciprocal(out=st2[:, 2:3], in_=st2[:, 1:2]), rd=[st2], wr=[st2])
    cx.V(lambda h: h.scalar_tensor_tensor(out=sq[:], in0=xt[:], scalar=st2[:, 2:3], in1=gs[:], op0=ALU.mult, op1=ALU.mult), rd=[xt, st2, gs], wr=[sq])
    if hf is not None:
        cx.V(lambda h: h.tensor_tensor(out=hf[:], in0=sq[:], in1=sh[:], op=ALU.add), rd=[sq, sh], wr=[hf])
        cx.G(lambda h: h.tensor_copy(out=hb[:], in_=hf[:]), rd=[hf], wr=[hb])
    else:
        cx.V(lambda h: h.tensor_tensor(out=hb[:], in0=sq[:], in1=sh[:], op=ALU.add), rd=[sq, sh], wr=[hb])
    for k in range(8):
        cx.M(lambda h, k=k: h.transpose(out=ptr[:, k, :], in_=hb[:, k * 128:(k + 1) * 128], identity=idb[:]), rd=[hb, idb], wr=[ptr])
    cx.A(lambda h: h.activation(out=hT_dst, in_=ptr[:], func=AF.Copy), rd=[ptr], wr=[hT_buf])


EPS_AP = [None]


def load_consts(cx, CN):
    idf = cx.T([128, 128], F32)
    idb = cx.T([128, 128], BF16)
    eps = cx.T([128, 1], F32)
    cx.dma("sp", idf[:], CN["ident"], wr=[idf])
    cx.V(lambda h: h.tensor_copy(out=idb[:], in_=idf[:]), rd=[idf], wr=[idb])
    cx.V(lambda h: h.memset(eps[:], EPS), wr=[eps])
    EPS_AP[0] = eps
    return idf, idb, eps


def phase_mods(P, nc, IN, MODS):
    with contextlib.ExitStack() as st:
        cx = Ctx(P, nc, st)
        cT = cx.T([128, 8], F32)
        cond = cx.T([128, 8], F32)
        crep = cx.T([128, 8, 128], BF16)
        cx.dma("sp", cT[:], IN["cT"], wr=[cT])
        cx.A(lambda h: h.activation(out=cond[:], in_=cT[:], func=AF.Silu), rd=[cT], wr=[cond])
        cx.V(lambda h: h.tensor_copy(out=crep[:], in_=cond[:].unsqueeze(2).broadcast_to([128, 8, 128])), rd=[cond], wr=[crep])
        wb = [cx.T([128, 8, 512], BF16) for _ in range(3)]
        ps = [cx.PS([128, 512]) for _ in range(2)]
        mod = cx.T([128, 6144], F32)
        ab = cx.T([128, 6144], F32)
        gm = cx.T([128, 1024], F32)
        gf = cx.T([128, 1024], F32)
        n = 0
        for i in range(2):
            cx.dma("act", ab[:], IN["ada_b"][i:i + 1, :].broadcast_to([128, 6144]), wr=[ab])
            cx.dma("act", gm[:], IN["norm_mix_g"][i:i + 1, :].broadcast_to([128, 1024]), wr=[gm])
            cx.dma("act", gf[:], IN["norm_ffn_g"][i:i + 1, :].broadcast_to([128, 1024]), wr=[gf])
            for nb in range(12):
                w = wb[n % 3]
                p = ps[n % 2]
                n += 1
                cx.dma("pool", w[:], IN["ada_w"][i, :, nb * 512:(nb + 1) * 512].rearrange("(k p) n -> p k n", p=128), wr=[w])
                for k in range(8):
                    cx.M(lambda h, k=k, w=w, p=p: h.matmul(p[:], lhsT=crep[:, k, :], rhs=w[:, k, :], start=(k == 0), stop=(k == 7)), rd=[crep, w], wr=[p])
                cx.V(lambda h, p=p, nb=nb: h.tensor_tensor(out=mod[:, nb * 512:(nb + 1) * 512], in0=p[:], in1=ab[:, nb * 512:(nb + 1) * 512], op=ALU.add), rd=[p, ab], wr=[mod])
            cx.V(lambda h: h.scalar_tensor_tensor(out=mod[:, 1024:2048], in0=mod[:, 1024:2048], scalar=1.0, in1=gm[:], op0=ALU.add, op1=ALU.mult), rd=[mod, gm], wr=[mod])
            cx.V(lambda h: h.scalar_tensor_tensor(out=mod[:, 4096:5120], in0=mod[:, 4096:5120], scalar=1.0, in1=gf[:], op0=ALU.add, op1=ALU.mult), rd=[mod, gf], wr=[mod])
            for j, off in enumerate([1024, 0, 2048, 4096, 3072, 5120]):
                cx.dma("sp", MODS.t[i, j], mod[:, off:off + 1024], rd=[mod], wr=[MODS])
        P.barrier()
        P.flush()


def load_mods(cx, MODS, i, js, q="act"):
    out = []
    for j in js:
        b = cx.T([128, 1024], F32)
        cx.dma(q, b[:], MODS.t[i, j], rd=[MODS], wr=[b])
        out.append(b)
    return out


def emit_hT(cx, Xin, gs, sh, idb, hT):
    xb = [cx.T([128, 1024], F32) for _ in range(2)]
    sq = cx.T([128, 1024], F32)
    hb = [cx.T([128, 1024], BF16) for _ in range(2)]
    st2 = [cx.T([128, 4], F32) for _ in range(2)]
    ptr = [cx.PS([128, 8, 128], BF16) for _ in range(2)]
    for i in range(NT):
        x = xb[i % 2]
        cx.dma("sp", x[:], Xin.t[i * 128:(i + 1) * 128, :], rd=[Xin], wr=[x])
        emit_norm_tile(cx, x, gs, sh, hb[i % 2], sq, st2[i % 2], idb, ptr[i % 2], hT[:, :, i * 128:(i + 1) * 128], hT)


def emit_outproj(cx, Xin, Xout, catT_src, w_ap, g1, stage_f, final=None, nx=4):
    wob = cx.T([128, 8, 1024], BF16)
    for k in range(8):
        cx.dma("pool", wob[:, k, :], w_ap[k * 128:(k + 1) * 128, :], wr=[wob])
    py = [cx.PS([128, 1024]) for _ in range(2)]
    NX = nx
    xb = [cx.T([128, 1024], F32) for _ in range(NX)]
    yb = [cx.T([128, 1024], F32) for _ in range(2)]
    PD = 2
    srcs = {}
    for it in range(NT + PD):
        if it < NT:
            srcs[it] = catT_src(it)
            cx.dma("act", xb[it % NX][:], Xin.t[it * 128:(it + 1) * 128, :], rd=[Xin], wr=[xb[it % NX]])
        i = it - PD
        if i < 0:
            continue
        ap, tok = srcs.pop(i)
        p = py[i % 2]
        x = xb[i % NX]
        y = yb[i % 2]
        for nb in range(2):
            for k in range(8):
                cx.M(lambda h, k=k, nb=nb, p=p, ap=ap: h.matmul(p[:, nb * 512:(nb + 1) * 512], lhsT=ap[:, k, :], rhs=wob[:, k, nb * 512:(nb + 1) * 512],
                                                               start=(k == 0), stop=(k == 7)), rd=[tok, wob], wr=[p])
        cx.V(lambda h, p=p, y=y: h.tensor_tensor(out=y[:], in0=p[:], in1=g1[:], op=ALU.mult), rd=[p, g1], wr=[y])
        cx.G(lambda h, x=x, y=y: h.tensor_tensor(out=y[:], in0=y[:], in1=x[:], op=ALU.add), rd=[y, x], wr=[y])
        cx.dma("sp", Xout.t[i * 128:(i + 1) * 128, :], y[:], rd=[y], wr=[Xout])


def phase_even(P, nc, IN, MODS, li, Xin, Xout, SC):
    CATT, V1D, SIGD, HFD = SC["CATT"], SC["V1D"], SC["SIGD"], SC["HFD"]
    with contextlib.ExitStack() as st0:
        c0 = Ctx(P, nc, st0)
        idf, idb, eps = load_consts(c0, IN)
        QK = c0.T([128, 8, S], BF16)
        GT = c0.T([128, NT, 16], F32)
        EB = c0.T([128, NT, 8], F32)
        ES = c0.T([128, NT, 8], F32)
        EE = c0.T([128, NT, 8], F32)
        with contextlib.ExitStack() as st1:
            cx1 = Ctx(P, nc, st1)
            hT = cx1.T([128, 8, S], BF16)
            with contextlib.ExitStack() as st2:
                c2 = Ctx(P, nc, st2)
                gs1, sh1 = load_mods(c2, MODS, li, [0, 1])
                emit_hT(c2, Xin, gs1, sh1, idb, hT)
                P.barrier()
            with contextlib.ExitStack() as stf:
                cx = Ctx(P, nc, stf)
                bT = cx.T([128, 20], F32)
                cw = cx.T([128, 8, 5], F32)
                cb = cx.T([128, 8], F32)
                edge = cx.T([128, 4, 32], F32)
                cx.dma("sp", bT[:], IN["ev_b_inT"], wr=[bT])
                cx.dma("sp", cw[:], IN["ev_conv_wT"], wr=[cw])
                cx.dma("sp", cb[:], IN["ev_conv_bT"], wr=[cb])
                cx.dma("sp", edge[:], IN["pooledge"].broadcast_to([128, 4, 32]), wr=[edge])
                wst = [cx.T([128, 8, 128], F32) for _ in range(2)]
                wcb = [cx.T([128, 8, 128], BF16) for _ in range(2)]
                zc = cx.T([128, S + 16], F32)
                pa = cx.T([128, S + 16], F32)
                yb = cx.T([128, S + 16], F32)
                ybf = cx.T([128, S], BF16)
                yo = [cx.T([128, 512], BF16) for _ in range(2)]
                pw = cx.T([128, 128], F32)
                psc = cx.T([128, 128], F32)
                pwb = cx.T([128, 128], BF16)
                pz = [cx.PS([128, 512]) for _ in range(2)]
                cx.V(lambda h: h.memset(zc[:], 0.0), wr=[zc])
                nps = 0
                for c in range(12):
                    col0 = c * 128 if c < 8 else 2048 + (c - 8) * 128
                    bcol = c if c < 8 else 16 + (c - 8)
                    ws, wc = wst[c % 2], wcb[c % 2]
                    cx.dma("pool", wc[:], IN["ev_w_in"][:, col0:col0 + 128].rearrange("(k p) n -> p k n", p=128), wr=[wc])
                    for tb in range(8):
                        p = pz[nps % 2]
                        nps += 1
                        for k in range(8):
                            cx.M(lambda h, k=k, p=p, wc=wc, tb=tb: h.matmul(p[:], lhsT=wc[:, k, :], rhs=hT[:, k, tb * 512:(tb + 1) * 512], start=(k == 0), stop=(k == 7)),
                                 rd=[wc, hT], wr=[p])
                        cx.A(lambda h, p=p, tb=tb, bcol=bcol: h.activation(out=zc[:, 8 + tb * 512:8 + (tb + 1) * 512], in_=p[:], func=AF.Identity, bias=bT[:, bcol:bcol + 1]),
                             rd=[p, bT], wr=[zc])
                    if c < 8:
                        cx.V(lambda h, c=c: h.tensor_scalar(out=yb[:, 0:S], in0=zc[:, 6:6 + S], scalar1=cw[:, c, 0:1], scalar2=None, op0=ALU.mult), rd=[zc, cw], wr=[yb])
                        for j in range(1, 5):
                            cx.V(lambda h, c=c, j=j: h.scalar_tensor_tensor(out=yb[:, 0:S], in0=zc[:, 6 + j:6 + j + S], scalar=cw[:, c, j:j + 1], in1=yb[:, 0:S], op0=ALU.mult, op1=ALU.add),
                                 rd=[zc, cw, yb], wr=[yb])
                        cx.A(lambda h, c=c: h.activation(out=QK[:, c, :], in_=yb[:, 0:S], func=AF.Silu, bias=cb[:, c:c + 1]), rd=[yb, cb], wr=[QK])
                    else:
                        g = c - 8
                        win = (2, 4, 8, 16)[g]
                        half = win // 2
                        n_el = S + 15
                        cur = zc
                        bufs = [pa, yb]
                        bi = 0
                        step = 1
                        while step < win:
                            d = bufs[bi % 2]
                            bi += 1
                            cx.V(lambda h, cur=cur, d=d, step=step, n_el=n_el: h.tensor_tensor(out=d[:, 0:n_el - step + 1], in0=cur[:, 0:n_el - step + 1], in1=cur[:, step:n_el + 1], op=ALU.add),
                                 rd=[cur], wr=[d])
                            n_el = n_el - step
                            cur = d
                            step *= 2
                        o = bufs[bi % 2]
                        cx.V(lambda h, cur=cur, half=half, o=o, win=win: h.tensor_scalar(out=o[:, 0:S], in0=cur[:, 8 - half:8 - half + S], scalar1=1.0 / win, scalar2=None, op0=ALU.mult), rd=[cur], wr=[o])
                        cx.V(lambda h, o=o, g=g: h.tensor_tensor(out=o[:, 0:16], in0=o[:, 0:16], in1=edge[:, g, 0:16], op=ALU.mult), rd=[o, edge], wr=[o])
                        cx.V(lambda h, o=o, g=g: h.tensor_tensor(out=o[:, S - 16:S], in0=o[:, S - 16:S], in1=edge[:, g, 16:32], op=ALU.mult), rd=[o, edge], wr=[o])
                        cx.V(lambda h, o=o: h.tensor_tensor(out=ybf[:], in0=o[:, 0:S], in1=zc[:, 8:8 + S], op=ALU.subtract), rd=[o, zc], wr=[ybf])
                        cx.dma("sp", pw[:], IN["ev_pool_w"][g], wr=[pw])
                        cx.dma("sp", psc[:], IN["ev_pool_scale"][0:1, g * 128:(g + 1) * 128].broadcast_to([128, 128]), wr=[psc])
                        cx.V(lambda h: h.tensor_tensor(out=pwb[:], in0=pw[:], in1=psc[:], op=ALU.mult), rd=[pw, psc], wr=[pwb])
                        for tb in range(8):
                            p = pz[nps % 2]
                            y_ = yo[nps % 2]
                            nps += 1
                            cx.M(lambda h, p=p, tb=tb: h.matmul(p[:], lhsT=pwb[:], rhs=ybf[:, tb * 512:(tb + 1) * 512], start=True, stop=True), rd=[pwb, ybf], wr=[p])
                            cx.A(lambda h, p=p, y_=y_: h.activation(out=y_[:], in_=p[:], func=AF.Copy), rd=[p], wr=[y_])
                            cx.dma("sp", CATT.t[tb * 4:(tb + 1) * 4, :, 4 + g, :].rearrange("n f t -> f n t"), y_[:].rearrange("f (n t) -> f n t", n=4), rd=[y_], wr=[CATT])
                P.barrier()
            with contextlib.ExitStack() as stt:
                cx = Ctx(P, nc, stt)
                stage_f = cx.T([128, 1024], F32)
                wtm = cx.T([128, 8, 1040], BF16)
                for k in range(8):
                    cx.dma("pool", wtm[:, k, 0:1024], IN["ev_w_in"][k * 128:(k + 1) * 128, 1024:2048], wr=[wtm])
                cx.dma("pool", wtm[:, :, 1024:1040], IN["ev_w_in"][:, 2560:2576].rearrange("(k p) n -> p k n", p=128), wr=[wtm])
                bvo = cx.T([128, 1024], F32)
                bg = cx.T([128, 16], F32)
                cx.dma("sp", bvo[:], IN["ev_b_in"][0:1, 1024:2048].broadcast_to([128, 1024]), wr=[bvo])
                cx.dma("sp", bg[:], IN["ev_b_in"][0:1, 2560:2576].broadcast_to([128, 16]), wr=[bg])
                pv = [cx.PS([128, 512]) for _ in range(2)]
                po = [cx.PS([128, 512]) for _ in range(2)]
                pg = [cx.PS([128, 16]) for _ in range(2)]
                v1 = [cx.T([128, 4, 130], BF16) for _ in range(2)]
                of = [cx.T([128, 512], F32) for _ in range(2)]
                ob = [cx.T([128, 512], BF16) for _ in range(2)]
                for b_ in v1:
                    cx.V(lambda h, b_=b_: h.memset(b_[:], 1.0), wr=[b_])
                for i in range(NT):
                    a = i % 2
                    for (p, c0_, c1_) in ((pv[a], 0, 512), (po[a], 512, 1024), (pg[a], 1024, 1040)):
                        for k in range(8):
                            cx.M(lambda h, k=k, p=p, c0_=c0_, c1_=c1_, i=i: h.matmul(p[:], lhsT=hT[:, k, i * 128:(i + 1) * 128], rhs=wtm[:, k, c0_:c1_], start=(k == 0), stop=(k == 7)),
                                 rd=[hT, wtm], wr=[p])
                    for hh in range(4):
                        cx.V(lambda h, a=a, hh=hh: h.tensor_tensor(out=v1[a][:, hh, 0:128], in0=pv[a][:, hh * 128:(hh + 1) * 128],
                                                                   in1=bvo[:, hh * 128:(hh + 1) * 128], op=ALU.add), rd=[pv[a], bvo], wr=[v1[a]])
                    cx.dma("sp", V1D.t[i], v1[a][:], rd=[v1[a]], wr=[V1D])
                    cx.V(lambda h, a=a: h.tensor_tensor(out=of[a][:], in0=po[a][:], in1=bvo[:, 512:1024], op=ALU.add), rd=[po[a], bvo], wr=[of[a]])
                    cx.A(lambda h, a=a: h.activation(out=ob[a][:], in_=of[a][:], func=AF.Sigmoid), rd=[of[a]], wr=[ob[a]])
                    if i == 0:
                        cx.dma("sp", SC["DBG2"].t, of[a][:], rd=[of[a]], wr=[SC["DBG2"]])
                    cx.dma("sp", SIGD.t[i], ob[a][:], rd=[ob[a]], wr=[SIGD])
                    cx.V(lambda h, a=a, i=i: h.tensor_tensor(out=GT[:, i, :], in0=pg[a][:], in1=bg[:], op=ALU.add), rd=[pg[a], bg], wr=[GT])
                cx.dma("sp", SC["DBG1"].t, GT[:], rd=[GT], wr=[SC["DBG1"]])
                P.barrier()
            with contextlib.ExitStack() as stg:
                cx = Ctx(P, nc, stg)
                LF = cx.T([128, NT, 8], F32)
                t8 = cx.T([128, NT, 8], F32)
                BC = cx.T([128, NT, 16], F32)
                cx.A(lambda h: h.activation(out=t8[:], in_=GT[:, :, 8:16], func=AF.Exp, scale=-1.0), rd=[GT], wr=[t8])
                cx.V(lambda h: h.tensor_scalar(out=t8[:], in0=t8[:], scalar1=1.0, scalar2=None, op0=ALU.add), rd=[t8], wr=[t8])
                cx.A(lambda h: h.activation(out=LF[:], in_=t8[:], func=AF.Ln), rd=[t8], wr=[LF])
                cx.V(lambda h: h.tensor_scalar(out=LF[:], in0=LF[:], scalar1=-1.0, scalar2=None, op0=ALU.mult), rd=[LF], wr=[LF])
                triU = cx.T([128, 128], F32)
                triL = cx.T([128, 128], F32)
                ones = cx.T([128, 128], F32)
                cx.dma("sp", triU[:], IN["triU"], wr=[triU])
                cx.dma("sp", triL[:], IN["triL"], wr=[triL])
                cx.V(lambda h: h.memset(ones[:], 1.0), wr=[ones])
                pc = cx.PS([128, NT, 16])
                for i in range(NT):
                    cx.M(lambda h, i=i: h.matmul(pc[:, i, 0:4], lhsT=triU[:], rhs=LF[:, i, 0:4], start=True, stop=True), rd=[triU, LF], wr=[pc])
                    cx.M(lambda h, i=i: h.matmul(pc[:, i, 4:8], lhsT=triL[:], rhs=LF[:, i, 4:8], start=True, stop=True), rd=[triL, LF], wr=[pc])
                    cx.M(lambda h, i=i: h.matmul(pc[:, i, 8:16], lhsT=ones[:], rhs=LF[:, i, 0:8], start=True, stop=True), rd=[ones, LF], wr=[pc])
                cx.V(lambda h: h.tensor_copy(out=BC[:], in_=pc[:]), rd=[pc], wr=[BC])
                cx.A(lambda h: h.activation(out=EB[:], in_=BC[:, :, 0:8], func=AF.Exp), rd=[BC], wr=[EB])
                cx.A(lambda h: h.activation(out=EE[:], in_=BC[:, :, 8:16], func=AF.Exp), rd=[BC], wr=[EE])
                cx.V(lambda h: h.tensor_tensor(out=t8[:], in0=GT[:, :, 0:8], in1=BC[:, :, 0:8], op=ALU.subtract), rd=[GT, BC], wr=[t8])
                cx.V(lambda h: h.tensor_scalar(out=t8[:], in0=t8[:], scalar1=float(-0.5 * np.log(128.0)), scalar2=None, op0=ALU.add), rd=[t8], wr=[t8])
                cx.A(lambda h: h.activation(out=ES[:], in_=t8[:], func=AF.Exp), rd=[t8], wr=[ES])
                P.barrier()
        with contextlib.ExitStack() as st3:
            cx = Ctx(P, nc, st3)
            mk = []
            for nm in ("triU", "triL"):
                f = cx.T([128, 128], F32)
                cx.dma("sp", f[:], IN[nm], wr=[f])
                mk.append(f)
            Cst = cx.T([128, 8, 129], F32)
            Cb = cx.T([128, 8, 129], BF16)
            cx.V(lambda h: h.memset(Cst[:], 0.0), wr=[Cst])
            cx.V(lambda h: h.memset(Cb[:], 0.0), wr=[Cb])
            NV = 6
            v1 = [cx.T([128, 4, 130], BF16) for _ in range(NV)]
            psS = [cx.PS([128, 4, 128]) for _ in range(2)]
            psA = [cx.PS([128, 3, 129]) for _ in range(3)]
            ps_t = cx.PS([128, 8, 128], BF16)
            ps_h = cx.PS([128, 4, 128], BF16)
            AT = cx.T([128, 8, 128], BF16)
            ksb = cx.T([128, 8, 128], BF16)
            sm = cx.T([128, 8, 4], F32)
            hacc = cx.T([128, NT, 512], F32)
            NSG = 6
            sgl = [cx.T([128, 512], BF16) for _ in range(NSG)]
            sq = cx.T([128, 512], F32)
            st4 = [cx.T([128, 12], F32) for _ in range(2)]
            mg = cx.T([128, 512], F32)
            hmb = [cx.T([128, 512], BF16) for _ in range(2)]
            hmT = [cx.T([128, 4, 128], BF16) for _ in range(2)]
            cx.dma("sp", mg[:], IN["ev_mnorm_g"][0:1, :].broadcast_to([128, 512]), wr=[mg])
            bg = SC.get("BG0")
            if bg is not None:
                bg.attach(cx)
            chains = [(d, hh) for d in range(2) for hh in range(4)]

            def aslot(c):
                return psA[c // 3], c % 3

            def tile_of(s, d):
                return s if d == 0 else NT - 1 - s

            PDV = 2
            nfin = 0
            for s in range(NT + PDV):
                if s < NT:
                    for d in range(2):
                        vb = v1[(2 * s + d) % NV]
                        cx.dma("sp", vb[:], V1D.t[tile_of(s, d)], rd=[V1D], wr=[vb])
                    if s >= NT // 2:
                        for d in range(2):
                            sg_ = sgl[(2 * s + d) % NSG]
                            cx.dma("act", sg_[:], SIGD.t[tile_of(s, d)], rd=[SIGD], wr=[sg_])
                s_ = s - PDV
                if s_ < 0:
                    continue
                s = s_
                if bg is not None:
                    bg.step(2)
                vbs = [v1[(2 * s + d) % NV] for d in range(2)]
                tls = [tile_of(s, d) for d in range(2)]
                for c, (d, hh) in enumerate(chains):
                    tsl = slice(tls[d] * 128, (tls[d] + 1) * 128)
                    cx.M(lambda h: h.matmul(psS[d][:, hh, :], lhsT=QK[:, 4 + hh, tsl], rhs=QK[:, hh, tsl], start=True, stop=True), rd=[QK], wr=[psS[d]])
                for c, (d, hh) in enumerate(chains):
                    col = d * 4 + hh
                    cx.V(lambda h: h.scalar_tensor_tensor(out=AT[:, c, :], in0=psS[d][:, hh, :], scalar=ES[:, tls[d], col:col + 1], in1=mk[d][:], op0=ALU.mult, op1=ALU.mult),
                         rd=[psS[d], ES, mk[d]], wr=[AT])
                for c, (d, hh) in enumerate(chains):
                    col = d * 4 + hh
                    tsl = slice(tls[d] * 128, (tls[d] + 1) * 128)
                    pa, sl = aslot(c)
                    cx.M(lambda h: h.matmul(pa[:, sl, :], lhsT=AT[:, c, :], rhs=vbs[d][:, hh, 0:129], start=True, stop=False), rd=[AT, vbs[d]], wr=[pa])
                    cx.M(lambda h: h.matmul(pa[:, sl, :], lhsT=QK[:, hh, tsl], rhs=Cb[:, col, :], start=False, stop=True), rd=[QK, Cb], wr=[pa])
                for c, (d, hh) in enumerate(chains):
                    col = d * 4 + hh
                    pa, sl = aslot(c)
                    cx.A(lambda h: h.activation(out=sm[:, c, 2:3], in_=pa[:, sl, 128:129], func=AF.Abs, scale=EB[:, tls[d], col:col + 1]), rd=[pa, EB], wr=[sm])
                cx.V(lambda h: h.tensor_scalar(out=sm[:, :, 0:1], in0=sm[:, :, 2:3], scalar1=1.0, scalar2=None, op0=ALU.max), rd=[sm], wr=[sm])
                cx.V(lambda h: h.reciprocal(out=sm[:, :, 3:4], in_=sm[:, :, 0:1]), rd=[sm], wr=[sm])
                for d in range(2):
                    cx.V(lambda h: h.tensor_tensor(out=sm[:, d * 4:(d + 1) * 4, 1:2], in0=EB[:, tls[d], d * 4:(d + 1) * 4].unsqueeze(2), in1=sm[:, d * 4:(d + 1) * 4, 3:4], op=ALU.mult),
                         rd=[sm, EB], wr=[sm])
                for c, (d, hh) in enumerate(chains):
                    pa, sl = aslot(c)
                    dst = hacc[:, tls[d], hh * 128:(hh + 1) * 128]
                    if s < NT // 2:
                        cx.A(lambda h: h.activation(out=dst, in_=pa[:, sl, 0:128], func=AF.Copy, scale=sm[:, c, 1:2]), rd=[pa, sm], wr=[hacc])
                    else:
                        cx.V(lambda h: h.scalar_tensor_tensor(out=dst, in0=pa[:, sl, 0:128], scalar=sm[:, c, 1:2], in1=dst, op0=ALU.mult, op1=ALU.add), rd=[pa, sm, hacc], wr=[hacc])
                for c, (d, hh) in enumerate(chains):
                    tsl = slice(tls[d] * 128, (tls[d] + 1) * 128)
                    cx.M(lambda h: h.transpose(out=ps_t[:, c, :], in_=QK[:, 4 + hh, tsl], identity=idb[:]), rd=[QK, idb], wr=[ps_t])
                for c, (d, hh) in enumerate(chains):
                    col = d * 4 + hh
                    cx.A(lambda h: h.activation(out=ksb[:, c, :], in_=ps_t[:, c, :], func=AF.Copy, scale=ES[:, tls[d], col:col + 1]), rd=[ps_t, ES], wr=[ksb])
                for c, (d, hh) in enumerate(chains):
                    pa, sl = aslot(c)
                    cx.M(lambda h: h.matmul(pa[:, sl, :], lhsT=ksb[:, c, :], rhs=vbs[d][:, hh, 0:129], start=True, stop=True), rd=[ksb, vbs[d]], wr=[pa])
                for c, (d, hh) in enumerate(chains):
                    col = d * 4 + hh
                    pa, sl = aslot(c)
                    cx.V(lambda h: h.tensor_scalar(out=Cst[:, col, :], in0=Cst[:, col, :], scalar1=EE[:, tls[d], col:col + 1], scalar2=None, op0=ALU.mult), rd=[Cst, EE], wr=[Cst])
                    cx.V(lambda h: h.scalar_tensor_tensor(out=Cst[:, col, :], in0=pa[:, sl, :], scalar=EE[:, tls[d], col:col + 1], in1=Cst[:, col, :], op0=ALU.mult, op1=ALU.add),
                         rd=[pa, EE, Cst], wr=[Cst])
                cx.A(lambda h: h.activation(out=Cb[:], in_=Cst[:], func=AF.Copy), rd=[Cst], wr=[Cb])
                if s >= NT // 2:
                    for d in range(2):
                        i = tls[d]
                        a = nfin % 2
                        nfin += 1
                        sg_ = sgl[(2 * s + d) % NSG]
                        hv = hacc[:, i, :]
                        cx.V(lambda h: h.tensor_tensor(out=sq[:], in0=hv, in1=hv, op=ALU.mult), rd=[hacc], wr=[sq])
                        cx.V(lambda h: h.tensor_reduce(out=st4[a][:, 0:4], in_=sq[:].rearrange("p (h d) -> p h d", h=4), axis=AX.X, op=ALU.add), rd=[sq], wr=[st4[a]])
                        cx.A(lambda h: h.activation(out=st4[a][:, 4:8], in_=st4[a][:, 0:4], func=AF.Sqrt, scale=1.0 / 128, bias=eps[:, 0:1]), rd=[st4[a], eps], wr=[st4[a]])
                        cx.V(lambda h: h.reciprocal(out=st4[a][:, 8:12], in_=st4[a][:, 4:8]), rd=[st4[a]], wr=[st4[a]])
                        cx.V(lambda h: h.tensor_tensor(out=hv.rearrange("p (h d) -> p h d", h=4), in0=hv.rearrange("p (h d) -> p h d", h=4),
                                                       in1=st4[a][:, 8:12].unsqueeze(2).broadcast_to([128, 4, 128]), op=ALU.mult), rd=[hacc, st4[a]], wr=[hacc])
                        cx.V(lambda h: h.tensor_tensor(out=sq[:], in0=mg[:], in1=sg_[:], op=ALU.mult), rd=[mg, sg_], wr=[sq])
                        cx.V(lambda h: h.tensor_tensor(out=hmb[a][:], in0=hv, in1=sq[:], op=ALU.mult), rd=[hacc, sq], wr=[hmb[a]])
                        for k in range(4):
                            cx.M(lambda h: h.transpose(out=ps_h[:, k, :], in_=hmb[a][:, k * 128:(k + 1) * 128], identity=idb[:]), rd=[hmb[a], idb], wr=[ps_h])
                        cx.A(lambda h: h.activation(out=hmT[a][:], in_=ps_h[:], func=AF.Copy), rd=[ps_h], wr=[hmT[a]])
                        cx.dma("act", CATT.t[i, :, 0:4, :], hmT[a][:], rd=[hmT[a]], wr=[CATT])
            if bg is not None:
                bg.finish()
                SC.setdefault("TABDONE", {})[0] = True
            P.barrier()
        with contextlib.ExitStack() as st4_:
            cx = Ctx(P, nc, st4_)
            cb_ = [cx.T([128, 8, 128], BF16) for _ in range(4)]
            g1, = load_mods(cx, MODS, li, [2])
            stage_f = cx.T([128, 1024], F32)

            def src(i):
                b = cb_[i % 4]
                cx.dma("pool", b[:], CATT.t[i], rd=[CATT], wr=[b])
                return b, b
            emit_outproj(cx, Xin, Xout, src, IN["ev_w_out"], g1, stage_f)
            P.barrier()
        P.flush()


W_SPECS = {
    "ada_w": [2, 1024, 6144], "ada_b": [2, 6144], "norm_mix_g": [2, 1024], "norm_ffn_g": [2, 1024],
    "ev_w_in": [1024, 2576], "ev_b_in": [1, 2576], "ev_b_inT": [128, 20], "ev_conv_wT": [128, 8, 5], "ev_conv_bT": [128, 8],
    "ev_mnorm_g": [1, 512], "ev_pool_w": [4, 128, 128], "ev_pool_scale": [1, 512], "ev_w_out": [1024, 1024],
    "od_w_in": [1024, 1536], "od_qnorm_g": [1, 128], "od_knorm_g": [1, 128], "od_w_out": [1024, 1024],
    "peer_w_q": [2, 1024, 2048], "peer_keys": [2, 2, 128, 128], "peer_u": [2, 16384, 1024], "peer_v": [2, 16384, 1024],
    "final_g": [1, 1024],
    "ident": [128, 128], "triU": [128, 128], "triL": [128, 128], "pooledge": [1, 4, 32], "ropecs": [128, 2, NT, 64], "iota16": [1, 16],
    "cT": [128, 8],
}


def host_consts():
    cn = {}
    cn["ident"] = np.eye(128, dtype=np.float32)
    s_ = np.arange(128)
    cn["triU"] = (s_[:, None] <= s_[None, :]).astype(np.float32)
    cn["triL"] = (s_[:, None] >= s_[None, :]).astype(np.float32)
    pe = np.zeros((1, 4, 32), np.float32)
    for g, w in enumerate((2, 4, 8, 16)):
        for j in range(32):
            t = j if j < 16 else S - 32 + j
            lo = max(t - w // 2, 0)
            hi = min(t + w // 2, S)
            pe[0, g, j] = w / float(hi - lo)
    cn["pooledge"] = pe
    t = np.arange(S)
    r, c = t // 64, t % 64
    freqs = (10000.0 ** (-np.arange(0, 64, 2, dtype=np.float32) / 64.0)).astype(np.float32)
    ang = np.concatenate([r[:, None].astype(np.float32) * freqs, c[:, None].astype(np.float32) * freqs], axis=-1).astype(np.float32)
    cs = np.stack([np.cos(ang), np.sin(ang)], 0).astype(np.float32)
    cn["ropecs"] = np.ascontiguousarray(cs.reshape(2, NT, 128, 64).transpose(2, 0, 1, 3))
    cn["iota16"] = np.arange(16, dtype=np.float32)[None, :]
    return cn


DEBUG = [False]


def build(first, last):
    nc = bass.Bass("TRN2", target_bir_lowering=False)
    IK = "ExternalOutput" if DEBUG[0] else "Internal"
    IN = {k: nc.dram_tensor(k, v, F32, kind="ExternalInput").ap() for k, v in W_SPECS.items()}
    xin = nc.dram_tensor("xin", [S, D], F32, kind="ExternalInput").ap()
    out = nc.dram_tensor("out", [S, D], F32, kind="ExternalOutput").ap()
    X = {}
    for k in range(0, 5):
        if k == first - 1:
            X[k] = Buf(xin)
        elif k == last:
            X[k] = Buf(out)
        elif first <= k < last:
            X[k] = Buf(nc.dram_tensor("X%d" % k, [S, D], F32, kind="Internal").ap())
    SC = {
        "CATT": Buf(nc.dram_tensor("CATT", [NT, 128, 8, 128], BF16, kind=IK).ap()),
        "V1D": Buf(nc.dram_tensor("V1D", [NT, 128, 4, 130], BF16, kind=IK).ap()),
        "SIGD": Buf(nc.dram_tensor("SIGD", [NT, 128, 512], BF16, kind=IK).ap()),
        "HFD": Buf(nc.dram_tensor("HFD", [NT, 128, 512], F32, kind=IK).ap()),
        "TAB": [Buf(nc.dram_tensor("TAB%d" % i, [16384, 2048], BF16, kind="Internal").ap()) for i in range(2)],
    }
    SC["DBG1"] = Buf(nc.dram_tensor("DBG1", [128, NT, 16], F32, kind=IK).ap())
    SC["DBG2"] = Buf(nc.dram_tensor("DBG2", [128, 512], F32, kind=IK).ap())
    MODS = Buf(nc.dram_tensor("MODS", [2, 6, 128, 1024], F32, kind=IK).ap())
    with contextlib.ExitStack() as st:
        P = Prog(nc, st)
        phase_mods(P, nc, IN, MODS)
        if first <= 1 and last >= 2:
            SC["BG0"] = TableBuilder(P, nc, IN, 0, SC["TAB"][0])
        if first <= 3 and last >= 4:
            SC["BG1"] = TableBuilder(P, nc, IN, 1, SC["TAB"][1])
        for ph in range(first, last + 1):
            if ph == 1:
                phase_even(P, nc, IN, MODS, 0, X[0], X[1], SC)
            elif ph == 2:
                phase_peer(P, nc, IN, MODS, 0, X[1], X[2], SC, final=False)
            elif ph == 3:
                phase_attn(P, nc, IN, MODS, 1, X[2], X[3], SC)
            elif ph == 4:
                phase_peer(P, nc, IN, MODS, 1, X[3], X[4], SC, final=True)
    return nc


def host_inputs(inputs):
    f = lambda a: np.ascontiguousarray(np.asarray(a, dtype=np.float32))
    sh = {}
    sh["ada_w"] = f(inputs["ada_w"]); sh["ada_b"] = f(inputs["ada_b"])
    sh["norm_mix_g"] = f(inputs["norm_mix_g"]); sh["norm_ffn_g"] = f(inputs["norm_ffn_g"])
    sh["ev_w_in"] = f(inputs["ev_w_in"][0]); sh["ev_b_in"] = f(inputs["ev_b_in"])
    sh["ev_b_inT"] = f(np.asarray(inputs["ev_b_in"])[0, :2560].reshape(20, 128).T)
    cw = np.asarray(inputs["ev_conv_w"])[0, :, 0, :]
    sh["ev_conv_wT"] = f(cw.T.reshape(8, 128, 5).transpose(1, 0, 2))
    sh["ev_conv_bT"] = f(np.asarray(inputs["ev_conv_b"])[0].reshape(8, 128).T)
    sh["ev_mnorm_g"] = f(inputs["ev_mnorm_g"]); sh["ev_pool_w"] = f(inputs["ev_pool_w"][0]); sh["ev_pool_scale"] = f(inputs["ev_pool_scale"])
    sh["ev_w_out"] = f(inputs["ev_w_out"][0])
    sh["od_w_in"] = f(inputs["od_w_in"][0]); sh["od_qnorm_g"] = f(inputs["od_qnorm_g"]); sh["od_knorm_g"] = f(inputs["od_knorm_g"])
    sh["od_w_out"] = f(inputs["od_w_out"][0])
    sh["peer_w_q"] = f(inputs["peer_w_q"]); sh["peer_keys"] = f(inputs["peer_keys"])
    sh["peer_u"] = f(inputs["peer_u"]); sh["peer_v"] = f(inputs["peer_v"])
    sh["final_g"] = f(np.asarray(inputs["final_g"])[None, :])
    sh.update(host_consts())
    return sh


def run_phases(inputs, first, last, xin_list, cores):
    nc = build(first, last)
    sh = host_inputs(inputs)
    c = np.asarray(inputs["c"], dtype=np.float32)
    in_maps = []
    for j, b in enumerate(cores):
        m = dict(sh)
        m["cT"] = np.ascontiguousarray(c[b].reshape(8, 128).T)
        m["xin"] = np.ascontiguousarray(xin_list[j], dtype=np.float32)
        in_maps.append(m)
    res = run_bass_kernel_spmd(nc, in_maps, core_ids=list(range(len(cores))))
    if DEBUG[0]:
        return res.results
    return [r["out"] for r in res.results]


def kernel(**inputs):
    x = np.asarray(inputs["x"], dtype=np.float32)
    outs = run_phases(inputs, 1, 4, [x[b] for b in range(8)], list(range(8)))
    return np.stack(outs, 0).astype(np.float32)


class TableBuilder:
    ROWS = 512

    def __init__(self, P, nc, IN, li, TAB):
        self.P, self.nc, self.IN, self.li, self.TAB = P, nc, IN, li, TAB
        nb = 16384 // self.ROWS
        self.blocks = [(half, blk) for blk in range(nb) for half in range(2)]
        self.k = 0
        self.cx = None

    def attach(self, cx):
        self.cx = cx

    def step(self, n=1):
        for _ in range(n):
            if self.k >= len(self.blocks):
                return
            half, blk = self.blocks[self.k]
            rows = slice(blk * self.ROWS, (blk + 1) * self.ROWS)
            self.cx.dma("pool", self.TAB.t[rows, half * 1024:(half + 1) * 1024], self.IN[("peer_u", "peer_v")[half]][self.li, rows, :], wr=[Tok()])
            self.k += 1
            self.last = True

    def finish(self):
        self.step(len(self.blocks))
        self.cx = None

    @property
    def done(self):
        return self.k >= len(self.blocks)


POOL_DOTS = False
PROD_DT = BF16
STT_SLOTS = ()
DIAG_ON_DVE = True


def phase_peer(P, nc, IN, MODS, li, Xin, Xout, SC, final):
    TAB = SC["TAB"][li]
    with contextlib.ExitStack() as st:
        cx = Ctx(P, nc, st)
        prebuilt = bool(SC.get("TABDONE", {}).get(li))
        sf = [cx.T([128, 4, 1024], F32) for _ in range(0 if prebuilt else 3)]
        sb = [cx.T([128, 4, 1024], BF16) for _ in range(0 if prebuilt else 3)]
        n = 0
        for half, nm in enumerate(("peer_u", "peer_v")):
            for blk in range(0 if prebuilt else 32):
                f, b = sf[n % 3], sb[n % 3]
                rows = slice(blk * 512, (blk + 1) * 512)
                cx.dma("sp", f[:], IN[nm][li, rows, :].rearrange("(n p) d -> p n d", p=128), wr=[f])
                if n % 3 == 0:
                    cx.A(lambda h: h.activation(out=b[:], in_=f[:], func=AF.Copy), rd=[f], wr=[b])
                elif n % 3 == 1:
                    cx.V(lambda h: h.tensor_copy(out=b[:], in_=f[:]), rd=[f], wr=[b])
                else:
                    cx.G(lambda h: h.tensor_copy(out=b[:], in_=f[:]), rd=[f], wr=[b])
                cx.dma("act", TAB.t[rows, half * 1024:(half + 1) * 1024].rearrange("(n p) d -> p n d", p=128), b[:], rd=[b], wr=[TAB])
                n += 1
        P.barrier()
        P.flush()
    with contextlib.ExitStack() as st:
        cx = Ctx(P, nc, st)
        idf, idb, eps = load_consts(cx, IN)
        gs2, sh2, g2 = load_mods(cx, MODS, li, [3, 4, 5])
        io16 = cx.T([128, 16], F32)
        th16 = cx.T([128, 16], F32)
        cx.dma("sp", io16[:], IN["iota16"].broadcast_to([128, 16]), wr=[io16])
        cx.V(lambda h: h.tensor_scalar(out=th16[:], in0=io16[:], scalar1=16.0, scalar2=None, op0=ALU.mult), rd=[io16], wr=[th16])
        if final:
            fg = cx.T([128, 1024], F32)
            cx.dma("sp", fg[:], IN["final_g"].broadcast_to([128, 1024]), wr=[fg])
        wq = cx.T([128, 8, 2048], BF16)
        ptr = cx.PS([128, 8, 128], BF16)
        keysT = cx.T([128, 2, 128], BF16)
        with contextlib.ExitStack() as stw:
            cw_ = Ctx(P, nc, stw)
            for k in range(8):
                cw_.dma("pool", wq[:, k, :], IN["peer_w_q"][li, k * 128:(k + 1) * 128, :], wr=[wq])
            kf = cw_.T([128, 2, 128], F32)
            kb = cw_.T([128, 2, 128], BF16)
            cw_.dma("sp", kf[:], IN["peer_keys"][li].rearrange("t n c -> n t c"), wr=[kf])
            cw_.V(lambda h: h.tensor_copy(out=kb[:], in_=kf[:]), rd=[kf], wr=[kb])
            for t in range(2):
                cw_.M(lambda h: h.transpose(out=ptr[:, t, :], in_=kb[:, t, :], identity=idb[:]), rd=[kb, idb], wr=[ptr])
            cw_.A(lambda h: h.activation(out=keysT[:], in_=ptr[:, 0:2, :], func=AF.Copy), rd=[ptr], wr=[keysT])
            P.barrier()
        pq = [cx.PS([128, 512]) for _ in range(2)]
        psc = [cx.PS([128, 4, 128]) for _ in range(2)]
        po = cx.PS([128, 1024])
        xb = [cx.T([128, 1024], F32) for _ in range(3)]
        sq = cx.T([128, 1024], F32)
        hb = cx.T([128, 1024], BF16)
        hTi = cx.T([128, 8, 128], BF16)
        st2 = cx.T([128, 4], F32)
        qb = cx.T([128, 2048], BF16)
        rs = cx.T([128, 48], F32)
        qT = cx.T([128, 16, 128], BF16)
        S1 = cx.T([128, 16, 128], F32)
        sqq = Buf(S1.t)
        sqq.k = S1.k
        sqq_ap = S1.t[:].rearrange("p g c -> p (g c)")
        wk = [cx.T([128, 128], F32) for _ in range(2)]
        m = cx.T([128, 16, 16], F32)
        ix = cx.T([128, 16, 16], U32)
        ixf = cx.T([128, 16, 16], F32)
        CS = cx.T([128, 8, 256], F32)
        wk2 = [cx.T([128, 256], F32) for _ in range(2)]
        tops = cx.T([128, 8, 16], F32)
        pos = cx.T([128, 8, 16], U32)
        posf = cx.T([128, 8, 16], F32)
        af = cx.T([128, 8, 16], F32)
        bf_ = cx.T([128, 8, 16], F32)
        oh = Buf(CS.t)
        oh.k = CS.k
        oh_ap = CS.t[:].rearrange("p h (a b) -> p h a b", a=16)
        i12 = cx.T([128, 2, 128], F32)
        idxf = cx.T([128, 128], F32)
        idx = cx.T([128, 128], I32)
        ge = cx.T([128, 8, 16], F32)
        gsum = cx.T([128, 16], F32)
        gate = cx.T([128, 128], F32)
        act = cx.T([128, 128], F32)
        gl = cx.T([128, 128], F32)
        coef = cx.T([128, 128], F32)
        NG = 5
        actT = [Tok() for _ in range(4)]
        glT = [Tok() for _ in range(4)]
        coefT = [Tok() for _ in range(4)]
        Gb = [cx.T([128, 4, 2048], BF16) for _ in range(NG)]
        Gtok = [[Tok() for _ in range(4)] for _ in range(NG)]
        diag = [cx.T([128, 4, 128], BF16) for _ in range(2)]
        junk = cx.T([128, 1024], BF16)
        NPR = 2
        junkv = cx.T([128, 1024], BF16) if STT_SLOTS else None
        prods = [cx.T([128, 1024], PROD_DT) for _ in range(NPR)]
        yb = [cx.T([128, 1024], F32) for _ in range(1)]
        sgn = 0
        hbs = [hb, cx.T([128, 1024], BF16)]
        idxs = [idx, cx.T([128, 128], I32)]
        gates = [gate, cx.T([128, 128], F32)]
        st3 = cx.T([128, 4], F32)
        sq2 = cx.T([128, 1024], F32) if final else None
        fg_ = fg if final else None
        FSTEP = 2

        def front(i):
            x = xb[i % 3]
            hb = hbs[i % 2]
            idx = idxs[i % 2]
            gate = gates[i % 2]
            yield
            cx.dma("sp", x[:], Xin.t[i * 128:(i + 1) * 128, :], rd=[Xin], wr=[x])
            yield
            emit_norm_tile(cx, x, gs2, sh2, hb, sq, st2, idb, ptr, hTi[:], hTi)
            for nb in range(4):
                p = pq[nb % 2]
                for k in range(8):
                    yield
                    cx.M(lambda h: h.matmul(p[:], lhsT=hTi[:, k, :], rhs=wq[:, k, nb * 512:(nb + 1) * 512], start=(k == 0), stop=(k == 7)), rd=[hTi, wq], wr=[p])
                yield
                cx.A(lambda h: h.activation(out=qb[:, nb * 512:(nb + 1) * 512], in_=p[:], func=AF.Copy), rd=[p], wr=[qb])
            yield
            cx.V(lambda h: h.tensor_tensor(out=sqq_ap, in0=qb[:], in1=qb[:], op=ALU.mult), rd=[qb], wr=[sqq])
            yield
            cx.V(lambda h: h.tensor_reduce(out=rs[:, 0:16], in_=S1.t[:], axis=AX.X, op=ALU.add), rd=[sqq], wr=[rs])
            yield
            cx.A(lambda h: h.activation(out=rs[:, 16:32], in_=rs[:, 0:16], func=AF.Sqrt, scale=1.0 / 128, bias=eps[:, 0:1]), rd=[rs, eps], wr=[rs])
            yield
            cx.V(lambda h: h.reciprocal(out=rs[:, 32:48], in_=rs[:, 16:32]), rd=[rs], wr=[rs])
            for r in range(2):
                for j in range(8):
                    g_ = r * 8 + j
                    yield
                    cx.M(lambda h: h.transpose(out=ptr[:, j, :], in_=qb[:, g_ * 128:(g_ + 1) * 128], identity=idb[:]), rd=[qb, idb], wr=[ptr])
                yield
                cx.A(lambda h: h.activation(out=qT[:, r * 8:(r + 1) * 8, :], in_=ptr[:], func=AF.Copy), rd=[ptr], wr=[qT])
            for r in range(4):
                ps_ = psc[r % 2]
                for j in range(4):
                    hp = r * 4 + j
                    yield
                    cx.M(lambda h: h.matmul(ps_[:, j, :], lhsT=qT[:, hp, :], rhs=keysT[:, hp % 2, :], start=True, stop=True), rd=[qT, keysT], wr=[ps_])
                yield
                cx.V(lambda h: h.tensor_tensor(out=S1[:, r * 4:(r + 1) * 4, :], in0=ps_[:], in1=rs[:, 32 + r * 4:32 + (r + 1) * 4].unsqueeze(2).broadcast_to([128, 4, 128]), op=ALU.mult),
                     rd=[ps_, rs], wr=[S1])
            for hp in range(16):
                w_ = wk[hp % 2]
                yield
                cx.V(lambda h: h.max(out=m[:, hp, 0:8], in_=S1[:, hp, :]), rd=[S1], wr=[m])
                yield
                cx.V(lambda h: h.max_index(out=ix[:, hp, 0:8], in_max=m[:, hp, 0:8], in_values=S1[:, hp, :]), rd=[m, S1], wr=[ix])
                yield
                cx.V(lambda h: h.match_replace(out=w_[:], in_to_replace=m[:, hp, 0:8], in_values=S1[:, hp, :], imm_value=-1e30), rd=[m, S1], wr=[w_])
                yield
                cx.V(lambda h: h.max(out=m[:, hp, 8:16], in_=w_[:]), rd=[w_], wr=[m])
                yield
                cx.V(lambda h: h.max_index(out=ix[:, hp, 8:16], in_max=m[:, hp, 8:16], in_values=w_[:]), rd=[m, w_], wr=[ix])
            mv = m[:].rearrange("p (h t) k -> p h t k", t=2)
            yield
            cx.V(lambda h: h.tensor_tensor(out=CS[:].rearrange("p h (a b) -> p h a b", a=16), in0=mv[:, :, 0, :].unsqueeze(3).broadcast_to([128, 8, 16, 16]),
                                           in1=mv[:, :, 1, :].unsqueeze(2).broadcast_to([128, 8, 16, 16]), op=ALU.add), rd=[m], wr=[CS])
            for hh in range(8):
                w_ = wk2[hh % 2]
                yield
                cx.V(lambda h: h.max(out=tops[:, hh, 0:8], in_=CS[:, hh, :]), rd=[CS], wr=[tops])
                yield
                cx.V(lambda h: h.max_index(out=pos[:, hh, 0:8], in_max=tops[:, hh, 0:8], in_values=CS[:, hh, :]), rd=[tops, CS], wr=[pos])
                yield
                cx.V(lambda h: h.match_replace(out=w_[:], in_to_replace=tops[:, hh, 0:8], in_values=CS[:, hh, :], imm_value=-1e30), rd=[tops, CS], wr=[w_])
                yield
                cx.V(lambda h: h.max(out=tops[:, hh, 8:16], in_=w_[:]), rd=[w_], wr=[tops])
                yield
                cx.V(lambda h: h.max_index(out=pos[:, hh, 8:16], in_max=tops[:, hh, 8:16], in_values=w_[:]), rd=[tops, w_], wr=[pos])
            yield
            cx.V(lambda h: h.tensor_copy(out=posf[:], in_=pos[:]), rd=[pos], wr=[posf])
            yield
            cx.V(lambda h: h.tensor_copy(out=ixf[:], in_=ix[:]), rd=[ix], wr=[ixf])
            bc4 = lambda ap3: ap3.unsqueeze(3).broadcast_to([128, 8, 16, 16])
            io4 = io16[:].unsqueeze(1).unsqueeze(1).broadcast_to([128, 8, 16, 16])
            th4 = th16[:].unsqueeze(1).unsqueeze(1).broadcast_to([128, 8, 16, 16])
            yield
            cx.V(lambda h: h.tensor_tensor(out=oh_ap, in0=bc4(posf[:]), in1=th4, op=ALU.is_ge), rd=[posf, th16], wr=[oh])
            yield
            cx.V(lambda h: h.tensor_reduce(out=af[:], in_=oh_ap, axis=AX.X, op=ALU.add), rd=[oh], wr=[af])
            yield
            cx.V(lambda h: h.tensor_scalar(out=af[:], in0=af[:], scalar1=-1.0, scalar2=None, op0=ALU.add), rd=[af], wr=[af])
            yield
            cx.V(lambda h: h.scalar_tensor_tensor(out=bf_[:], in0=af[:], scalar=-16.0, in1=posf[:], op0=ALU.mult, op1=ALU.add), rd=[af, posf], wr=[bf_])
            ixv = ixf[:].rearrange("p (h t) k -> p h t k", t=2)
            for t, src in ((0, af), (1, bf_)):
                yield
                cx.V(lambda h: h.tensor_tensor(out=oh_ap, in0=bc4(src[:]), in1=io4, op=ALU.is_equal), rd=[src, io16], wr=[oh])
                yield
                cx.V(lambda h: h.tensor_tensor(out=oh_ap, in0=oh_ap, in1=ixv[:, :, t, :].unsqueeze(2).broadcast_to([128, 8, 16, 16]), op=ALU.mult), rd=[oh, ixf], wr=[oh])
                yield
                cx.V(lambda h: h.tensor_reduce(out=i12[:, t, :].rearrange("p (h k) -> p h k", h=8), in_=oh_ap, axis=AX.X, op=ALU.add), rd=[oh], wr=[i12])
            yield
            cx.V(lambda h: h.scalar_tensor_tensor(out=idxf[:], in0=i12[:, 0, :], scalar=128.0, in1=i12[:, 1, :], op0=ALU.mult, op1=ALU.add), rd=[i12], wr=[idxf])
            yield
            cx.V(lambda h: h.tensor_copy(out=idx[:], in_=idxf[:]), rd=[idxf], wr=[idx])
            yield
            cx.V(lambda h: h.tensor_tensor(out=ge[:], in0=tops[:], in1=tops[:, :, 0:1].broadcast_to([128, 8, 16]), op=ALU.subtract), rd=[tops], wr=[ge])
            yield
            cx.A(lambda h: h.activation(out=ge[:], in_=ge[:], func=AF.Exp), rd=[ge], wr=[ge])
            yield
            cx.V(lambda h: h.tensor_reduce(out=gsum[:, 0:8], in_=ge[:], axis=AX.X, op=ALU.add), rd=[ge], wr=[gsum])
            yield
            cx.V(lambda h: h.reciprocal(out=gsum[:, 8:16], in_=gsum[:, 0:8]), rd=[gsum], wr=[gsum])
            yield
            cx.V(lambda h: h.tensor_tensor(out=gate[:].rearrange("p (h k) -> p h k", h=8), in0=ge[:], in1=gsum[:, 8:16].unsqueeze(2).broadcast_to([128, 8, 16]), op=ALU.mult),
                 rd=[ge, gsum], wr=[gate])


        PF = NG - 2
        NGRP = NT * 32
        fgens = {}

        def drain(i):
            g = fgens.pop(i, None)
            if g is not None:
                for _ in g:
                    pass

        fgens[0] = front(0)
        drain(0)
        for gn in range(NGRP + PF + 1):
            if gn < NGRP:
                i, sg = divmod(gn, 32)
                if sg == 0:
                    drain(i)
                idx = idxs[i % 2]
                bq_ = gn % NG
                for jj in range(4):
                    j = sg * 4 + jj
                    P.dma("pool", lambda h: h.indirect_dma_start(out=Gb[bq_][:, jj, :], out_offset=None, in_=TAB.t,
                                                                 in_offset=bass.IndirectOffsetOnAxis(ap=idx[:, j:j + 1], axis=0)),
                          _ks([idx, TAB]), [Gtok[bq_][jj]])
            gm = gn - PF
            if 0 <= gm < NGRP:
                i, sg = divmod(gm, 32)
                hb = hbs[i % 2]
                if sg == 0:
                    cx.V(lambda h: h.memset(act[:], 0.0), wr=actT)
                    if i + 1 < NT:
                        fgens[i + 1] = front(i + 1)
                fg = fgens.get(i + 1)
                b_ = gm % NG
                G_ = Gb[b_]
                for jj in range(4):
                    j = sg * 4 + jj
                    if jj in STT_SLOTS:
                        cx.V(lambda h: h.scalar_tensor_tensor(out=junkv[:], in0=G_[:, jj, 0:1024], scalar=1.0, in1=hb[:], op0=ALU.mult, op1=ALU.mult, accum_out=act[:, j:j + 1]),
                             rd=[Gtok[b_][jj], hb], wr=[junkv, actT[sg % 4]])
                    else:
                        pr = prods[j % NPR]
                        cx.V(lambda h: h.tensor_tensor(out=pr[:], in0=G_[:, jj, 0:1024], in1=hb[:], op=ALU.mult), rd=[Gtok[b_][jj], hb], wr=[pr])
                        cx.A(lambda h: h.activation(out=junk[:], in_=pr[:], func=AF.Copy, accum_out=act[:, j:j + 1]), rd=[pr], wr=[junk, actT[sg % 4]])
                    if fg is not None:
                        for _ in range(FSTEP):
                            next(fg, None)
                cs = slice(sg * 4, (sg + 1) * 4)
                cx.A(lambda h: h.activation(out=gl[:, cs], in_=act[:, cs], func=AF.Gelu), rd=[actT[sg % 4]], wr=[glT[sg % 4]])
            gc = gn - PF - 1
            if 0 <= gc < NGRP:
                i, sg = divmod(gc, 32)
                gate = gates[i % 2]
                x = xb[i % 3]
                b_ = gc % NG
                G_ = Gb[b_]
                dg = diag[sg % 2]
                cs = slice(sg * 4, (sg + 1) * 4)
                cx.V(lambda h: h.tensor_tensor(out=coef[:, cs], in0=gl[:, cs], in1=gate[:, cs], op=ALU.mult), rd=[glT[sg % 4], gate], wr=[coefT[sg % 4]])
                for jj in range(4):
                    j = sg * 4 + jj
                    if DIAG_ON_DVE:
                        cx.V(lambda h: h.tensor_scalar(out=dg[:, jj, :], in0=idf[:], scalar1=coef[:, j:j + 1], scalar2=None, op0=ALU.mult), rd=[idf, coefT[sg % 4]], wr=[dg])
                    else:
                        cx.A(lambda h: h.activation(out=dg[:, jj, :], in_=idf[:], func=AF.Copy, scale=coef[:, j:j + 1]), rd=[idf, coefT[sg % 4]], wr=[dg])
                for jj in range(4):
                    j = sg * 4 + jj
                    for hv in range(2):
                        cx.M(lambda h: h.matmul(po[:, hv * 512:(hv + 1) * 512], lhsT=dg[:, jj, :], rhs=G_[:, jj, 1024 + hv * 512:1024 + (hv + 1) * 512],
                                                start=(j == 0), stop=(j == 127)), rd=[dg, Gtok[b_][jj]], wr=[po])
                if sg == 31:
                    y = yb[0]
                    cx.V(lambda h: h.tensor_tensor(out=y[:], in0=po[:], in1=g2[:], op=ALU.mult), rd=[po, g2], wr=[y])
                    cx.V(lambda h: h.tensor_tensor(out=y[:], in0=y[:], in1=x[:], op=ALU.add), rd=[y, x], wr=[y])
                    if final:
                        cx.V(lambda h: h.memset(st3[:, 0:1], 0.0), wr=[st3])
                        cx.A(lambda h: h.activation(out=sq2[:], in_=y[:], func=AF.Square, accum_out=st3[:, 0:1]), rd=[y, st3], wr=[sq2, st3])
                        cx.A(lambda h: h.activation(out=st3[:, 1:2], in_=st3[:, 0:1], func=AF.Sqrt, scale=1.0 / D, bias=eps[:, 0:1]), rd=[st3, eps], wr=[st3])
                        cx.V(lambda h: h.reciprocal(out=st3[:, 2:3], in_=st3[:, 1:2]), rd=[st3], wr=[st3])
                        cx.V(lambda h: h.scalar_tensor_tensor(out=y[:], in0=y[:], scalar=st3[:, 2:3], in1=fg_[:], op0=ALU.mult, op1=ALU.mult), rd=[y, st3, fg_], wr=[y])
                    cx.dma("sp", Xout.t[i * 128:(i + 1) * 128, :], y[:], rd=[y], wr=[Xout])
        P.barrier()
        P.flush()


def phase_attn(P, nc, IN, MODS, li, Xin, Xout, SC):
    with contextlib.ExitStack() as st0:
        c0 = Ctx(P, nc, st0)
        idf, idb, eps = load_consts(c0, IN)
        bigT = c0.T([128, 8, S], BF16)
        qT = c0.T([128, 8, S], BF16)
        kT = c0.T([128, 2, S], BF16)
        V1 = c0.T([128, NT, 2, 130], BF16)
        with contextlib.ExitStack() as st1:
            c1 = Ctx(P, nc, st1)
            gs1, sh1 = load_mods(c1, MODS, li, [0, 1])
            emit_hT(c1, Xin, gs1, sh1, idb, bigT)
            P.barrier()
        with contextlib.ExitStack() as st2:
            cx = Ctx(P, nc, st2)
            w = cx.T([128, 8, 1536], BF16)
            with contextlib.ExitStack() as stw:
                cw_ = Ctx(P, nc, stw)
                for k in range(8):
                    cw_.dma("pool", w[:, k, :], IN["od_w_in"][k * 128:(k + 1) * 128, :], wr=[w])
                P.barrier()
            csb = [cx.T([128, 2, 64], F32) for _ in range(2)]
            gq = cx.T([128, 10, 128], F32)
            g1_ = cx.T([128, 128], F32)
            g2_ = cx.T([128, 128], F32)
            cx.dma("sp", g1_[:], IN["od_qnorm_g"].broadcast_to([128, 128]), wr=[g1_])
            cx.dma("sp", g2_[:], IN["od_knorm_g"].broadcast_to([128, 128]), wr=[g2_])
            cx.V(lambda h: h.tensor_scalar(out=g1_[:], in0=g1_[:], scalar1=float(128 ** -0.5), scalar2=None, op0=ALU.mult), rd=[g1_], wr=[g1_])
            cx.V(lambda h: h.tensor_copy(out=gq[:, 0:8, :], in_=g1_[:].unsqueeze(1).broadcast_to([128, 8, 128])), rd=[g1_], wr=[gq])
            cx.V(lambda h: h.tensor_copy(out=gq[:, 8:10, :], in_=g2_[:].unsqueeze(1).broadcast_to([128, 2, 128])), rd=[g2_], wr=[gq])
            cx.V(lambda h: h.memset(V1[:], 1.0), wr=[V1])
            pz = [cx.PS([128, 512]) for _ in range(3)]
            ptq = cx.PS([128, 8, 128], BF16)
            ptk = cx.PS([128, 2, 128], BF16)
            rs = cx.T([128, 32], F32)
            qn = cx.T([128, 10, 128], F32)
            qr = cx.T([128, 10, 128], BF16)
            t1 = cx.T([128, 10, 64], F32)
            t2 = cx.T([128, 10, 64], F32)
            for i in range(NT):
                tsl = slice(i * 128, (i + 1) * 128)
                for nb in range(3):
                    for k in range(8):
                        cx.M(lambda h: h.matmul(pz[nb][:], lhsT=bigT[:, k, tsl], rhs=w[:, k, nb * 512:(nb + 1) * 512], start=(k == 0), stop=(k == 7)), rd=[bigT, w], wr=[pz[nb]])
                cx.A(lambda h: h.activation(out=V1[:, i, :, 0:128], in_=pz[2][:, 256:512].rearrange("p (g d) -> p g d", g=2), func=AF.Copy), rd=[pz[2]], wr=[V1])
                cs = csb[i % 2]
                cx.dma("act", cs[:], IN["ropecs"][:, :, i, :], wr=[cs])
                zsrc = ((pz[0], 0, 4, 512), (pz[1], 4, 8, 512), (pz[2], 8, 10, 256))
                for (pp, g0, g1x, wd) in zsrc:
                    cx.A(lambda h: h.activation(out=qn[:, g0:g1x, :], in_=pp[:, 0:wd].rearrange("p (g d) -> p g d", d=128), func=AF.Square), rd=[pp], wr=[qn])
                cx.V(lambda h: h.tensor_reduce(out=rs[:, 0:10], in_=qn[:], axis=AX.X, op=ALU.add), rd=[qn], wr=[rs])
                cx.A(lambda h: h.activation(out=rs[:, 10:20], in_=rs[:, 0:10], func=AF.Sqrt, scale=1.0 / 128, bias=eps[:, 0:1]), rd=[rs, eps], wr=[rs])
                cx.V(lambda h: h.reciprocal(out=rs[:, 20:30], in_=rs[:, 10:20]), rd=[rs], wr=[rs])
                for (pp, g0, g1x, wd) in zsrc:
                    cx.V(lambda h: h.tensor_tensor(out=qn[:, g0:g1x, :], in0=pp[:, 0:wd].rearrange("p (g d) -> p g d", d=128),
                                                   in1=rs[:, 20 + g0:20 + g1x].unsqueeze(2).broadcast_to([128, g1x - g0, 128]), op=ALU.mult), rd=[pp, rs], wr=[qn])
                cx.V(lambda h: h.tensor_tensor(out=qn[:], in0=qn[:], in1=gq[:], op=ALU.mult), rd=[qn, gq], wr=[qn])
                qv = qn[:].rearrange("p g (d t) -> p g d t", t=2)
                qo = qr[:].rearrange("p g (d t) -> p g d t", t=2)
                cc = cs[:, 0, :].unsqueeze(1).broadcast_to([128, 10, 64])
                ss_ = cs[:, 1, :].unsqueeze(1).broadcast_to([128, 10, 64])
                cx.V(lambda h: h.tensor_tensor(out=t1[:], in0=qv[:, :, :, 0], in1=cc, op=ALU.mult), rd=[qn, cs], wr=[t1])
                cx.V(lambda h: h.tensor_tensor(out=t2[:], in0=qv[:, :, :, 1], in1=ss_, op=ALU.mult), rd=[qn, cs], wr=[t2])
                cx.V(lambda h: h.tensor_tensor(out=qo[:, :, :, 0], in0=t1[:], in1=t2[:], op=ALU.subtract), rd=[t1, t2], wr=[qr])
                cx.V(lambda h: h.tensor_tensor(out=t1[:], in0=qv[:, :, :, 0], in1=ss_, op=ALU.mult), rd=[qn, cs, qr], wr=[t1])
                cx.V(lambda h: h.tensor_tensor(out=t2[:], in0=qv[:, :, :, 1], in1=cc, op=ALU.mult), rd=[qn, cs, qr], wr=[t2])
                cx.V(lambda h: h.tensor_tensor(out=qo[:, :, :, 1], in0=t1[:], in1=t2[:], op=ALU.add), rd=[t1, t2], wr=[qr])
                for g_ in range(8):
                    cx.M(lambda h: h.transpose(out=ptq[:, g_, :], in_=qr[:, g_, :], identity=idb[:]), rd=[qr, idb], wr=[ptq])
                for g_ in range(2):
                    cx.M(lambda h: h.transpose(out=ptk[:, g_, :], in_=qr[:, 8 + g_, :], identity=idb[:]), rd=[qr, idb], wr=[ptk])
                cx.A(lambda h: h.activation(out=qT[:, :, tsl], in_=ptq[:], func=AF.Copy), rd=[ptq], wr=[qT])
                cx.A(lambda h: h.activation(out=kT[:, :, tsl], in_=ptk[:], func=AF.Copy), rd=[ptk], wr=[kT])
            P.barrier()
        import os as _os
        if _os.environ.get("ATT_STOP") == "2":
            P.barrier()
            P.flush()
            return
        with contextlib.ExitStack() as st3:
            cx = Ctx(P, nc, st3)
            pss = [cx.PS([128, 512]) for _ in range(2)]
            pacc = [cx.PS([128, 512]) for _ in range(4)]
            pto = cx.PS([128, 8, 128], BF16)
            pT = [cx.T([128, 512], BF16) for _ in range(3)]
            ao = [cx.T([128, 8, 128], BF16) for _ in range(4)]
            rinv = cx.T([128, 8], F32)
            steps = [(qb_, hd_, sj) for qb_ in range(8) for hd_ in range(8) for sj in range(NT)]
            accs = pacc

            def emit_score(n):
                qb_, hd_, sj = steps[n]
                ps_ = pss[n % 2]
                cx.M(lambda h: h.matmul(ps_[:], lhsT=kT[:, hd_ // 4, sj * 128:(sj + 1) * 128], rhs=qT[:, hd_, qb_ * 512:(qb_ + 1) * 512], start=True, stop=True), rd=[kT, qT], wr=[ps_])

            bg = SC.get("BG1")
            if bg is not None:
                bg.attach(cx)
            emit_score(0)
            for n, (qb_, hd_, sj) in enumerate(steps):
                g_ = hd_ // 4
                if bg is not None and n % 32 == 0:
                    bg.step(1)
                if n + 1 < len(steps):
                    emit_score(n + 1)
                ps_ = pss[n % 2]
                pt_ = pT[n % 3]
                cx.A(lambda h: h.activation(out=pt_[:], in_=ps_[:], func=AF.Exp), rd=[ps_], wr=[pt_])
                for qs in range(4):
                    cx.M(lambda h: h.matmul(accs[qs][:, 0:129], lhsT=pt_[:, qs * 128:(qs + 1) * 128], rhs=V1[:, sj, g_, 0:129], start=(sj == 0), stop=(sj == NT - 1)),
                         rd=[pt_, V1], wr=[accs[qs]])
                if sj == NT - 1:
                    for qs in range(4):
                        a_ = accs[qs]
                        cx.V(lambda h: h.reciprocal(out=rinv[:, qs:qs + 1], in_=a_[:, 128:129]), rd=[a_], wr=[rinv])
                        cx.V(lambda h: h.tensor_scalar(out=ao[qs][:, hd_, :], in0=a_[:, 0:128], scalar1=rinv[:, qs:qs + 1], scalar2=None, op0=ALU.mult), rd=[a_, rinv], wr=[ao[qs]])
                    if hd_ == 7:
                        for qs in range(4):
                            ti = qb_ * 4 + qs
                            for k in range(8):
                                cx.M(lambda h: h.transpose(out=pto[:, k, :], in_=ao[qs][:, k, :], identity=idb[:]), rd=[ao[qs], idb], wr=[pto])
                            cx.V(lambda h: h.tensor_copy(out=bigT[:, :, ti * 128:(ti + 1) * 128], in_=pto[:]), rd=[pto], wr=[bigT])
            if bg is not None:
                bg.finish()
                SC.setdefault("TABDONE", {})[1] = True
            P.barrier()
        if _os.environ.get("ATT_STOP") == "3":
            P.barrier()
            P.flush()
            return
        with contextlib.ExitStack() as st4:
            cx = Ctx(P, nc, st4)
            g1, = load_mods(cx, MODS, li, [2])
            stage_f = cx.T([128, 1024], F32)
            emit_outproj(cx, Xin, Xout, lambda i: (bigT[:, :, i * 128:(i + 1) * 128], bigT), IN["od_w_out"], g1, stage_f, nx=3)
            P.barrier()
        P.flush()
```
